# Optimizing a Trainium2 kernel written in Bass

```python
import math
import numpy as np
import jax
import jax.numpy as jnp
from jax import lax

D_MODEL = 1024
BATCH = 2
SEQ = 8192
DEPTH = 2

GRID_W = 64
CTX_LEN = 256
DN_HEADS = 8
DN_DK = 64
DN_DV = 64
DN_CONV = 5
DN_CHUNK = 64
SWA_Q_HEADS = 8
SWA_KV_HEADS = 2
SWA_HD = 64
SWA_WINDOW = 128
SWA_BLOCK = 128
ROPE_THETA = 10000.0
ROPE_FREQS = SWA_HD // 4
DN_QK = DN_HEADS * DN_DK
D_DN = DN_HEADS * DN_DV
D_SWA = SWA_Q_HEADS * SWA_HD
D_KV = SWA_KV_HEADS * SWA_HD
D_MIX = D_DN + D_SWA
DN_CONV_CH = 2 * DN_QK + D_DN
D_IN = 2 * DN_QK + 2 * D_DN + 4 * DN_HEADS + D_SWA + 2 * D_KV
N_EXPERTS = 32
TOP_K = 4
D_EXPERT = D_MODEL
SWIGLU_LIMIT = 7.0
SWIGLU_ALPHA = 1.702
MOE_BLOCK = 128
NORM_EPS = 1e-6
NEG_INF = -1e30

kernel_name = "hybrid_deltanet_swa_moe_dit"


def rmsnorm(x, g):
    xf = x.astype(jnp.float32)
    y = xf * lax.rsqrt(jnp.mean(xf * xf, axis=-1, keepdims=True) + NORM_EPS)
    return (y * g.astype(jnp.float32)).astype(x.dtype)


def l2norm(x):
    xf = x.astype(jnp.float32)
    return (xf * lax.rsqrt(jnp.sum(xf * xf, axis=-1, keepdims=True) + NORM_EPS)).astype(x.dtype)


def modulate(x, g, shift, scale):
    return rmsnorm(x, g) * (1.0 + scale) + shift


def split_proj(p):
    sizes = (DN_QK, DN_QK, D_DN, D_DN, 2 * DN_HEADS, 2 * DN_HEADS, D_SWA, D_KV, D_KV)
    idx = np.cumsum(np.array(sizes))[:-1].tolist()
    return jnp.split(p, idx, axis=-1)


def axial_rope_tables(n_tokens):
    rows = n_tokens // GRID_W
    row = jnp.repeat(jnp.arange(rows, dtype=jnp.float32), GRID_W)
    col = jnp.tile(jnp.arange(GRID_W, dtype=jnp.float32), rows)
    inv_freq = jnp.power(ROPE_THETA, -jnp.arange(ROPE_FREQS, dtype=jnp.float32) / ROPE_FREQS)
    ang_r = row[:, None] * inv_freq
    ang_c = col[:, None] * inv_freq
    return (jnp.cos(ang_r), jnp.sin(ang_r), jnp.cos(ang_c), jnp.sin(ang_c))


def rope_rotate(x, cos, sin):
    x1, x2 = jnp.split(x, 2, axis=-1)
    cos = cos[:, None, :].astype(x.dtype)
    sin = sin[:, None, :].astype(x.dtype)
    return jnp.concatenate([x1 * cos - x2 * sin, x2 * cos + x1 * sin], axis=-1)


def axial_rope(x, tabs):
    cos_r, sin_r, cos_c, sin_c = tabs
    x_row, x_col = jnp.split(x, 2, axis=-1)
    return jnp.concatenate([rope_rotate(x_row, cos_r, sin_r), rope_rotate(x_col, cos_c, sin_c)], axis=-1)


def short_conv(x, w):
    pad = DN_CONV // 2
    y = lax.conv_general_dilated(x, w[:, None, :].astype(x.dtype), window_strides=(1,), padding=[(pad, pad)],
                                 dimension_numbers=('NWC', 'WIO', 'NWC'), feature_group_count=x.shape[-1])
    return jax.nn.silu(y)


def dn_prepare(p_q, p_k, p_v, p_a, p_b, conv_w, a_log, dt_bias):
    B, T, _ = p_q.shape
    qkv = short_conv(jnp.concatenate([p_q, p_k, p_v], axis=-1), conv_w)
    q, k, v = jnp.split(qkv, [DN_QK, 2 * DN_QK], axis=-1)
    q = l2norm(q.reshape(B, T, DN_HEADS, DN_DK)) * (DN_DK ** -0.5)
    k = l2norm(k.reshape(B, T, DN_HEADS, DN_DK))
    v = v.reshape(B, T, DN_HEADS, DN_DV)
    a = p_a.reshape(B, T, 2, DN_HEADS).astype(jnp.float32)
    b = p_b.reshape(B, T, 2, DN_HEADS).astype(jnp.float32)
    g = -jnp.exp(a_log.astype(jnp.float32)) * jax.nn.softplus(a + dt_bias.astype(jnp.float32))
    beta = jax.nn.sigmoid(b)
    return q, k, v, g, beta


def gated_delta_chunked(q, k, v, g, beta, s0):
    B, T, H, DK = q.shape
    DV = v.shape[-1]
    C = DN_CHUNK
    N = T // C
    f32 = jnp.float32

    def chunks(t):
        return t.astype(f32).reshape(B, N, C, H, -1).transpose(0, 3, 1, 2, 4)

    qc, kc, vc = chunks(q), chunks(k), chunks(v)
    gch = chunks(g[..., None])[..., 0]
    bch = chunks(beta[..., None])[..., 0]
    gcum = jnp.cumsum(gch, axis=-1)
    tri = jnp.tril(jnp.ones((C, C), bool))
    strict = jnp.tril(jnp.ones((C, C), bool), -1)
    decay = jnp.where(tri, jnp.exp(jnp.where(tri, gcum[..., :, None] - gcum[..., None, :], 0.0)), 0.0)
    kb = kc * bch[..., None]
    vb = vc * bch[..., None]
    eye = jnp.eye(C, dtype=f32)
    a_mat = eye + jnp.where(strict, jnp.einsum('bhnid,bhnjd->bhnij', kb, kc) * decay, 0.0)
    t_inv = lax.linalg.triangular_solve(a_mat, jnp.broadcast_to(eye, a_mat.shape), left_side=True,
                                        lower=True, unit_diagonal=True)
    eg = jnp.exp(gcum)
    u = t_inv @ vb
    w = t_inv @ (kb * eg[..., None])
    a_qk = jnp.einsum('bhnid,bhnjd->bhnij', qc, kc) * decay
    qg = qc * eg[..., None]
    kd = kc * jnp.exp(gcum[..., -1:] - gcum)[..., None]
    g_last = eg[..., -1]

    def step(state, xs):
        a_i, u_i, w_i, qg_i, kd_i, gl_i = xs
        v_new = u_i - w_i @ state
        o_i = qg_i @ state + a_i @ v_new
        state = state * gl_i[..., None, None] + jnp.einsum('bhck,bhcv->bhkv', kd_i, v_new)
        return state, o_i

    to_front = lambda t: jnp.moveaxis(t, 2, 0)
    s_final, o = lax.scan(step, s0.astype(f32), (to_front(a_qk), to_front(u), to_front(w), to_front(qg),
                                                  to_front(kd), to_front(g_last)))
    o = jnp.moveaxis(o, 0, 2).transpose(0, 2, 3, 1, 4).reshape(B, T, H, DV)
    return o.astype(v.dtype), s_final


def dn_bidirectional(q, k, v, g, beta, s0_fwd, s0_bwd):
    flip = lambda t: jnp.flip(t, axis=1)
    o_f, s_f = gated_delta_chunked(q, k, v, g[:, :, 0], beta[:, :, 0], s0_fwd)
    o_b, s_b = gated_delta_chunked(flip(q), flip(k), flip(v), flip(g[:, :, 1]), flip(beta[:, :, 1]), s0_bwd)
    return o_f + flip(o_b), s_f, s_b


def dn_gated_out(o, z, g):
    B, T = o.shape[:2]
    z = z.reshape(B, T, DN_HEADS, DN_DV)
    return (rmsnorm(o, g) * jax.nn.silu(z)).reshape(B, T, D_DN)


def swa_latent(q, k, v, kc, vc, sinks):
    B, S = q.shape[:2]
    BL = SWA_BLOCK
    NB = S // BL
    G = SWA_Q_HEADS // SWA_KV_HEADS
    scale = SWA_HD ** -0.5
    qb = q.reshape(B, NB, BL, SWA_KV_HEADS, G, SWA_HD)

    def band(t):
        tp = jnp.pad(t, ((0, 0), (BL, BL), (0, 0), (0, 0))).reshape(B, NB + 2, BL, SWA_KV_HEADS, SWA_HD)
        return jnp.concatenate([tp[:, :-2], tp[:, 1:-1], tp[:, 2:]], axis=2)

    kb, vb = band(k), band(v)
    s_loc = jnp.einsum('bnqhgd,bnkhd->bnhgqk', qb, kb).astype(jnp.float32) * scale
    rel = jnp.arange(3 * BL)[None, :] - jnp.arange(BL)[:, None]
    in_window = (rel >= BL - SWA_WINDOW) & (rel <= BL + SWA_WINDOW)
    key_pos = jnp.arange(NB)[:, None] * BL - BL + jnp.arange(3 * BL)[None, :]
    in_range = (key_pos >= 0) & (key_pos < S)
    mask = in_window[None] & in_range[:, None, :]
    s_loc = jnp.where(mask[None, :, None, None], s_loc, NEG_INF)
    s_ctx = jnp.einsum('bnqhgd,bkhd->bnhgqk', qb, kc).astype(jnp.float32) * scale
    sink = jnp.broadcast_to(sinks.astype(jnp.float32).reshape(SWA_KV_HEADS, G)[None, None, :, :, None, None],
                            s_loc.shape[:-1] + (1,))
    p = jax.nn.softmax(jnp.concatenate([s_loc, s_ctx, sink], axis=-1), axis=-1)
    n_loc = 3 * BL
    n_ctx = kc.shape[1]
    p_loc = p[..., :n_loc].astype(v.dtype)
    p_ctx = p[..., n_loc:n_loc + n_ctx].astype(v.dtype)
    o = jnp.einsum('bnhgqk,bnkhd->bnqhgd', p_loc, vb) + jnp.einsum('bnhgqk,bkhd->bnqhgd', p_ctx, vc)
    return o.reshape(B, S, D_SWA)


def swa_context(qc, kc, vc, sinks):
    B, CL = qc.shape[:2]
    G = SWA_Q_HEADS // SWA_KV_HEADS
    qg = qc.reshape(B, CL, SWA_KV_HEADS, G, SWA_HD)
    s = jnp.einsum('bqhgd,bkhd->bhgqk', qg, kc).astype(jnp.float32) * (SWA_HD ** -0.5)
    sink = jnp.broadcast_to(sinks.astype(jnp.float32).reshape(SWA_KV_HEADS, G)[None, :, :, None, None],
                            s.shape[:-1] + (1,))
    p = jax.nn.softmax(jnp.concatenate([s, sink], axis=-1), axis=-1)[..., :CL].astype(vc.dtype)
    o = jnp.einsum('bhgqk,bkhd->bqhgd', p, vc)
    return o.reshape(B, CL, D_SWA)


def moe(h, router_w, router_b, w_gate_up, b_gate_up, w_down, b_down):
    T, D = h.shape
    logits = (h @ router_w + router_b).astype(jnp.float32)
    top_val, top_idx = lax.top_k(logits, TOP_K)
    gates = jax.nn.softmax(top_val, axis=-1)
    n_assign = T * TOP_K
    flat_e = top_idx.reshape(-1)
    order = jnp.argsort(flat_e)
    e_sorted = flat_e[order]
    tok_sorted = (order // TOP_K).astype(jnp.int32)
    gate_sorted = gates.reshape(-1)[order]
    counts = jnp.bincount(flat_e, length=N_EXPERTS)
    start = jnp.cumsum(counts) - counts
    padded = (counts + MOE_BLOCK - 1) // MOE_BLOCK * MOE_BLOCK
    pad_end = jnp.cumsum(padded)
    pad_start = pad_end - padded
    dest = pad_start[e_sorted] + (jnp.arange(n_assign) - start[e_sorted])
    n_blocks = (n_assign + MOE_BLOCK - 1) // MOE_BLOCK + N_EXPERTS
    n_pad = n_blocks * MOE_BLOCK
    buf_tok = jnp.full((n_pad,), T, jnp.int32).at[dest].set(tok_sorted)
    buf_gate = jnp.zeros((n_pad,), jnp.float32).at[dest].set(gate_sorted)
    block_expert = jnp.clip(jnp.searchsorted(pad_end, jnp.arange(n_blocks) * MOE_BLOCK, side='right'),
                            0, N_EXPERTS - 1)
    h_pad = jnp.concatenate([h, jnp.zeros((1, D), h.dtype)], axis=0)
    xb = h_pad[buf_tok].reshape(n_blocks, MOE_BLOCK, D)

    def expert_block(args):
        xe, e = args
        gu = xe @ w_gate_up[e] + b_gate_up[e]
        gate, up = jnp.split(gu, 2, axis=-1)
        gate = jnp.minimum(gate, SWIGLU_LIMIT)
        up = jnp.clip(up, -SWIGLU_LIMIT, SWIGLU_LIMIT)
        act = (up + 1.0) * gate * jax.nn.sigmoid(SWIGLU_ALPHA * gate)
        return act @ w_down[e] + b_down[e]

    yb = lax.map(expert_block, (xb, block_expert))
    y = yb.reshape(n_pad, D) * buf_gate[:, None].astype(h.dtype)
    return jax.ops.segment_sum(y, buf_tok, num_segments=T + 1)[:T]


def hybrid_layer(x, xc, c, c_ctx, rope_tabs, ada_w, ada_b, norm1_g, w_in, dn_conv_w, dn_a_log, dn_dt_bias,
                 dn_out_g, q_norm_g, k_norm_g, sinks, w_out, norm2_g, router_w, router_b,
                 w_gate_up, b_gate_up, w_down, b_down, last):
    B, S, D = x.shape
    CL = xc.shape[1]
    mod_x = (jax.nn.silu(c) @ ada_w + ada_b)[:, None, :]
    mod_c = jax.nn.silu(c_ctx) @ ada_w + ada_b
    sh1, sc1, gt1, sh2, sc2, gt2 = jnp.split(mod_x, 6, axis=-1)
    csh1, csc1, cgt1, csh2, csc2, cgt2 = jnp.split(mod_c, 6, axis=-1)

    hx = modulate(x, norm1_g, sh1, sc1)
    hc = modulate(xc, norm1_g, csh1, csc1)
    xq, xk, xv, xz, xa, xb, xsq, xsk, xsv = split_proj(hx @ w_in)
    cq, ck, cv, cz, ca, cb, csq, csk, csv = split_proj(hc @ w_in)

    dn_c = dn_prepare(cq, ck, cv, ca, cb, dn_conv_w, dn_a_log, dn_dt_bias)
    dn_x = dn_prepare(xq, xk, xv, xa, xb, dn_conv_w, dn_a_log, dn_dt_bias)
    s0 = jnp.zeros((B, DN_HEADS, DN_DK, DN_DV), jnp.float32)
    o_dn_c, s_fwd, s_bwd = dn_bidirectional(*dn_c, s0, s0)
    o_dn_x, _, _ = dn_bidirectional(*dn_x, s_fwd, s_bwd)
    o_dn_x = dn_gated_out(o_dn_x, xz, dn_out_g)

    qx = axial_rope(rmsnorm(xsq.reshape(B, S, SWA_Q_HEADS, SWA_HD), q_norm_g), rope_tabs)
    kx = axial_rope(rmsnorm(xsk.reshape(B, S, SWA_KV_HEADS, SWA_HD), k_norm_g), rope_tabs)
    vx = xsv.reshape(B, S, SWA_KV_HEADS, SWA_HD)
    kc = rmsnorm(csk.reshape(B, CL, SWA_KV_HEADS, SWA_HD), k_norm_g)
    vc = csv.reshape(B, CL, SWA_KV_HEADS, SWA_HD)
    o_sw_x = swa_latent(qx, kx, vx, kc, vc, sinks)

    x = x + gt1 * (jnp.concatenate([o_dn_x, o_sw_x], axis=-1) @ w_out)
    if not last:
        qc = rmsnorm(csq.reshape(B, CL, SWA_Q_HEADS, SWA_HD), q_norm_g)
        o_sw_c = swa_context(qc, kc, vc, sinks)
        o_dn_c = dn_gated_out(o_dn_c, cz, dn_out_g)
        xc = xc + cgt1 * (jnp.concatenate([o_dn_c, o_sw_c], axis=-1) @ w_out)

    hx2 = modulate(x, norm2_g, sh2, sc2)
    if last:
        y = moe(hx2.reshape(B * S, D), router_w, router_b, w_gate_up, b_gate_up, w_down, b_down)
        x = x + gt2 * y.reshape(B, S, D)
    else:
        hc2 = modulate(xc, norm2_g, csh2, csc2)
        tokens = jnp.concatenate([hx2.reshape(B * S, D), hc2.reshape(B * CL, D)], axis=0)
        y = moe(tokens, router_w, router_b, w_gate_up, b_gate_up, w_down, b_down)
        x = x + gt2 * y[:B * S].reshape(B, S, D)
        xc = xc + cgt2 * y[B * S:].reshape(B, CL, D)
    return x, xc


def setup_inputs(seed: int = 0) -> dict:
    key = jax.random.key(seed)
    ks = jax.random.split(key, 24)
    f32 = jnp.float32
    L = DEPTH

    def nrm(k, shape, s):
        return jax.random.normal(k, shape, f32) * s

    dt = jnp.exp(jax.random.uniform(ks[10], (L, 2, DN_HEADS), f32, math.log(1e-3), math.log(1e-1)))
    return {
        "x": nrm(ks[0], (BATCH, SEQ, D_MODEL), 1.0),
        "c": nrm(ks[1], (BATCH, D_MODEL), 1.0),
        "ctx": nrm(ks[2], (BATCH, CTX_LEN, D_MODEL), 1.0),
        "c_ctx": nrm(ks[3], (D_MODEL,), 1.0),
        "ada_w": nrm(ks[4], (L, D_MODEL, 6 * D_MODEL), 0.5 * D_MODEL ** -0.5),
        "ada_b": nrm(ks[5], (L, 6 * D_MODEL), 0.01),
        "norm1_g": 1.0 + nrm(ks[6], (L, D_MODEL), 0.02),
        "w_in": nrm(ks[7], (L, D_MODEL, D_IN), D_MODEL ** -0.5),
        "dn_conv_w": nrm(ks[8], (L, DN_CONV, DN_CONV_CH), DN_CONV ** -0.5),
        "dn_a_log": jnp.log(jax.random.uniform(ks[9], (L, 2, DN_HEADS), f32, 1.0, 16.0)),
        "dn_dt_bias": jnp.log(jnp.expm1(dt)),
        "dn_out_g": 1.0 + nrm(ks[11], (L, DN_DV), 0.02),
        "q_norm_g": 1.0 + nrm(ks[12], (L, SWA_HD), 0.02),
        "k_norm_g": 1.0 + nrm(ks[13], (L, SWA_HD), 0.02),
        "sinks": nrm(ks[14], (L, SWA_Q_HEADS), 0.5),
        "w_out": nrm(ks[15], (L, D_MIX, D_MODEL), D_MIX ** -0.5),
        "norm2_g": 1.0 + nrm(ks[16], (L, D_MODEL), 0.02),
        "router_w": nrm(ks[17], (L, D_MODEL, N_EXPERTS), D_MODEL ** -0.5),
        "router_b": nrm(ks[18], (L, N_EXPERTS), 0.01),
        "w_gate_up": nrm(ks[19], (L, N_EXPERTS, D_MODEL, 2 * D_EXPERT), D_MODEL ** -0.5),
        "b_gate_up": nrm(ks[20], (L, N_EXPERTS, 2 * D_EXPERT), 0.01),
        "w_down": nrm(ks[21], (L, N_EXPERTS, D_EXPERT, D_MODEL), D_EXPERT ** -0.5),
        "b_down": nrm(ks[22], (L, N_EXPERTS, D_MODEL), 0.01),
    }


def reference(x, c, ctx, c_ctx, ada_w, ada_b, norm1_g, w_in, dn_conv_w, dn_a_log, dn_dt_bias, dn_out_g,
              q_norm_g, k_norm_g, sinks, w_out, norm2_g, router_w, router_b, w_gate_up, b_gate_up,
              w_down, b_down):
    rope_tabs = axial_rope_tables(x.shape[1])
    xc = ctx
    for l in range(DEPTH):
        x, xc = hybrid_layer(x, xc, c, c_ctx, rope_tabs, ada_w[l], ada_b[l], norm1_g[l], w_in[l], dn_conv_w[l],
                             dn_a_log[l], dn_dt_bias[l], dn_out_g[l], q_norm_g[l], k_norm_g[l], sinks[l],
                             w_out[l], norm2_g[l], router_w[l], router_b[l], w_gate_up[l], b_gate_up[l],
                             w_down[l], b_down[l], last=(l == DEPTH - 1))
    return x
```

```python
import os
import numpy as np
from contextlib import ExitStack
import concourse.bass as bass
import concourse.mybir as mybir
from concourse.bass_utils import run_bass_kernel_spmd

F32 = mybir.dt.float32
BF16 = mybir.dt.bfloat16
AF = mybir.ActivationFunctionType
ALU = mybir.AluOpType
AX = mybir.AxisListType

SAME_ENGINE_SYNC = True
NRING = 8
EPOCH = 30000


class Res:
    __slots__ = ("name", "w", "r")

    def __init__(self, name=""):
        self.name = name
        self.w = None
        self.r = {}


class Prog:
    def __init__(self, nc, stack):
        self.nc = nc
        self.e = {"pe": nc.tensor, "act": nc.scalar, "dve": nc.vector, "pool": nc.gpsimd, "sp": nc.sync}
        self.ops = {k: [] for k in self.e}
        self.cnt = {k: 0 for k in self.e}
        self.sem = {k: [stack.enter_context(nc.semaphore("s_" + k + "0"))] for k in self.e}
        self.ring = {q: [stack.enter_context(nc.semaphore("d_%s_%d" % (q, i))) for i in range(NRING)]
                     for q in ("sp", "act", "pool")}
        self.dcnt = {q: 0 for q in self.ring}
        self.waited = {k: {} for k in self.e}
        self.stack = stack
        self.n_wait = 0

    def sb(self, name, shape, dt=F32, stack=None):
        self.n_alloc = getattr(self, "n_alloc", 0) + 1
        return (stack or self.stack).enter_context(self.nc.sbuf_tensor("%s_%d" % (name, self.n_alloc), list(shape), dt))

    def fence(self):
        for E in self.e:
            for F in self.e:
                if F != E and self.cnt[F] > 0:
                    self._wait(E, ("c", F, self.cnt[F]))
            for q in self.ring:
                n = self.dcnt[q]
                for k in range(max(0, n - NRING), n):
                    self._wait(E, ("d", q, k))

    def ps(self, name, shape, dt=F32):
        return self.stack.enter_context(self.nc.psum_tensor(name, list(shape), dt))

    def _semval(self, ev):
        if ev[0] == "c":
            ep, v = divmod(ev[2] - 1, EPOCH)
            return ("c", ev[1]), self.sem[ev[1]][ep], (ep, v + 1)
        if ev[0] == "x":
            return ("x", ev[1]), self.ccsems[ev[1]], (0, 1)
        _, q, n = ev
        slot = n % NRING
        return ("d", q, slot), self.ring[q][slot], (0, 16 * (n // NRING + 1))

    def _wait(self, eng, ev):
        if ev[0] == "c" and ev[1] == eng:
            if eng == "pe" or not SAME_ENGINE_SYNC:
                return
        key, sem, val = self._semval(ev)
        if self.waited[eng].get(key, (0, 0)) >= val:
            return
        self.waited[eng][key] = val
        self.n_wait += 1
        self.ops[eng].append(lambda h, sem=sem, val=val[1]: h.wait_ge(sem, val))

    def _deps(self, eng, reads, writes):
        deps = []
        for r in reads:
            if r.w is not None:
                deps.append(r.w)
        for w in writes:
            if w.w is not None:
                deps.append(w.w)
            deps.extend(w.r.values())
        for ev in deps:
            self._wait(eng, ev)

    def _record(self, ev, reads, writes):
        if ev[0] == "c":
            key = ("c", ev[1])
        elif ev[0] == "x":
            key = ("x", ev[1])
        else:
            key = ("d", ev[1], ev[2] % NRING)
        for r in reads:
            r.r[key] = ev
        for w in writes:
            w.w = ev
            w.r = {}

    def op(self, eng, fn, reads=(), writes=()):
        self._deps(eng, reads, writes)
        self.cnt[eng] += 1
        ev = ("c", eng, self.cnt[eng])
        ep = (self.cnt[eng] - 1) // EPOCH
        if ep >= len(self.sem[eng]):
            self.sem[eng].append(self.stack.enter_context(self.nc.semaphore("s_%s%d" % (eng, ep))))
        sem = self.sem[eng][ep]
        self.ops[eng].append(lambda h, fn=fn, sem=sem: fn(h).then_inc(sem, 1))
        self._record(ev, reads, writes)

    def dma(self, q, out, in_, reads=(), writes=(), **kw):
        self._deps(q, reads, writes)
        n = self.dcnt[q]
        self.dcnt[q] += 1
        if n >= NRING:
            self._wait(q, ("d", q, n - NRING))
        sem = self.ring[q][n % NRING]
        self.ops[q].append(lambda h, out=out, in_=in_, sem=sem, kw=kw: h.dma_start(out=out, in_=in_, **kw).then_inc(sem, 16))
        ev = ("d", q, n)
        self._record(ev, reads, writes)
        return ev

    def coll(self, kind, op, groups, in_ap, out_ap, reads=(), writes=()):
        q = "pool"
        self._deps(q, reads, writes)
        if not hasattr(self, "ccsems"):
            self.ccsems = []
        sem = self.stack.enter_context(self.nc.semaphore("cc%d" % len(self.ccsems)))
        self.ccsems.append(sem)
        self.ops[q].append(lambda h, sem=sem: h.collective_compute(kind, op, replica_groups=groups, ins=[in_ap.opt()], outs=[out_ap.opt()]).then_inc(sem))
        ev = ("x", len(self.ccsems) - 1, 0)
        self._record(ev, reads, writes)
        return ev

    def finish(self, final_res):
        for r in final_res:
            if r.w is not None:
                self._wait("sp", r.w)
        for q in self.ring:
            n = self.dcnt[q]
            for k in range(max(0, n - NRING), n):
                self._wait("sp", ("d", q, k))
        with self.nc.Block() as block:
            @block.tensor
            def _(h):
                for f in self.ops["pe"]:
                    f(h)

            @block.scalar
            def _(h):
                for f in self.ops["act"]:
                    f(h)

            @block.vector
            def _(h):
                for f in self.ops["dve"]:
                    f(h)

            @block.gpsimd
            def _(h):
                for f in self.ops["pool"]:
                    f(h)

            @block.sync
            def _(h):
                for f in self.ops["sp"]:
                    f(h)

    def mm(self, out, lhsT, rhs, start=True, stop=True, reads=(), writes=(), **kw):
        self.op("pe", lambda h: h.matmul(out, lhsT, rhs, start=start, stop=stop, **kw), reads, writes)

    def act(self, out, in_, func, reads=(), writes=(), **kw):
        self.op("act", lambda h: h.activation(out=out, in_=in_, func=func, **kw), reads, writes)


D = 1024
NB = 66
NCH = 132


def emit_globals(P, IN):
    G = {}
    ID = P.sb("ID", [128, 128]); rID = Res(); P.dma("sp", ID[:], IN["ident"], writes=[rID])
    ONES = P.sb("ONES", [128, 128]); rONES = Res()
    P.op("dve", lambda h: h.memset(ONES[:], 1.0), writes=[rONES])
    ONESB = P.sb("ONESB", [1, 128], BF16); rONESB = Res()
    P.op("dve", lambda h: h.memset(ONESB[:], 1.0), writes=[rONESB])
    PS = [P.ps("PS%d" % i, [128, 512]) for i in range(8)]; rPS = [Res() for _ in range(8)]
    G.update(ID=ID, rID=rID, ONES=ONES, rONES=rONES, ONESB=ONESB, rONESB=rONESB, PS=PS, rPS=rPS)
    return G


def emit_common(P, G, IN, l, stk):
    cvT = IN["cvT"]; adaw = IN["adaw1", l]; adabT = IN["adab1T", l]; g1T = IN["g1T", l]
    C = dict(G)
    PS, rPS = G["PS"], G["rPS"]
    MODF = P.sb("MODF", [128, 16, 2], stack=stk); rMODF = Res()
    SCALE1 = P.sb("SCALE1", [128, 8, 2], stack=stk); rSCALE1 = Res()
    G1 = P.sb("G1", [128, 8], stack=stk); rG1 = Res(); P.dma("sp", G1[:], g1T, writes=[rG1])
    ABT = P.sb("ABT", [128, 16], stack=stk); rABT = Res(); P.dma("sp", ABT[:], adabT, writes=[rABT])
    sub = ExitStack()
    CV = P.sb("CV", [128, 8, 2], stack=sub); rCV = Res(); P.dma("sp", CV[:], cvT, writes=[rCV])
    SCV = P.sb("SCV", [128, 8, 2], stack=sub); rSCV = Res()
    P.act(SCV[:], CV[:], AF.Silu, reads=[rCV], writes=[rSCV])
    AW = [P.sb("AW%d" % i, [128, 8, 512], stack=sub) for i in range(2)]; rAW = [Res(), Res()]
    for blk in range(4):
        aw, raw = AW[blk % 2], rAW[blk % 2]
        P.dma("sp", aw[:], adaw[:, blk * 512:(blk + 1) * 512].rearrange("(k p) f -> p k f", p=128), writes=[raw])
        ps, rps = PS[blk % 2], rPS[blk % 2]
        for fcl in range(4):
            for k in range(8):
                P.mm(ps[:, fcl * 2:fcl * 2 + 2], aw[:, k, fcl * 128:(fcl + 1) * 128], SCV[:, k, :],
                     start=(k == 0), stop=(k == 7), reads=[rSCV, raw], writes=[rps])
        for fcl in range(4):
            fcg = blk * 4 + fcl
            P.act(MODF[:, fcg, :], ps[:, fcl * 2:fcl * 2 + 2], AF.Identity, reads=[rps, rABT], writes=[rMODF],
                  bias=ABT[:, fcg:fcg + 1], scale=1.0)
    P.op("dve", lambda h: h.tensor_scalar(out=SCALE1[:], in0=MODF[:, 8:16, :], scalar1=1.0, scalar2=None, op0=ALU.add),
         reads=[rMODF], writes=[rSCALE1])
    P.op("dve", lambda h: h.tensor_tensor(out=SCALE1[:], in0=SCALE1[:], in1=G1[:].unsqueeze(2).to_broadcast([128, 8, 2]), op=ALU.mult),
         reads=[rSCALE1, rG1], writes=[rSCALE1])
    P.fence()
    sub.close()
    C.update(MODF=MODF, rMODF=rMODF, SCALE1=SCALE1, rSCALE1=rSCALE1)
    return C


def emit_frontend(P, C, xsrc, rsrc, consume, blocks, stk, tbanks=(0, 1)):
    PS, rPS = C["PS"], C["rPS"]
    XT = [P.sb("XT%d" % i, [128, D], stack=stk) for i in range(2)]; rXT = [Res(), Res()]
    XN = P.sb("XN", [128, D], stack=stk); rXN = Res()
    HX = [P.sb("HX%d" % i, [128, 8, 128], BF16, stack=stk) for i in range(2)]; rHX = [Res(), Res()]
    SM = P.sb("FSM", [128, 8], stack=stk); rSM = Res()
    for idx, n in enumerate(blocks):
        j = 1 if n < 2 else 0
        xt, rxt = XT[idx % 2], rXT[idx % 2]
        hx, rhx = HX[idx % 2], rHX[idx % 2]
        for (p0, np_, src) in xsrc(n):
            P.dma("sp", xt[p0:p0 + np_, :], src, reads=rsrc, writes=[rxt])
        P.op("dve", lambda h: h.memset(SM[:, 0:1], 0.0), writes=[rSM])
        P.act(XN[:], xt[:], AF.Square, reads=[rxt], writes=[rXN, rSM], accum_out=SM[:, 0:1])
        P.act(SM[:, 1:2], SM[:, 0:1], AF.Sqrt, reads=[rSM], writes=[rSM], bias=1e-6, scale=1.0 / D)
        P.op("dve", lambda h: h.reciprocal(out=SM[:, 2:3], in_=SM[:, 1:2]), reads=[rSM], writes=[rSM])
        P.op("dve", lambda h, xt=xt: h.tensor_scalar(out=XN[:], in0=xt[:], scalar1=SM[:, 2:3], scalar2=None, op0=ALU.mult),
             reads=[rxt, rSM], writes=[rXN])
        for half in range(2):
            ps, rps = PS[tbanks[half]], rPS[tbanks[half]]
            for k in range(4 * half, 4 * half + 4):
                P.op("pe", lambda h, ps=ps, k=k: h.transpose(ps[:, (k % 4) * 128:(k % 4 + 1) * 128], XN[:, k * 128:(k + 1) * 128], C["ID"][:]),
                     reads=[rXN, C["rID"]], writes=[rps])
            for k in range(4 * half, 4 * half + 4):
                P.act(hx[:, k, :], ps[:, (k % 4) * 128:(k % 4 + 1) * 128], AF.Identity, reads=[rps, C["rSCALE1"], C["rMODF"]], writes=[rhx],
                      scale=C["SCALE1"][:, k, j:j + 1], bias=C["MODF"][:, k, j:j + 1])
        consume(n, hx, rhx)


def emit_swa(P, C, IN, l, last, xsrc, rsrc, o_sw, rOUT):
    ws = IN["ws", l]; gqk = IN["gqk", l]; ropet = IN["ropet"]; sinkb = IN["sinkb", l]; maskw = IN["maskw"]
    with ExitStack() as st0:
        _sb = P.sb
        P_sb = lambda name, shape, dt=F32: _sb("sw_" + name, shape, dt, stack=st0)
        PS, rPS, ID, rID = C["PS"], C["rPS"], C["ID"], C["rID"]
        WS = P_sb("WS", [128, 8, 256], BF16); rWS = Res()
        P.dma("pool", WS[:], ws.rearrange("(k p) f -> p k f", p=128), writes=[rWS])
        GQK = P_sb("GQK", [128, 3, 64]); rGQK = Res(); P.dma("sp", GQK[:], gqk, writes=[rGQK])
        ROPE = P_sb("ROPE", [128, 64, 2, 2, 16]); rROPE = Res(); P.dma("sp", ROPE[:], ropet, writes=[rROPE])
        SINK = P_sb("SINK", [128, 2]); rSINK = Res(); P.dma("sp", SINK[:], sinkb, writes=[rSINK])
        MASKW = P_sb("MASKW", [128, 384]); rMASKW = Res(); P.dma("sp", MASKW[:], maskw, writes=[rMASKW])
        SQT = P_sb("SQT", [128, NB * 128], BF16); rSQT = [Res() for _ in range(NB)]
        SKT = P_sb("SKT", [128, NB * 128], BF16); rSKT = [Res() for _ in range(NB)]
        SV = P_sb("SV", [128, NB, 64], BF16); rSV = [Res() for _ in range(NB)]
        QK = P_sb("QK", [128, 3, 64]); rQK = Res()
        QKR = P_sb("QKR", [128, 4, 64]); rQKR = Res()
        SQ = P_sb("SQ", [128, 3, 64]); rSQ = Res()
        T1 = P_sb("T1", [128, 3, 2, 16]); rT1 = Res()
        T2 = P_sb("T2", [128, 3, 2, 16]); rT2 = Res()
        SM = P_sb("SM", [128, 16]); rSM = Res()
        S = P_sb("S", [128, 640]); rS = Res()
        E = P_sb("E", [128, 640]); rE = Res()
        ET = P_sb("ET", [128, 5, 128], BF16); rET = Res()
        OSW = [P_sb("OSW%d" % i, [128, 128]) for i in range(2)]; rOSW = [Res(), Res()]

        HL = []
        for h in range(2):
            Ld = dict(S=P_sb("S%d" % h, [128, 640]), rS=Res(), E=P_sb("E%d" % h, [128, 640]), rE=Res(),
                      ET=P_sb("ET%d" % h, [128, 5, 128], BF16), rET=Res(), SM=P_sb("SMh%d" % h, [128, 8]), rSM=Res(),
                      b0=PS[2 + 3 * h], r0=rPS[2 + 3 * h], b1=PS[3 + 3 * h], r1=rPS[3 + 3 * h], b2=PS[4 + 3 * h], r2=rPS[4 + 3 * h])
            HL.append(Ld)

        def att_gen(n, h, osw, rosw):
            Ld = HL[h]
            S_, rS_, E_, rE_, ET_, rET_, SM_, rSM_ = Ld["S"], Ld["rS"], Ld["E"], Ld["rE"], Ld["ET"], Ld["rET"], Ld["SM"], Ld["rSM"]
            b0, r0, b1, r1, b2, r2 = Ld["b0"], Ld["r0"], Ld["b1"], Ld["r1"], Ld["b2"], Ld["r2"]
            if n >= 2:
                lo, hi = max(2, n - 1), min(NB - 1, n + 1)
                nl = (hi - lo + 1) * 128
                m0 = (lo - (n - 1)) * 128
            else:
                lo, hi, nl, m0 = 0, -1, 0, 0
            ntot = nl + 256
            kblocks = list(range(lo, hi + 1)) + [0, 1]
            hs = slice(h * 64, (h + 1) * 64)
            if nl:
                P.mm(b0[:, 0:nl], SQT[hs, n * 128:(n + 1) * 128], SKT[hs, lo * 128:(hi + 1) * 128],
                     reads=[rSQT[n]] + [rSKT[b] for b in range(lo, hi + 1)], writes=[r0])
            P.mm(b1[:, 0:256], SQT[hs, n * 128:(n + 1) * 128], SKT[hs, 0:256], reads=[rSQT[n], rSKT[0], rSKT[1]], writes=[r1])
            yield
            if nl:
                P.op("dve", lambda hh: hh.scalar_tensor_tensor(out=S_[:, 0:nl], in0=b0[:, 0:nl], scalar=0.125,
                                                              in1=MASKW[:, m0:m0 + nl], op0=ALU.mult, op1=ALU.add),
                     reads=[r0, rMASKW], writes=[rS_])
            P.act(S_[:, nl:ntot], b1[:, 0:256], AF.Copy, reads=[r1], writes=[rS_], scale=0.125)
            yield
            P.op("dve", lambda hh: hh.reduce_max(out=SM_[:, 0:1], in_=S_[:, 0:ntot], axis=AX.X), reads=[rS_], writes=[rSM_])
            yield
            P.op("dve", lambda hh: hh.tensor_tensor(out=SM_[:, 1:2], in0=SM_[:, 0:1], in1=SINK[:, h:h + 1], op=ALU.max),
                 reads=[rSM_, rSINK], writes=[rSM_])
            yield
            P.op("dve", lambda hh: hh.tensor_scalar(out=SM_[:, 2:3], in0=SM_[:, 1:2], scalar1=-1.0, scalar2=None, op0=ALU.mult),
                 reads=[rSM_], writes=[rSM_])
            P.op("dve", lambda hh: hh.memset(SM_[:, 3:4], 0.0), writes=[rSM_])
            yield
            P.act(E_[:, 0:ntot], S_[:, 0:ntot], AF.Exp, reads=[rS_, rSM_], writes=[rE_, rSM_], bias=SM_[:, 2:3], scale=1.0, accum_out=SM_[:, 3:4])
            P.act(SM_[:, 4:5], SINK[:, h:h + 1], AF.Exp, reads=[rSINK, rSM_], writes=[rSM_], bias=SM_[:, 2:3], scale=1.0)
            yield
            P.op("dve", lambda hh: hh.tensor_tensor(out=SM_[:, 5:6], in0=SM_[:, 3:4], in1=SM_[:, 4:5], op=ALU.add), reads=[rSM_], writes=[rSM_])
            nk = ntot // 128
            n4 = min(nk, 4)
            for c in range(n4):
                P.op("pe", lambda hh, c=c: hh.transpose(b2[:, c * 128:(c + 1) * 128], E_[:, c * 128:(c + 1) * 128], ID[:]),
                     reads=[rE_, rID], writes=[r2])
            if nk > 4:
                P.op("pe", lambda hh: hh.transpose(b1[:, 384:512], E_[:, 512:640], ID[:]), reads=[rE_, rID], writes=[r1])
            yield
            P.op("dve", lambda hh: hh.reciprocal(out=SM_[:, 6:7], in_=SM_[:, 5:6]), reads=[rSM_], writes=[rSM_])
            P.act(ET_[:, 0:n4, :], b2[:, 0:n4 * 128], AF.Copy, reads=[r2], writes=[rET_])
            if nk > 4:
                P.act(ET_[:, 4, :], b1[:, 384:512], AF.Copy, reads=[r1], writes=[rET_])
            yield
            for c in range(nk):
                kb = kblocks[c]
                P.mm(b1[:, 256:320], ET_[:, c, :], SV[:, kb, :], start=(c == 0), stop=(c == nk - 1), reads=[rET_, rSV[kb]], writes=[r1])
            yield
            P.op("dve", lambda hh: hh.tensor_scalar(out=osw[:, h * 64:(h + 1) * 64], in0=b1[:, 256:320], scalar1=SM_[:, 6:7], scalar2=None, op0=ALU.mult),
                 reads=[r1, rSM_], writes=[rosw])
            yield

        def attention(n):
            osw, rosw = OSW[n % 2], rOSW[n % 2]
            gs = [att_gen(n, 0, osw, rosw), att_gen(n, 1, osw, rosw)]
            alive = [True, True]
            while any(alive):
                for i in range(2):
                    if alive[i]:
                        try:
                            next(gs[i])
                        except StopIteration:
                            alive[i] = False
            P.dma("sp", o_sw[n * 128:(n + 1) * 128, :], osw[:], reads=[rosw], writes=[rOUT[n]])

        def consume(n, hx, rhx):
            ps, rps = PS[1], rPS[1]
            for k in range(8):
                P.mm(ps[:, 0:256], hx[:, k, :], WS[:, k, :], start=(k == 0), stop=(k == 7), reads=[rhx, rWS], writes=[rps])
            P.act(QK[:], ps[:, 0:192], AF.Copy, reads=[rps], writes=[rQK])
            P.act(SV[:, n, :], ps[:, 192:256], AF.Copy, reads=[rps], writes=[rSV[n]])
            P.op("dve", lambda h: h.tensor_tensor(out=SQ[:], in0=QK[:], in1=QK[:], op=ALU.mult), reads=[rQK], writes=[rSQ])
            P.op("dve", lambda h: h.reduce_sum(out=SM[:, 8:11], in_=SQ[:], axis=AX.X), reads=[rSQ], writes=[rSM])
            P.act(SM[:, 11:14], SM[:, 8:11], AF.Sqrt, reads=[rSM], writes=[rSM], bias=1e-6, scale=1.0 / 64)
            P.op("dve", lambda h: h.reciprocal(out=SM[:, 8:11], in_=SM[:, 11:14]), reads=[rSM], writes=[rSM])
            P.op("dve", lambda h: h.tensor_tensor(out=QK[:], in0=QK[:], in1=SM[:, 8:11].unsqueeze(2).to_broadcast([128, 3, 64]), op=ALU.mult),
                 reads=[rQK, rSM], writes=[rQK])
            P.op("dve", lambda h: h.tensor_tensor(out=QK[:], in0=QK[:], in1=GQK[:], op=ALU.mult), reads=[rQK, rGQK], writes=[rQK])
            if n >= 2:
                bi = n - 2
                q5 = QK[:].rearrange("p s (a t f) -> p s a t f", a=2, t=2)
                o5 = QKR[:, 0:3, :].rearrange("p s (a t f) -> p s a t f", a=2, t=2)
                X1, X2 = q5[:, :, :, 0, :], q5[:, :, :, 1, :]
                Cc = ROPE[:, bi, 0, :, :].unsqueeze(1).to_broadcast([128, 3, 2, 16])
                Sn = ROPE[:, bi, 1, :, :].unsqueeze(1).to_broadcast([128, 3, 2, 16])
                tt = lambda out, a, b, op, reads, writes: P.op("dve", lambda h: h.tensor_tensor(out=out, in0=a, in1=b, op=op), reads=reads, writes=writes)
                tt(T1[:], X1, Cc, ALU.mult, [rQK, rROPE], [rT1])
                tt(T2[:], X2, Sn, ALU.mult, [rQK, rROPE], [rT2])
                tt(o5[:, :, :, 0, :], T1[:], T2[:], ALU.subtract, [rT1, rT2], [rQKR])
                tt(T1[:], X2, Cc, ALU.mult, [rQK, rROPE], [rT1])
                tt(T2[:], X1, Sn, ALU.mult, [rQK, rROPE], [rT2])
                tt(o5[:, :, :, 1, :], T1[:], T2[:], ALU.add, [rT1, rT2], [rQKR])
            else:
                P.op("dve", lambda h: h.tensor_copy(out=QKR[:, 0:3, :], in_=QK[:]), reads=[rQK], writes=[rQKR])
            P.op("dve", lambda h: h.tensor_copy(out=QKR[:, 3, :], in_=QKR[:, 2, :]), reads=[rQKR], writes=[rQKR])
            ps, rps = PS[1], rPS[1]
            P.op("pe", lambda h: h.transpose(ps[:, 256:384], QKR[:, 0:2, :].rearrange("p a f -> p (a f)"), ID[:]), reads=[rQKR, rID], writes=[rps])
            P.op("pe", lambda h: h.transpose(ps[:, 384:512], QKR[:, 2:4, :].rearrange("p a f -> p (a f)"), ID[:]), reads=[rQKR, rID], writes=[rps])
            P.act(SQT[:, n * 128:(n + 1) * 128], ps[:, 256:384], AF.Copy, reads=[rps], writes=[rSQT[n]])
            P.act(SKT[:, n * 128:(n + 1) * 128], ps[:, 384:512], AF.Copy, reads=[rps], writes=[rSKT[n]])
            if n == 1 and not last:
                attention(0); attention(1)
            if n >= 3:
                attention(n - 1)
            if n == NB - 1:
                attention(n)

        emit_frontend(P, C, xsrc, rsrc, consume, list(range(NB)), st0, tbanks=(0, 0))
        P.fence()


def emit_dn(P, C, IN, l, last, xsrc, rsrc, o_dn, rOUT):
    wa = IN["wa", l]; wz = IN["wz", l]; cw = IN["cw", l]; nega = IN["nega", l]; dtb = IN["dtb", l]; gdn = IN["gdn", l]
    blk1 = IN["blk1"]; masks = IN["masks"]
    NS = NCH + 1
    with ExitStack() as st0:
        _sb = P.sb
        P_sb = lambda name, shape, dt=F32: _sb("dn_" + name, shape, dt, stack=st0)
        PS, rPS, ID, rID, ONES, rONES = C["PS"], C["rPS"], C["ID"], C["rID"], C["ONES"], C["rONES"]
        WA = P_sb("WA", [128, 8, 384], BF16); rWA = Res(); P.dma("pool", WA[:], wa.rearrange("(k p) f -> p k f", p=128), writes=[rWA])
        WZ = P_sb("WZ", [128, 8, 2, 68], BF16); rWZ = Res(); P.dma("pool", WZ[:], wz.rearrange("(k p) h f -> p k h f", p=128), writes=[rWZ])
        CW = P_sb("CW", [128, 3, 5]); rCW = Res(); P.dma("sp", CW[:], cw, writes=[rCW])
        NEGA = P_sb("NEGA", [128, 2]); rNEGA = Res(); P.dma("sp", NEGA[:], nega, writes=[rNEGA])
        DTB = P_sb("DTB", [128, 2]); rDTB = Res(); P.dma("sp", DTB[:], dtb, writes=[rDTB])
        GDN = P_sb("GDN", [128, 64]); rGDN = Res(); P.dma("sp", GDN[:], gdn, writes=[rGDN])
        BLK = P_sb("BLK", [128, 128]); rBLK = Res(); P.dma("sp", BLK[:], blk1, writes=[rBLK])
        MSK = P_sb("MSK", [128, 6, 2, 64]); rMSK = Res(); P.dma("sp", MSK[:], masks, writes=[rMSK])
        TRI, NEGMT, NEGM, NSTT, NST, ID2 = [MSK[:, i, :, :] for i in range(6)]
        ONES3 = P_sb("ONES3", [128, 2, 64]); rONES3 = Res()
        P.op("dve", lambda h: h.memset(ONES3[:], 1.0), writes=[rONES3])
        QT = P_sb("QT", [128, NCH * 64], BF16); KT = P_sb("KT", [128, NCH * 64], BF16)
        KTM = P_sb("KTM", [128, NCH, 64], BF16); VTM = P_sb("VTM", [128, NCH, 64], BF16)
        SZ = P_sb("SZ", [128, NCH, 64], BF16)
        Gs = P_sb("Gs", [128, NS, 2]); Bs = P_sb("Bs", [128, NS, 2])
        O = P_sb("O", [128, NCH, 64])
        rCH = [Res() for _ in range(NCH)]
        rO = [Res() for _ in range(NCH)]
        P.op("dve", lambda h: h.memset(O[:], 0.0), writes=rO)
        CB = P_sb("CB", [128, 3, 260]); rCB = Res()
        CVb = P_sb("CVb", [128, 3, 128]); rCVb = Res()
        SQ2 = P_sb("SQ2", [128, 2, 128]); rSQ2 = Res()
        RS2 = P_sb("RS2", [128, 2, 128]); rRS2 = Res()
        KN = P_sb("KN", [128, 128]); rKN = Res()
        GT_ = P_sb("GT_", [128, 2, 2, 8]); rGT_ = Res()
        P.op("dve", lambda h: h.memset(CB[:], 0.0), writes=[rCB])

        def step_of(c, d):
            if d == 0:
                return c
            return 4 - c if c < 4 else 136 - c

        def conv_block(m):
            for s in range(3):
                eng = "dve"
                P.op(eng, lambda h, s=s: h.tensor_scalar(out=CVb[:, s, :], in0=CB[:, s, 0:128], scalar1=CW[:, s, 0:1], scalar2=None, op0=ALU.mult),
                     reads=[rCB, rCW], writes=[rCVb])
                for tap in range(1, 5):
                    P.op(eng, lambda h, s=s, tap=tap: h.scalar_tensor_tensor(out=CVb[:, s, :], in0=CB[:, s, tap:tap + 128], scalar=CW[:, s, tap:tap + 1],
                                                                            in1=CVb[:, s, :], op0=ALU.mult, op1=ALU.add),
                         reads=[rCB, rCW, rCVb], writes=[rCVb])
            P.act(CVb[:], CVb[:], AF.Silu, reads=[rCVb], writes=[rCVb])
            P.op("dve", lambda h: h.tensor_tensor(out=SQ2[:], in0=CVb[:, 0:2, :], in1=CVb[:, 0:2, :], op=ALU.mult), reads=[rCVb], writes=[rSQ2])
            ps, rps = PS[2], rPS[2]
            P.mm(ps[:, 0:256], BLK[:], SQ2[:].rearrange("p a t -> p (a t)"), reads=[rBLK, rSQ2], writes=[rps])
            P.act(RS2[:].rearrange("p a t -> p (a t)"), ps[:, 0:256], AF.Sqrt, reads=[rps], writes=[rRS2], bias=1e-6, scale=1.0)
            P.op("dve", lambda h: h.reciprocal(out=RS2[:], in_=RS2[:]), reads=[rRS2], writes=[rRS2])
            rc = [rCH[2 * m], rCH[2 * m + 1]]
            P.op("dve", lambda h: h.scalar_tensor_tensor(out=QT[:, m * 128:(m + 1) * 128], in0=CVb[:, 0, :], scalar=0.125, in1=RS2[:, 0, :], op0=ALU.mult, op1=ALU.mult),
                 reads=[rCVb, rRS2], writes=rc)
            P.op("dve", lambda h: h.tensor_tensor(out=KN[:], in0=CVb[:, 1, :], in1=RS2[:, 1, :], op=ALU.mult), reads=[rCVb, rRS2], writes=[rKN])
            P.op("dve", lambda h: h.tensor_copy(out=KT[:, m * 128:(m + 1) * 128], in_=KN[:]), reads=[rKN], writes=rc)
            ps, rps = PS[3], rPS[3]
            for which, src in ((0, KN), (1, None)):
                for cc in range(2):
                    for hh in range(2):
                        hs = slice(hh * 64, (hh + 1) * 64)
                        srcap = KN[hs, cc * 64:(cc + 1) * 64] if which == 0 else CVb[hs, 2, cc * 64:(cc + 1) * 64]
                        P.op("pe", lambda h, hs=hs, cc=cc, which=which, srcap=srcap: h.matmul(ps[hs, which * 128 + cc * 64: which * 128 + (cc + 1) * 64], srcap, ID[hs, hs], start=True, stop=True),
                             reads=[rKN, rCVb, rID], writes=[rps])
            P.act(KTM[:, 2 * m:2 * m + 2, :], ps[:, 0:128].rearrange("p (c f) -> p c f", c=2), AF.Copy, reads=[rps], writes=rc)
            P.act(VTM[:, 2 * m:2 * m + 2, :], ps[:, 128:256].rearrange("p (c f) -> p c f", c=2), AF.Copy, reads=[rps], writes=rc)

        def consume(n, hx, rhx):
            first = n in (0, 2)
            if first:
                P.op("dve", lambda h: h.memset(CB[:, :, 0:2], 0.0), writes=[rCB])
            ps, rps = PS[2], rPS[2]
            for s in range(3):
                for k in range(8):
                    P.mm(ps[:, s * 128:(s + 1) * 128], WA[:, k, s * 128:(s + 1) * 128], hx[:, k, :], start=(k == 0), stop=(k == 7), reads=[rWA, rhx], writes=[rps])
            dst = 2 if first else 130
            P.act(CB[:, :, dst:dst + 128], ps[:, 0:384].rearrange("p (s t) -> p s t", s=3), AF.Copy, reads=[rps], writes=[rCB])
            if not first:
                conv_block(n - 1)
                P.op("dve", lambda h: h.tensor_copy(out=CB[:, :, 0:2], in_=CB[:, :, 128:130]), reads=[rCB], writes=[rCB])
                P.op("dve", lambda h: h.tensor_copy(out=CB[:, :, 2:130], in_=CB[:, :, 130:258]), reads=[rCB], writes=[rCB])
            if n in (1, NB - 1):
                P.op("dve", lambda h: h.memset(CB[:, :, 130:132], 0.0), writes=[rCB])
                conv_block(n)
            ps, rps = PS[4], rPS[4]
            for cc in range(2):
                for hh in range(2):
                    hs = slice(hh * 64, (hh + 1) * 64)
                    for k in range(8):
                        P.mm(ps[hs, cc * 68:(cc + 1) * 68], hx[:, k, cc * 64:(cc + 1) * 64], WZ[:, k, hh, :], start=(k == 0), stop=(k == 7),
                             reads=[rhx, rWZ], writes=[rps])
            rc = [rCH[2 * n], rCH[2 * n + 1]]
            pz = ps[:, 0:136].rearrange("p (c f) -> p c f", c=2)
            P.act(SZ[:, 2 * n:2 * n + 2, :], pz[:, :, 0:64], AF.Silu, reads=[rps], writes=rc)
            xa = GT_[:, :, :, 0]; ab = GT_[:, :, :, 1]; ee = GT_[:, :, :, 2]; ll = GT_[:, :, :, 3]; rr = GT_[:, :, :, 4]; gg = GT_[:, :, :, 5]; bb = GT_[:, :, :, 6]
            P.op("dve", lambda h: h.tensor_tensor(out=xa, in0=pz[:, :, 64:66], in1=DTB[:].unsqueeze(1).to_broadcast([128, 2, 2]), op=ALU.add), reads=[rps, rDTB], writes=[rGT_])
            P.act(ab, xa, AF.Abs, reads=[rGT_], writes=[rGT_])
            P.act(ee, ab, AF.Exp, reads=[rGT_], writes=[rGT_], scale=-1.0)
            P.act(ll, ee, AF.Ln, reads=[rGT_], writes=[rGT_], bias=1.0, scale=1.0)
            P.op("dve", lambda h: h.tensor_scalar(out=rr, in0=xa, scalar1=0.0, scalar2=None, op0=ALU.max), reads=[rGT_], writes=[rGT_])
            P.op("dve", lambda h: h.tensor_tensor(out=rr, in0=rr, in1=ll, op=ALU.add), reads=[rGT_], writes=[rGT_])
            P.op("dve", lambda h: h.tensor_tensor(out=gg, in0=rr, in1=NEGA[:].unsqueeze(1).to_broadcast([128, 2, 2]), op=ALU.mult), reads=[rGT_, rNEGA], writes=[rGT_])
            P.act(bb, pz[:, :, 66:68], AF.Sigmoid, reads=[rps], writes=[rGT_])
            for cc in range(2):
                c = 2 * n + cc
                for d in range(2):
                    s_ = step_of(c, d)
                    P.op("dve", lambda h, cc=cc, d=d, s_=s_: h.tensor_copy(out=Gs[:, s_, d:d + 1], in_=GT_[:, cc, d, 5:6]), reads=[rGT_], writes=[rCH[c]])
                    P.op("dve", lambda h, cc=cc, d=d, s_=s_: h.tensor_copy(out=Bs[:, s_, d:d + 1], in_=GT_[:, cc, d, 6:7]), reads=[rGT_], writes=[rCH[c]])

        emit_frontend(P, C, xsrc, rsrc, consume, list(range(NB)), st0)

        Sst = [(P_sb("S0", [128, 2, 64]), Res()), (P_sb("S1", [128, 2, 64]), Res())]
        for (t, r) in Sst:
            P.op("dve", lambda h, t=t: h.memset(t[:], 0.0), writes=[r])
        HS = [slice(0, 64), slice(64, 128)]
        dve = lambda fn, reads, writes: P.op("dve", fn, reads, writes)

        def make_lane(li):
            L = {}
            for nm in ("GBC", "BBC", "EB", "DT1", "TA", "DECT", "DEC", "DECS", "DECTS", "VB", "KBE", "KD", "QGT", "AQM", "NWT", "VN", "SGL", "C0", "C1"):
                L[nm] = (P_sb("%s_%d" % (nm, li), [128, 2, 64]), Res())
            L["W0"] = (P_sb("W0_%d" % li, [128, 2, 128]), Res()); L["W1"] = (P_sb("W1_%d" % li, [128, 2, 128]), Res())
            L["SC"] = (P_sb("SC_%d" % li, [128, 8]), Res())
            a, b, c = PS[3 * li], PS[3 * li + 1], PS[3 * li + 2]
            v = lambda bank, lo, n: bank[:, lo:lo + 2 * n].rearrange("p (d f) -> p d f", d=2)
            ra, rb, rc = rPS[3 * li], rPS[3 * li + 1], rPS[3 * li + 2]
            L["ps1"] = (v(a, 0, 64), ra); L["ps4"] = (v(a, 128, 64), ra); L["ps2"] = (v(a, 256, 64), ra); L["ps3"] = (v(a, 384, 64), ra)
            L["psI"] = (v(b, 0, 128), rb); L["psC"] = (v(b, 256, 64), rb); L["pcol"] = (b[:, 384:386], rb)
            L["ps5"] = (v(c, 0, 64), rc); L["pswT"] = (v(c, 128, 64), rc); L["ps6"] = (v(c, 256, 64), rc); L["ps7"] = (v(c, 384, 64), rc)
            return L

        lanes = [make_lane(0), make_lane(1)]

        def step_gen(s, L):
            GBC, rGBC = L["GBC"]; BBC, rBBC = L["BBC"]; EB, rEB = L["EB"]; DT1, rDT1 = L["DT1"]; TA, rTA = L["TA"]
            DECT, rDECT = L["DECT"]; DEC, rDEC = L["DEC"]; DECS, rDECS = L["DECS"]; DECTS, rDECTS = L["DECTS"]
            VB, rVB = L["VB"]; KBE, rKBE = L["KBE"]; KD, rKD = L["KD"]; QGT, rQGT = L["QGT"]; AQM, rAQM = L["AQM"]
            NWT, rNWT = L["NWT"]; VN, rVN = L["VN"]; SGL, rSGL = L["SGL"]; SC, rSC = L["SC"]
            Cb = [L["C0"], L["C1"]]; Wb = [L["W0"], L["W1"]]
            ps1, r1 = L["ps1"]; ps4, r4 = L["ps4"]; ps2, r2 = L["ps2"]; ps3, r3 = L["ps3"]
            psI, rI = L["psI"]; psC, rC = L["psC"]; pcol, rcol = L["pcol"]
            ps5, r5 = L["ps5"]; pswT, rwT = L["pswT"]; ps6, r6 = L["ps6"]; ps7, r7 = L["ps7"]
            dirs = [d for d in range(2) if (d == 0 and s <= NCH - 1) or (d == 1 and s >= 1)]
            ch = {0: s, 1: (4 - s if s <= 4 else 136 - s)}
            d0, d1 = dirs[0], dirs[-1] + 1
            ds = slice(d0, d1)
            nd = d1 - d0
            rch = [rCH[ch[d]] for d in dirs]
            Scur, rScur = Sst[s % 2]; Snew, rSnew = Sst[(s + 1) % 2]
            tok = {d: slice(ch[d] * 64, ch[d] * 64 + 64) for d in dirs}
            dve(lambda h: h.tensor_tensor(out=GBC[:, ds, :], in0=ONES3[:, ds, :], in1=Gs[:, s, ds].unsqueeze(2).to_broadcast([128, nd, 64]), op=ALU.mult), [rONES3] + rch, [rGBC])
            dve(lambda h: h.tensor_tensor(out=BBC[:, ds, :], in0=ONES3[:, ds, :], in1=Bs[:, s, ds].unsqueeze(2).to_broadcast([128, nd, 64]), op=ALU.mult), [rONES3] + rch, [rBBC])
            yield
            for d in dirs:
                for hs in HS:
                    P.mm(ps1[hs, d, :], GBC[hs, d, :], TRI[hs, d, :], reads=[rGBC, rMSK], writes=[r1])
            for d in dirs:
                for hs in HS:
                    P.mm(pcol[hs, d:d + 1], TRI[hs, d, :], Gs[hs, s, d:d + 1], reads=[rMSK] + rch, writes=[rcol])
            for d in dirs:
                for hs in HS:
                    P.mm(ps4[hs, d, :], BBC[hs, d, :], ID2[hs, d, :], reads=[rBBC, rMSK], writes=[r4])
            for d in dirs:
                for hs in HS:
                    P.mm(ps2[hs, d, :], KT[hs, tok[d]], KT[hs, tok[d]], reads=rch, writes=[r2])
            for d in dirs:
                for hs in HS:
                    P.mm(ps3[hs, d, :], KT[hs, tok[d]], QT[hs, tok[d]], reads=rch, writes=[r3])
            yield
            dve(lambda h: h.tensor_copy(out=SC[:, 0:2][:, ds], in_=pcol[:, ds]), [rcol], [rSC])
            P.act(EB[:, ds, :], ps1[:, ds, :], AF.Exp, reads=[r1], writes=[rEB])
            yield
            P.act(SC[:, 2:4][:, ds], SC[:, 0:2][:, ds], AF.Exp, reads=[rSC], writes=[rSC])
            dve(lambda h: h.tensor_tensor(out=DT1[:, ds, :], in0=ps1[:, ds, :], in1=SC[:, 0:2][:, ds].unsqueeze(2).to_broadcast([128, nd, 64]), op=ALU.subtract), [r1, rSC], [rDT1])
            yield
            dve(lambda h: h.tensor_tensor(out=TA[:, ds, :], in0=DT1[:, ds, :], in1=NEGMT[:, ds, :], op=ALU.add), [rDT1, rMSK], [rTA])
            yield
            P.act(DECT[:, ds, :], TA[:, ds, :], AF.Exp, reads=[rTA], writes=[rDECT])
            yield
            dve(lambda h: h.scalar_tensor_tensor(out=TA[:, ds, :], in0=DT1[:, ds, :], scalar=-1.0, in1=NEGM[:, ds, :], op0=ALU.mult, op1=ALU.add), [rDT1, rMSK, rDECT], [rTA])
            yield
            P.act(DEC[:, ds, :], TA[:, ds, :], AF.Exp, reads=[rTA], writes=[rDEC])
            for d in dirs:
                lastc = 63 if d == 0 else 0
                dve(lambda h, d=d, lastc=lastc: h.tensor_tensor(out=SC[:, 4 + d:5 + d], in0=ps1[:, d, lastc:lastc + 1], in1=SC[:, d:d + 1], op=ALU.subtract), [r1, rSC], [rSC])
            yield
            P.act(SC[:, 4:6][:, ds], SC[:, 4:6][:, ds], AF.Exp, reads=[rSC], writes=[rSC])
            C0, rC0 = Cb[0]; W0, rW0 = Wb[0]
            dve(lambda h: h.tensor_tensor(out=DECS[:, ds, :], in0=DEC[:, ds, :], in1=NST[:, ds, :], op=ALU.mult), [rDEC, rMSK], [rDECS])
            yield
            for d in dirs:
                dve(lambda h, d=d: h.scalar_tensor_tensor(out=C0[:, d, :], in0=ps2[:, d, :], scalar=Bs[:, s, d:d + 1], in1=DECS[:, d, :], op0=ALU.mult, op1=ALU.mult),
                    [r2, rDECS] + rch, [rC0])
            yield
            dve(lambda h: h.tensor_tensor(out=DECTS[:, ds, :], in0=DECT[:, ds, :], in1=NSTT[:, ds, :], op=ALU.mult), [rDECT, rMSK], [rDECTS])
            yield
            dve(lambda h: h.tensor_tensor(out=DECTS[:, ds, :], in0=ps2[:, ds, :], in1=DECTS[:, ds, :], op=ALU.mult), [r2, rDECTS], [rDECTS])
            yield
            dve(lambda h: h.tensor_tensor(out=W0[:, ds, 0:64], in0=ps4[:, ds, :], in1=DECTS[:, ds, :], op=ALU.mult), [r4, rDECTS], [rW0])
            dve(lambda h: h.tensor_copy(out=W0[:, ds, 64:128], in_=ID2[:, ds, :]), [rMSK], [rW0])
            yield
            for m in range(6):
                (Wc, rWc), (Wn, rWn) = Wb[m % 2], Wb[(m + 1) % 2]
                (Cc, rCc), (Cn, rCn) = Cb[m % 2], Cb[(m + 1) % 2]
                for d in dirs:
                    for hs in HS:
                        P.mm(psI[hs, d, :], Cc[hs, d, :], Wc[hs, d, :], reads=[rCc, rWc], writes=[rI])
                if m < 5:
                    for d in dirs:
                        for hs in HS:
                            P.mm(psC[hs, d, :], Wc[hs, d, 0:64], Cc[hs, d, :], reads=[rCc, rWc], writes=[rC])
                yield
                dve(lambda h, Wn=Wn, Wc=Wc: h.tensor_tensor(out=Wn[:, ds, 64:128], in0=Wc[:, ds, 64:128], in1=psI[:, ds, 64:128], op=ALU.add), [rWc, rI], [rWn])
                if m < 5:
                    P.act(Wn[:, ds, 0:64], psI[:, ds, 0:64], AF.Copy, reads=[rI], writes=[rWn])
                    P.act(Cn[:, ds, :], psC[:, ds, :], AF.Copy, reads=[rC], writes=[rCn])
                yield
                if m == 2:
                    yield "HALF"
            TITt, rTIT = Wb[0]
            TIT = TITt[:, :, 64:128]
            for d in dirs:
                c = ch[d]
                dve(lambda h, d=d, c=c: h.tensor_scalar(out=VB[:, d, :], in0=VTM[:, c, :], scalar1=Bs[:, s, d:d + 1], scalar2=None, op0=ALU.mult), rch, [rVB])
                dve(lambda h, d=d, c=c: h.tensor_scalar(out=KBE[:, d, :], in0=KTM[:, c, :], scalar1=Bs[:, s, d:d + 1], scalar2=SC[:, 2 + d:3 + d], op0=ALU.mult, op1=ALU.mult), rch + [rSC], [rKBE])
                yield
                dve(lambda h, d=d, c=c: h.tensor_scalar(out=KD[:, d, :], in0=KTM[:, c, :], scalar1=SC[:, 4 + d:5 + d], scalar2=None, op0=ALU.mult), rch + [rSC], [rKD])
                dve(lambda h, d=d: h.tensor_tensor(out=QGT[:, d, :], in0=QT[:, tok[d]], in1=EB[:, d, :], op=ALU.mult), rch + [rEB], [rQGT])
                yield
            dve(lambda h: h.tensor_tensor(out=AQM[:, ds, :], in0=ps3[:, ds, :], in1=DECT[:, ds, :], op=ALU.mult), [r3, rDECT], [rAQM])
            for d in dirs:
                for hs in HS:
                    P.mm(pswT[hs, d, :], KBE[hs, d, :], TIT[hs, d, :], reads=[rKBE, rTIT], writes=[rwT])
            yield
            dve(lambda h: h.tensor_scalar(out=NWT[:, ds, :], in0=pswT[:, ds, :], scalar1=-1.0, scalar2=None, op0=ALU.mult), [rwT], [rNWT])
            yield
            for d in dirs:
                for hs in HS:
                    P.mm(ps5[hs, d, :], TIT[hs, d, :], VB[hs, d, :], start=True, stop=False, reads=[rTIT, rVB], writes=[r5])
                    P.mm(ps5[hs, d, :], NWT[hs, d, :], Scur[hs, d, :], start=False, stop=True, reads=[rNWT, rScur], writes=[r5])
            yield
            dve(lambda h: h.tensor_copy(out=VN[:, ds, :], in_=ps5[:, ds, :]), [r5], [rVN])
            yield
            for d in dirs:
                for hs in HS:
                    P.mm(ps6[hs, d, :], QGT[hs, d, :], Scur[hs, d, :], start=True, stop=False, reads=[rQGT, rScur], writes=[r6])
                    P.mm(ps6[hs, d, :], AQM[hs, d, :], VN[hs, d, :], start=False, stop=True, reads=[rAQM, rVN], writes=[r6])
            for d in dirs:
                for hs in HS:
                    P.mm(ps7[hs, d, :], KD[hs, d, :], VN[hs, d, :], reads=[rKD, rVN], writes=[r7])
            yield
            for d in range(2):
                if d in dirs:
                    lastc = 63 if d == 0 else 0
                    dve(lambda h, d=d, lastc=lastc: h.tensor_scalar(out=SGL[:, d, :], in0=Scur[:, d, :], scalar1=EB[:, d, lastc:lastc + 1], scalar2=None, op0=ALU.mult), [rScur, rEB], [rSGL])
                    dve(lambda h, d=d: h.tensor_tensor(out=Snew[:, d, :], in0=SGL[:, d, :], in1=ps7[:, d, :], op=ALU.add), [rSGL, r7], [rSnew])
                else:
                    dve(lambda h, d=d: h.tensor_copy(out=Snew[:, d, :], in_=Scur[:, d, :]), [rScur], [rSnew])
            yield
            for d in dirs:
                c = ch[d]
                dve(lambda h, d=d, c=c: h.tensor_tensor(out=O[:, c, :], in0=O[:, c, :], in1=ps6[:, d, :], op=ALU.add), [r6, rO[c]], [rO[c]])
            yield

        def drive(older, newer):
            a_done = older is None
            b_done = newer is None
            while not (a_done and b_done):
                if not a_done:
                    try:
                        next(older)
                    except StopIteration:
                        a_done = True
                if not b_done:
                    try:
                        if next(newer) == "HALF":
                            b_done = True
                    except StopIteration:
                        b_done = True

        prev = None
        for s in range(NS):
            g = step_gen(s, lanes[s % 2])
            drive(prev, g)
            prev = g
        drive(prev, None)

        P.fence()
        G = 33
        OSQ = P_sb("OSQ", [128, G, 64]); rOSQ = Res()
        OSS = P_sb("OSS", [128, G]); rOSS = Res()
        for g in range(NCH // G):
            cs = slice(g * G, (g + 1) * G)
            ro = [rO[c] for c in range(g * G, (g + 1) * G)]
            dve(lambda h, cs=cs: h.tensor_tensor(out=OSQ[:], in0=O[:, cs, :], in1=O[:, cs, :], op=ALU.mult), ro, [rOSQ])
            dve(lambda h: h.reduce_sum(out=OSS[:], in_=OSQ[:], axis=AX.X), [rOSQ], [rOSS])
            P.act(OSS[:], OSS[:], AF.Sqrt, reads=[rOSS], writes=[rOSS], bias=1e-6, scale=1.0 / 64)
            dve(lambda h: h.reciprocal(out=OSS[:], in_=OSS[:]), [rOSS], [rOSS])
            dve(lambda h, cs=cs: h.tensor_tensor(out=O[:, cs, :], in0=O[:, cs, :], in1=OSS[:].unsqueeze(2).to_broadcast([128, G, 64]), op=ALU.mult), ro + [rOSS], ro)
            dve(lambda h, cs=cs: h.tensor_tensor(out=O[:, cs, :], in0=O[:, cs, :], in1=GDN[:].unsqueeze(1).to_broadcast([128, G, 64]), op=ALU.mult), ro + [rGDN], ro)
            dve(lambda h, cs=cs: h.tensor_tensor(out=O[:, cs, :], in0=O[:, cs, :], in1=SZ[:, cs, :], op=ALU.mult), ro + [rCH[c] for c in range(g * G, (g + 1) * G)], ro)
        ov = o_dn.rearrange("(c t) (h d) -> h t c d", t=64, h=2)
        for hh in range(2):
            P.dma("sp", ov[hh], O[hh * 64:(hh + 1) * 64, :, :], reads=rO, writes=[rOUT[hh]])
        P.fence()


NE = 32


def emit_b(P, G, IN, l, NL, NC, xs, rxs, omall, romall, xo, rOUT):
    NT = NL + NC
    cvT = IN["cvT"]; adaw = IN["adaw2", l]; adab = IN["adab2", l]; adabT = IN["adab2T", l]
    g2T = IN["g2T", l]; wout = IN["wout", l]; rw = IN["rw", l]; rb = IN["rb", l]
    wgu = IN["wgu", l]; bguT = IN["bguT", l]; wd = IN["wd", l]; bd = IN["bd", l]; seli = IN["seli"]

    tiles = [(i * 128, 128, 0) for i in range(NL // 128)]
    if NC:
        tiles.append((NL, NC, 1))
    nlt = NL // 128
    passes = [list(range(0, nlt // 2)), list(range(nlt // 2, len(tiles)))]
    MAXT = max(len(p) for p in passes)

    with ExitStack() as st0:
        _sb = P.sb
        P_sb = lambda name, shape, dt=F32, stack=None: _sb("b_" + name, shape, dt, stack=(stack or st0))
        ID, rID, ONES, rONES, ONESB, rONESB, PS, rPS = G["ID"], G["rID"], G["ONES"], G["rONES"], G["ONESB"], G["rONESB"], G["PS"], G["rPS"]
        SELI = P_sb("SELI", [128, 4, 128], BF16); rSELI = Res()
        P.dma("pool", SELI[:], seli, writes=[rSELI])

        GT = P_sb("GT", [128, 2, 2, D]); rGT = Res()
        MODF = P_sb("MODF", [128, 16, 2]); rMODF = Res()
        SCALE2 = P_sb("SCALE2", [128, 8, 2]); rSCALE2 = Res()
        G2 = P_sb("G2", [128, 8]); rG2 = Res()
        P.dma("sp", G2[:], g2T, writes=[rG2])
        ABT = P_sb("ABT", [128, 16]); rABT = Res()
        P.dma("sp", ABT[:], adabT, writes=[rABT])
        RW = P_sb("RW", [128, 8, NE]); rRW = Res()
        P.dma("sp", RW[:], rw.rearrange("(k p) e -> p k e", p=128), writes=[rRW])
        RB = P_sb("RB", [1, NE]); rRB = Res()
        P.dma("sp", RB[:], rb, writes=[rRB])
        WO = P_sb("WO", [128, 8, D], BF16); rWO = Res()
        P.dma("pool", WO[:], wout.rearrange("(k p) f -> p k f", p=128), writes=[rWO])
        sub = ExitStack()
        ABR = P_sb("ABR", [1, 4096], stack=sub); rABR = Res()
        P.dma("sp", ABR[:], adab, writes=[rABR])
        CV = P_sb("CV", [128, 8, 2], stack=sub); rCV = Res()
        P.dma("sp", CV[:], cvT, writes=[rCV])
        SCV = P_sb("SCV", [128, 8, 2], stack=sub); rSCV = Res()
        P.act(SCV[:], CV[:], AF.Silu, reads=[rCV], writes=[rSCV])
        SCB = P_sb("SCB", [128, 8, 2, 128], stack=sub); rSCB = Res()
        P.op("dve", lambda h: h.tensor_tensor(out=SCB[:], in0=ONES[:].unsqueeze(1).unsqueeze(1).to_broadcast([128, 8, 2, 128]),
                                              in1=SCV[:].unsqueeze(3).to_broadcast([128, 8, 2, 128]), op=ALU.mult),
             reads=[rONES, rSCV], writes=[rSCB])

        AW = [P_sb("AW%d" % i, [128, 8, 512], stack=sub) for i in range(2)]; rAW = [Res(), Res()]
        for blk in range(8):
            aw, raw = AW[blk % 2], rAW[blk % 2]
            P.dma("sp", aw[:], adaw[:, blk * 512:(blk + 1) * 512].rearrange("(k p) f -> p k f", p=128), writes=[raw])
            if blk in (0, 1, 6, 7):
                which = 0 if blk < 2 else 1
                half = blk % 2
                for j in range(2):
                    ps, rps = PS[j], rPS[j]
                    for k in range(8):
                        P.mm(ps[:], SCB[:, k, j, :], aw[:, k, :], start=(k == 0), stop=False,
                             reads=[rSCB, raw], writes=[rps])
                    P.mm(ps[:], ONES[0:1, :], ABR[0:1, blk * 512:(blk + 1) * 512], start=False, stop=True,
                         reads=[rONES, rABR], writes=[rps])
                    P.act(GT[:, which, j, half * 512:(half + 1) * 512], ps[:], AF.Copy, reads=[rps], writes=[rGT])
            else:
                ps, rps = PS[2 + blk % 2], rPS[2 + blk % 2]
                for fcl in range(4):
                    fcg = (blk - 2) * 4 + fcl
                    for k in range(8):
                        P.mm(ps[:, fcl * 2:fcl * 2 + 2], aw[:, k, fcl * 128:(fcl + 1) * 128], SCV[:, k, :],
                             start=(k == 0), stop=(k == 7), reads=[rSCV, raw], writes=[rps])
                for fcl in range(4):
                    fcg = (blk - 2) * 4 + fcl
                    P.act(MODF[:, fcg, :], ps[:, fcl * 2:fcl * 2 + 2], AF.Identity, reads=[rps, rABT], writes=[rMODF],
                          bias=ABT[:, fcg:fcg + 1], scale=1.0)
        P.op("dve", lambda h: h.tensor_scalar(out=SCALE2[:], in0=MODF[:, 8:16, :], scalar1=1.0, scalar2=None, op0=ALU.add),
             reads=[rMODF], writes=[rSCALE2])
        P.op("dve", lambda h: h.tensor_tensor(out=SCALE2[:], in0=SCALE2[:], in1=G2[:].unsqueeze(2).to_broadcast([128, 8, 2]), op=ALU.mult),
             reads=[rSCALE2, rG2], writes=[rSCALE2])

        P.fence()
        sub.close()
        X1 = P_sb("X1", [128, MAXT, D]); rX1 = [Res() for _ in range(MAXT)]
        H2B = P_sb("H2B", [128, 8, MAXT * 128], BF16); rH2B = [Res() for _ in range(MAXT)]
        GATES = P_sb("GATES", [128, MAXT, NE]); rGATES = [Res() for _ in range(MAXT)]
        XT = [P_sb("XT%d" % i, [128, D]) for i in range(2)]; rXT = [Res(), Res()]
        OM = [P_sb("OM%d" % i, [128, 8, 128], BF16) for i in range(2)]; rOM = [Res(), Res()]
        OMX = P_sb("OMX", [128, 4, 4, 256], BF16); rOMX = Res()
        TMP = [P_sb("TMP%d" % i, [128, D]) for i in range(2)]; rTMP = [Res(), Res()]
        H2F = P_sb("H2F", [128, 8, 128]); rH2F = Res()
        SMALL = P_sb("SMALL", [128, 64]); rSM = Res()
        LG = P_sb("LG", [128, NE]); rLG = Res()
        EX = P_sb("EX", [128, NE]); rEX = Res()
        MK = P_sb("MK", [128, NE]); rMK = Res()
        WGU = P_sb("WGU", [128, 8, 2 * D], BF16); rWG = [Res() for _ in range(8)]; rWU = [Res() for _ in range(8)]
        WD = P_sb("WD", [128, 8, D], BF16); rWD = [Res() for _ in range(8)]
        BGU = [P_sb("BGU%d" % i, [128, 16]) for i in range(2)]; rBGU = [Res(), Res()]
        BD = [P_sb("BD%d" % i, [1, D], BF16) for i in range(2)]; rBD = [Res(), Res()]
        ACTT = [P_sb("ACTT%d" % i, [128, 8, 512], BF16) for i in range(2)]; rACTT = [Res(), Res()]
        GP = [P_sb("GP%d" % i, [128, 512]) for i in range(2)]; rGP = [Res(), Res()]
        SG = [P_sb("SG%d" % i, [128, 512]) for i in range(2)]; rSG = [Res(), Res()]
        UP = [P_sb("UP%d" % i, [128, 512]) for i in range(2)]; rUP = [Res(), Res()]
        wcount = 0
        cnt = 0

        for pss in passes:
            for li, ti in enumerate(pss):
                r0, nr, j = tiles[ti]
                xt, rxt = XT[li % 2], rXT[li % 2]
                om, rom = OM[li % 2], rOM[li % 2]
                tmp, rtmp = TMP[li % 2], rTMP[li % 2]
                P.dma("sp", xt[:nr, :], xs[r0:r0 + nr, :], reads=rxs, writes=[rxt])
                for q in range(4):
                    grow = (256 + q * NL + r0) if j == 0 else (q * NC)
                    for jj in range(4):
                        P.dma("pool", OMX[:nr, q, jj, :], omall(jj, grow, nr), reads=romall, writes=[rOMX])
                for k in range(8):
                    ps, rps = PS[2 + k // 4], rPS[2 + k // 4]
                    for q in range(4):
                        P.mm(ps[:, (k % 4) * 128:(k % 4) * 128 + nr], OMX[:nr, q, k % 4, (k // 4) * 128:(k // 4 + 1) * 128], SELI[:nr, q, :nr],
                             start=(q == 0), stop=(q == 3), reads=[rOMX, rSELI], writes=[rps])
                for kk in range(2):
                    P.act(om[:, 4 * kk:4 * kk + 4, :nr], PS[2 + kk][:, :].rearrange("p (a t) -> p a t", a=4)[:, :, :nr], AF.Copy, reads=[rPS[2 + kk]], writes=[rom])
                for half in range(2):
                    ps, rps = PS[half], rPS[half]
                    for k in range(8):
                        P.mm(ps[:nr, :], om[:, k, :nr], WO[:, k, half * 512:(half + 1) * 512], start=(k == 0), stop=(k == 7),
                             reads=[rom, rWO], writes=[rps])
                    P.op("dve", lambda h, ps=ps, tmp=tmp, half=half, j=j, nr=nr: h.tensor_tensor(
                        out=tmp[:nr, half * 512:(half + 1) * 512], in0=ps[:nr, :], in1=GT[:nr, 0, j, half * 512:(half + 1) * 512], op=ALU.mult),
                        reads=[rps, rGT], writes=[rtmp])
                P.op("dve", lambda h, tmp=tmp, xt=xt, li=li, nr=nr: h.tensor_tensor(out=X1[:nr, li, :], in0=tmp[:nr, :], in1=xt[:nr, :], op=ALU.add),
                     reads=[rtmp, rxt], writes=[rX1[li]])
                P.act(tmp[:nr, :], X1[:nr, li, :], AF.Square, reads=[rX1[li]], writes=[rtmp, rSM], accum_out=SMALL[:nr, 0:1])
                P.act(SMALL[:nr, 1:2], SMALL[:nr, 0:1], AF.Sqrt, reads=[rSM], writes=[rSM], bias=1e-6, scale=1.0 / D)
                P.op("dve", lambda h, nr=nr: h.reciprocal(out=SMALL[:nr, 2:3], in_=SMALL[:nr, 1:2]), reads=[rSM], writes=[rSM])
                P.op("dve", lambda h, tmp=tmp, li=li, nr=nr: h.tensor_scalar(out=tmp[:nr, :], in0=X1[:nr, li, :], scalar1=SMALL[:nr, 2:3], scalar2=None, op0=ALU.mult),
                     reads=[rX1[li], rSM], writes=[rtmp])
                for k in range(8):
                    ps, rps = PS[2 + k // 4], rPS[2 + k // 4]
                    P.op("pe", lambda h, ps=ps, tmp=tmp, k=k, nr=nr: h.transpose(ps[:, (k % 4) * 128:(k % 4) * 128 + nr], tmp[:nr, k * 128:(k + 1) * 128], ID[:nr, :nr]),
                         reads=[rtmp, rID], writes=[rps])
                for k in range(8):
                    ps, rps = PS[2 + k // 4], rPS[2 + k // 4]
                    P.act(H2F[:, k, :nr], ps[:, (k % 4) * 128:(k % 4) * 128 + nr], AF.Identity, reads=[rps, rSCALE2, rMODF], writes=[rH2F],
                          scale=SCALE2[:, k, j:j + 1], bias=MODF[:, k, j:j + 1])
                P.op("dve", lambda h, li=li, nr=nr: h.tensor_copy(out=H2B[:, :, li * 128:li * 128 + nr], in_=H2F[:, :, :nr]),
                     reads=[rH2F], writes=[rH2B[li]])
                ps, rps = PS[4], rPS[4]
                for k in range(8):
                    P.mm(ps[:nr, 0:NE], H2F[:, k, :nr], RW[:, k, :], start=(k == 0), stop=False, reads=[rH2F, rRW], writes=[rps])
                P.mm(ps[:nr, 0:NE], ONES[0:1, :nr], RB[0:1, :], start=False, stop=True, reads=[rONES, rRB], writes=[rps])
                P.op("dve", lambda h, ps=ps, nr=nr: h.tensor_copy(out=LG[:nr, :], in_=ps[:nr, 0:NE]), reads=[rps], writes=[rLG])
                P.op("dve", lambda h, nr=nr: h.max(out=SMALL[:nr, 8:16], in_=LG[:nr, :]), reads=[rLG], writes=[rSM])
                P.op("dve", lambda h, nr=nr: h.tensor_scalar(out=SMALL[:nr, 16:17], in0=SMALL[:nr, 8:9], scalar1=-1.0, scalar2=None, op0=ALU.mult),
                     reads=[rSM], writes=[rSM])
                P.act(EX[:nr, :], LG[:nr, :], AF.Exp, reads=[rLG, rSM], writes=[rEX], bias=SMALL[:nr, 16:17], scale=1.0)
                P.op("dve", lambda h, nr=nr: h.tensor_scalar(out=MK[:nr, :], in0=LG[:nr, :], scalar1=SMALL[:nr, 11:12], scalar2=None, op0=ALU.is_ge),
                     reads=[rLG, rSM], writes=[rMK])
                P.op("dve", lambda h, nr=nr: h.tensor_tensor(out=EX[:nr, :], in0=EX[:nr, :], in1=MK[:nr, :], op=ALU.mult),
                     reads=[rEX, rMK], writes=[rEX])
                P.op("dve", lambda h, nr=nr: h.reduce_sum(out=SMALL[:nr, 17:18], in_=EX[:nr, :], axis=AX.X), reads=[rEX], writes=[rSM])
                P.op("dve", lambda h, nr=nr: h.reciprocal(out=SMALL[:nr, 18:19], in_=SMALL[:nr, 17:18]), reads=[rSM], writes=[rSM])
                P.op("dve", lambda h, li=li, nr=nr: h.tensor_scalar(out=GATES[:nr, li, :], in0=EX[:nr, :], scalar1=SMALL[:nr, 18:19], scalar2=None, op0=ALU.mult),
                     reads=[rEX, rSM], writes=[rGATES[li]])

            groups = []
            li = 0
            while li < len(pss):
                g = []
                while li < len(pss) and len(g) < 4 and tiles[pss[li]][1] == 128:
                    g.append(li); li += 1
                if not g:
                    g = [li]; li += 1
                groups.append(g)
            for e in range(IN.get("nex", NE)):
                wb = wcount % 2
                wcount += 1
                for fc in range(8):
                    P.dma("pool", WGU[:, :, fc * 128:(fc + 1) * 128], wgu[e, :, fc * 128:(fc + 1) * 128].rearrange("(k p) f -> p k f", p=128),
                          writes=[rWG[fc]])
                    P.dma("pool", WGU[:, :, D + fc * 128:D + (fc + 1) * 128], wgu[e, :, D + fc * 128:D + (fc + 1) * 128].rearrange("(k p) f -> p k f", p=128),
                          writes=[rWU[fc]])
                for fc in range(8):
                    P.dma("pool", WD[:, fc, :], wd[e, fc * 128:(fc + 1) * 128, :], writes=[rWD[fc]])
                P.dma("sp", BGU[wb][:], bguT[e], writes=[rBGU[wb]])
                P.dma("pool", BD[wb][:], bd[e:e + 1, :], writes=[rBD[wb]])
                for g in groups:
                    ntok = sum(tiles[pss[l]][1] for l in g)
                    c0 = g[0] * 128
                    ab = cnt % 2
                    cnt += 1
                    actt, ractt = ACTT[ab], rACTT[ab]
                    rh = [rH2B[l] for l in g]
                    for fc in range(8):
                        pb = (fc % 2) * 2
                        psg, rpsg = PS[pb], rPS[pb]
                        psu, rpsu = PS[pb + 1], rPS[pb + 1]
                        for k in range(8):
                            P.mm(psg[:, :ntok], WGU[:, k, fc * 128:(fc + 1) * 128], H2B[:, k, c0:c0 + ntok], start=(k == 0), stop=(k == 7),
                                 reads=[rWG[fc]] + rh, writes=[rpsg])
                        for k in range(8):
                            P.mm(psu[:, :ntok], WGU[:, k, D + fc * 128:D + (fc + 1) * 128], H2B[:, k, c0:c0 + ntok], start=(k == 0), stop=(k == 7),
                                 reads=[rWU[fc]] + rh, writes=[rpsu])
                        tb = fc % 2
                        gp, sg, up = GP[tb], SG[tb], UP[tb]
                        P.op("dve", lambda h, gp=gp, psg=psg, fc=fc, wb=wb, ntok=ntok: h.tensor_scalar(
                            out=gp[:, :ntok], in0=psg[:, :ntok], scalar1=BGU[wb][:, fc:fc + 1], scalar2=7.0, op0=ALU.add, op1=ALU.min),
                            reads=[rpsg, rBGU[wb]], writes=[rGP[tb]])
                        P.act(sg[:, :ntok], gp[:, :ntok], AF.Sigmoid, reads=[rGP[tb]], writes=[rSG[tb]], scale=1.702)
                        P.op("dve", lambda h, up=up, psu=psu, fc=fc, wb=wb, ntok=ntok: h.tensor_scalar(
                            out=up[:, :ntok], in0=psu[:, :ntok], scalar1=BGU[wb][:, 8 + fc:9 + fc], scalar2=7.0, op0=ALU.add, op1=ALU.min),
                            reads=[rpsu, rBGU[wb]], writes=[rUP[tb]])
                        P.op("dve", lambda h, up=up, ntok=ntok: h.tensor_scalar(
                            out=up[:, :ntok], in0=up[:, :ntok], scalar1=-7.0, scalar2=1.0, op0=ALU.max, op1=ALU.add),
                            reads=[rUP[tb]], writes=[rUP[tb]])
                        P.op("dve", lambda h, gp=gp, sg=sg, ntok=ntok: h.tensor_tensor(out=gp[:, :ntok], in0=gp[:, :ntok], in1=sg[:, :ntok], op=ALU.mult),
                             reads=[rGP[tb], rSG[tb]], writes=[rGP[tb]])
                        P.op("dve", lambda h, gp=gp, up=up, actt=actt, fc=fc, ntok=ntok: h.tensor_tensor(out=actt[:, fc, :ntok], in0=gp[:, :ntok], in1=up[:, :ntok], op=ALU.mult),
                             reads=[rGP[tb], rUP[tb]], writes=[ractt])
                    for gi, l in enumerate(g):
                        r0, nr, j = tiles[pss[l]]
                        yb = 4 + (l % 2) * 2
                        for half in range(2):
                            ps, rps = PS[yb + half], rPS[yb + half]
                            for fc in range(8):
                                P.mm(ps[:nr, :], actt[:, fc, gi * 128:gi * 128 + nr], WD[:, fc, half * 512:(half + 1) * 512], start=(fc == 0), stop=False,
                                     reads=[ractt, rWD[fc]], writes=[rps])
                            P.mm(ps[:nr, :], ONESB[0:1, :nr], BD[wb][0:1, half * 512:(half + 1) * 512], start=False, stop=True,
                                 reads=[rONESB, rBD[wb]], writes=[rps])
                        tmp, rtmp = TMP[l % 2], rTMP[l % 2]
                        for half in range(2):
                            ps, rps = PS[yb + half], rPS[yb + half]
                            P.op("dve", lambda h, ps=ps, tmp=tmp, half=half, l=l, e=e, j=j, nr=nr: h.scalar_tensor_tensor(
                                out=tmp[:nr, half * 512:(half + 1) * 512], in0=ps[:nr, :], scalar=GATES[:nr, l, e:e + 1],
                                in1=GT[:nr, 1, j, half * 512:(half + 1) * 512], op0=ALU.mult, op1=ALU.mult),
                                reads=[rps, rGATES[l], rGT], writes=[rtmp])
                        P.op("dve", lambda h, tmp=tmp, l=l, nr=nr: h.tensor_tensor(out=X1[:nr, l, :], in0=X1[:nr, l, :], in1=tmp[:nr, :], op=ALU.add),
                             reads=[rtmp, rX1[l]], writes=[rX1[l]])
            for li, ti in enumerate(pss):
                r0, nr, j = tiles[ti]
                P.dma("sp", xo[r0:r0 + nr, :], X1[:nr, li, :], reads=[rX1[li]], writes=[rOUT[ti]])
        P.fence()


PER_LAYER = [("adaw1", [1024, 2048]), ("adab1T", [128, 16]), ("g1T", [128, 8]), ("wa", [1024, 384]), ("wz", [1024, 2, 68]),
             ("cw", [128, 3, 5]), ("nega", [128, 2]), ("dtb", [128, 2]), ("gdn", [128, 64]), ("ws", [1024, 256]),
             ("gqk", [128, 3, 64]), ("sinkb", [128, 2]),
             ("adaw2", [1024, 4096]), ("adab2", [1, 4096]), ("adab2T", [128, 16]), ("g2T", [128, 8]), ("wout", [1024, 1024]),
             ("rw", [1024, 32]), ("rb", [1, 32]), ("bguT", [32, 128, 16]), ("bd", [32, 1024])]
SHARED = [("ident", [128, 128]), ("cvT", [128, 8, 2]), ("blk1", [128, 128]), ("masks", [128, 6, 2, 64]), ("maskw", [128, 384]),
          ("ropet", [128, 64, 2, 2, 16]), ("seli", [128, 4, 128])]
NLOC = 2112


def build_fused(nex=32):
    nc = bass.Bass("TRN2", target_bir_lowering=False)
    dr = lambda name, shape: nc.dram_tensor(name, list(shape), F32, kind="ExternalInput").ap()
    IN = {}
    IN["xall"] = dr("xall", [8448, 1024]); IN["xs0"] = dr("xs0", [NLOC, 1024])
    for nm, shp in SHARED:
        IN[nm] = dr(nm, shp)
    for l in range(2):
        for nm, shp in PER_LAYER:
            IN[nm, l] = dr("%s_%d" % (nm, l), shp)
    for l in range(2):
        IN["wgu", l] = dr("wgu_%d" % l, [nex, 1024, 2048]); IN["wd", l] = dr("wd_%d" % l, [nex, 1024, 1024])
    IN["nex"] = nex
    xo = nc.dram_tensor("xo", [2048, 1024], F32, kind="ExternalOutput").ap()
    omloc = [nc.dram_tensor("omloc%d" % l, [8448, 256], F32).ap() for l in range(2)]
    OCH = 1024
    och = [(r, min(OCH, 8448 - r)) for r in range(0, 8448, OCH)]
    omall = [[nc.dram_tensor("omall%d_%d" % (l, k), [4 * n, 256], F32).ap() for k, (r, n) in enumerate(och)] for l in range(2)]
    xloc = nc.dram_tensor("xloc", [NLOC, 1024], F32).ap()
    XCH = 256
    xch = [(r, min(XCH, NLOC - r)) for r in range(0, NLOC, XCH)]
    xgat = [nc.dram_tensor("xgat_%d" % k, [4 * n, 1024], F32).ap() for k, (r, n) in enumerate(xch)]

    def om_rows(l, jj, grow, nr):
        k = grow // OCH
        n = och[k][1]
        off = jj * n + (grow - och[k][0])
        return omall[l][k][off:off + nr, :]

    def xg_rows(q, r, nr):
        k = r // XCH
        n = xch[k][1]
        off = q * n + (r - xch[k][0])
        return xgat[k][off:off + nr, :]
    groups = [[0, 1, 2, 3], [4, 5, 6, 7]]
    with ExitStack() as st:
        P = Prog(nc, st)
        G = emit_globals(P, IN)
        rxloc = [Res() for _ in range(17)]
        rxgat = Res()
        rcc = Res()
        rxo = [Res() for _ in range(16)]
        for l in range(2):
            last = (l == 1)
            if l == 0:
                xsrc = lambda n: [(0, 128, IN["xall"][n * 128:(n + 1) * 128, :])]
                rsrc = []
            else:
                def xsrc(n):
                    if n < 2:
                        a, b = 2 * n, 2 * n + 1
                        return [(0, 64, xg_rows(a, 2048, 64)), (64, 64, xg_rows(b, 2048, 64))]
                    i = n - 2
                    q, r = i // 16, (i % 16) * 128
                    return [(0, 128, xg_rows(q, r, 128))]
                rsrc = [rxgat]
            rdn = [Res(), Res()]
            rsw = [Res() for _ in range(66)]
            with ExitStack() as lst:
                C = emit_common(P, G, IN, l, lst)
                emit_dn(P, C, IN, l, last, xsrc, rsrc, omloc[l][:, 0:128], rdn)
                emit_swa(P, C, IN, l, last, xsrc, rsrc, omloc[l][:, 128:256], rsw)
                P.fence()
            romall = Res()
            if os.environ.get("NOCOLL") != "1":
                for k, (r, n) in enumerate(och):
                    P.coll("AllGather", ALU.bypass, groups, omloc[l][r:r + n, :], omall[l][k], reads=rdn + rsw, writes=[romall, rcc])
            if l == 0:
                emit_b(P, G, IN, 0, 2048, 64, IN["xs0"], [], lambda jj, grow, nr: om_rows(0, jj, grow, nr), [romall], xloc, rxloc)
                if os.environ.get("NOCOLL") not in ("1", "2"):
                    for k, (r, n) in enumerate(xch):
                        P.coll("AllGather", ALU.bypass, groups, xloc[r:r + n, :], xgat[k], reads=rxloc, writes=[rxgat, rcc])
            else:
                emit_b(P, G, IN, 1, 2048, 0, xloc, rxloc, lambda jj, grow, nr: om_rows(1, jj, grow, nr), [romall], xo, rxo)
        P.finish(rxo)
        print("fused instr counts", P.cnt, P.dcnt, "waits", P.n_wait, "sems", {k: len(v) for k, v in P.sem.items()})
    return nc


NEG = -1e30
def consts():
    f = np.float32
    i = np.arange(64)
    k_le_f = (i[:, None] <= i[None, :]).astype(f)
    k_ge_f = (i[:, None] >= i[None, :]).astype(f)
    m = np.zeros((64, 6, 2, 64), f)
    m[:, 0, 0] = k_le_f; m[:, 0, 1] = k_ge_f
    m[:, 1, 0] = np.where(i[None, :] >= i[:, None], 0, NEG)
    m[:, 1, 1] = np.where(i[None, :] <= i[:, None], 0, NEG)
    m[:, 2, 0] = np.where(i[None, :] <= i[:, None], 0, NEG)
    m[:, 2, 1] = np.where(i[None, :] >= i[:, None], 0, NEG)
    m[:, 3, 0] = np.where(i[None, :] > i[:, None], -1, 0)
    m[:, 3, 1] = np.where(i[None, :] < i[:, None], -1, 0)
    m[:, 4, 0] = np.where(i[None, :] < i[:, None], -1, 0)
    m[:, 4, 1] = np.where(i[None, :] > i[:, None], -1, 0)
    m[:, 5, 0] = np.eye(64); m[:, 5, 1] = np.eye(64)
    masks = np.concatenate([m, m], 0)
    blk1 = np.zeros((128, 128), f); blk1[:64, :64] = 1; blk1[64:, 64:] = 1
    qi = np.arange(128)[:, None]; kc = np.arange(384)[None, :]
    maskw = np.where((kc >= qi) & (kc <= qi + 256), 0, NEG).astype(f)
    t = np.arange(8192); row = (t // 64).astype(f); col = (t % 64).astype(f)
    inv = np.power(f(10000.0), -np.arange(16, dtype=f) / f(16)).astype(f)
    ar = row[:, None] * inv; ac = col[:, None] * inv
    rope = np.stack([np.stack([np.cos(ar), np.cos(ac)], 1), np.stack([np.sin(ar), np.sin(ac)], 1)], 1).astype(f)
    ropet = np.ascontiguousarray(rope.reshape(64, 128, 2, 2, 16).transpose(1, 0, 2, 3, 4))
    return dict(masks=masks, blk1=blk1, maskw=maskw, ropet=ropet, ident=np.eye(128, dtype=f))
def prep_a(inp, l, x, xc, K):
    f = np.float32
    w_in = inp["w_in"][l]; cwl = inp["dn_conv_w"][l]
    ada_w = np.ascontiguousarray(inp["ada_w"][l][:, 0:2048]); ada_b = inp["ada_b"][l][0:2048]
    base = dict(adaw=ada_w, adabT=np.ascontiguousarray(ada_b.reshape(16, 128).T), g1T=np.ascontiguousarray(inp["norm1_g"][l].reshape(8, 128).T), ident=K["ident"])
    dn, sw = [], []
    for c in range(8):
        b, j = c // 4, c % 4
        xall = np.ascontiguousarray(np.concatenate([xc[b], x[b]], 0))
        cv = np.stack([inp["c"][b], inp["c_ctx"]], -1)
        m = dict(base); m.update(xall=xall, cvT=np.ascontiguousarray(cv.reshape(8, 128, 2).transpose(1, 0, 2)))
        hd = [2 * j, 2 * j + 1]
        wa = np.concatenate([w_in[:, s * 512 + 128 * j: s * 512 + 128 * j + 128] for s in range(3)], 1)
        wz = np.stack([np.concatenate([w_in[:, 1536 + h * 64:1536 + (h + 1) * 64], w_in[:, [2048 + h, 2048 + 8 + h, 2064 + h, 2064 + 8 + h]]], 1) for h in hd], 1)
        cw = np.stack([cwl[:, s * 512 + 128 * j: s * 512 + 128 * j + 128].T for s in range(3)], 1)
        hp = np.repeat(np.array(hd), 64)
        nega = -np.exp(inp["dn_a_log"][l][:, hp]).T; dtb = inp["dn_dt_bias"][l][:, hp].T
        md = dict(m); md.update(wa=np.ascontiguousarray(wa), wz=np.ascontiguousarray(wz), cw=np.ascontiguousarray(cw), nega=np.ascontiguousarray(nega.astype(f)),
                                dtb=np.ascontiguousarray(dtb), gdn=np.ascontiguousarray(np.broadcast_to(inp["dn_out_g"][l], (128, 64))), blk1=K["blk1"], masks=K["masks"])
        dn.append(md)
        kv = j // 2
        ws = np.concatenate([w_in[:, 2080 + hd[0] * 64:2080 + hd[0] * 64 + 128], w_in[:, 2592 + kv * 64:2592 + (kv + 1) * 64], w_in[:, 2720 + kv * 64:2720 + (kv + 1) * 64]], 1)
        gqk = np.broadcast_to(np.stack([inp["q_norm_g"][l], inp["q_norm_g"][l], inp["k_norm_g"][l]], 0), (128, 3, 64))
        ms = dict(m); ms.update(ws=np.ascontiguousarray(ws), gqk=np.ascontiguousarray(gqk), ropet=K["ropet"],
                                sinkb=np.ascontiguousarray(np.broadcast_to(inp["sinks"][l][hd], (128, 2))), maskw=K["maskw"])
        sw.append(ms)
    return dn, sw
def gather_a(res_dn, res_sw):
    om_x = np.zeros((2, 8192, 1024), np.float32); om_c = np.zeros((2, 256, 1024), np.float32)
    for c in range(8):
        b, j = c // 4, c % 4
        od = res_dn[c]["o_dn"]; os_ = res_sw[c]["o_sw"]
        om_c[b, :, 128 * j:128 * j + 128] = od[:256]; om_x[b, :, 128 * j:128 * j + 128] = od[256:]
        om_c[b, :, 512 + 128 * j:512 + 128 * j + 128] = os_[:256]; om_x[b, :, 512 + 128 * j:512 + 128 * j + 128] = os_[256:]
    return om_x, om_c


def prep_b(inp, l, x, xc, om_x, om_c, last):
    f = np.float32
    ada_w = np.ascontiguousarray(inp["ada_w"][l][:, 2048:6144]); ada_b = inp["ada_b"][l][2048:6144]
    common = dict(
        adaw=ada_w, adab=np.ascontiguousarray(ada_b[None, :]),
        adabT=np.ascontiguousarray(ada_b[1024:3072].reshape(16, 128).T),
        g2T=np.ascontiguousarray(inp["norm2_g"][l].reshape(8, 128).T),
        wout=inp["w_out"][l], rw=inp["router_w"][l], rb=np.ascontiguousarray(inp["router_b"][l][None, :]),
        wgu=inp["w_gate_up"][l], bguT=np.ascontiguousarray(inp["b_gate_up"][l].reshape(32, 16, 128).transpose(0, 2, 1)),
        wd=inp["w_down"][l], bd=inp["b_down"][l], ident=np.eye(128, dtype=f))
    maps = []
    for c in range(8):
        b, q = c // 4, c % 4
        rows = [x[b, q * 2048:(q + 1) * 2048]]; oms = [om_x[b, q * 2048:(q + 1) * 2048]]
        if not last:
            rows.append(xc[b, q * 64:(q + 1) * 64]); oms.append(om_c[b, q * 64:(q + 1) * 64])
        xs = np.ascontiguousarray(np.concatenate(rows, 0)); om = np.concatenate(oms, 0)
        cv = np.stack([inp["c"][b], inp["c_ctx"]], -1)
        m = dict(common)
        m.update(xs=xs, omT=np.ascontiguousarray(om.T), cvT=np.ascontiguousarray(cv.reshape(8, 128, 2).transpose(1, 0, 2)))
        maps.append(m)
    return maps
def gather_b(results, last):
    x = np.zeros((2, 8192, 1024), np.float32); xc = np.zeros((2, 256, 1024), np.float32)
    for c in range(8):
        b, q = c // 4, c % 4
        xo = results[c]["xo"]
        x[b, q * 2048:(q + 1) * 2048] = xo[:2048]
        if not last:
            xc[b, q * 64:(q + 1) * 64] = xo[2048:]
    return x, xc


def prep_fused(inp, nex=32):
    f = np.float32
    K = consts()
    x = np.ascontiguousarray(inp["x"], dtype=f); xc = np.ascontiguousarray(inp["ctx"], dtype=f)
    A = [prep_a(inp, l, x, xc, K) for l in range(2)]
    zx = np.zeros((2, 8192, 1024), f); zc = np.zeros((2, 256, 1024), f)
    B = [prep_b(inp, l, zx, zc, zx, zc, False) for l in range(2)]
    wgu = [np.ascontiguousarray(inp["w_gate_up"][l][:nex], dtype=f) for l in range(2)]; wd = [np.ascontiguousarray(inp["w_down"][l][:nex], dtype=f) for l in range(2)]
    maps = []
    for c in range(8):
        b, q = c // 4, c % 4
        m = dict(xall=A[0][0][c]["xall"], cvT=A[0][0][c]["cvT"], ident=K["ident"], blk1=K["blk1"], masks=K["masks"], maskw=K["maskw"], ropet=K["ropet"])
        m["xs0"] = np.ascontiguousarray(np.concatenate([x[b, q * 2048:(q + 1) * 2048], xc[b, q * 64:(q + 1) * 64]], 0))
        seli = np.zeros((128, 4, 128), f); seli[:, q, :] = np.eye(128, dtype=f); m["seli"] = seli
        for l in range(2):
            dn, sw = A[l][0][c], A[l][1][c]; bb = B[l][c]
            for nm, src, key in [("adaw1", dn, "adaw"), ("adab1T", dn, "adabT"), ("g1T", dn, "g1T"), ("wa", dn, "wa"), ("wz", dn, "wz"), ("cw", dn, "cw"),
                                 ("nega", dn, "nega"), ("dtb", dn, "dtb"), ("gdn", dn, "gdn"), ("ws", sw, "ws"), ("gqk", sw, "gqk"), ("sinkb", sw, "sinkb"),
                                 ("adaw2", bb, "adaw"), ("adab2", bb, "adab"), ("adab2T", bb, "adabT"), ("g2T", bb, "g2T"), ("wout", bb, "wout"),
                                 ("rw", bb, "rw"), ("rb", bb, "rb"), ("bguT", bb, "bguT"), ("bd", bb, "bd")]:
                m["%s_%d" % (nm, l)] = np.ascontiguousarray(src[key], dtype=f)
        for l in range(2):
            m["wgu_%d" % l] = wgu[l]; m["wd_%d" % l] = wd[l]
        maps.append(m)
    return maps
def gather_fused(results):
    x = np.zeros((2, 8192, 1024), np.float32)
    for c in range(8):
        b, q = c // 4, c % 4
        x[b, q * 2048:(q + 1) * 2048] = results[c]["xo"]
    return x


_NC = None


def kernel(**inputs):
    global _NC
    inp = {k: np.asarray(v) for k, v in inputs.items()}
    if _NC is None:
        _NC = build_fused(32)
    maps = prep_fused(inp, 32)
    res = run_bass_kernel_spmd(_NC, maps, core_ids=list(range(8)))
    return gather_fused(res.results).astype(np.float32)
```

```python
import os
import numpy as np
from contextlib import ExitStack
import concourse.bass as bass
import concourse.mybir as mybir
from concourse.bass_utils import run_bass_kernel_spmd

F32 = mybir.dt.float32
BF16 = mybir.dt.bfloat16
AF = mybir.ActivationFunctionType
ALU = mybir.AluOpType
AX = mybir.AxisListType

SAME_ENGINE_SYNC = True
NRING = 8
EPOCH = 30000


class Res:
    __slots__ = ("name", "w", "r", "x")

    def __init__(self, name="", x=False):
        self.name = name
        self.w = None
        self.r = {}
        self.x = x


class Prog:
    def __init__(self, nc, stack):
        self.nc = nc
        self.e = {"pe": nc.tensor, "act": nc.scalar, "dve": nc.vector, "pool": nc.gpsimd, "sp": nc.sync}
        self.ops = {k: [] for k in self.e}
        self.cnt = {k: 0 for k in self.e}
        self.sem = {k: [stack.enter_context(nc.semaphore("s_" + k + "0"))] for k in self.e}
        self.ring = {q: [stack.enter_context(nc.semaphore("d_%s_%d" % (q, i))) for i in range(NRING)]
                     for q in ("sp", "act", "pool")}
        self.dcnt = {q: 0 for q in self.ring}
        self.waited = {k: {} for k in self.e}
        self.stack = stack
        self.n_wait = 0

    def sb(self, name, shape, dt=F32, stack=None):
        self.n_alloc = getattr(self, "n_alloc", 0) + 1
        return (stack or self.stack).enter_context(self.nc.sbuf_tensor("%s_%d" % (name, self.n_alloc), list(shape), dt))

    def fence(self):
        for E in self.e:
            for F in self.e:
                if F != E and self.cnt[F] > 0:
                    self._wait(E, ("c", F, self.cnt[F]))
            for q in self.ring:
                n = self.dcnt[q]
                for k in range(max(0, n - NRING), n):
                    self._wait(E, ("d", q, k))

    def ps(self, name, shape, dt=F32):
        return self.stack.enter_context(self.nc.psum_tensor(name, list(shape), dt))

    def _semval(self, ev):
        if ev[0] == "c":
            ep, v = divmod(ev[2] - 1, EPOCH)
            return ("c", ev[1]), self.sem[ev[1]][ep], (ep, v + 1)
        if ev[0] == "x":
            return ("x", ev[1]), self.ccsems[ev[1]], (0, 1)
        _, q, n = ev
        slot = n % NRING
        return ("d", q, slot), self.ring[q][slot], (0, 16 * (n // NRING + 1))

    def _wait(self, eng, ev):
        if ev[0] == "c" and ev[1] == eng:
            if eng == "pe" or not SAME_ENGINE_SYNC:
                return
        key, sem, val = self._semval(ev)
        if self.waited[eng].get(key, (0, 0)) >= val:
            return
        self.waited[eng][key] = val
        self.n_wait += 1
        self.ops[eng].append(lambda h, sem=sem, val=val[1]: h.wait_ge(sem, val))

    def _deps(self, eng, reads, writes):
        deps = []
        for r in reads:
            if r.w is not None:
                deps.append(r.w)
            if r.x:
                for k, ev in r.r.items():
                    if not (k[0] == "c" and k[1] == eng):
                        deps.append(ev)
        for w in writes:
            if w.w is not None:
                deps.append(w.w)
            deps.extend(w.r.values())
        for ev in deps:
            self._wait(eng, ev)

    def _record(self, ev, reads, writes):
        if ev[0] == "c":
            key = ("c", ev[1])
        elif ev[0] == "x":
            key = ("x", ev[1])
        else:
            key = ("d", ev[1], ev[2] % NRING)
        for r in reads:
            r.r[key] = ev
        for w in writes:
            w.w = ev
            w.r = {}

    def op(self, eng, fn, reads=(), writes=()):
        self._deps(eng, reads, writes)
        self.cnt[eng] += 1
        ev = ("c", eng, self.cnt[eng])
        ep = (self.cnt[eng] - 1) // EPOCH
        if ep >= len(self.sem[eng]):
            self.sem[eng].append(self.stack.enter_context(self.nc.semaphore("s_%s%d" % (eng, ep))))
        sem = self.sem[eng][ep]
        self.ops[eng].append(lambda h, fn=fn, sem=sem: fn(h).then_inc(sem, 1))
        self._record(ev, reads, writes)

    def dma(self, q, out, in_, reads=(), writes=(), **kw):
        self._deps(q, reads, writes)
        n = self.dcnt[q]
        self.dcnt[q] += 1
        if n >= NRING:
            self._wait(q, ("d", q, n - NRING))
        sem = self.ring[q][n % NRING]
        self.ops[q].append(lambda h, out=out, in_=in_, sem=sem, kw=kw: h.dma_start(out=out, in_=in_, **kw).then_inc(sem, 16))
        ev = ("d", q, n)
        self._record(ev, reads, writes)
        return ev

    def coll(self, kind, op, groups, in_ap, out_ap, reads=(), writes=()):
        q = "pool"
        self._deps(q, reads, writes)
        if not hasattr(self, "ccsems"):
            self.ccsems = []
        sem = self.stack.enter_context(self.nc.semaphore("cc%d" % len(self.ccsems)))
        self.ccsems.append(sem)
        self.ops[q].append(lambda h, sem=sem: h.collective_compute(kind, op, replica_groups=groups, ins=[in_ap.opt()], outs=[out_ap.opt()]).then_inc(sem))
        ev = ("x", len(self.ccsems) - 1, 0)
        self._record(ev, reads, writes)
        return ev

    def finish(self, final_res):
        for r in final_res:
            if r.w is not None:
                self._wait("sp", r.w)
        for q in self.ring:
            n = self.dcnt[q]
            for k in range(max(0, n - NRING), n):
                self._wait("sp", ("d", q, k))
        with self.nc.Block() as block:
            @block.tensor
            def _(h):
                for f in self.ops["pe"]:
                    f(h)

            @block.scalar
            def _(h):
                for f in self.ops["act"]:
                    f(h)

            @block.vector
            def _(h):
                for f in self.ops["dve"]:
                    f(h)

            @block.gpsimd
            def _(h):
                for f in self.ops["pool"]:
                    f(h)

            @block.sync
            def _(h):
                for f in self.ops["sp"]:
                    f(h)

    def mm(self, out, lhsT, rhs, start=True, stop=True, reads=(), writes=(), **kw):
        self.op("pe", lambda h: h.matmul(out, lhsT, rhs, start=start, stop=stop, **kw), reads, writes)

    def act(self, out, in_, func, reads=(), writes=(), **kw):
        self.op("act", lambda h: h.activation(out=out, in_=in_, func=func, **kw), reads, writes)


D = 1024
NB = 66
NCH = 132


def emit_globals(P, IN):
    G = {}
    ID = P.sb("ID", [128, 128]); rID = Res(); P.dma("sp", ID[:], IN["ident"], writes=[rID])
    ONES = P.sb("ONES", [128, 128]); rONES = Res()
    P.op("dve", lambda h: h.memset(ONES[:], 1.0), writes=[rONES])
    ONESB = P.sb("ONESB", [1, 128], BF16); rONESB = Res()
    P.op("dve", lambda h: h.memset(ONESB[:], 1.0), writes=[rONESB])
    PS = [P.ps("PS%d" % i, [128, 512]) for i in range(8)]; rPS = [Res("ps%d" % i, x=True) for i in range(8)]
    G.update(ID=ID, rID=rID, ONES=ONES, rONES=rONES, ONESB=ONESB, rONESB=rONESB, PS=PS, rPS=rPS)
    return G


def emit_common(P, G, IN, l, stk):
    cvT = IN["cvT"]; adaw = IN["adaw1", l]; adabT = IN["adab1T", l]; g1T = IN["g1T", l]
    C = dict(G)
    PS, rPS = G["PS"], G["rPS"]
    MODF = P.sb("MODF", [128, 16, 2], stack=stk); rMODF = Res()
    SCALE1 = P.sb("SCALE1", [128, 8, 2], stack=stk); rSCALE1 = Res()
    G1 = P.sb("G1", [128, 8], stack=stk); rG1 = Res(); P.dma("sp", G1[:], g1T, writes=[rG1])
    ABT = P.sb("ABT", [128, 16], stack=stk); rABT = Res(); P.dma("sp", ABT[:], adabT, writes=[rABT])
    sub = ExitStack()
    CV = P.sb("CV", [128, 8, 2], stack=sub); rCV = Res(); P.dma("sp", CV[:], cvT, writes=[rCV])
    SCV = P.sb("SCV", [128, 8, 2], stack=sub); rSCV = Res()
    P.act(SCV[:], CV[:], AF.Silu, reads=[rCV], writes=[rSCV])
    AW = [P.sb("AW%d" % i, [128, 8, 512], stack=sub) for i in range(2)]; rAW = [Res(), Res()]
    for blk in range(4):
        aw, raw = AW[blk % 2], rAW[blk % 2]
        P.dma("sp", aw[:], adaw[:, blk * 512:(blk + 1) * 512].rearrange("(k p) f -> p k f", p=128), writes=[raw])
        ps, rps = PS[blk % 2], rPS[blk % 2]
        for fcl in range(4):
            for k in range(8):
                P.mm(ps[:, fcl * 2:fcl * 2 + 2], aw[:, k, fcl * 128:(fcl + 1) * 128], SCV[:, k, :],
                     start=(k == 0), stop=(k == 7), reads=[rSCV, raw], writes=[rps])
        for fcl in range(4):
            fcg = blk * 4 + fcl
            P.act(MODF[:, fcg, :], ps[:, fcl * 2:fcl * 2 + 2], AF.Identity, reads=[rps, rABT], writes=[rMODF],
                  bias=ABT[:, fcg:fcg + 1], scale=1.0)
    P.op("dve", lambda h: h.tensor_scalar(out=SCALE1[:], in0=MODF[:, 8:16, :], scalar1=1.0, scalar2=None, op0=ALU.add),
         reads=[rMODF], writes=[rSCALE1])
    P.op("dve", lambda h: h.tensor_tensor(out=SCALE1[:], in0=SCALE1[:], in1=G1[:].unsqueeze(2).to_broadcast([128, 8, 2]), op=ALU.mult),
         reads=[rSCALE1, rG1], writes=[rSCALE1])
    P.fence()
    sub.close()
    C.update(MODF=MODF, rMODF=rMODF, SCALE1=SCALE1, rSCALE1=rSCALE1)
    return C


def emit_frontend(P, C, xsrc, rsrc, consume, blocks, stk, tbanks=(0, 1)):
    PS, rPS = C["PS"], C["rPS"]
    XT = [P.sb("XT%d" % i, [128, D], stack=stk) for i in range(2)]; rXT = [Res(), Res()]
    XN = P.sb("XN", [128, D], stack=stk); rXN = Res()
    HX = [P.sb("HX%d" % i, [128, 8, 128], BF16, stack=stk) for i in range(2)]; rHX = [Res(), Res()]
    SM = P.sb("FSM", [128, 8], stack=stk); rSM = Res()
    for idx, n in enumerate(blocks):
        j = 1 if n < 2 else 0
        xt, rxt = XT[idx % 2], rXT[idx % 2]
        hx, rhx = HX[idx % 2], rHX[idx % 2]
        for (p0, np_, src) in xsrc(n):
            P.dma("sp", xt[p0:p0 + np_, :], src, reads=rsrc, writes=[rxt])
        P.op("dve", lambda h: h.memset(SM[:, 0:1], 0.0), writes=[rSM])
        P.act(XN[:], xt[:], AF.Square, reads=[rxt], writes=[rXN, rSM], accum_out=SM[:, 0:1])
        P.act(SM[:, 1:2], SM[:, 0:1], AF.Ln, reads=[rSM], writes=[rSM], bias=1e-6, scale=1.0 / D)
        P.act(SM[:, 2:3], SM[:, 1:2], AF.Exp, reads=[rSM], writes=[rSM], scale=-0.5)
        P.op("dve", lambda h, xt=xt: h.tensor_scalar(out=XN[:], in0=xt[:], scalar1=SM[:, 2:3], scalar2=None, op0=ALU.mult),
             reads=[rxt, rSM], writes=[rXN])
        for half in range(2):
            ps, rps = PS[tbanks[half]], rPS[tbanks[half]]
            for k in range(4 * half, 4 * half + 4):
                P.op("pe", lambda h, ps=ps, k=k: h.transpose(ps[:, (k % 4) * 128:(k % 4 + 1) * 128], XN[:, k * 128:(k + 1) * 128], C["ID"][:]),
                     reads=[rXN, C["rID"]], writes=[rps])
            for k in range(4 * half, 4 * half + 4):
                P.act(hx[:, k, :], ps[:, (k % 4) * 128:(k % 4 + 1) * 128], AF.Identity, reads=[rps, C["rSCALE1"], C["rMODF"]], writes=[rhx],
                      scale=C["SCALE1"][:, k, j:j + 1], bias=C["MODF"][:, k, j:j + 1])
        consume(n, hx, rhx)


def emit_swa(P, C, IN, l, last, xsrc, rsrc, o_sw, rOUT):
    ws = IN["ws", l]; gqk = IN["gqk", l]; ropet = IN["ropet"]; sinkb = IN["sinkb", l]; maskw = IN["maskw"]
    with ExitStack() as st0:
        _sb = P.sb
        P_sb = lambda name, shape, dt=F32: _sb("sw_" + name, shape, dt, stack=st0)
        PS, rPS, ID, rID = C["PS"], C["rPS"], C["ID"], C["rID"]
        WS = P_sb("WS", [128, 8, 256], BF16); rWS = Res()
        P.dma("pool", WS[:], ws.rearrange("(k p) f -> p k f", p=128), writes=[rWS])
        GQK = P_sb("GQK", [128, 3, 64]); rGQK = Res(); P.dma("sp", GQK[:], gqk, writes=[rGQK])
        ROPE = P_sb("ROPE", [128, 64, 2, 2, 16]); rROPE = Res(); P.dma("sp", ROPE[:], ropet, writes=[rROPE])
        SINK = P_sb("SINK", [128, 2]); rSINK = Res(); P.dma("sp", SINK[:], sinkb, writes=[rSINK])
        MASKW = P_sb("MASKW", [128, 384]); rMASKW = Res(); P.dma("sp", MASKW[:], maskw, writes=[rMASKW])
        SQT = P_sb("SQT", [128, NB * 128], BF16); rSQT = [Res() for _ in range(NB)]
        SKT = P_sb("SKT", [128, NB * 128], BF16); rSKT = [Res() for _ in range(NB)]
        SV = P_sb("SV", [128, NB, 64], BF16); rSV = [Res() for _ in range(NB)]
        QK = P_sb("QK", [128, 3, 64]); rQK = Res()
        QKR = P_sb("QKR", [128, 4, 64]); rQKR = Res()
        SQ = P_sb("SQ", [128, 3, 64]); rSQ = Res()
        T1 = P_sb("T1", [128, 3, 2, 16]); rT1 = Res()
        T2 = P_sb("T2", [128, 3, 2, 16]); rT2 = Res()
        SM = P_sb("SM", [128, 16]); rSM = Res()
        S = P_sb("S", [128, 640]); rS = Res()
        E = P_sb("E", [128, 640]); rE = Res()
        ET = P_sb("ET", [128, 5, 128], BF16); rET = Res()
        OSW = [P_sb("OSW%d" % i, [128, 128]) for i in range(2)]; rOSW = [Res(), Res()]

        HL = []
        for h in range(2):
            Ld = dict(S=P_sb("S%d" % h, [128, 640]), rS=Res(), E=P_sb("E%d" % h, [128, 640]), rE=Res(),
                      ET=P_sb("ET%d" % h, [128, 5, 128], BF16), rET=Res(), SM=P_sb("SMh%d" % h, [128, 8]), rSM=Res(),
                      b0=PS[2 + 3 * h], r0=rPS[2 + 3 * h], b1=PS[3 + 3 * h], r1=rPS[3 + 3 * h], b2=PS[4 + 3 * h], r2=rPS[4 + 3 * h])
            HL.append(Ld)

        def att_gen(n, h, osw, rosw):
            Ld = HL[h]
            S_, rS_, E_, rE_, ET_, rET_, SM_, rSM_ = Ld["S"], Ld["rS"], Ld["E"], Ld["rE"], Ld["ET"], Ld["rET"], Ld["SM"], Ld["rSM"]
            b0, r0, b1, r1, b2, r2 = Ld["b0"], Ld["r0"], Ld["b1"], Ld["r1"], Ld["b2"], Ld["r2"]
            if n >= 2:
                lo, hi = max(2, n - 1), min(NB - 1, n + 1)
                nl = (hi - lo + 1) * 128
                m0 = (lo - (n - 1)) * 128
            else:
                lo, hi, nl, m0 = 0, -1, 0, 0
            ntot = nl + 256
            kblocks = list(range(lo, hi + 1)) + [0, 1]
            hs = slice(h * 64, (h + 1) * 64)
            if nl:
                P.mm(b0[:, 0:nl], SQT[hs, n * 128:(n + 1) * 128], SKT[hs, lo * 128:(hi + 1) * 128],
                     reads=[rSQT[n]] + [rSKT[b] for b in range(lo, hi + 1)], writes=[r0])
            P.mm(b1[:, 0:256], SQT[hs, n * 128:(n + 1) * 128], SKT[hs, 0:256], reads=[rSQT[n], rSKT[0], rSKT[1]], writes=[r1])
            yield
            if nl:
                P.op("dve", lambda hh: hh.scalar_tensor_tensor(out=S_[:, 0:nl], in0=b0[:, 0:nl], scalar=0.125,
                                                              in1=MASKW[:, m0:m0 + nl], op0=ALU.mult, op1=ALU.add),
                     reads=[r0, rMASKW], writes=[rS_])
            P.act(S_[:, nl:ntot], b1[:, 0:256], AF.Copy, reads=[r1], writes=[rS_], scale=0.125)
            yield
            P.op("dve", lambda hh: hh.reduce_max(out=SM_[:, 0:1], in_=S_[:, 0:ntot], axis=AX.X), reads=[rS_], writes=[rSM_])
            yield
            P.op("dve", lambda hh: hh.tensor_tensor(out=SM_[:, 1:2], in0=SM_[:, 0:1], in1=SINK[:, h:h + 1], op=ALU.max),
                 reads=[rSM_, rSINK], writes=[rSM_])
            yield
            P.op("dve", lambda hh: hh.tensor_scalar(out=SM_[:, 2:3], in0=SM_[:, 1:2], scalar1=-1.0, scalar2=None, op0=ALU.mult),
                 reads=[rSM_], writes=[rSM_])
            P.op("dve", lambda hh: hh.memset(SM_[:, 3:4], 0.0), writes=[rSM_])
            yield
            P.act(E_[:, 0:ntot], S_[:, 0:ntot], AF.Exp, reads=[rS_, rSM_], writes=[rE_, rSM_], bias=SM_[:, 2:3], scale=1.0, accum_out=SM_[:, 3:4])
            P.act(SM_[:, 4:5], SINK[:, h:h + 1], AF.Exp, reads=[rSINK, rSM_], writes=[rSM_], bias=SM_[:, 2:3], scale=1.0)
            yield
            P.op("dve", lambda hh: hh.tensor_tensor(out=SM_[:, 5:6], in0=SM_[:, 3:4], in1=SM_[:, 4:5], op=ALU.add), reads=[rSM_], writes=[rSM_])
            nk = ntot // 128
            n4 = min(nk, 4)
            for c in range(n4):
                P.op("pe", lambda hh, c=c: hh.transpose(b2[:, c * 128:(c + 1) * 128], E_[:, c * 128:(c + 1) * 128], ID[:]),
                     reads=[rE_, rID], writes=[r2])
            if nk > 4:
                P.op("pe", lambda hh: hh.transpose(b1[:, 384:512], E_[:, 512:640], ID[:]), reads=[rE_, rID], writes=[r1])
            yield
            P.op("dve", lambda hh: hh.reciprocal(out=SM_[:, 6:7], in_=SM_[:, 5:6]), reads=[rSM_], writes=[rSM_])
            P.act(ET_[:, 0:n4, :], b2[:, 0:n4 * 128], AF.Copy, reads=[r2], writes=[rET_])
            if nk > 4:
                P.act(ET_[:, 4, :], b1[:, 384:512], AF.Copy, reads=[r1], writes=[rET_])
            yield
            for c in range(nk):
                kb = kblocks[c]
                P.mm(b1[:, 256:320], ET_[:, c, :], SV[:, kb, :], start=(c == 0), stop=(c == nk - 1), reads=[rET_, rSV[kb]], writes=[r1])
            yield
            P.op("dve", lambda hh: hh.tensor_scalar(out=osw[:, h * 64:(h + 1) * 64], in0=b1[:, 256:320], scalar1=SM_[:, 6:7], scalar2=None, op0=ALU.mult),
                 reads=[r1, rSM_], writes=[rosw])
            yield

        def attention(n):
            osw, rosw = OSW[n % 2], rOSW[n % 2]
            gs = [att_gen(n, 0, osw, rosw), att_gen(n, 1, osw, rosw)]
            alive = [True, True]
            while any(alive):
                for i in range(2):
                    if alive[i]:
                        try:
                            next(gs[i])
                        except StopIteration:
                            alive[i] = False
            P.dma("sp", o_sw[n * 128:(n + 1) * 128, :], osw[:], reads=[rosw], writes=[rOUT[n]])

        def consume(n, hx, rhx):
            ps, rps = PS[1], rPS[1]
            for k in range(8):
                P.mm(ps[:, 0:256], hx[:, k, :], WS[:, k, :], start=(k == 0), stop=(k == 7), reads=[rhx, rWS], writes=[rps])
            P.act(QK[:], ps[:, 0:192], AF.Copy, reads=[rps], writes=[rQK])
            P.act(SV[:, n, :], ps[:, 192:256], AF.Copy, reads=[rps], writes=[rSV[n]])
            P.op("dve", lambda h: h.tensor_tensor(out=SQ[:], in0=QK[:], in1=QK[:], op=ALU.mult), reads=[rQK], writes=[rSQ])
            P.op("dve", lambda h: h.reduce_sum(out=SM[:, 8:11], in_=SQ[:], axis=AX.X), reads=[rSQ], writes=[rSM])
            P.act(SM[:, 11:14], SM[:, 8:11], AF.Ln, reads=[rSM], writes=[rSM], bias=1e-6, scale=1.0 / 64)
            P.act(SM[:, 8:11], SM[:, 11:14], AF.Exp, reads=[rSM], writes=[rSM], scale=-0.5)
            P.op("dve", lambda h: h.tensor_tensor(out=QK[:], in0=QK[:], in1=SM[:, 8:11].unsqueeze(2).to_broadcast([128, 3, 64]), op=ALU.mult),
                 reads=[rQK, rSM], writes=[rQK])
            P.op("dve", lambda h: h.tensor_tensor(out=QK[:], in0=QK[:], in1=GQK[:], op=ALU.mult), reads=[rQK, rGQK], writes=[rQK])
            if n >= 2:
                bi = n - 2
                q5 = QK[:].rearrange("p s (a t f) -> p s a t f", a=2, t=2)
                o5 = QKR[:, 0:3, :].rearrange("p s (a t f) -> p s a t f", a=2, t=2)
                X1, X2 = q5[:, :, :, 0, :], q5[:, :, :, 1, :]
                Cc = ROPE[:, bi, 0, :, :].unsqueeze(1).to_broadcast([128, 3, 2, 16])
                Sn = ROPE[:, bi, 1, :, :].unsqueeze(1).to_broadcast([128, 3, 2, 16])
                tt = lambda out, a, b, op, reads, writes: P.op("dve", lambda h: h.tensor_tensor(out=out, in0=a, in1=b, op=op), reads=reads, writes=writes)
                tt(T1[:], X1, Cc, ALU.mult, [rQK, rROPE], [rT1])
                tt(T2[:], X2, Sn, ALU.mult, [rQK, rROPE], [rT2])
                tt(o5[:, :, :, 0, :], T1[:], T2[:], ALU.subtract, [rT1, rT2], [rQKR])
                tt(T1[:], X2, Cc, ALU.mult, [rQK, rROPE], [rT1])
                tt(T2[:], X1, Sn, ALU.mult, [rQK, rROPE], [rT2])
                tt(o5[:, :, :, 1, :], T1[:], T2[:], ALU.add, [rT1, rT2], [rQKR])
            else:
                P.op("dve", lambda h: h.tensor_copy(out=QKR[:, 0:3, :], in_=QK[:]), reads=[rQK], writes=[rQKR])
            P.op("dve", lambda h: h.tensor_copy(out=QKR[:, 3, :], in_=QKR[:, 2, :]), reads=[rQKR], writes=[rQKR])
            ps, rps = PS[1], rPS[1]
            P.op("pe", lambda h: h.transpose(ps[:, 256:384], QKR[:, 0:2, :].rearrange("p a f -> p (a f)"), ID[:]), reads=[rQKR, rID], writes=[rps])
            P.op("pe", lambda h: h.transpose(ps[:, 384:512], QKR[:, 2:4, :].rearrange("p a f -> p (a f)"), ID[:]), reads=[rQKR, rID], writes=[rps])
            P.act(SQT[:, n * 128:(n + 1) * 128], ps[:, 256:384], AF.Copy, reads=[rps], writes=[rSQT[n]])
            P.act(SKT[:, n * 128:(n + 1) * 128], ps[:, 384:512], AF.Copy, reads=[rps], writes=[rSKT[n]])
            if n == 1 and not last:
                attention(0); attention(1)
            if n >= 3:
                attention(n - 1)
            if n == NB - 1:
                attention(n)

        emit_frontend(P, C, xsrc, rsrc, consume, list(range(NB)), st0, tbanks=(0, 0))
        P.fence()


def emit_dn(P, C, IN, l, last, xsrc, rsrc, o_dn, rOUT):
    wa = IN["wa", l]; wz = IN["wz", l]; cw = IN["cw", l]; nega = IN["nega", l]; dtb = IN["dtb", l]; gdn = IN["gdn", l]
    blk1 = IN["blk1"]; masks = IN["masks"]
    NS = NCH + 1
    with ExitStack() as st0:
        _sb = P.sb
        P_sb = lambda name, shape, dt=F32: _sb("dn_" + name, shape, dt, stack=st0)
        PS, rPS, ID, rID, ONES, rONES = C["PS"], C["rPS"], C["ID"], C["rID"], C["ONES"], C["rONES"]
        WA = P_sb("WA", [128, 8, 384], BF16); rWA = Res(); P.dma("pool", WA[:], wa.rearrange("(k p) f -> p k f", p=128), writes=[rWA])
        WZ = P_sb("WZ", [128, 8, 2, 68], BF16); rWZ = Res(); P.dma("pool", WZ[:], wz.rearrange("(k p) h f -> p k h f", p=128), writes=[rWZ])
        CW = P_sb("CW", [128, 3, 5]); rCW = Res(); P.dma("sp", CW[:], cw, writes=[rCW])
        NEGA = P_sb("NEGA", [128, 2]); rNEGA = Res(); P.dma("sp", NEGA[:], nega, writes=[rNEGA])
        DTB = P_sb("DTB", [128, 2]); rDTB = Res(); P.dma("sp", DTB[:], dtb, writes=[rDTB])
        GDN = P_sb("GDN", [128, 64]); rGDN = Res(); P.dma("sp", GDN[:], gdn, writes=[rGDN])
        BLK = P_sb("BLK", [128, 128]); rBLK = Res(); P.dma("sp", BLK[:], blk1, writes=[rBLK])
        MSK = P_sb("MSK", [128, 6, 2, 64]); rMSK = Res(); P.dma("sp", MSK[:], masks, writes=[rMSK])
        TRI, NEGMT, NEGM, NSTT, NST, ID2 = [MSK[:, i, :, :] for i in range(6)]
        ONES3 = P_sb("ONES3", [128, 2, 64]); rONES3 = Res()
        P.op("dve", lambda h: h.memset(ONES3[:], 1.0), writes=[rONES3])
        QT = P_sb("QT", [128, NCH * 64], BF16); KT = P_sb("KT", [128, NCH * 64], BF16)
        KTM = P_sb("KTM", [128, NCH, 64], BF16); VTM = P_sb("VTM", [128, NCH, 64], BF16)
        SZ = P_sb("SZ", [128, NCH, 64], BF16)
        Gs = P_sb("Gs", [128, NS, 2]); Bs = P_sb("Bs", [128, NS, 2])
        O = P_sb("O", [128, NCH, 64])
        rCH = [Res() for _ in range(NCH)]
        rO = [Res() for _ in range(NCH)]
        P.op("dve", lambda h: h.memset(O[:], 0.0), writes=rO)
        CBL = [P_sb("CBL%d" % i, [128, 3, 132]) for i in range(4)]; rCBL = [Res() for _ in range(4)]
        for i in range(4):
            P.op("dve", lambda h, i=i: h.memset(CBL[i][:], 0.0), writes=[rCBL[i]])

        def step_of(c, d):
            if d == 0:
                return c
            return 4 - c if c < 4 else 136 - c

        FL = []
        PSL = int(os.environ.get("PSL", "2"))
        for li in range(2):
            F = dict(XT=P_sb("XT%d" % li, [128, D]), rXT=Res(), XN=P_sb("XN%d" % li, [128, D]), rXN=Res(),
                     HX=P_sb("HX%d" % li, [128, 8, 128], BF16), rHX=Res(), SM=P_sb("FSM%d" % li, [128, 8]), rSM=Res(),
                     CVb=P_sb("CVb%d" % li, [128, 3, 128]), rCVb=Res(), SQ2=P_sb("SQ2%d" % li, [128, 2, 128]), rSQ2=Res(),
                     RS2=P_sb("RS2%d" % li, [128, 2, 128]), rRS2=Res(), KN=P_sb("KN%d" % li, [128, 128]), rKN=Res(),
                     GT_=P_sb("GT_%d" % li, [128, 2, 2, 8]), rGT_=Res(), EXb=P_sb("EXb%d" % li, [128, 3, 128]), rEXb=Res(),
                     EZ=P_sb("EZ%d" % li, [128, 2, 64]), rEZ=Res(),
                     bT=PS[4 * (li % PSL)], rT=rPS[4 * (li % PSL)], bA=PS[4 * (li % PSL) + 1], rA=rPS[4 * (li % PSL) + 1], bB=PS[4 * (li % PSL) + 2], rB=rPS[4 * (li % PSL) + 2],
                     bZ=PS[4 * (li % PSL) + 3], rZ=rPS[4 * (li % PSL) + 3])
            FL.append(F)

        def conv_gen(m, F):
            CVb, rCVb, SQ2, rSQ2, RS2, rRS2, KN, rKN = F["CVb"], F["rCVb"], F["SQ2"], F["rSQ2"], F["RS2"], F["rRS2"], F["KN"], F["rKN"]
            CB, rCB = CBL[m % 4], rCBL[m % 4]
            rcv = [Res(), Res(), Res()]
            for s_ in range(3):
                P.op("dve", lambda h, s_=s_: h.tensor_scalar(out=CVb[:, s_, :], in0=CB[:, s_, 0:128], scalar1=CW[:, s_, 0:1], scalar2=None, op0=ALU.mult),
                     reads=[rCB, rCW], writes=[rcv[s_], rCVb])
            yield
            for tap in range(1, 5):
                for s_ in range(3):
                    P.op("dve", lambda h, s_=s_, tap=tap: h.scalar_tensor_tensor(out=CVb[:, s_, :], in0=CB[:, s_, tap:tap + 128], scalar=CW[:, s_, tap:tap + 1],
                                                                              in1=CVb[:, s_, :], op0=ALU.mult, op1=ALU.add),
                         reads=[rCB, rCW, rcv[s_]], writes=[rcv[s_]] + ([rCVb] if tap == 4 else []))
                yield
            EXb, rEXb = F["EXb"], F["rEXb"]
            P.act(EXb[:], CVb[:], AF.Exp, reads=[rCVb], writes=[rEXb], scale=-1.0)
            yield
            P.op("dve", lambda h: h.tensor_scalar(out=EXb[:], in0=EXb[:], scalar1=1.0, scalar2=None, op0=ALU.add), reads=[rEXb], writes=[rEXb])
            yield
            P.op("dve", lambda h: h.reciprocal(out=EXb[:], in_=EXb[:]), reads=[rEXb], writes=[rEXb])
            yield
            P.op("dve", lambda h: h.tensor_tensor(out=CVb[:], in0=CVb[:], in1=EXb[:], op=ALU.mult), reads=[rCVb, rEXb], writes=[rCVb])
            yield
            P.op("dve", lambda h: h.tensor_tensor(out=SQ2[:], in0=CVb[:, 0:2, :], in1=CVb[:, 0:2, :], op=ALU.mult), reads=[rCVb], writes=[rSQ2])
            yield
            ps, rps = F["bB"], F["rB"]
            P.mm(ps[:, 0:256], BLK[:], SQ2[:].rearrange("p a t -> p (a t)"), reads=[rBLK, rSQ2], writes=[rps])
            yield
            P.act(RS2[:].rearrange("p a t -> p (a t)"), ps[:, 0:256], AF.Ln, reads=[rps], writes=[rRS2], bias=1e-6, scale=1.0)
            yield
            P.act(RS2[:], RS2[:], AF.Exp, reads=[rRS2], writes=[rRS2], scale=-0.5)
            yield
            rc = [rCH[2 * m], rCH[2 * m + 1]]
            P.op("dve", lambda h: h.scalar_tensor_tensor(out=QT[:, m * 128:(m + 1) * 128], in0=CVb[:, 0, :], scalar=0.125, in1=RS2[:, 0, :], op0=ALU.mult, op1=ALU.mult),
                 reads=[rCVb, rRS2], writes=rc)
            P.op("dve", lambda h: h.tensor_tensor(out=KN[:], in0=CVb[:, 1, :], in1=RS2[:, 1, :], op=ALU.mult), reads=[rCVb, rRS2], writes=[rKN])
            yield
            P.op("dve", lambda h: h.tensor_copy(out=KT[:, m * 128:(m + 1) * 128], in_=KN[:]), reads=[rKN], writes=rc)
            for which in range(2):
                for cc in range(2):
                    for hh in range(2):
                        hs = slice(hh * 64, (hh + 1) * 64)
                        srcap = KN[hs, cc * 64:(cc + 1) * 64] if which == 0 else CVb[hs, 2, cc * 64:(cc + 1) * 64]
                        P.op("pe", lambda h, hs=hs, cc=cc, which=which, srcap=srcap: h.matmul(ps[hs, 256 + which * 128 + cc * 64: 256 + which * 128 + (cc + 1) * 64], srcap, ID[hs, hs], start=True, stop=True),
                             reads=[rKN, rCVb, rID], writes=[rps])
            yield
            P.act(KTM[:, 2 * m:2 * m + 2, :], ps[:, 256:384].rearrange("p (c f) -> p c f", c=2), AF.Copy, reads=[rps], writes=rc)
            P.act(VTM[:, 2 * m:2 * m + 2, :], ps[:, 384:512].rearrange("p (c f) -> p c f", c=2), AF.Copy, reads=[rps], writes=rc)
            yield

        def blk_gen(n):
            F = FL[n % 2]
            j = 1 if n < 2 else 0
            xt, rxt, XN, rXN, hx, rhx, SM, rSM = F["XT"], F["rXT"], F["XN"], F["rXN"], F["HX"], F["rHX"], F["SM"], F["rSM"]
            for (p0, np_, src) in xsrc(n):
                P.dma("sp", xt[p0:p0 + np_, :], src, reads=rsrc, writes=[rxt])
            P.op("dve", lambda h: h.memset(SM[:, 0:1], 0.0), writes=[rSM])
            yield
            P.act(XN[:], xt[:], AF.Square, reads=[rxt], writes=[rXN, rSM], accum_out=SM[:, 0:1])
            yield
            P.act(SM[:, 1:2], SM[:, 0:1], AF.Ln, reads=[rSM], writes=[rSM], bias=1e-6, scale=1.0 / D)
            yield
            P.act(SM[:, 2:3], SM[:, 1:2], AF.Exp, reads=[rSM], writes=[rSM], scale=-0.5)
            yield
            P.op("dve", lambda h: h.tensor_scalar(out=XN[:], in0=xt[:], scalar1=SM[:, 2:3], scalar2=None, op0=ALU.mult),
                 reads=[rxt, rSM], writes=[rXN])
            yield
            ps, rps = F["bT"], F["rT"]
            for half in range(2):
                for k in range(4 * half, 4 * half + 4):
                    P.op("pe", lambda h, k=k, ps=ps: h.transpose(ps[:, (k % 4) * 128:(k % 4 + 1) * 128], XN[:, k * 128:(k + 1) * 128], ID[:]),
                         reads=[rXN, rID], writes=[rps])
                yield
                for k in range(4 * half, 4 * half + 4):
                    P.act(hx[:, k, :], ps[:, (k % 4) * 128:(k % 4 + 1) * 128], AF.Identity, reads=[rps, C["rSCALE1"], C["rMODF"]], writes=[rhx],
                          scale=C["SCALE1"][:, k, j:j + 1], bias=C["MODF"][:, k, j:j + 1])
                yield
            ps, rps = F["bA"], F["rA"]
            for s_ in range(3):
                for k in range(8):
                    P.mm(ps[:, s_ * 128:(s_ + 1) * 128], WA[:, k, s_ * 128:(s_ + 1) * 128], hx[:, k, :], start=(k == 0), stop=(k == 7), reads=[rWA, rhx], writes=[rps])
            yield
            CB, rCB = CBL[n % 4], rCBL[n % 4]
            first = n in (0, 2)
            lastb = n in (1, NB - 1)
            P.act(CB[:, :, 2:130], ps[:, 0:384].rearrange("p (s t) -> p s t", s=3), AF.Copy, reads=[rps], writes=[rCB])
            if first:
                P.op("dve", lambda h: h.memset(CB[:, :, 0:2], 0.0), writes=[rCB])
            else:
                Pv, rPv = CBL[(n - 1) % 4], rCBL[(n - 1) % 4]
                P.op("dve", lambda h: h.tensor_copy(out=CB[:, :, 0:2], in_=Pv[:, :, 128:130]), reads=[rPv], writes=[rCB])
                P.op("dve", lambda h: h.tensor_copy(out=Pv[:, :, 130:132], in_=CB[:, :, 2:4]), reads=[rCB], writes=[rPv])
            if lastb:
                P.op("dve", lambda h: h.memset(CB[:, :, 130:132], 0.0), writes=[rCB])
            yield
            ps, rps = F["bZ"], F["rZ"]
            for cc in range(2):
                for hh in range(2):
                    hs = slice(hh * 64, (hh + 1) * 64)
                    for k in range(8):
                        P.mm(ps[hs, cc * 68:(cc + 1) * 68], hx[:, k, cc * 64:(cc + 1) * 64], WZ[:, k, hh, :], start=(k == 0), stop=(k == 7),
                             reads=[rhx, rWZ], writes=[rps])
            yield
            rc = [rCH[2 * n], rCH[2 * n + 1]]
            pz = ps[:, 0:136].rearrange("p (c f) -> p c f", c=2)
            EZ, rEZ = F["EZ"], F["rEZ"]
            P.act(EZ[:], pz[:, :, 0:64], AF.Exp, reads=[rps], writes=[rEZ], scale=-1.0)
            P.op("dve", lambda h: h.tensor_scalar(out=EZ[:], in0=EZ[:], scalar1=1.0, scalar2=None, op0=ALU.add), reads=[rEZ], writes=[rEZ])
            yield
            P.op("dve", lambda h: h.reciprocal(out=EZ[:], in_=EZ[:]), reads=[rEZ], writes=[rEZ])
            yield
            P.op("dve", lambda h: h.tensor_tensor(out=SZ[:, 2 * n:2 * n + 2, :], in0=pz[:, :, 0:64], in1=EZ[:], op=ALU.mult), reads=[rps, rEZ], writes=rc)
            GT_, rGT_ = F["GT_"], F["rGT_"]
            xa = GT_[:, :, :, 0]; ab = GT_[:, :, :, 1]; ee = GT_[:, :, :, 2]; ll = GT_[:, :, :, 3]; rr = GT_[:, :, :, 4]; gg = GT_[:, :, :, 5]; bb = GT_[:, :, :, 6]
            P.op("dve", lambda h: h.tensor_tensor(out=xa, in0=pz[:, :, 64:66], in1=DTB[:].unsqueeze(1).to_broadcast([128, 2, 2]), op=ALU.add), reads=[rps, rDTB], writes=[rGT_])
            yield
            P.act(bb, pz[:, :, 66:68], AF.Exp, reads=[rps], writes=[rGT_], scale=-1.0)
            P.act(ab, xa, AF.Abs, reads=[rGT_], writes=[rGT_])
            yield
            P.op("dve", lambda h: h.tensor_scalar(out=bb, in0=bb, scalar1=1.0, scalar2=None, op0=ALU.add), reads=[rGT_], writes=[rGT_])
            P.op("dve", lambda h: h.reciprocal(out=bb, in_=bb), reads=[rGT_], writes=[rGT_])
            yield
            P.act(ee, ab, AF.Exp, reads=[rGT_], writes=[rGT_], scale=-1.0)
            yield
            P.act(ll, ee, AF.Ln, reads=[rGT_], writes=[rGT_], bias=1.0, scale=1.0)
            P.op("dve", lambda h: h.tensor_scalar(out=rr, in0=xa, scalar1=0.0, scalar2=None, op0=ALU.max), reads=[rGT_], writes=[rGT_])
            yield
            P.op("dve", lambda h: h.tensor_tensor(out=rr, in0=rr, in1=ll, op=ALU.add), reads=[rGT_], writes=[rGT_])
            yield
            P.op("dve", lambda h: h.tensor_tensor(out=gg, in0=rr, in1=NEGA[:].unsqueeze(1).to_broadcast([128, 2, 2]), op=ALU.mult), reads=[rGT_, rNEGA], writes=[rGT_])
            yield
            for cc in range(2):
                c = 2 * n + cc
                for d in range(2):
                    s2 = step_of(c, d)
                    P.op("dve", lambda h, cc=cc, d=d, s2=s2: h.tensor_copy(out=Gs[:, s2, d:d + 1], in_=GT_[:, cc, d, 5:6]), reads=[rGT_], writes=[rCH[c]])
                    P.op("dve", lambda h, cc=cc, d=d, s2=s2: h.tensor_copy(out=Bs[:, s2, d:d + 1], in_=GT_[:, cc, d, 6:7]), reads=[rGT_], writes=[rCH[c]])
                yield
            if not first:
                yield from conv_gen(n - 1, F)
            if lastb:
                yield from conv_gen(n, F)

        active = []
        nxt = 0
        GRAN = int(os.environ.get("GRAN", "1"))
        while nxt < NB or active:
            while len(active) < int(os.environ.get("DNLANES", "2")) and nxt < NB:
                active.append(blk_gen(nxt)); nxt += 1
            for g in list(active):
                try:
                    for _ in range(GRAN):
                        next(g)
                except StopIteration:
                    active.remove(g)

        Sst = [(P_sb("S0", [128, 2, 64]), Res()), (P_sb("S1", [128, 2, 64]), Res())]
        for (t, r) in Sst:
            P.op("dve", lambda h, t=t: h.memset(t[:], 0.0), writes=[r])
        HS = [slice(0, 64), slice(64, 128)]
        dve = lambda fn, reads, writes: P.op("dve", fn, reads, writes)

        def make_lane(li):
            L = {}
            for nm in ("GBC", "BBC", "EB", "DT1", "TA", "DECT", "DEC", "DECS", "DECTS", "VB", "KBE", "KD", "QGT", "AQM", "NWT", "VN", "SGL", "C0", "C1"):
                L[nm] = (P_sb("%s_%d" % (nm, li), [128, 2, 64]), Res())
            L["W0"] = (P_sb("W0_%d" % li, [128, 2, 128]), Res()); L["W1"] = (P_sb("W1_%d" % li, [128, 2, 128]), Res())
            L["SC"] = (P_sb("SC_%d" % li, [128, 8]), Res())
            a, b, c = PS[3 * li], PS[3 * li + 1], PS[3 * li + 2]
            v = lambda bank, lo, n: bank[:, lo:lo + 2 * n].rearrange("p (d f) -> p d f", d=2)
            ra, rb, rc = rPS[3 * li], rPS[3 * li + 1], rPS[3 * li + 2]
            L["ps1"] = (v(a, 0, 64), ra); L["ps4"] = (v(a, 128, 64), ra); L["ps2"] = (v(a, 256, 64), ra); L["ps3"] = (v(a, 384, 64), ra)
            L["psI"] = (v(b, 0, 128), rb); L["psC"] = (v(b, 256, 64), rb); L["pcol"] = (b[:, 384:386], rb)
            L["ps5"] = (v(c, 0, 64), rc); L["pswT"] = (v(c, 128, 64), rc); L["ps6"] = (v(c, 256, 64), rc); L["ps7"] = (v(c, 384, 64), rc)
            return L

        lanes = [make_lane(0), make_lane(1)]

        def step_gen(s, L):
            GBC, rGBC = L["GBC"]; BBC, rBBC = L["BBC"]; EB, rEB = L["EB"]; DT1, rDT1 = L["DT1"]; TA, rTA = L["TA"]
            DECT, rDECT = L["DECT"]; DEC, rDEC = L["DEC"]; DECS, rDECS = L["DECS"]; DECTS, rDECTS = L["DECTS"]
            VB, rVB = L["VB"]; KBE, rKBE = L["KBE"]; KD, rKD = L["KD"]; QGT, rQGT = L["QGT"]; AQM, rAQM = L["AQM"]
            NWT, rNWT = L["NWT"]; VN, rVN = L["VN"]; SGL, rSGL = L["SGL"]; SC, rSC = L["SC"]
            Cb = [L["C0"], L["C1"]]; Wb = [L["W0"], L["W1"]]
            ps1, r1 = L["ps1"]; ps4, r4 = L["ps4"]; ps2, r2 = L["ps2"]; ps3, r3 = L["ps3"]
            psI, rI = L["psI"]; psC, rC = L["psC"]; pcol, rcol = L["pcol"]
            ps5, r5 = L["ps5"]; pswT, rwT = L["pswT"]; ps6, r6 = L["ps6"]; ps7, r7 = L["ps7"]
            dirs = [d for d in range(2) if (d == 0 and s <= NCH - 1) or (d == 1 and s >= 1)]
            ch = {0: s, 1: (4 - s if s <= 4 else 136 - s)}
            d0, d1 = dirs[0], dirs[-1] + 1
            ds = slice(d0, d1)
            nd = d1 - d0
            rch = [rCH[ch[d]] for d in dirs]
            Scur, rScur = Sst[s % 2]; Snew, rSnew = Sst[(s + 1) % 2]
            tok = {d: slice(ch[d] * 64, ch[d] * 64 + 64) for d in dirs}
            dve(lambda h: h.tensor_tensor(out=GBC[:, ds, :], in0=ONES3[:, ds, :], in1=Gs[:, s, ds].unsqueeze(2).to_broadcast([128, nd, 64]), op=ALU.mult), [rONES3] + rch, [rGBC])
            dve(lambda h: h.tensor_tensor(out=BBC[:, ds, :], in0=ONES3[:, ds, :], in1=Bs[:, s, ds].unsqueeze(2).to_broadcast([128, nd, 64]), op=ALU.mult), [rONES3] + rch, [rBBC])
            yield
            for d in dirs:
                for hs in HS:
                    P.mm(ps1[hs, d, :], GBC[hs, d, :], TRI[hs, d, :], reads=[rGBC, rMSK], writes=[r1])
            for d in dirs:
                for hs in HS:
                    P.mm(pcol[hs, d:d + 1], TRI[hs, d, :], Gs[hs, s, d:d + 1], reads=[rMSK] + rch, writes=[rcol])
            for d in dirs:
                for hs in HS:
                    P.mm(ps4[hs, d, :], BBC[hs, d, :], ID2[hs, d, :], reads=[rBBC, rMSK], writes=[r4])
            for d in dirs:
                for hs in HS:
                    P.mm(ps2[hs, d, :], KT[hs, tok[d]], KT[hs, tok[d]], reads=rch, writes=[r2])
            for d in dirs:
                for hs in HS:
                    P.mm(ps3[hs, d, :], KT[hs, tok[d]], QT[hs, tok[d]], reads=rch, writes=[r3])
            yield
            dve(lambda h: h.tensor_copy(out=SC[:, 0:2][:, ds], in_=pcol[:, ds]), [rcol], [rSC])
            P.act(EB[:, ds, :], ps1[:, ds, :], AF.Exp, reads=[r1], writes=[rEB])
            yield
            P.act(SC[:, 2:4][:, ds], SC[:, 0:2][:, ds], AF.Exp, reads=[rSC], writes=[rSC])
            dve(lambda h: h.tensor_tensor(out=DT1[:, ds, :], in0=ps1[:, ds, :], in1=SC[:, 0:2][:, ds].unsqueeze(2).to_broadcast([128, nd, 64]), op=ALU.subtract), [r1, rSC], [rDT1])
            yield
            dve(lambda h: h.tensor_tensor(out=TA[:, ds, :], in0=DT1[:, ds, :], in1=NEGMT[:, ds, :], op=ALU.add), [rDT1, rMSK], [rTA])
            yield
            P.act(DECT[:, ds, :], TA[:, ds, :], AF.Exp, reads=[rTA], writes=[rDECT])
            yield
            dve(lambda h: h.scalar_tensor_tensor(out=TA[:, ds, :], in0=DT1[:, ds, :], scalar=-1.0, in1=NEGM[:, ds, :], op0=ALU.mult, op1=ALU.add), [rDT1, rMSK, rDECT], [rTA])
            yield
            P.act(DEC[:, ds, :], TA[:, ds, :], AF.Exp, reads=[rTA], writes=[rDEC])
            for d in dirs:
                lastc = 63 if d == 0 else 0
                dve(lambda h, d=d, lastc=lastc: h.tensor_tensor(out=SC[:, 4 + d:5 + d], in0=ps1[:, d, lastc:lastc + 1], in1=SC[:, d:d + 1], op=ALU.subtract), [r1, rSC], [rSC])
            yield
            P.act(SC[:, 4:6][:, ds], SC[:, 4:6][:, ds], AF.Exp, reads=[rSC], writes=[rSC])
            C0, rC0 = Cb[0]; W0, rW0 = Wb[0]
            dve(lambda h: h.tensor_tensor(out=DECS[:, ds, :], in0=DEC[:, ds, :], in1=NST[:, ds, :], op=ALU.mult), [rDEC, rMSK], [rDECS])
            yield
            for d in dirs:
                dve(lambda h, d=d: h.scalar_tensor_tensor(out=C0[:, d, :], in0=ps2[:, d, :], scalar=Bs[:, s, d:d + 1], in1=DECS[:, d, :], op0=ALU.mult, op1=ALU.mult),
                    [r2, rDECS] + rch, [rC0])
            yield
            dve(lambda h: h.tensor_tensor(out=DECTS[:, ds, :], in0=DECT[:, ds, :], in1=NSTT[:, ds, :], op=ALU.mult), [rDECT, rMSK], [rDECTS])
            yield
            dve(lambda h: h.tensor_tensor(out=DECTS[:, ds, :], in0=ps2[:, ds, :], in1=DECTS[:, ds, :], op=ALU.mult), [r2, rDECTS], [rDECTS])
            yield
            dve(lambda h: h.tensor_tensor(out=W0[:, ds, 0:64], in0=ps4[:, ds, :], in1=DECTS[:, ds, :], op=ALU.mult), [r4, rDECTS], [rW0])
            dve(lambda h: h.tensor_copy(out=W0[:, ds, 64:128], in_=ID2[:, ds, :]), [rMSK], [rW0])
            yield
            for m in range(6):
                (Wc, rWc), (Wn, rWn) = Wb[m % 2], Wb[(m + 1) % 2]
                (Cc, rCc), (Cn, rCn) = Cb[m % 2], Cb[(m + 1) % 2]
                for d in dirs:
                    for hs in HS:
                        P.mm(psI[hs, d, :], Cc[hs, d, :], Wc[hs, d, :], reads=[rCc, rWc], writes=[rI])
                if m < 5:
                    for d in dirs:
                        for hs in HS:
                            P.mm(psC[hs, d, :], Wc[hs, d, 0:64], Cc[hs, d, :], reads=[rCc, rWc], writes=[rC])
                yield
                dve(lambda h, Wn=Wn, Wc=Wc: h.tensor_tensor(out=Wn[:, ds, 64:128], in0=Wc[:, ds, 64:128], in1=psI[:, ds, 64:128], op=ALU.add), [rWc, rI], [rWn])
                if m < 5:
                    P.act(Wn[:, ds, 0:64], psI[:, ds, 0:64], AF.Copy, reads=[rI], writes=[rWn])
                    P.act(Cn[:, ds, :], psC[:, ds, :], AF.Copy, reads=[rC], writes=[rCn])
                yield
                if m == 2:
                    yield "HALF"
            TITt, rTIT = Wb[0]
            TIT = TITt[:, :, 64:128]
            for d in dirs:
                c = ch[d]
                dve(lambda h, d=d, c=c: h.tensor_scalar(out=VB[:, d, :], in0=VTM[:, c, :], scalar1=Bs[:, s, d:d + 1], scalar2=None, op0=ALU.mult), rch, [rVB])
                dve(lambda h, d=d, c=c: h.tensor_scalar(out=KBE[:, d, :], in0=KTM[:, c, :], scalar1=Bs[:, s, d:d + 1], scalar2=SC[:, 2 + d:3 + d], op0=ALU.mult, op1=ALU.mult), rch + [rSC], [rKBE])
                yield
                dve(lambda h, d=d, c=c: h.tensor_scalar(out=KD[:, d, :], in0=KTM[:, c, :], scalar1=SC[:, 4 + d:5 + d], scalar2=None, op0=ALU.mult), rch + [rSC], [rKD])
                dve(lambda h, d=d: h.tensor_tensor(out=QGT[:, d, :], in0=QT[:, tok[d]], in1=EB[:, d, :], op=ALU.mult), rch + [rEB], [rQGT])
                yield
            dve(lambda h: h.tensor_tensor(out=AQM[:, ds, :], in0=ps3[:, ds, :], in1=DECT[:, ds, :], op=ALU.mult), [r3, rDECT], [rAQM])
            for d in dirs:
                for hs in HS:
                    P.mm(pswT[hs, d, :], KBE[hs, d, :], TIT[hs, d, :], reads=[rKBE, rTIT], writes=[rwT])
            yield
            dve(lambda h: h.tensor_scalar(out=NWT[:, ds, :], in0=pswT[:, ds, :], scalar1=-1.0, scalar2=None, op0=ALU.mult), [rwT], [rNWT])
            yield
            for d in dirs:
                for hs in HS:
                    P.mm(ps5[hs, d, :], TIT[hs, d, :], VB[hs, d, :], start=True, stop=False, reads=[rTIT, rVB], writes=[r5])
                    P.mm(ps5[hs, d, :], NWT[hs, d, :], Scur[hs, d, :], start=False, stop=True, reads=[rNWT, rScur], writes=[r5])
            yield
            dve(lambda h: h.tensor_copy(out=VN[:, ds, :], in_=ps5[:, ds, :]), [r5], [rVN])
            yield
            for d in dirs:
                for hs in HS:
                    P.mm(ps6[hs, d, :], QGT[hs, d, :], Scur[hs, d, :], start=True, stop=False, reads=[rQGT, rScur], writes=[r6])
                    P.mm(ps6[hs, d, :], AQM[hs, d, :], VN[hs, d, :], start=False, stop=True, reads=[rAQM, rVN], writes=[r6])
            for d in dirs:
                for hs in HS:
                    P.mm(ps7[hs, d, :], KD[hs, d, :], VN[hs, d, :], reads=[rKD, rVN], writes=[r7])
            yield
            for d in range(2):
                if d in dirs:
                    lastc = 63 if d == 0 else 0
                    dve(lambda h, d=d, lastc=lastc: h.tensor_scalar(out=SGL[:, d, :], in0=Scur[:, d, :], scalar1=EB[:, d, lastc:lastc + 1], scalar2=None, op0=ALU.mult), [rScur, rEB], [rSGL])
                    dve(lambda h, d=d: h.tensor_tensor(out=Snew[:, d, :], in0=SGL[:, d, :], in1=ps7[:, d, :], op=ALU.add), [rSGL, r7], [rSnew])
                else:
                    dve(lambda h, d=d: h.tensor_copy(out=Snew[:, d, :], in_=Scur[:, d, :]), [rScur], [rSnew])
            yield
            for d in dirs:
                c = ch[d]
                dve(lambda h, d=d, c=c: h.tensor_tensor(out=O[:, c, :], in0=O[:, c, :], in1=ps6[:, d, :], op=ALU.add), [r6, rO[c]], [rO[c]])
            yield

        def drive(older, newer):
            a_done = older is None
            b_done = newer is None
            while not (a_done and b_done):
                if not a_done:
                    try:
                        next(older)
                    except StopIteration:
                        a_done = True
                if not b_done:
                    try:
                        if next(newer) == "HALF":
                            b_done = True
                    except StopIteration:
                        b_done = True

        prev = None
        for s in range(NS):
            g = step_gen(s, lanes[s % 2])
            drive(prev, g)
            prev = g
        drive(prev, None)

        P.fence()
        G = 33
        OSQ = P_sb("OSQ", [128, G, 64]); rOSQ = Res()
        OSS = P_sb("OSS", [128, G]); rOSS = Res()
        for g in range(NCH // G):
            cs = slice(g * G, (g + 1) * G)
            ro = [rO[c] for c in range(g * G, (g + 1) * G)]
            dve(lambda h, cs=cs: h.tensor_tensor(out=OSQ[:], in0=O[:, cs, :], in1=O[:, cs, :], op=ALU.mult), ro, [rOSQ])
            dve(lambda h: h.reduce_sum(out=OSS[:], in_=OSQ[:], axis=AX.X), [rOSQ], [rOSS])
            P.act(OSS[:], OSS[:], AF.Ln, reads=[rOSS], writes=[rOSS], bias=1e-6, scale=1.0 / 64)
            P.act(OSS[:], OSS[:], AF.Exp, reads=[rOSS], writes=[rOSS], scale=-0.5)
            dve(lambda h, cs=cs: h.tensor_tensor(out=O[:, cs, :], in0=O[:, cs, :], in1=OSS[:].unsqueeze(2).to_broadcast([128, G, 64]), op=ALU.mult), ro + [rOSS], ro)
            dve(lambda h, cs=cs: h.tensor_tensor(out=O[:, cs, :], in0=O[:, cs, :], in1=GDN[:].unsqueeze(1).to_broadcast([128, G, 64]), op=ALU.mult), ro + [rGDN], ro)
            dve(lambda h, cs=cs: h.tensor_tensor(out=O[:, cs, :], in0=O[:, cs, :], in1=SZ[:, cs, :], op=ALU.mult), ro + [rCH[c] for c in range(g * G, (g + 1) * G)], ro)
        ov = o_dn.rearrange("(c t) (h d) -> h t c d", t=64, h=2)
        for hh in range(2):
            P.dma("sp", ov[hh], O[hh * 64:(hh + 1) * 64, :, :], reads=rO, writes=[rOUT[hh]])
        P.fence()


NE = 32


def emit_b(P, G, IN, l, NL, NC, xs, rxs, omall, romall, xo, rOUT):
    NT = NL + NC
    cvT = IN["cvT"]; adaw = IN["adaw2", l]; adab = IN["adab2", l]; adabT = IN["adab2T", l]
    g2T = IN["g2T", l]; wout = IN["wout", l]; rw = IN["rw", l]; rb = IN["rb", l]
    wgu = IN["wgu", l]; bguT = IN["bguT", l]; wd = IN["wd", l]; bd = IN["bd", l]; seli = IN["seli"]

    tiles = [(i * 128, 128, 0) for i in range(NL // 128)]
    if NC:
        tiles.append((NL, NC, 1))
    nlt = NL // 128
    passes = [list(range(0, nlt // 2)), list(range(nlt // 2, len(tiles)))]
    MAXT = max(len(p) for p in passes)

    with ExitStack() as st0:
        _sb = P.sb
        P_sb = lambda name, shape, dt=F32, stack=None: _sb("b_" + name, shape, dt, stack=(stack or st0))
        ID, rID, ONES, rONES, ONESB, rONESB, PS, rPS = G["ID"], G["rID"], G["ONES"], G["rONES"], G["ONESB"], G["rONESB"], G["PS"], G["rPS"]
        SELI = P_sb("SELI", [128, 4, 128], BF16); rSELI = Res()
        P.dma("pool", SELI[:], seli, writes=[rSELI])

        GT = P_sb("GT", [128, 2, 2, D]); rGT = Res()
        MODF = P_sb("MODF", [128, 16, 2]); rMODF = Res()
        SCALE2 = P_sb("SCALE2", [128, 8, 2]); rSCALE2 = Res()
        G2 = P_sb("G2", [128, 8]); rG2 = Res()
        P.dma("sp", G2[:], g2T, writes=[rG2])
        ABT = P_sb("ABT", [128, 16]); rABT = Res()
        P.dma("sp", ABT[:], adabT, writes=[rABT])
        RW = P_sb("RW", [128, 8, NE]); rRW = Res()
        P.dma("sp", RW[:], rw.rearrange("(k p) e -> p k e", p=128), writes=[rRW])
        RB = P_sb("RB", [1, NE]); rRB = Res()
        P.dma("sp", RB[:], rb, writes=[rRB])
        WO = P_sb("WO", [128, 8, D], BF16); rWO = Res()
        P.dma("pool", WO[:], wout.rearrange("(k p) f -> p k f", p=128), writes=[rWO])
        sub = ExitStack()
        ABR = P_sb("ABR", [1, 4096], stack=sub); rABR = Res()
        P.dma("sp", ABR[:], adab, writes=[rABR])
        CV = P_sb("CV", [128, 8, 2], stack=sub); rCV = Res()
        P.dma("sp", CV[:], cvT, writes=[rCV])
        SCV = P_sb("SCV", [128, 8, 2], stack=sub); rSCV = Res()
        P.act(SCV[:], CV[:], AF.Silu, reads=[rCV], writes=[rSCV])
        SCB = P_sb("SCB", [128, 8, 2, 128], stack=sub); rSCB = Res()
        P.op("dve", lambda h: h.tensor_tensor(out=SCB[:], in0=ONES[:].unsqueeze(1).unsqueeze(1).to_broadcast([128, 8, 2, 128]),
                                              in1=SCV[:].unsqueeze(3).to_broadcast([128, 8, 2, 128]), op=ALU.mult),
             reads=[rONES, rSCV], writes=[rSCB])

        AW = [P_sb("AW%d" % i, [128, 8, 512], stack=sub) for i in range(2)]; rAW = [Res(), Res()]
        for blk in range(8):
            aw, raw = AW[blk % 2], rAW[blk % 2]
            P.dma("sp", aw[:], adaw[:, blk * 512:(blk + 1) * 512].rearrange("(k p) f -> p k f", p=128), writes=[raw])
            if blk in (0, 1, 6, 7):
                which = 0 if blk < 2 else 1
                half = blk % 2
                for j in range(2):
                    ps, rps = PS[j], rPS[j]
                    for k in range(8):
                        P.mm(ps[:], SCB[:, k, j, :], aw[:, k, :], start=(k == 0), stop=False,
                             reads=[rSCB, raw], writes=[rps])
                    P.mm(ps[:], ONES[0:1, :], ABR[0:1, blk * 512:(blk + 1) * 512], start=False, stop=True,
                         reads=[rONES, rABR], writes=[rps])
                    P.act(GT[:, which, j, half * 512:(half + 1) * 512], ps[:], AF.Copy, reads=[rps], writes=[rGT])
            else:
                ps, rps = PS[2 + blk % 2], rPS[2 + blk % 2]
                for fcl in range(4):
                    fcg = (blk - 2) * 4 + fcl
                    for k in range(8):
                        P.mm(ps[:, fcl * 2:fcl * 2 + 2], aw[:, k, fcl * 128:(fcl + 1) * 128], SCV[:, k, :],
                             start=(k == 0), stop=(k == 7), reads=[rSCV, raw], writes=[rps])
                for fcl in range(4):
                    fcg = (blk - 2) * 4 + fcl
                    P.act(MODF[:, fcg, :], ps[:, fcl * 2:fcl * 2 + 2], AF.Identity, reads=[rps, rABT], writes=[rMODF],
                          bias=ABT[:, fcg:fcg + 1], scale=1.0)
        P.op("dve", lambda h: h.tensor_scalar(out=SCALE2[:], in0=MODF[:, 8:16, :], scalar1=1.0, scalar2=None, op0=ALU.add),
             reads=[rMODF], writes=[rSCALE2])
        P.op("dve", lambda h: h.tensor_tensor(out=SCALE2[:], in0=SCALE2[:], in1=G2[:].unsqueeze(2).to_broadcast([128, 8, 2]), op=ALU.mult),
             reads=[rSCALE2, rG2], writes=[rSCALE2])

        P.fence()
        sub.close()
        X1 = P_sb("X1", [128, MAXT, D]); rX1 = [Res() for _ in range(MAXT)]
        H2B = P_sb("H2B", [128, 8, MAXT * 128], BF16); rH2B = [Res() for _ in range(MAXT)]
        GATES = P_sb("GATES", [128, MAXT, NE]); rGATES = [Res() for _ in range(MAXT)]
        XT = [P_sb("XT%d" % i, [128, D]) for i in range(2)]; rXT = [Res(), Res()]
        OM = [P_sb("OM%d" % i, [128, 8, 128], BF16) for i in range(2)]; rOM = [Res(), Res()]
        OMX = P_sb("OMX", [128, 4, 4, 256], BF16); rOMX = Res()
        TMP = [P_sb("TMP%d" % i, [128, D]) for i in range(2)]; rTMP = [Res(), Res()]
        H2F = P_sb("H2F", [128, 8, 128]); rH2F = Res()
        SMALL = P_sb("SMALL", [128, 64]); rSM = Res()
        LG = P_sb("LG", [128, NE]); rLG = Res()
        EX = P_sb("EX", [128, NE]); rEX = Res()
        MK = P_sb("MK", [128, NE]); rMK = Res()
        WGU = P_sb("WGU", [128, 8, 2 * D], BF16); rWG = [Res() for _ in range(8)]; rWU = [Res() for _ in range(8)]
        WD = P_sb("WD", [128, 8, D], BF16); rWD = [Res() for _ in range(8)]
        BGU = [P_sb("BGU%d" % i, [128, 16]) for i in range(2)]; rBGU = [Res(), Res()]
        BD = [P_sb("BD%d" % i, [1, D], BF16) for i in range(2)]; rBD = [Res(), Res()]
        BGX = [P_sb("BGX%d" % i, [128, 16]) for i in range(2)]; rBGX = [Res(), Res()]
        SIGC = float(1.0 / (1.0 + np.exp(np.float64(-1.702 * 7.0))))
        ACTT = [P_sb("ACTT%d" % i, [128, 8, 512], BF16) for i in range(2)]; rACTT = [Res(), Res()]
        GP = [P_sb("GP%d" % i, [128, 512]) for i in range(2)]; rGP = [Res(), Res()]
        SG = [P_sb("SG%d" % i, [128, 512]) for i in range(2)]; rSG = [Res(), Res()]
        UP = [P_sb("UP%d" % i, [128, 512]) for i in range(2)]; rUP = [Res(), Res()]
        wcount = 0
        cnt = 0

        for pss in passes:
            for li, ti in enumerate(pss):
                r0, nr, j = tiles[ti]
                xt, rxt = XT[li % 2], rXT[li % 2]
                om, rom = OM[li % 2], rOM[li % 2]
                tmp, rtmp = TMP[li % 2], rTMP[li % 2]
                P.dma("sp", xt[:nr, :], xs[r0:r0 + nr, :], reads=rxs, writes=[rxt])
                for q in range(4):
                    grow = (256 + q * NL + r0) if j == 0 else (q * NC)
                    for jj in range(4):
                        P.dma("pool", OMX[:nr, q, jj, :], omall(jj, grow, nr), reads=romall, writes=[rOMX])
                for k in range(8):
                    ps, rps = PS[2 + k // 4], rPS[2 + k // 4]
                    for q in range(4):
                        P.mm(ps[:, (k % 4) * 128:(k % 4) * 128 + nr], OMX[:nr, q, k % 4, (k // 4) * 128:(k // 4 + 1) * 128], SELI[:nr, q, :nr],
                             start=(q == 0), stop=(q == 3), reads=[rOMX, rSELI], writes=[rps])
                for kk in range(2):
                    P.act(om[:, 4 * kk:4 * kk + 4, :nr], PS[2 + kk][:, :].rearrange("p (a t) -> p a t", a=4)[:, :, :nr], AF.Copy, reads=[rPS[2 + kk]], writes=[rom])
                for half in range(2):
                    ps, rps = PS[half], rPS[half]
                    for k in range(8):
                        P.mm(ps[:nr, :], om[:, k, :nr], WO[:, k, half * 512:(half + 1) * 512], start=(k == 0), stop=(k == 7),
                             reads=[rom, rWO], writes=[rps])
                    P.op("dve", lambda h, ps=ps, tmp=tmp, half=half, j=j, nr=nr: h.tensor_tensor(
                        out=tmp[:nr, half * 512:(half + 1) * 512], in0=ps[:nr, :], in1=GT[:nr, 0, j, half * 512:(half + 1) * 512], op=ALU.mult),
                        reads=[rps, rGT], writes=[rtmp])
                P.op("dve", lambda h, tmp=tmp, xt=xt, li=li, nr=nr: h.tensor_tensor(out=X1[:nr, li, :], in0=tmp[:nr, :], in1=xt[:nr, :], op=ALU.add),
                     reads=[rtmp, rxt], writes=[rX1[li]])
                P.act(tmp[:nr, :], X1[:nr, li, :], AF.Square, reads=[rX1[li]], writes=[rtmp, rSM], accum_out=SMALL[:nr, 0:1])
                P.act(SMALL[:nr, 1:2], SMALL[:nr, 0:1], AF.Sqrt, reads=[rSM], writes=[rSM], bias=1e-6, scale=1.0 / D)
                P.op("dve", lambda h, nr=nr: h.reciprocal(out=SMALL[:nr, 2:3], in_=SMALL[:nr, 1:2]), reads=[rSM], writes=[rSM])
                P.op("dve", lambda h, tmp=tmp, li=li, nr=nr: h.tensor_scalar(out=tmp[:nr, :], in0=X1[:nr, li, :], scalar1=SMALL[:nr, 2:3], scalar2=None, op0=ALU.mult),
                     reads=[rX1[li], rSM], writes=[rtmp])
                for k in range(8):
                    ps, rps = PS[2 + k // 4], rPS[2 + k // 4]
                    P.op("pe", lambda h, ps=ps, tmp=tmp, k=k, nr=nr: h.transpose(ps[:, (k % 4) * 128:(k % 4) * 128 + nr], tmp[:nr, k * 128:(k + 1) * 128], ID[:nr, :nr]),
                         reads=[rtmp, rID], writes=[rps])
                for k in range(8):
                    ps, rps = PS[2 + k // 4], rPS[2 + k // 4]
                    P.act(H2F[:, k, :nr], ps[:, (k % 4) * 128:(k % 4) * 128 + nr], AF.Identity, reads=[rps, rSCALE2, rMODF], writes=[rH2F],
                          scale=SCALE2[:, k, j:j + 1], bias=MODF[:, k, j:j + 1])
                P.op("dve", lambda h, li=li, nr=nr: h.tensor_copy(out=H2B[:, :, li * 128:li * 128 + nr], in_=H2F[:, :, :nr]),
                     reads=[rH2F], writes=[rH2B[li]])
                ps, rps = PS[4], rPS[4]
                for k in range(8):
                    P.mm(ps[:nr, 0:NE], H2F[:, k, :nr], RW[:, k, :], start=(k == 0), stop=False, reads=[rH2F, rRW], writes=[rps])
                P.mm(ps[:nr, 0:NE], ONES[0:1, :nr], RB[0:1, :], start=False, stop=True, reads=[rONES, rRB], writes=[rps])
                P.op("dve", lambda h, ps=ps, nr=nr: h.tensor_copy(out=LG[:nr, :], in_=ps[:nr, 0:NE]), reads=[rps], writes=[rLG])
                P.op("dve", lambda h, nr=nr: h.max(out=SMALL[:nr, 8:16], in_=LG[:nr, :]), reads=[rLG], writes=[rSM])
                P.op("dve", lambda h, nr=nr: h.tensor_scalar(out=SMALL[:nr, 16:17], in0=SMALL[:nr, 8:9], scalar1=-1.0, scalar2=None, op0=ALU.mult),
                     reads=[rSM], writes=[rSM])
                P.act(EX[:nr, :], LG[:nr, :], AF.Exp, reads=[rLG, rSM], writes=[rEX], bias=SMALL[:nr, 16:17], scale=1.0)
                P.op("dve", lambda h, nr=nr: h.tensor_scalar(out=MK[:nr, :], in0=LG[:nr, :], scalar1=SMALL[:nr, 11:12], scalar2=None, op0=ALU.is_ge),
                     reads=[rLG, rSM], writes=[rMK])
                P.op("dve", lambda h, nr=nr: h.tensor_tensor(out=EX[:nr, :], in0=EX[:nr, :], in1=MK[:nr, :], op=ALU.mult),
                     reads=[rEX, rMK], writes=[rEX])
                P.op("dve", lambda h, nr=nr: h.reduce_sum(out=SMALL[:nr, 17:18], in_=EX[:nr, :], axis=AX.X), reads=[rEX], writes=[rSM])
                P.op("dve", lambda h, nr=nr: h.reciprocal(out=SMALL[:nr, 18:19], in_=SMALL[:nr, 17:18]), reads=[rSM], writes=[rSM])
                P.op("dve", lambda h, li=li, nr=nr: h.tensor_scalar(out=GATES[:nr, li, :], in0=EX[:nr, :], scalar1=SMALL[:nr, 18:19], scalar2=None, op0=ALU.mult),
                     reads=[rEX, rSM], writes=[rGATES[li]])

            groups = []
            li = 0
            while li < len(pss):
                g = []
                while li < len(pss) and len(g) < 4 and tiles[pss[li]][1] == 128:
                    g.append(li); li += 1
                if not g:
                    g = [li]; li += 1
                groups.append(g)
            for e in range(IN.get("nex", NE)):
                wb = wcount % 2
                wcount += 1
                for fc in range(8):
                    P.dma("pool", WGU[:, :, fc * 128:(fc + 1) * 128], wgu[e, :, fc * 128:(fc + 1) * 128].rearrange("(k p) f -> p k f", p=128),
                          writes=[rWG[fc]])
                    P.dma("pool", WGU[:, :, D + fc * 128:D + (fc + 1) * 128], wgu[e, :, D + fc * 128:D + (fc + 1) * 128].rearrange("(k p) f -> p k f", p=128),
                          writes=[rWU[fc]])
                for fc in range(8):
                    P.dma("pool", WD[:, fc, :], wd[e, fc * 128:(fc + 1) * 128, :], writes=[rWD[fc]])
                P.dma("sp", BGU[wb][:], bguT[e], writes=[rBGU[wb]])
                P.op("dve", lambda h, wb=wb: h.tensor_scalar(out=BGX[wb][:, 0:8], in0=BGU[wb][:, 0:8], scalar1=1.702, scalar2=None, op0=ALU.mult),
                     reads=[rBGU[wb]], writes=[rBGX[wb]])
                P.op("dve", lambda h, wb=wb: h.tensor_scalar(out=BGX[wb][:, 8:16], in0=BGU[wb][:, 8:16], scalar1=1.0, scalar2=None, op0=ALU.add),
                     reads=[rBGU[wb]], writes=[rBGX[wb]])
                P.dma("pool", BD[wb][:], bd[e:e + 1, :], writes=[rBD[wb]])
                for g in groups:
                    ntok = sum(tiles[pss[l]][1] for l in g)
                    c0 = g[0] * 128
                    ab = cnt % 2
                    cnt += 1
                    actt, ractt = ACTT[ab], rACTT[ab]
                    rh = [rH2B[l] for l in g]
                    for fc in range(8):
                        pb = (fc % 2) * 2
                        psg, rpsg = PS[pb], rPS[pb]
                        psu, rpsu = PS[pb + 1], rPS[pb + 1]
                        for k in range(8):
                            P.mm(psg[:, :ntok], WGU[:, k, fc * 128:(fc + 1) * 128], H2B[:, k, c0:c0 + ntok], start=(k == 0), stop=(k == 7),
                                 reads=[rWG[fc]] + rh, writes=[rpsg])
                        for k in range(8):
                            P.mm(psu[:, :ntok], WGU[:, k, D + fc * 128:D + (fc + 1) * 128], H2B[:, k, c0:c0 + ntok], start=(k == 0), stop=(k == 7),
                                 reads=[rWU[fc]] + rh, writes=[rpsu])
                        tb = fc % 2
                        gp, sg, up = GP[tb], SG[tb], UP[tb]
                        P.act(sg[:, :ntok], psg[:, :ntok], AF.Sigmoid, reads=[rpsg, rBGX[wb]], writes=[rSG[tb]], scale=1.702, bias=BGX[wb][:, fc:fc + 1])
                        P.op("dve", lambda h, gp=gp, psg=psg, fc=fc, wb=wb, ntok=ntok: h.tensor_scalar(
                            out=gp[:, :ntok], in0=psg[:, :ntok], scalar1=BGU[wb][:, fc:fc + 1], scalar2=7.0, op0=ALU.add, op1=ALU.min),
                            reads=[rpsg, rBGU[wb]], writes=[rGP[tb]])
                        P.op("dve", lambda h, up=up, psu=psu, fc=fc, wb=wb, ntok=ntok: h.tensor_scalar(
                            out=up[:, :ntok], in0=psu[:, :ntok], scalar1=BGX[wb][:, 8 + fc:9 + fc], scalar2=8.0, op0=ALU.add, op1=ALU.min),
                            reads=[rpsu, rBGX[wb]], writes=[rUP[tb]])
                        P.op("dve", lambda h, gp=gp, sg=sg, ntok=ntok: h.scalar_tensor_tensor(out=gp[:, :ntok], in0=sg[:, :ntok], scalar=SIGC, in1=gp[:, :ntok], op0=ALU.min, op1=ALU.mult),
                             reads=[rGP[tb], rSG[tb]], writes=[rGP[tb]])
                        P.op("dve", lambda h, gp=gp, up=up, actt=actt, fc=fc, ntok=ntok: h.scalar_tensor_tensor(out=actt[:, fc, :ntok], in0=up[:, :ntok], scalar=-6.0, in1=gp[:, :ntok], op0=ALU.max, op1=ALU.mult),
                             reads=[rGP[tb], rUP[tb]], writes=[ractt])
                    for gi, l in enumerate(g):
                        r0, nr, j = tiles[pss[l]]
                        yb = 4 + (l % 2) * 2
                        for half in range(2):
                            ps, rps = PS[yb + half], rPS[yb + half]
                            for fc in range(8):
                                P.mm(ps[:nr, :], actt[:, fc, gi * 128:gi * 128 + nr], WD[:, fc, half * 512:(half + 1) * 512], start=(fc == 0), stop=False,
                                     reads=[ractt, rWD[fc]], writes=[rps])
                            P.mm(ps[:nr, :], ONESB[0:1, :nr], BD[wb][0:1, half * 512:(half + 1) * 512], start=False, stop=True,
                                 reads=[rONESB, rBD[wb]], writes=[rps])
                        tmp, rtmp = TMP[l % 2], rTMP[l % 2]
                        for half in range(2):
                            ps, rps = PS[yb + half], rPS[yb + half]
                            P.op("dve", lambda h, ps=ps, tmp=tmp, half=half, l=l, e=e, j=j, nr=nr: h.scalar_tensor_tensor(
                                out=tmp[:nr, half * 512:(half + 1) * 512], in0=ps[:nr, :], scalar=GATES[:nr, l, e:e + 1],
                                in1=GT[:nr, 1, j, half * 512:(half + 1) * 512], op0=ALU.mult, op1=ALU.mult),
                                reads=[rps, rGATES[l], rGT], writes=[rtmp])
                        P.op("dve", lambda h, tmp=tmp, l=l, nr=nr: h.tensor_tensor(out=X1[:nr, l, :], in0=X1[:nr, l, :], in1=tmp[:nr, :], op=ALU.add),
                             reads=[rtmp, rX1[l]], writes=[rX1[l]])
            for li, ti in enumerate(pss):
                r0, nr, j = tiles[ti]
                P.dma("sp", xo[r0:r0 + nr, :], X1[:nr, li, :], reads=[rX1[li]], writes=[rOUT[ti]])
        P.fence()


PER_LAYER = [("adaw1", [1024, 2048]), ("adab1T", [128, 16]), ("g1T", [128, 8]), ("wa", [1024, 384]), ("wz", [1024, 2, 68]),
             ("cw", [128, 3, 5]), ("nega", [128, 2]), ("dtb", [128, 2]), ("gdn", [128, 64]), ("ws", [1024, 256]),
             ("gqk", [128, 3, 64]), ("sinkb", [128, 2]),
             ("adaw2", [1024, 4096]), ("adab2", [1, 4096]), ("adab2T", [128, 16]), ("g2T", [128, 8]), ("wout", [1024, 1024]),
             ("rw", [1024, 32]), ("rb", [1, 32]), ("bguT", [32, 128, 16]), ("bd", [32, 1024])]
SHARED = [("ident", [128, 128]), ("cvT", [128, 8, 2]), ("blk1", [128, 128]), ("masks", [128, 6, 2, 64]), ("maskw", [128, 384]),
          ("ropet", [128, 64, 2, 2, 16]), ("seli", [128, 4, 128])]
NLOC = 2112


def build_fused(nex=32):
    nc = bass.Bass("TRN2", target_bir_lowering=False)
    dr = lambda name, shape: nc.dram_tensor(name, list(shape), F32, kind="ExternalInput").ap()
    IN = {}
    IN["xall"] = dr("xall", [8448, 1024]); IN["xs0"] = dr("xs0", [NLOC, 1024])
    for nm, shp in SHARED:
        IN[nm] = dr(nm, shp)
    for l in range(2):
        for nm, shp in PER_LAYER:
            IN[nm, l] = dr("%s_%d" % (nm, l), shp)
    for l in range(2):
        IN["wgu", l] = dr("wgu_%d" % l, [nex, 1024, 2048]); IN["wd", l] = dr("wd_%d" % l, [nex, 1024, 1024])
    IN["nex"] = nex
    xo = nc.dram_tensor("xo", [2048, 1024], F32, kind="ExternalOutput").ap()
    omloc = [nc.dram_tensor("omloc%d" % l, [8448, 256], F32).ap() for l in range(2)]
    OCH = 1024
    och = [(r, min(OCH, 8448 - r)) for r in range(0, 8448, OCH)]
    omall = [[nc.dram_tensor("omall%d_%d" % (l, k), [4 * n, 256], F32).ap() for k, (r, n) in enumerate(och)] for l in range(2)]
    xloc = nc.dram_tensor("xloc", [NLOC, 1024], F32).ap()
    XCH = 256
    xch = [(r, min(XCH, NLOC - r)) for r in range(0, NLOC, XCH)]
    xgat = [nc.dram_tensor("xgat_%d" % k, [4 * n, 1024], F32).ap() for k, (r, n) in enumerate(xch)]

    def om_rows(l, jj, grow, nr):
        k = grow // OCH
        n = och[k][1]
        off = jj * n + (grow - och[k][0])
        return omall[l][k][off:off + nr, :]

    def xg_rows(q, r, nr):
        k = r // XCH
        n = xch[k][1]
        off = q * n + (r - xch[k][0])
        return xgat[k][off:off + nr, :]
    groups = [[0, 1, 2, 3], [4, 5, 6, 7]]
    with ExitStack() as st:
        P = Prog(nc, st)
        G = emit_globals(P, IN)
        rxloc = [Res() for _ in range(17)]
        rxgat = Res()
        rcc = Res()
        rxo = [Res() for _ in range(16)]
        for l in range(2):
            last = (l == 1)
            if l == 0:
                xsrc = lambda n: [(0, 128, IN["xall"][n * 128:(n + 1) * 128, :])]
                rsrc = []
            else:
                def xsrc(n):
                    if n < 2:
                        a, b = 2 * n, 2 * n + 1
                        return [(0, 64, xg_rows(a, 2048, 64)), (64, 64, xg_rows(b, 2048, 64))]
                    i = n - 2
                    q, r = i // 16, (i % 16) * 128
                    return [(0, 128, xg_rows(q, r, 128))]
                rsrc = [rxgat]
            rdn = [Res(), Res()]
            rsw = [Res() for _ in range(66)]
            with ExitStack() as lst:
                C = emit_common(P, G, IN, l, lst)
                emit_dn(P, C, IN, l, last, xsrc, rsrc, omloc[l][:, 0:128], rdn)
                emit_swa(P, C, IN, l, last, xsrc, rsrc, omloc[l][:, 128:256], rsw)
                P.fence()
            romall = Res()
            if os.environ.get("NOCOLL") != "1":
                for k, (r, n) in enumerate(och):
                    P.coll("AllGather", ALU.bypass, groups, omloc[l][r:r + n, :], omall[l][k], reads=rdn + rsw, writes=[romall, rcc])
            if l == 0:
                emit_b(P, G, IN, 0, 2048, 64, IN["xs0"], [], lambda jj, grow, nr: om_rows(0, jj, grow, nr), [romall], xloc, rxloc)
                if os.environ.get("NOCOLL") not in ("1", "2"):
                    for k, (r, n) in enumerate(xch):
                        P.coll("AllGather", ALU.bypass, groups, xloc[r:r + n, :], xgat[k], reads=rxloc, writes=[rxgat, rcc])
            else:
                emit_b(P, G, IN, 1, 2048, 0, xloc, rxloc, lambda jj, grow, nr: om_rows(1, jj, grow, nr), [romall], xo, rxo)
        P.finish(rxo)
        print("fused instr counts", P.cnt, P.dcnt, "waits", P.n_wait, "sems", {k: len(v) for k, v in P.sem.items()})
    return nc


NEG = -1e30
def consts():
    f = np.float32
    i = np.arange(64)
    k_le_f = (i[:, None] <= i[None, :]).astype(f)
    k_ge_f = (i[:, None] >= i[None, :]).astype(f)
    m = np.zeros((64, 6, 2, 64), f)
    m[:, 0, 0] = k_le_f; m[:, 0, 1] = k_ge_f
    m[:, 1, 0] = np.where(i[None, :] >= i[:, None], 0, NEG)
    m[:, 1, 1] = np.where(i[None, :] <= i[:, None], 0, NEG)
    m[:, 2, 0] = np.where(i[None, :] <= i[:, None], 0, NEG)
    m[:, 2, 1] = np.where(i[None, :] >= i[:, None], 0, NEG)
    m[:, 3, 0] = np.where(i[None, :] > i[:, None], -1, 0)
    m[:, 3, 1] = np.where(i[None, :] < i[:, None], -1, 0)
    m[:, 4, 0] = np.where(i[None, :] < i[:, None], -1, 0)
    m[:, 4, 1] = np.where(i[None, :] > i[:, None], -1, 0)
    m[:, 5, 0] = np.eye(64); m[:, 5, 1] = np.eye(64)
    masks = np.concatenate([m, m], 0)
    blk1 = np.zeros((128, 128), f); blk1[:64, :64] = 1; blk1[64:, 64:] = 1
    qi = np.arange(128)[:, None]; kc = np.arange(384)[None, :]
    maskw = np.where((kc >= qi) & (kc <= qi + 256), 0, NEG).astype(f)
    t = np.arange(8192); row = (t // 64).astype(f); col = (t % 64).astype(f)
    inv = np.power(f(10000.0), -np.arange(16, dtype=f) / f(16)).astype(f)
    ar = row[:, None] * inv; ac = col[:, None] * inv
    rope = np.stack([np.stack([np.cos(ar), np.cos(ac)], 1), np.stack([np.sin(ar), np.sin(ac)], 1)], 1).astype(f)
    ropet = np.ascontiguousarray(rope.reshape(64, 128, 2, 2, 16).transpose(1, 0, 2, 3, 4))
    return dict(masks=masks, blk1=blk1, maskw=maskw, ropet=ropet, ident=np.eye(128, dtype=f))
def prep_a(inp, l, x, xc, K):
    f = np.float32
    w_in = inp["w_in"][l]; cwl = inp["dn_conv_w"][l]
    ada_w = np.ascontiguousarray(inp["ada_w"][l][:, 0:2048]); ada_b = inp["ada_b"][l][0:2048]
    base = dict(adaw=ada_w, adabT=np.ascontiguousarray(ada_b.reshape(16, 128).T), g1T=np.ascontiguousarray(inp["norm1_g"][l].reshape(8, 128).T), ident=K["ident"])
    dn, sw = [], []
    for c in range(8):
        b, j = c // 4, c % 4
        xall = np.ascontiguousarray(np.concatenate([xc[b], x[b]], 0))
        cv = np.stack([inp["c"][b], inp["c_ctx"]], -1)
        m = dict(base); m.update(xall=xall, cvT=np.ascontiguousarray(cv.reshape(8, 128, 2).transpose(1, 0, 2)))
        hd = [2 * j, 2 * j + 1]
        wa = np.concatenate([w_in[:, s * 512 + 128 * j: s * 512 + 128 * j + 128] for s in range(3)], 1)
        wz = np.stack([np.concatenate([w_in[:, 1536 + h * 64:1536 + (h + 1) * 64], w_in[:, [2048 + h, 2048 + 8 + h, 2064 + h, 2064 + 8 + h]]], 1) for h in hd], 1)
        cw = np.stack([cwl[:, s * 512 + 128 * j: s * 512 + 128 * j + 128].T for s in range(3)], 1)
        hp = np.repeat(np.array(hd), 64)
        nega = -np.exp(inp["dn_a_log"][l][:, hp]).T; dtb = inp["dn_dt_bias"][l][:, hp].T
        md = dict(m); md.update(wa=np.ascontiguousarray(wa), wz=np.ascontiguousarray(wz), cw=np.ascontiguousarray(cw), nega=np.ascontiguousarray(nega.astype(f)),
                                dtb=np.ascontiguousarray(dtb), gdn=np.ascontiguousarray(np.broadcast_to(inp["dn_out_g"][l], (128, 64))), blk1=K["blk1"], masks=K["masks"])
        dn.append(md)
        kv = j // 2
        ws = np.concatenate([w_in[:, 2080 + hd[0] * 64:2080 + hd[0] * 64 + 128], w_in[:, 2592 + kv * 64:2592 + (kv + 1) * 64], w_in[:, 2720 + kv * 64:2720 + (kv + 1) * 64]], 1)
        gqk = np.broadcast_to(np.stack([inp["q_norm_g"][l], inp["q_norm_g"][l], inp["k_norm_g"][l]], 0), (128, 3, 64))
        ms = dict(m); ms.update(ws=np.ascontiguousarray(ws), gqk=np.ascontiguousarray(gqk), ropet=K["ropet"],
                                sinkb=np.ascontiguousarray(np.broadcast_to(inp["sinks"][l][hd], (128, 2))), maskw=K["maskw"])
        sw.append(ms)
    return dn, sw
def gather_a(res_dn, res_sw):
    om_x = np.zeros((2, 8192, 1024), np.float32); om_c = np.zeros((2, 256, 1024), np.float32)
    for c in range(8):
        b, j = c // 4, c % 4
        od = res_dn[c]["o_dn"]; os_ = res_sw[c]["o_sw"]
        om_c[b, :, 128 * j:128 * j + 128] = od[:256]; om_x[b, :, 128 * j:128 * j + 128] = od[256:]
        om_c[b, :, 512 + 128 * j:512 + 128 * j + 128] = os_[:256]; om_x[b, :, 512 + 128 * j:512 + 128 * j + 128] = os_[256:]
    return om_x, om_c


def prep_b(inp, l, x, xc, om_x, om_c, last):
    f = np.float32
    ada_w = np.ascontiguousarray(inp["ada_w"][l][:, 2048:6144]); ada_b = inp["ada_b"][l][2048:6144]
    common = dict(
        adaw=ada_w, adab=np.ascontiguousarray(ada_b[None, :]),
        adabT=np.ascontiguousarray(ada_b[1024:3072].reshape(16, 128).T),
        g2T=np.ascontiguousarray(inp["norm2_g"][l].reshape(8, 128).T),
        wout=inp["w_out"][l], rw=inp["router_w"][l], rb=np.ascontiguousarray(inp["router_b"][l][None, :]),
        wgu=inp["w_gate_up"][l], bguT=np.ascontiguousarray(inp["b_gate_up"][l].reshape(32, 16, 128).transpose(0, 2, 1)),
        wd=inp["w_down"][l], bd=inp["b_down"][l], ident=np.eye(128, dtype=f))
    maps = []
    for c in range(8):
        b, q = c // 4, c % 4
        rows = [x[b, q * 2048:(q + 1) * 2048]]; oms = [om_x[b, q * 2048:(q + 1) * 2048]]
        if not last:
            rows.append(xc[b, q * 64:(q + 1) * 64]); oms.append(om_c[b, q * 64:(q + 1) * 64])
        xs = np.ascontiguousarray(np.concatenate(rows, 0)); om = np.concatenate(oms, 0)
        cv = np.stack([inp["c"][b], inp["c_ctx"]], -1)
        m = dict(common)
        m.update(xs=xs, omT=np.ascontiguousarray(om.T), cvT=np.ascontiguousarray(cv.reshape(8, 128, 2).transpose(1, 0, 2)))
        maps.append(m)
    return maps
def gather_b(results, last):
    x = np.zeros((2, 8192, 1024), np.float32); xc = np.zeros((2, 256, 1024), np.float32)
    for c in range(8):
        b, q = c // 4, c % 4
        xo = results[c]["xo"]
        x[b, q * 2048:(q + 1) * 2048] = xo[:2048]
        if not last:
            xc[b, q * 64:(q + 1) * 64] = xo[2048:]
    return x, xc


def prep_fused(inp, nex=32):
    f = np.float32
    K = consts()
    x = np.ascontiguousarray(inp["x"], dtype=f); xc = np.ascontiguousarray(inp["ctx"], dtype=f)
    A = [prep_a(inp, l, x, xc, K) for l in range(2)]
    zx = np.zeros((2, 8192, 1024), f); zc = np.zeros((2, 256, 1024), f)
    B = [prep_b(inp, l, zx, zc, zx, zc, False) for l in range(2)]
    wgu = [np.ascontiguousarray(inp["w_gate_up"][l][:nex], dtype=f) for l in range(2)]; wd = [np.ascontiguousarray(inp["w_down"][l][:nex], dtype=f) for l in range(2)]
    maps = []
    for c in range(8):
        b, q = c // 4, c % 4
        m = dict(xall=A[0][0][c]["xall"], cvT=A[0][0][c]["cvT"], ident=K["ident"], blk1=K["blk1"], masks=K["masks"], maskw=K["maskw"], ropet=K["ropet"])
        m["xs0"] = np.ascontiguousarray(np.concatenate([x[b, q * 2048:(q + 1) * 2048], xc[b, q * 64:(q + 1) * 64]], 0))
        seli = np.zeros((128, 4, 128), f); seli[:, q, :] = np.eye(128, dtype=f); m["seli"] = seli
        for l in range(2):
            dn, sw = A[l][0][c], A[l][1][c]; bb = B[l][c]
            for nm, src, key in [("adaw1", dn, "adaw"), ("adab1T", dn, "adabT"), ("g1T", dn, "g1T"), ("wa", dn, "wa"), ("wz", dn, "wz"), ("cw", dn, "cw"),
                                 ("nega", dn, "nega"), ("dtb", dn, "dtb"), ("gdn", dn, "gdn"), ("ws", sw, "ws"), ("gqk", sw, "gqk"), ("sinkb", sw, "sinkb"),
                                 ("adaw2", bb, "adaw"), ("adab2", bb, "adab"), ("adab2T", bb, "adabT"), ("g2T", bb, "g2T"), ("wout", bb, "wout"),
                                 ("rw", bb, "rw"), ("rb", bb, "rb"), ("bguT", bb, "bguT"), ("bd", bb, "bd")]:
                m["%s_%d" % (nm, l)] = np.ascontiguousarray(src[key], dtype=f)
        for l in range(2):
            m["wgu_%d" % l] = wgu[l]; m["wd_%d" % l] = wd[l]
        maps.append(m)
    return maps
def gather_fused(results):
    x = np.zeros((2, 8192, 1024), np.float32)
    for c in range(8):
        b, q = c // 4, c % 4
        x[b, q * 2048:(q + 1) * 2048] = results[c]["xo"]
    return x


_NC = None


def kernel(**inputs):
    global _NC
    inp = {k: np.asarray(v) for k, v in inputs.items()}
    if _NC is None:
        _NC = build_fused(32)
    maps = prep_fused(inp, 32)
    res = run_bass_kernel_spmd(_NC, maps, core_ids=list(range(8)))
    return gather_fused(res.results).astype(np.float32)
```

```python
import os
import numpy as np
from contextlib import ExitStack
import concourse.bass as bass
import concourse.mybir as mybir
from concourse.bass_utils import run_bass_kernel_spmd

F32 = mybir.dt.float32
BF16 = mybir.dt.bfloat16
AF = mybir.ActivationFunctionType
ALU = mybir.AluOpType
AX = mybir.AxisListType

SAME_ENGINE_SYNC = True
NRING = 8
EPOCH = 30000


class Res:
    __slots__ = ("name", "w", "r", "x")

    def __init__(self, name="", x=False):
        self.name = name
        self.w = None
        self.r = {}
        self.x = x


class Prog:
    def __init__(self, nc, stack):
        self.nc = nc
        self.e = {"pe": nc.tensor, "act": nc.scalar, "dve": nc.vector, "pool": nc.gpsimd, "sp": nc.sync}
        self.ops = {k: [] for k in self.e}
        self.cnt = {k: 0 for k in self.e}
        self.sem = {k: [stack.enter_context(nc.semaphore("s_" + k + "0"))] for k in self.e}
        self.ring = {q: [stack.enter_context(nc.semaphore("d_%s_%d" % (q, i))) for i in range(NRING)]
                     for q in ("sp", "act", "pool")}
        self.dcnt = {q: 0 for q in self.ring}
        self.waited = {k: {} for k in self.e}
        self.stack = stack
        self.n_wait = 0

    def sb(self, name, shape, dt=F32, stack=None):
        self.n_alloc = getattr(self, "n_alloc", 0) + 1
        return (stack or self.stack).enter_context(self.nc.sbuf_tensor("%s_%d" % (name, self.n_alloc), list(shape), dt))

    def fence(self):
        for E in self.e:
            for F in self.e:
                if F != E and self.cnt[F] > 0:
                    self._wait(E, ("c", F, self.cnt[F]))
            for q in self.ring:
                n = self.dcnt[q]
                for k in range(max(0, n - NRING), n):
                    self._wait(E, ("d", q, k))

    def ps(self, name, shape, dt=F32):
        return self.stack.enter_context(self.nc.psum_tensor(name, list(shape), dt))

    def _semval(self, ev):
        if ev[0] == "c":
            ep, v = divmod(ev[2] - 1, EPOCH)
            return ("c", ev[1]), self.sem[ev[1]][ep], (ep, v + 1)
        if ev[0] == "x":
            return ("x", ev[1]), self.ccsems[ev[1]], (0, 1)
        _, q, n = ev
        slot = n % NRING
        return ("d", q, slot), self.ring[q][slot], (0, 16 * (n // NRING + 1))

    def _wait(self, eng, ev):
        if ev[0] == "c" and ev[1] == eng:
            if eng == "pe" or not SAME_ENGINE_SYNC:
                return
        key, sem, val = self._semval(ev)
        if self.waited[eng].get(key, (0, 0)) >= val:
            return
        self.waited[eng][key] = val
        self.n_wait += 1
        self.ops[eng].append(lambda h, sem=sem, val=val[1]: h.wait_ge(sem, val))

    def _deps(self, eng, reads, writes):
        deps = []
        for r in reads:
            if r.w is not None:
                deps.append(r.w)
            if r.x:
                for k, ev in r.r.items():
                    if not (k[0] == "c" and k[1] == eng):
                        deps.append(ev)
        for w in writes:
            if w.w is not None:
                deps.append(w.w)
            deps.extend(w.r.values())
        for ev in deps:
            self._wait(eng, ev)

    def _record(self, ev, reads, writes):
        if ev[0] == "c":
            key = ("c", ev[1])
        elif ev[0] == "x":
            key = ("x", ev[1])
        else:
            key = ("d", ev[1], ev[2] % NRING)
        for r in reads:
            r.r[key] = ev
        for w in writes:
            w.w = ev
            w.r = {}

    def op(self, eng, fn, reads=(), writes=()):
        self._deps(eng, reads, writes)
        self.cnt[eng] += 1
        ev = ("c", eng, self.cnt[eng])
        ep = (self.cnt[eng] - 1) // EPOCH
        if ep >= len(self.sem[eng]):
            self.sem[eng].append(self.stack.enter_context(self.nc.semaphore("s_%s%d" % (eng, ep))))
        sem = self.sem[eng][ep]
        self.ops[eng].append(lambda h, fn=fn, sem=sem: fn(h).then_inc(sem, 1))
        self._record(ev, reads, writes)

    def dma(self, q, out, in_, reads=(), writes=(), **kw):
        self._deps(q, reads, writes)
        n = self.dcnt[q]
        self.dcnt[q] += 1
        if n >= NRING:
            self._wait(q, ("d", q, n - NRING))
        sem = self.ring[q][n % NRING]
        self.ops[q].append(lambda h, out=out, in_=in_, sem=sem, kw=kw: h.dma_start(out=out, in_=in_, **kw).then_inc(sem, 16))
        ev = ("d", q, n)
        self._record(ev, reads, writes)
        return ev

    def coll(self, kind, op, groups, in_ap, out_ap, reads=(), writes=()):
        q = "pool"
        self._deps(q, reads, writes)
        if not hasattr(self, "ccsems"):
            self.ccsems = []
        sem = self.stack.enter_context(self.nc.semaphore("cc%d" % len(self.ccsems)))
        self.ccsems.append(sem)
        self.ops[q].append(lambda h, sem=sem: h.collective_compute(kind, op, replica_groups=groups, ins=[in_ap.opt()], outs=[out_ap.opt()]).then_inc(sem))
        ev = ("x", len(self.ccsems) - 1, 0)
        self._record(ev, reads, writes)
        return ev

    def finish(self, final_res):
        for r in final_res:
            if r.w is not None:
                self._wait("sp", r.w)
        for q in self.ring:
            n = self.dcnt[q]
            for k in range(max(0, n - NRING), n):
                self._wait("sp", ("d", q, k))
        with self.nc.Block() as block:
            @block.tensor
            def _(h):
                for f in self.ops["pe"]:
                    f(h)

            @block.scalar
            def _(h):
                for f in self.ops["act"]:
                    f(h)

            @block.vector
            def _(h):
                for f in self.ops["dve"]:
                    f(h)

            @block.gpsimd
            def _(h):
                for f in self.ops["pool"]:
                    f(h)

            @block.sync
            def _(h):
                for f in self.ops["sp"]:
                    f(h)

    def mm(self, out, lhsT, rhs, start=True, stop=True, reads=(), writes=(), **kw):
        self.op("pe", lambda h: h.matmul(out, lhsT, rhs, start=start, stop=stop, **kw), reads, writes)

    def act(self, out, in_, func, reads=(), writes=(), **kw):
        self.op("act", lambda h: h.activation(out=out, in_=in_, func=func, **kw), reads, writes)


D = 1024
NB = 66
NCH = 132


def emit_globals(P, IN):
    G = {}
    ID = P.sb("ID", [128, 128]); rID = Res(); P.dma("sp", ID[:], IN["ident"], writes=[rID])
    ONES = P.sb("ONES", [128, 128]); rONES = Res()
    P.op("dve", lambda h: h.memset(ONES[:], 1.0), writes=[rONES])
    ONESB = P.sb("ONESB", [1, 128], BF16); rONESB = Res()
    P.op("dve", lambda h: h.memset(ONESB[:], 1.0), writes=[rONESB])
    PS = [P.ps("PS%d" % i, [128, 512]) for i in range(8)]; rPS = [Res("ps%d" % i, x=True) for i in range(8)]
    G.update(ID=ID, rID=rID, ONES=ONES, rONES=rONES, ONESB=ONESB, rONESB=rONESB, PS=PS, rPS=rPS)
    return G


def emit_common(P, G, IN, l, stk):
    cvT = IN["cvT"]; adaw = IN["adaw1", l]; adabT = IN["adab1T", l]; g1T = IN["g1T", l]
    C = dict(G)
    PS, rPS = G["PS"], G["rPS"]
    MODF = P.sb("MODF", [128, 16, 2], stack=stk); rMODF = Res()
    SCALE1 = P.sb("SCALE1", [128, 8, 2], stack=stk); rSCALE1 = Res()
    G1 = P.sb("G1", [128, 8], stack=stk); rG1 = Res(); P.dma("sp", G1[:], g1T, writes=[rG1])
    ABT = P.sb("ABT", [128, 16], stack=stk); rABT = Res(); P.dma("sp", ABT[:], adabT, writes=[rABT])
    sub = ExitStack()
    CV = P.sb("CV", [128, 8, 2], stack=sub); rCV = Res(); P.dma("sp", CV[:], cvT, writes=[rCV])
    SCV = P.sb("SCV", [128, 8, 2], stack=sub); rSCV = Res()
    P.act(SCV[:], CV[:], AF.Silu, reads=[rCV], writes=[rSCV])
    AW = [P.sb("AW%d" % i, [128, 8, 512], stack=sub) for i in range(2)]; rAW = [Res(), Res()]
    for blk in range(4):
        aw, raw = AW[blk % 2], rAW[blk % 2]
        P.dma("sp", aw[:], adaw[:, blk * 512:(blk + 1) * 512].rearrange("(k p) f -> p k f", p=128), writes=[raw])
        ps, rps = PS[blk % 2], rPS[blk % 2]
        for fcl in range(4):
            for k in range(8):
                P.mm(ps[:, fcl * 2:fcl * 2 + 2], aw[:, k, fcl * 128:(fcl + 1) * 128], SCV[:, k, :],
                     start=(k == 0), stop=(k == 7), reads=[rSCV, raw], writes=[rps])
        for fcl in range(4):
            fcg = blk * 4 + fcl
            P.act(MODF[:, fcg, :], ps[:, fcl * 2:fcl * 2 + 2], AF.Identity, reads=[rps, rABT], writes=[rMODF],
                  bias=ABT[:, fcg:fcg + 1], scale=1.0)
    P.op("dve", lambda h: h.tensor_scalar(out=SCALE1[:], in0=MODF[:, 8:16, :], scalar1=1.0, scalar2=None, op0=ALU.add),
         reads=[rMODF], writes=[rSCALE1])
    P.op("dve", lambda h: h.tensor_tensor(out=SCALE1[:], in0=SCALE1[:], in1=G1[:].unsqueeze(2).to_broadcast([128, 8, 2]), op=ALU.mult),
         reads=[rSCALE1, rG1], writes=[rSCALE1])
    P.fence()
    sub.close()
    C.update(MODF=MODF, rMODF=rMODF, SCALE1=SCALE1, rSCALE1=rSCALE1)
    return C


def emit_frontend(P, C, xsrc, rsrc, consume, blocks, stk, tbanks=(0, 1)):
    PS, rPS = C["PS"], C["rPS"]
    XT = [P.sb("XT%d" % i, [128, D], stack=stk) for i in range(2)]; rXT = [Res(), Res()]
    XN = P.sb("XN", [128, D], stack=stk); rXN = Res()
    HX = [P.sb("HX%d" % i, [128, 8, 128], BF16, stack=stk) for i in range(2)]; rHX = [Res(), Res()]
    SM = P.sb("FSM", [128, 8], stack=stk); rSM = Res()
    for idx, n in enumerate(blocks):
        j = 1 if n < 2 else 0
        xt, rxt = XT[idx % 2], rXT[idx % 2]
        hx, rhx = HX[idx % 2], rHX[idx % 2]
        for (p0, np_, src) in xsrc(n):
            P.dma("sp", xt[p0:p0 + np_, :], src, reads=rsrc, writes=[rxt])
        P.op("dve", lambda h: h.memset(SM[:, 0:1], 0.0), writes=[rSM])
        P.act(XN[:], xt[:], AF.Square, reads=[rxt], writes=[rXN, rSM], accum_out=SM[:, 0:1])
        P.act(SM[:, 1:2], SM[:, 0:1], AF.Ln, reads=[rSM], writes=[rSM], bias=1e-6, scale=1.0 / D)
        P.act(SM[:, 2:3], SM[:, 1:2], AF.Exp, reads=[rSM], writes=[rSM], scale=-0.5)
        P.op("dve", lambda h, xt=xt: h.tensor_scalar(out=XN[:], in0=xt[:], scalar1=SM[:, 2:3], scalar2=None, op0=ALU.mult),
             reads=[rxt, rSM], writes=[rXN])
        for half in range(2):
            ps, rps = PS[tbanks[half]], rPS[tbanks[half]]
            for k in range(4 * half, 4 * half + 4):
                P.op("pe", lambda h, ps=ps, k=k: h.transpose(ps[:, (k % 4) * 128:(k % 4 + 1) * 128], XN[:, k * 128:(k + 1) * 128], C["ID"][:]),
                     reads=[rXN, C["rID"]], writes=[rps])
            for k in range(4 * half, 4 * half + 4):
                P.act(hx[:, k, :], ps[:, (k % 4) * 128:(k % 4 + 1) * 128], AF.Identity, reads=[rps, C["rSCALE1"], C["rMODF"]], writes=[rhx],
                      scale=C["SCALE1"][:, k, j:j + 1], bias=C["MODF"][:, k, j:j + 1])
        consume(n, hx, rhx)


def emit_swa(P, C, IN, l, last, xsrc, rsrc, o_sw, rOUT):
    ws = IN["ws", l]; gqk = IN["gqk", l]; ropet = IN["ropet"]; sinkb = IN["sinkb", l]; maskw = IN["maskw"]
    with ExitStack() as st0:
        _sb = P.sb
        P_sb = lambda name, shape, dt=F32: _sb("sw_" + name, shape, dt, stack=st0)
        PS, rPS, ID, rID = C["PS"], C["rPS"], C["ID"], C["rID"]
        WS = P_sb("WS", [128, 8, 256], BF16); rWS = Res()
        P.dma("pool", WS[:], ws.rearrange("(k p) f -> p k f", p=128), writes=[rWS])
        GQK = P_sb("GQK", [128, 3, 64]); rGQK = Res(); P.dma("sp", GQK[:], gqk, writes=[rGQK])
        ROPE = P_sb("ROPE", [128, 64, 2, 2, 16]); rROPE = Res(); P.dma("sp", ROPE[:], ropet, writes=[rROPE])
        SINK = P_sb("SINK", [128, 2]); rSINK = Res(); P.dma("sp", SINK[:], sinkb, writes=[rSINK])
        MASKW = P_sb("MASKW", [128, 384]); rMASKW = Res(); P.dma("sp", MASKW[:], maskw, writes=[rMASKW])
        SQT = P_sb("SQT", [128, NB * 128], BF16); rSQT = [Res() for _ in range(NB)]
        SKT = P_sb("SKT", [128, NB * 128], BF16); rSKT = [Res() for _ in range(NB)]
        SV = P_sb("SV", [128, NB, 64], BF16); rSV = [Res() for _ in range(NB)]
        QK = P_sb("QK", [128, 3, 64]); rQK = Res()
        QKR = P_sb("QKR", [128, 4, 64]); rQKR = Res()
        SQ = P_sb("SQ", [128, 3, 64]); rSQ = Res()
        T1 = P_sb("T1", [128, 3, 2, 16]); rT1 = Res()
        T2 = P_sb("T2", [128, 3, 2, 16]); rT2 = Res()
        SM = P_sb("SM", [128, 16]); rSM = Res()
        S = P_sb("S", [128, 640]); rS = Res()
        E = P_sb("E", [128, 640]); rE = Res()
        ET = P_sb("ET", [128, 5, 128], BF16); rET = Res()
        OSW = [P_sb("OSW%d" % i, [128, 128]) for i in range(2)]; rOSW = [Res(), Res()]

        HL = []
        for h in range(2):
            Ld = dict(S=P_sb("S%d" % h, [128, 640]), rS=Res(), E=P_sb("E%d" % h, [128, 640]), rE=Res(),
                      ET=P_sb("ET%d" % h, [128, 5, 128], BF16), rET=Res(), SM=P_sb("SMh%d" % h, [128, 8]), rSM=Res(),
                      b0=PS[4 + 2 * h], r0=rPS[4 + 2 * h], b1=PS[5 + 2 * h], r1=rPS[5 + 2 * h])
            HL.append(Ld)

        def att_gen(n, h, osw, rosw):
            Ld = HL[h]
            S_, rS_, E_, rE_, ET_, rET_, SM_, rSM_ = Ld["S"], Ld["rS"], Ld["E"], Ld["rE"], Ld["ET"], Ld["rET"], Ld["SM"], Ld["rSM"]
            b0, r0, b1, r1 = Ld["b0"], Ld["r0"], Ld["b1"], Ld["r1"]
            if n >= 2:
                lo, hi = max(2, n - 1), min(NB - 1, n + 1)
                nl = (hi - lo + 1) * 128
                m0 = (lo - (n - 1)) * 128
            else:
                lo, hi, nl, m0 = 0, -1, 0, 0
            ntot = nl + 256
            kblocks = list(range(lo, hi + 1)) + [0, 1]
            hs = slice(h * 64, (h + 1) * 64)
            if nl:
                P.mm(b0[:, 0:nl], SQT[hs, n * 128:(n + 1) * 128], SKT[hs, lo * 128:(hi + 1) * 128],
                     reads=[rSQT[n]] + [rSKT[b] for b in range(lo, hi + 1)], writes=[r0])
            P.mm(b1[:, 0:256], SQT[hs, n * 128:(n + 1) * 128], SKT[hs, 0:256], reads=[rSQT[n], rSKT[0], rSKT[1]], writes=[r1])
            yield
            if nl:
                P.op("dve", lambda hh: hh.scalar_tensor_tensor(out=S_[:, 0:nl], in0=b0[:, 0:nl], scalar=0.125,
                                                              in1=MASKW[:, m0:m0 + nl], op0=ALU.mult, op1=ALU.add),
                     reads=[r0, rMASKW], writes=[rS_])
            P.act(S_[:, nl:ntot], b1[:, 0:256], AF.Copy, reads=[r1], writes=[rS_], scale=0.125)
            yield
            P.op("dve", lambda hh: hh.reduce_max(out=SM_[:, 0:1], in_=S_[:, 0:ntot], axis=AX.X), reads=[rS_], writes=[rSM_])
            yield
            P.op("dve", lambda hh: hh.tensor_tensor(out=SM_[:, 1:2], in0=SM_[:, 0:1], in1=SINK[:, h:h + 1], op=ALU.max),
                 reads=[rSM_, rSINK], writes=[rSM_])
            yield
            P.op("dve", lambda hh: hh.tensor_scalar(out=SM_[:, 2:3], in0=SM_[:, 1:2], scalar1=-1.0, scalar2=None, op0=ALU.mult),
                 reads=[rSM_], writes=[rSM_])
            P.op("dve", lambda hh: hh.memset(SM_[:, 3:4], 0.0), writes=[rSM_])
            yield
            P.act(E_[:, 0:ntot], S_[:, 0:ntot], AF.Exp, reads=[rS_, rSM_], writes=[rE_, rSM_], bias=SM_[:, 2:3], scale=1.0, accum_out=SM_[:, 3:4])
            P.act(SM_[:, 4:5], SINK[:, h:h + 1], AF.Exp, reads=[rSINK, rSM_], writes=[rSM_], bias=SM_[:, 2:3], scale=1.0)
            yield
            P.op("dve", lambda hh: hh.tensor_tensor(out=SM_[:, 5:6], in0=SM_[:, 3:4], in1=SM_[:, 4:5], op=ALU.add), reads=[rSM_], writes=[rSM_])
            nk = ntot // 128
            n4 = min(nk, 4)
            for c in range(n4):
                P.op("pe", lambda hh, c=c: hh.transpose(b1[:, c * 128:(c + 1) * 128], E_[:, c * 128:(c + 1) * 128], ID[:]),
                     reads=[rE_, rID], writes=[r1])
            if nk > 4:
                P.op("pe", lambda hh: hh.transpose(b0[:, 384:512], E_[:, 512:640], ID[:]), reads=[rE_, rID], writes=[r0])
            yield
            P.op("dve", lambda hh: hh.reciprocal(out=SM_[:, 6:7], in_=SM_[:, 5:6]), reads=[rSM_], writes=[rSM_])
            P.act(ET_[:, 0:n4, :], b1[:, 0:n4 * 128], AF.Copy, reads=[r1], writes=[rET_])
            if nk > 4:
                P.act(ET_[:, 4, :], b0[:, 384:512], AF.Copy, reads=[r0], writes=[rET_])
            yield
            for c in range(nk):
                kb = kblocks[c]
                P.mm(b1[:, 0:64], ET_[:, c, :], SV[:, kb, :], start=(c == 0), stop=(c == nk - 1), reads=[rET_, rSV[kb]], writes=[r1])
            yield
            P.op("dve", lambda hh: hh.tensor_scalar(out=osw[:, h * 64:(h + 1) * 64], in0=b1[:, 0:64], scalar1=SM_[:, 6:7], scalar2=None, op0=ALU.mult),
                 reads=[r1, rSM_], writes=[rosw])
            yield

        BL = []
        for li in range(2):
            F = dict(XT=P_sb("XT%d" % li, [128, D]), rXT=Res(), XN=P_sb("XN%d" % li, [128, D]), rXN=Res(),
                     HX=P_sb("HX%d" % li, [128, 8, 128], BF16), rHX=Res(), SM=P_sb("BSM%d" % li, [128, 16]), rSM=Res(),
                     QK=P_sb("QK%d" % li, [128, 3, 64]), rQK=Res(), QKR=P_sb("QKR%d" % li, [128, 4, 64]), rQKR=Res(),
                     SQ=P_sb("SQ%d" % li, [128, 3, 64]), rSQ=Res(), T1=P_sb("T1%d" % li, [128, 3, 2, 16]), rT1=Res(),
                     T2=P_sb("T2%d" % li, [128, 3, 2, 16]), rT2=Res(),
                     bT=PS[2 * li], rT=rPS[2 * li], bA=PS[2 * li + 1], rA=rPS[2 * li + 1])
            BL.append(F)

        def blk_gen(n):
            F = BL[n % 2]
            j = 1 if n < 2 else 0
            xt, rxt, XN_, rXN_, hx, rhx, SMb, rSMb = F["XT"], F["rXT"], F["XN"], F["rXN"], F["HX"], F["rHX"], F["SM"], F["rSM"]
            QK_, rQK_, QKR_, rQKR_, SQ_, rSQ_, T1_, rT1_, T2_, rT2_ = F["QK"], F["rQK"], F["QKR"], F["rQKR"], F["SQ"], F["rSQ"], F["T1"], F["rT1"], F["T2"], F["rT2"]
            for (p0, np_, src) in xsrc(n):
                P.dma("sp", xt[p0:p0 + np_, :], src, reads=rsrc, writes=[rxt])
            P.op("dve", lambda h: h.memset(SMb[:, 0:1], 0.0), writes=[rSMb])
            yield
            P.act(XN_[:], xt[:], AF.Square, reads=[rxt], writes=[rXN_, rSMb], accum_out=SMb[:, 0:1])
            yield
            P.act(SMb[:, 1:2], SMb[:, 0:1], AF.Ln, reads=[rSMb], writes=[rSMb], bias=1e-6, scale=1.0 / D)
            yield
            P.act(SMb[:, 2:3], SMb[:, 1:2], AF.Exp, reads=[rSMb], writes=[rSMb], scale=-0.5)
            yield
            P.op("dve", lambda h: h.tensor_scalar(out=XN_[:], in0=xt[:], scalar1=SMb[:, 2:3], scalar2=None, op0=ALU.mult),
                 reads=[rxt, rSMb], writes=[rXN_])
            yield
            ps, rps = F["bT"], F["rT"]
            for half in range(2):
                for k in range(4 * half, 4 * half + 4):
                    P.op("pe", lambda h, k=k, ps=ps: h.transpose(ps[:, (k % 4) * 128:(k % 4 + 1) * 128], XN_[:, k * 128:(k + 1) * 128], ID[:]),
                         reads=[rXN_, rID], writes=[rps])
                yield
                for k in range(4 * half, 4 * half + 4):
                    P.act(hx[:, k, :], ps[:, (k % 4) * 128:(k % 4 + 1) * 128], AF.Identity, reads=[rps, C["rSCALE1"], C["rMODF"]], writes=[rhx],
                          scale=C["SCALE1"][:, k, j:j + 1], bias=C["MODF"][:, k, j:j + 1])
                yield
            pa, rpa = F["bA"], F["rA"]
            for k in range(8):
                P.mm(pa[:, 0:256], hx[:, k, :], WS[:, k, :], start=(k == 0), stop=(k == 7), reads=[rhx, rWS], writes=[rpa])
            yield
            P.act(QK_[:], pa[:, 0:192], AF.Copy, reads=[rpa], writes=[rQK_])
            P.act(SV[:, n, :], pa[:, 192:256], AF.Copy, reads=[rpa], writes=[rSV[n]])
            yield
            P.op("dve", lambda h: h.tensor_tensor(out=SQ_[:], in0=QK_[:], in1=QK_[:], op=ALU.mult), reads=[rQK_], writes=[rSQ_])
            yield
            P.op("dve", lambda h: h.reduce_sum(out=SMb[:, 8:11], in_=SQ_[:], axis=AX.X), reads=[rSQ_], writes=[rSMb])
            yield
            P.act(SMb[:, 11:14], SMb[:, 8:11], AF.Ln, reads=[rSMb], writes=[rSMb], bias=1e-6, scale=1.0 / 64)
            yield
            P.act(SMb[:, 8:11], SMb[:, 11:14], AF.Exp, reads=[rSMb], writes=[rSMb], scale=-0.5)
            yield
            P.op("dve", lambda h: h.tensor_tensor(out=QK_[:], in0=QK_[:], in1=SMb[:, 8:11].unsqueeze(2).to_broadcast([128, 3, 64]), op=ALU.mult),
                 reads=[rQK_, rSMb], writes=[rQK_])
            yield
            P.op("dve", lambda h: h.tensor_tensor(out=QK_[:], in0=QK_[:], in1=GQK[:], op=ALU.mult), reads=[rQK_, rGQK], writes=[rQK_])
            yield
            if n >= 2:
                bi = n - 2
                q5 = QK_[:].rearrange("p s (a t f) -> p s a t f", a=2, t=2)
                o5 = QKR_[:, 0:3, :].rearrange("p s (a t f) -> p s a t f", a=2, t=2)
                X1, X2 = q5[:, :, :, 0, :], q5[:, :, :, 1, :]
                Cc = ROPE[:, bi, 0, :, :].unsqueeze(1).to_broadcast([128, 3, 2, 16])
                Sn = ROPE[:, bi, 1, :, :].unsqueeze(1).to_broadcast([128, 3, 2, 16])
                tt = lambda out, a, b, op, reads, writes: P.op("dve", lambda h: h.tensor_tensor(out=out, in0=a, in1=b, op=op), reads=reads, writes=writes)
                tt(T1_[:], X1, Cc, ALU.mult, [rQK_, rROPE], [rT1_])
                tt(T2_[:], X2, Sn, ALU.mult, [rQK_, rROPE], [rT2_])
                yield
                tt(o5[:, :, :, 0, :], T1_[:], T2_[:], ALU.subtract, [rT1_, rT2_], [rQKR_])
                yield
                tt(T1_[:], X2, Cc, ALU.mult, [rQK_, rROPE], [rT1_])
                tt(T2_[:], X1, Sn, ALU.mult, [rQK_, rROPE], [rT2_])
                yield
                tt(o5[:, :, :, 1, :], T1_[:], T2_[:], ALU.add, [rT1_, rT2_], [rQKR_])
                yield
            else:
                P.op("dve", lambda h: h.tensor_copy(out=QKR_[:, 0:3, :], in_=QK_[:]), reads=[rQK_], writes=[rQKR_])
                yield
            P.op("dve", lambda h: h.tensor_copy(out=QKR_[:, 3, :], in_=QKR_[:, 2, :]), reads=[rQKR_], writes=[rQKR_])
            yield
            P.op("pe", lambda h: h.transpose(pa[:, 256:384], QKR_[:, 0:2, :].rearrange("p a f -> p (a f)"), ID[:]), reads=[rQKR_, rID], writes=[rpa])
            P.op("pe", lambda h: h.transpose(pa[:, 384:512], QKR_[:, 2:4, :].rearrange("p a f -> p (a f)"), ID[:]), reads=[rQKR_, rID], writes=[rpa])
            yield
            P.act(SQT[:, n * 128:(n + 1) * 128], pa[:, 256:384], AF.Copy, reads=[rpa], writes=[rSQT[n]])
            P.act(SKT[:, n * 128:(n + 1) * 128], pa[:, 384:512], AF.Copy, reads=[rpa], writes=[rSKT[n]])
            yield

        qblocks = ([] if last else [0, 1]) + list(range(2, NB))
        done_blk = set()
        blk_active = {}
        att_active = None
        nxt = 0
        qi = 0
        while nxt < NB or blk_active or att_active is not None or qi < len(qblocks):
            while len(blk_active) < 2 and nxt < NB:
                blk_active[nxt] = blk_gen(nxt); nxt += 1
            if att_active is None and qi < len(qblocks):
                m = qblocks[qi]
                need = [b for b in (0, 1, m - 1, m, m + 1) if 0 <= b < NB and (b < 2 or b >= 2)]
                if m < 2:
                    need = [0, 1]
                if all(b in done_blk for b in need):
                    osw, rosw = OSW[m % 2], rOSW[m % 2]
                    att_active = (m, [att_gen(m, 0, osw, rosw), att_gen(m, 1, osw, rosw)], [True, True])
                    qi += 1
            progressed = False
            for nb_ in list(blk_active):
                try:
                    next(blk_active[nb_]); progressed = True
                except StopIteration:
                    del blk_active[nb_]; done_blk.add(nb_); progressed = True
            if att_active is not None:
                m, gs, alive = att_active
                for i in range(2):
                    if alive[i]:
                        try:
                            next(gs[i]); progressed = True
                        except StopIteration:
                            alive[i] = False; progressed = True
                if not any(alive):
                    P.dma("sp", o_sw[m * 128:(m + 1) * 128, :], OSW[m % 2][:], reads=[rOSW[m % 2]], writes=[rOUT[m]])
                    att_active = None
            assert progressed or att_active is None
        P.fence()


def emit_dn(P, C, IN, l, last, xsrc, rsrc, o_dn, rOUT):
    wa = IN["wa", l]; wz = IN["wz", l]; cw = IN["cw", l]; nega = IN["nega", l]; dtb = IN["dtb", l]; gdn = IN["gdn", l]
    blk1 = IN["blk1"]; masks = IN["masks"]
    NS = NCH + 1
    with ExitStack() as st0:
        _sb = P.sb
        P_sb = lambda name, shape, dt=F32: _sb("dn_" + name, shape, dt, stack=st0)
        PS, rPS, ID, rID, ONES, rONES = C["PS"], C["rPS"], C["ID"], C["rID"], C["ONES"], C["rONES"]
        WA = P_sb("WA", [128, 8, 384], BF16); rWA = Res(); P.dma("pool", WA[:], wa.rearrange("(k p) f -> p k f", p=128), writes=[rWA])
        WZ = P_sb("WZ", [128, 8, 2, 68], BF16); rWZ = Res(); P.dma("pool", WZ[:], wz.rearrange("(k p) h f -> p k h f", p=128), writes=[rWZ])
        CW = P_sb("CW", [128, 3, 5]); rCW = Res(); P.dma("sp", CW[:], cw, writes=[rCW])
        NEGA = P_sb("NEGA", [128, 2]); rNEGA = Res(); P.dma("sp", NEGA[:], nega, writes=[rNEGA])
        DTB = P_sb("DTB", [128, 2]); rDTB = Res(); P.dma("sp", DTB[:], dtb, writes=[rDTB])
        GDN = P_sb("GDN", [128, 64]); rGDN = Res(); P.dma("sp", GDN[:], gdn, writes=[rGDN])
        BLK = P_sb("BLK", [128, 128]); rBLK = Res(); P.dma("sp", BLK[:], blk1, writes=[rBLK])
        MSK = P_sb("MSK", [128, 6, 2, 64]); rMSK = Res(); P.dma("sp", MSK[:], masks, writes=[rMSK])
        TRI, NEGMT, NEGM, NSTT, NST, ID2 = [MSK[:, i, :, :] for i in range(6)]
        ONES3 = P_sb("ONES3", [128, 2, 64]); rONES3 = Res()
        P.op("dve", lambda h: h.memset(ONES3[:], 1.0), writes=[rONES3])
        QT = P_sb("QT", [128, NCH * 64], BF16); KT = P_sb("KT", [128, NCH * 64], BF16)
        KTM = P_sb("KTM", [128, NCH, 64], BF16); VTM = P_sb("VTM", [128, NCH, 64], BF16)
        SZ = P_sb("SZ", [128, NCH, 64], BF16)
        Gs = P_sb("Gs", [128, NS, 2]); Bs = P_sb("Bs", [128, NS, 2])
        O = P_sb("O", [128, NCH, 64])
        rCH = [Res() for _ in range(NCH)]
        rO = [Res() for _ in range(NCH)]
        P.op("dve", lambda h: h.memset(O[:], 0.0), writes=rO)
        CBL = [P_sb("CBL%d" % i, [128, 3, 132]) for i in range(4)]; rCBL = [Res() for _ in range(4)]
        for i in range(4):
            P.op("dve", lambda h, i=i: h.memset(CBL[i][:], 0.0), writes=[rCBL[i]])

        def step_of(c, d):
            if d == 0:
                return c
            return 4 - c if c < 4 else 136 - c

        FL = []
        PSL = int(os.environ.get("PSL", "2"))
        for li in range(2):
            F = dict(XT=P_sb("XT%d" % li, [128, D]), rXT=Res(), XN=P_sb("XN%d" % li, [128, D]), rXN=Res(),
                     HX=P_sb("HX%d" % li, [128, 8, 128], BF16), rHX=Res(), SM=P_sb("FSM%d" % li, [128, 8]), rSM=Res(),
                     CVb=P_sb("CVb%d" % li, [128, 3, 128]), rCVb=Res(), SQ2=P_sb("SQ2%d" % li, [128, 2, 128]), rSQ2=Res(),
                     RS2=P_sb("RS2%d" % li, [128, 2, 128]), rRS2=Res(), KN=P_sb("KN%d" % li, [128, 128]), rKN=Res(),
                     GT_=P_sb("GT_%d" % li, [128, 2, 2, 8]), rGT_=Res(), EXb=P_sb("EXb%d" % li, [128, 3, 128]), rEXb=Res(),
                     EZ=P_sb("EZ%d" % li, [128, 2, 64]), rEZ=Res(),
                     bT=PS[4 * (li % PSL)], rT=rPS[4 * (li % PSL)], bA=PS[4 * (li % PSL) + 1], rA=rPS[4 * (li % PSL) + 1], bB=PS[4 * (li % PSL) + 2], rB=rPS[4 * (li % PSL) + 2],
                     bZ=PS[4 * (li % PSL) + 3], rZ=rPS[4 * (li % PSL) + 3])
            FL.append(F)

        def conv_gen(m, F):
            CVb, rCVb, SQ2, rSQ2, RS2, rRS2, KN, rKN = F["CVb"], F["rCVb"], F["SQ2"], F["rSQ2"], F["RS2"], F["rRS2"], F["KN"], F["rKN"]
            CB, rCB = CBL[m % 4], rCBL[m % 4]
            rcv = [Res(), Res(), Res()]
            for s_ in range(3):
                P.op("dve", lambda h, s_=s_: h.tensor_scalar(out=CVb[:, s_, :], in0=CB[:, s_, 0:128], scalar1=CW[:, s_, 0:1], scalar2=None, op0=ALU.mult),
                     reads=[rCB, rCW], writes=[rcv[s_], rCVb])
            yield
            for tap in range(1, 5):
                for s_ in range(3):
                    P.op("dve", lambda h, s_=s_, tap=tap: h.scalar_tensor_tensor(out=CVb[:, s_, :], in0=CB[:, s_, tap:tap + 128], scalar=CW[:, s_, tap:tap + 1],
                                                                              in1=CVb[:, s_, :], op0=ALU.mult, op1=ALU.add),
                         reads=[rCB, rCW, rcv[s_]], writes=[rcv[s_]] + ([rCVb] if tap == 4 else []))
                yield
            EXb, rEXb = F["EXb"], F["rEXb"]
            P.act(EXb[:], CVb[:], AF.Exp, reads=[rCVb], writes=[rEXb], scale=-1.0)
            yield
            P.op("dve", lambda h: h.tensor_scalar(out=EXb[:], in0=EXb[:], scalar1=1.0, scalar2=None, op0=ALU.add), reads=[rEXb], writes=[rEXb])
            yield
            P.op("dve", lambda h: h.reciprocal(out=EXb[:], in_=EXb[:]), reads=[rEXb], writes=[rEXb])
            yield
            P.op("dve", lambda h: h.tensor_tensor(out=CVb[:], in0=CVb[:], in1=EXb[:], op=ALU.mult), reads=[rCVb, rEXb], writes=[rCVb])
            yield
            P.op("dve", lambda h: h.tensor_tensor(out=SQ2[:], in0=CVb[:, 0:2, :], in1=CVb[:, 0:2, :], op=ALU.mult), reads=[rCVb], writes=[rSQ2])
            yield
            ps, rps = F["bB"], F["rB"]
            P.mm(ps[:, 0:256], BLK[:], SQ2[:].rearrange("p a t -> p (a t)"), reads=[rBLK, rSQ2], writes=[rps])
            yield
            P.act(RS2[:].rearrange("p a t -> p (a t)"), ps[:, 0:256], AF.Ln, reads=[rps], writes=[rRS2], bias=1e-6, scale=1.0)
            yield
            P.act(RS2[:], RS2[:], AF.Exp, reads=[rRS2], writes=[rRS2], scale=-0.5)
            yield
            rc = [rCH[2 * m], rCH[2 * m + 1]]
            P.op("dve", lambda h: h.scalar_tensor_tensor(out=QT[:, m * 128:(m + 1) * 128], in0=CVb[:, 0, :], scalar=0.125, in1=RS2[:, 0, :], op0=ALU.mult, op1=ALU.mult),
                 reads=[rCVb, rRS2], writes=rc)
            P.op("dve", lambda h: h.tensor_tensor(out=KN[:], in0=CVb[:, 1, :], in1=RS2[:, 1, :], op=ALU.mult), reads=[rCVb, rRS2], writes=[rKN])
            yield
            P.op("dve", lambda h: h.tensor_copy(out=KT[:, m * 128:(m + 1) * 128], in_=KN[:]), reads=[rKN], writes=rc)
            for which in range(2):
                for cc in range(2):
                    for hh in range(2):
                        hs = slice(hh * 64, (hh + 1) * 64)
                        srcap = KN[hs, cc * 64:(cc + 1) * 64] if which == 0 else CVb[hs, 2, cc * 64:(cc + 1) * 64]
                        P.op("pe", lambda h, hs=hs, cc=cc, which=which, srcap=srcap: h.matmul(ps[hs, 256 + which * 128 + cc * 64: 256 + which * 128 + (cc + 1) * 64], srcap, ID[hs, hs], start=True, stop=True),
                             reads=[rKN, rCVb, rID], writes=[rps])
            yield
            P.act(KTM[:, 2 * m:2 * m + 2, :], ps[:, 256:384].rearrange("p (c f) -> p c f", c=2), AF.Copy, reads=[rps], writes=rc)
            P.act(VTM[:, 2 * m:2 * m + 2, :], ps[:, 384:512].rearrange("p (c f) -> p c f", c=2), AF.Copy, reads=[rps], writes=rc)
            yield

        def blk_gen(n):
            F = FL[n % 2]
            j = 1 if n < 2 else 0
            xt, rxt, XN, rXN, hx, rhx, SM, rSM = F["XT"], F["rXT"], F["XN"], F["rXN"], F["HX"], F["rHX"], F["SM"], F["rSM"]
            for (p0, np_, src) in xsrc(n):
                P.dma("sp", xt[p0:p0 + np_, :], src, reads=rsrc, writes=[rxt])
            P.op("dve", lambda h: h.memset(SM[:, 0:1], 0.0), writes=[rSM])
            yield
            P.act(XN[:], xt[:], AF.Square, reads=[rxt], writes=[rXN, rSM], accum_out=SM[:, 0:1])
            yield
            P.act(SM[:, 1:2], SM[:, 0:1], AF.Ln, reads=[rSM], writes=[rSM], bias=1e-6, scale=1.0 / D)
            yield
            P.act(SM[:, 2:3], SM[:, 1:2], AF.Exp, reads=[rSM], writes=[rSM], scale=-0.5)
            yield
            P.op("dve", lambda h: h.tensor_scalar(out=XN[:], in0=xt[:], scalar1=SM[:, 2:3], scalar2=None, op0=ALU.mult),
                 reads=[rxt, rSM], writes=[rXN])
            yield
            ps, rps = F["bT"], F["rT"]
            for half in range(2):
                for k in range(4 * half, 4 * half + 4):
                    P.op("pe", lambda h, k=k, ps=ps: h.transpose(ps[:, (k % 4) * 128:(k % 4 + 1) * 128], XN[:, k * 128:(k + 1) * 128], ID[:]),
                         reads=[rXN, rID], writes=[rps])
                yield
                for k in range(4 * half, 4 * half + 4):
                    P.act(hx[:, k, :], ps[:, (k % 4) * 128:(k % 4 + 1) * 128], AF.Identity, reads=[rps, C["rSCALE1"], C["rMODF"]], writes=[rhx],
                          scale=C["SCALE1"][:, k, j:j + 1], bias=C["MODF"][:, k, j:j + 1])
                yield
            ps, rps = F["bA"], F["rA"]
            for s_ in range(3):
                for k in range(8):
                    P.mm(ps[:, s_ * 128:(s_ + 1) * 128], WA[:, k, s_ * 128:(s_ + 1) * 128], hx[:, k, :], start=(k == 0), stop=(k == 7), reads=[rWA, rhx], writes=[rps])
            yield
            CB, rCB = CBL[n % 4], rCBL[n % 4]
            first = n in (0, 2)
            lastb = n in (1, NB - 1)
            P.act(CB[:, :, 2:130], ps[:, 0:384].rearrange("p (s t) -> p s t", s=3), AF.Copy, reads=[rps], writes=[rCB])
            if first:
                P.op("dve", lambda h: h.memset(CB[:, :, 0:2], 0.0), writes=[rCB])
            else:
                Pv, rPv = CBL[(n - 1) % 4], rCBL[(n - 1) % 4]
                P.op("dve", lambda h: h.tensor_copy(out=CB[:, :, 0:2], in_=Pv[:, :, 128:130]), reads=[rPv], writes=[rCB])
                P.op("dve", lambda h: h.tensor_copy(out=Pv[:, :, 130:132], in_=CB[:, :, 2:4]), reads=[rCB], writes=[rPv])
            if lastb:
                P.op("dve", lambda h: h.memset(CB[:, :, 130:132], 0.0), writes=[rCB])
            yield
            ps, rps = F["bZ"], F["rZ"]
            for cc in range(2):
                for hh in range(2):
                    hs = slice(hh * 64, (hh + 1) * 64)
                    for k in range(8):
                        P.mm(ps[hs, cc * 68:(cc + 1) * 68], hx[:, k, cc * 64:(cc + 1) * 64], WZ[:, k, hh, :], start=(k == 0), stop=(k == 7),
                             reads=[rhx, rWZ], writes=[rps])
            yield
            rc = [rCH[2 * n], rCH[2 * n + 1]]
            pz = ps[:, 0:136].rearrange("p (c f) -> p c f", c=2)
            EZ, rEZ = F["EZ"], F["rEZ"]
            P.act(EZ[:], pz[:, :, 0:64], AF.Exp, reads=[rps], writes=[rEZ], scale=-1.0)
            P.op("dve", lambda h: h.tensor_scalar(out=EZ[:], in0=EZ[:], scalar1=1.0, scalar2=None, op0=ALU.add), reads=[rEZ], writes=[rEZ])
            yield
            P.op("dve", lambda h: h.reciprocal(out=EZ[:], in_=EZ[:]), reads=[rEZ], writes=[rEZ])
            yield
            P.op("dve", lambda h: h.tensor_tensor(out=SZ[:, 2 * n:2 * n + 2, :], in0=pz[:, :, 0:64], in1=EZ[:], op=ALU.mult), reads=[rps, rEZ], writes=rc)
            GT_, rGT_ = F["GT_"], F["rGT_"]
            xa = GT_[:, :, :, 0]; ab = GT_[:, :, :, 1]; ee = GT_[:, :, :, 2]; ll = GT_[:, :, :, 3]; rr = GT_[:, :, :, 4]; gg = GT_[:, :, :, 5]; bb = GT_[:, :, :, 6]
            P.op("dve", lambda h: h.tensor_tensor(out=xa, in0=pz[:, :, 64:66], in1=DTB[:].unsqueeze(1).to_broadcast([128, 2, 2]), op=ALU.add), reads=[rps, rDTB], writes=[rGT_])
            yield
            P.act(bb, pz[:, :, 66:68], AF.Exp, reads=[rps], writes=[rGT_], scale=-1.0)
            P.act(ab, xa, AF.Abs, reads=[rGT_], writes=[rGT_])
            yield
            P.op("dve", lambda h: h.tensor_scalar(out=bb, in0=bb, scalar1=1.0, scalar2=None, op0=ALU.add), reads=[rGT_], writes=[rGT_])
            P.op("dve", lambda h: h.reciprocal(out=bb, in_=bb), reads=[rGT_], writes=[rGT_])
            yield
            P.act(ee, ab, AF.Exp, reads=[rGT_], writes=[rGT_], scale=-1.0)
            yield
            P.act(ll, ee, AF.Ln, reads=[rGT_], writes=[rGT_], bias=1.0, scale=1.0)
            P.op("dve", lambda h: h.tensor_scalar(out=rr, in0=xa, scalar1=0.0, scalar2=None, op0=ALU.max), reads=[rGT_], writes=[rGT_])
            yield
            P.op("dve", lambda h: h.tensor_tensor(out=rr, in0=rr, in1=ll, op=ALU.add), reads=[rGT_], writes=[rGT_])
            yield
            P.op("dve", lambda h: h.tensor_tensor(out=gg, in0=rr, in1=NEGA[:].unsqueeze(1).to_broadcast([128, 2, 2]), op=ALU.mult), reads=[rGT_, rNEGA], writes=[rGT_])
            yield
            for cc in range(2):
                c = 2 * n + cc
                for d in range(2):
                    s2 = step_of(c, d)
                    P.op("dve", lambda h, cc=cc, d=d, s2=s2: h.tensor_copy(out=Gs[:, s2, d:d + 1], in_=GT_[:, cc, d, 5:6]), reads=[rGT_], writes=[rCH[c]])
                    P.op("dve", lambda h, cc=cc, d=d, s2=s2: h.tensor_copy(out=Bs[:, s2, d:d + 1], in_=GT_[:, cc, d, 6:7]), reads=[rGT_], writes=[rCH[c]])
                yield
            if not first:
                yield from conv_gen(n - 1, F)
            if lastb:
                yield from conv_gen(n, F)

        active = []
        nxt = 0
        GRAN = int(os.environ.get("GRAN", "1"))
        while nxt < NB or active:
            while len(active) < int(os.environ.get("DNLANES", "2")) and nxt < NB:
                active.append(blk_gen(nxt)); nxt += 1
            for g in list(active):
                try:
                    for _ in range(GRAN):
                        next(g)
                except StopIteration:
                    active.remove(g)

        Sst = [(P_sb("S0", [128, 2, 64]), Res()), (P_sb("S1", [128, 2, 64]), Res())]
        for (t, r) in Sst:
            P.op("dve", lambda h, t=t: h.memset(t[:], 0.0), writes=[r])
        HS = [slice(0, 64), slice(64, 128)]
        dve = lambda fn, reads, writes: P.op("dve", fn, reads, writes)

        def make_lane(li):
            L = {}
            for nm in ("GBC", "BBC", "EB", "DT1", "TA", "DECT", "DEC", "DECS", "DECTS", "VB", "KBE", "KD", "QGT", "AQM", "NWT", "VN", "SGL", "C0", "C1"):
                L[nm] = (P_sb("%s_%d" % (nm, li), [128, 2, 64]), Res())
            L["W0"] = (P_sb("W0_%d" % li, [128, 2, 128]), Res()); L["W1"] = (P_sb("W1_%d" % li, [128, 2, 128]), Res())
            L["SC"] = (P_sb("SC_%d" % li, [128, 8]), Res())
            a, b, c = PS[3 * li], PS[3 * li + 1], PS[3 * li + 2]
            v = lambda bank, lo, n: bank[:, lo:lo + 2 * n].rearrange("p (d f) -> p d f", d=2)
            ra, rb, rc = rPS[3 * li], rPS[3 * li + 1], rPS[3 * li + 2]
            L["ps1"] = (v(a, 0, 64), ra); L["ps4"] = (v(a, 128, 64), ra); L["ps2"] = (v(a, 256, 64), ra); L["ps3"] = (v(a, 384, 64), ra)
            L["psI"] = (v(b, 0, 128), rb); L["psC"] = (v(b, 256, 64), rb); L["pcol"] = (b[:, 384:386], rb)
            L["ps5"] = (v(c, 0, 64), rc); L["pswT"] = (v(c, 128, 64), rc); L["ps6"] = (v(c, 256, 64), rc); L["ps7"] = (v(c, 384, 64), rc)
            return L

        lanes = [make_lane(0), make_lane(1)]

        def step_gen(s, L):
            GBC, rGBC = L["GBC"]; BBC, rBBC = L["BBC"]; EB, rEB = L["EB"]; DT1, rDT1 = L["DT1"]; TA, rTA = L["TA"]
            DECT, rDECT = L["DECT"]; DEC, rDEC = L["DEC"]; DECS, rDECS = L["DECS"]; DECTS, rDECTS = L["DECTS"]
            VB, rVB = L["VB"]; KBE, rKBE = L["KBE"]; KD, rKD = L["KD"]; QGT, rQGT = L["QGT"]; AQM, rAQM = L["AQM"]
            NWT, rNWT = L["NWT"]; VN, rVN = L["VN"]; SGL, rSGL = L["SGL"]; SC, rSC = L["SC"]
            Cb = [L["C0"], L["C1"]]; Wb = [L["W0"], L["W1"]]
            ps1, r1 = L["ps1"]; ps4, r4 = L["ps4"]; ps2, r2 = L["ps2"]; ps3, r3 = L["ps3"]
            psI, rI = L["psI"]; psC, rC = L["psC"]; pcol, rcol = L["pcol"]
            ps5, r5 = L["ps5"]; pswT, rwT = L["pswT"]; ps6, r6 = L["ps6"]; ps7, r7 = L["ps7"]
            dirs = [d for d in range(2) if (d == 0 and s <= NCH - 1) or (d == 1 and s >= 1)]
            ch = {0: s, 1: (4 - s if s <= 4 else 136 - s)}
            d0, d1 = dirs[0], dirs[-1] + 1
            ds = slice(d0, d1)
            nd = d1 - d0
            rch = [rCH[ch[d]] for d in dirs]
            Scur, rScur = Sst[s % 2]; Snew, rSnew = Sst[(s + 1) % 2]
            tok = {d: slice(ch[d] * 64, ch[d] * 64 + 64) for d in dirs}
            dve(lambda h: h.tensor_tensor(out=GBC[:, ds, :], in0=ONES3[:, ds, :], in1=Gs[:, s, ds].unsqueeze(2).to_broadcast([128, nd, 64]), op=ALU.mult), [rONES3] + rch, [rGBC])
            dve(lambda h: h.tensor_tensor(out=BBC[:, ds, :], in0=ONES3[:, ds, :], in1=Bs[:, s, ds].unsqueeze(2).to_broadcast([128, nd, 64]), op=ALU.mult), [rONES3] + rch, [rBBC])
            yield
            for d in dirs:
                for hs in HS:
                    P.mm(ps1[hs, d, :], GBC[hs, d, :], TRI[hs, d, :], reads=[rGBC, rMSK], writes=[r1])
            for d in dirs:
                for hs in HS:
                    P.mm(pcol[hs, d:d + 1], TRI[hs, d, :], Gs[hs, s, d:d + 1], reads=[rMSK] + rch, writes=[rcol])
            for d in dirs:
                for hs in HS:
                    P.mm(ps4[hs, d, :], BBC[hs, d, :], ID2[hs, d, :], reads=[rBBC, rMSK], writes=[r4])
            for d in dirs:
                for hs in HS:
                    P.mm(ps2[hs, d, :], KT[hs, tok[d]], KT[hs, tok[d]], reads=rch, writes=[r2])
            for d in dirs:
                for hs in HS:
                    P.mm(ps3[hs, d, :], KT[hs, tok[d]], QT[hs, tok[d]], reads=rch, writes=[r3])
            yield
            dve(lambda h: h.tensor_copy(out=SC[:, 0:2][:, ds], in_=pcol[:, ds]), [rcol], [rSC])
            P.act(EB[:, ds, :], ps1[:, ds, :], AF.Exp, reads=[r1], writes=[rEB])
            yield
            P.act(SC[:, 2:4][:, ds], SC[:, 0:2][:, ds], AF.Exp, reads=[rSC], writes=[rSC])
            dve(lambda h: h.tensor_tensor(out=DT1[:, ds, :], in0=ps1[:, ds, :], in1=SC[:, 0:2][:, ds].unsqueeze(2).to_broadcast([128, nd, 64]), op=ALU.subtract), [r1, rSC], [rDT1])
            yield
            dve(lambda h: h.tensor_tensor(out=TA[:, ds, :], in0=DT1[:, ds, :], in1=NEGMT[:, ds, :], op=ALU.add), [rDT1, rMSK], [rTA])
            yield
            P.act(DECT[:, ds, :], TA[:, ds, :], AF.Exp, reads=[rTA], writes=[rDECT])
            yield
            dve(lambda h: h.scalar_tensor_tensor(out=TA[:, ds, :], in0=DT1[:, ds, :], scalar=-1.0, in1=NEGM[:, ds, :], op0=ALU.mult, op1=ALU.add), [rDT1, rMSK, rDECT], [rTA])
            yield
            P.act(DEC[:, ds, :], TA[:, ds, :], AF.Exp, reads=[rTA], writes=[rDEC])
            for d in dirs:
                lastc = 63 if d == 0 else 0
                dve(lambda h, d=d, lastc=lastc: h.tensor_tensor(out=SC[:, 4 + d:5 + d], in0=ps1[:, d, lastc:lastc + 1], in1=SC[:, d:d + 1], op=ALU.subtract), [r1, rSC], [rSC])
            yield
            P.act(SC[:, 4:6][:, ds], SC[:, 4:6][:, ds], AF.Exp, reads=[rSC], writes=[rSC])
            C0, rC0 = Cb[0]; W0, rW0 = Wb[0]
            dve(lambda h: h.tensor_tensor(out=DECS[:, ds, :], in0=DEC[:, ds, :], in1=NST[:, ds, :], op=ALU.mult), [rDEC, rMSK], [rDECS])
            yield
            for d in dirs:
                dve(lambda h, d=d: h.scalar_tensor_tensor(out=C0[:, d, :], in0=ps2[:, d, :], scalar=Bs[:, s, d:d + 1], in1=DECS[:, d, :], op0=ALU.mult, op1=ALU.mult),
                    [r2, rDECS] + rch, [rC0])
            yield
            dve(lambda h: h.tensor_tensor(out=DECTS[:, ds, :], in0=DECT[:, ds, :], in1=NSTT[:, ds, :], op=ALU.mult), [rDECT, rMSK], [rDECTS])
            yield
            dve(lambda h: h.tensor_tensor(out=DECTS[:, ds, :], in0=ps2[:, ds, :], in1=DECTS[:, ds, :], op=ALU.mult), [r2, rDECTS], [rDECTS])
            yield
            dve(lambda h: h.tensor_tensor(out=W0[:, ds, 0:64], in0=ps4[:, ds, :], in1=DECTS[:, ds, :], op=ALU.mult), [r4, rDECTS], [rW0])
            dve(lambda h: h.tensor_copy(out=W0[:, ds, 64:128], in_=ID2[:, ds, :]), [rMSK], [rW0])
            yield
            for m in range(6):
                (Wc, rWc), (Wn, rWn) = Wb[m % 2], Wb[(m + 1) % 2]
                (Cc, rCc), (Cn, rCn) = Cb[m % 2], Cb[(m + 1) % 2]
                for d in dirs:
                    for hs in HS:
                        P.mm(psI[hs, d, :], Cc[hs, d, :], Wc[hs, d, :], reads=[rCc, rWc], writes=[rI])
                if m < 5:
                    for d in dirs:
                        for hs in HS:
                            P.mm(psC[hs, d, :], Wc[hs, d, 0:64], Cc[hs, d, :], reads=[rCc, rWc], writes=[rC])
                yield
                dve(lambda h, Wn=Wn, Wc=Wc: h.tensor_tensor(out=Wn[:, ds, 64:128], in0=Wc[:, ds, 64:128], in1=psI[:, ds, 64:128], op=ALU.add), [rWc, rI], [rWn])
                if m < 5:
                    P.act(Wn[:, ds, 0:64], psI[:, ds, 0:64], AF.Copy, reads=[rI], writes=[rWn])
                    P.act(Cn[:, ds, :], psC[:, ds, :], AF.Copy, reads=[rC], writes=[rCn])
                yield
                if m == 2:
                    yield "HALF"
            TITt, rTIT = Wb[0]
            TIT = TITt[:, :, 64:128]
            for d in dirs:
                c = ch[d]
                dve(lambda h, d=d, c=c: h.tensor_scalar(out=VB[:, d, :], in0=VTM[:, c, :], scalar1=Bs[:, s, d:d + 1], scalar2=None, op0=ALU.mult), rch, [rVB])
                dve(lambda h, d=d, c=c: h.tensor_scalar(out=KBE[:, d, :], in0=KTM[:, c, :], scalar1=Bs[:, s, d:d + 1], scalar2=SC[:, 2 + d:3 + d], op0=ALU.mult, op1=ALU.mult), rch + [rSC], [rKBE])
                yield
                dve(lambda h, d=d, c=c: h.tensor_scalar(out=KD[:, d, :], in0=KTM[:, c, :], scalar1=SC[:, 4 + d:5 + d], scalar2=None, op0=ALU.mult), rch + [rSC], [rKD])
                dve(lambda h, d=d: h.tensor_tensor(out=QGT[:, d, :], in0=QT[:, tok[d]], in1=EB[:, d, :], op=ALU.mult), rch + [rEB], [rQGT])
                yield
            dve(lambda h: h.tensor_tensor(out=AQM[:, ds, :], in0=ps3[:, ds, :], in1=DECT[:, ds, :], op=ALU.mult), [r3, rDECT], [rAQM])
            for d in dirs:
                for hs in HS:
                    P.mm(pswT[hs, d, :], KBE[hs, d, :], TIT[hs, d, :], reads=[rKBE, rTIT], writes=[rwT])
            yield
            dve(lambda h: h.tensor_scalar(out=NWT[:, ds, :], in0=pswT[:, ds, :], scalar1=-1.0, scalar2=None, op0=ALU.mult), [rwT], [rNWT])
            yield
            for d in dirs:
                for hs in HS:
                    P.mm(ps5[hs, d, :], TIT[hs, d, :], VB[hs, d, :], start=True, stop=False, reads=[rTIT, rVB], writes=[r5])
                    P.mm(ps5[hs, d, :], NWT[hs, d, :], Scur[hs, d, :], start=False, stop=True, reads=[rNWT, rScur], writes=[r5])
            yield
            dve(lambda h: h.tensor_copy(out=VN[:, ds, :], in_=ps5[:, ds, :]), [r5], [rVN])
            yield
            for d in dirs:
                for hs in HS:
                    P.mm(ps6[hs, d, :], QGT[hs, d, :], Scur[hs, d, :], start=True, stop=False, reads=[rQGT, rScur], writes=[r6])
                    P.mm(ps6[hs, d, :], AQM[hs, d, :], VN[hs, d, :], start=False, stop=True, reads=[rAQM, rVN], writes=[r6])
            for d in dirs:
                for hs in HS:
                    P.mm(ps7[hs, d, :], KD[hs, d, :], VN[hs, d, :], reads=[rKD, rVN], writes=[r7])
            yield
            for d in range(2):
                if d in dirs:
                    lastc = 63 if d == 0 else 0
                    dve(lambda h, d=d, lastc=lastc: h.tensor_scalar(out=SGL[:, d, :], in0=Scur[:, d, :], scalar1=EB[:, d, lastc:lastc + 1], scalar2=None, op0=ALU.mult), [rScur, rEB], [rSGL])
                    dve(lambda h, d=d: h.tensor_tensor(out=Snew[:, d, :], in0=SGL[:, d, :], in1=ps7[:, d, :], op=ALU.add), [rSGL, r7], [rSnew])
                else:
                    dve(lambda h, d=d: h.tensor_copy(out=Snew[:, d, :], in_=Scur[:, d, :]), [rScur], [rSnew])
            yield
            for d in dirs:
                c = ch[d]
                dve(lambda h, d=d, c=c: h.tensor_tensor(out=O[:, c, :], in0=O[:, c, :], in1=ps6[:, d, :], op=ALU.add), [r6, rO[c]], [rO[c]])
            yield

        def drive(older, newer):
            a_done = older is None
            b_done = newer is None
            while not (a_done and b_done):
                if not a_done:
                    try:
                        next(older)
                    except StopIteration:
                        a_done = True
                if not b_done:
                    try:
                        if next(newer) == "HALF":
                            b_done = True
                    except StopIteration:
                        b_done = True

        prev = None
        for s in range(NS):
            g = step_gen(s, lanes[s % 2])
            drive(prev, g)
            prev = g
        drive(prev, None)

        P.fence()
        G = 33
        OSQ = P_sb("OSQ", [128, G, 64]); rOSQ = Res()
        OSS = P_sb("OSS", [128, G]); rOSS = Res()
        for g in range(NCH // G):
            cs = slice(g * G, (g + 1) * G)
            ro = [rO[c] for c in range(g * G, (g + 1) * G)]
            dve(lambda h, cs=cs: h.tensor_tensor(out=OSQ[:], in0=O[:, cs, :], in1=O[:, cs, :], op=ALU.mult), ro, [rOSQ])
            dve(lambda h: h.reduce_sum(out=OSS[:], in_=OSQ[:], axis=AX.X), [rOSQ], [rOSS])
            P.act(OSS[:], OSS[:], AF.Ln, reads=[rOSS], writes=[rOSS], bias=1e-6, scale=1.0 / 64)
            P.act(OSS[:], OSS[:], AF.Exp, reads=[rOSS], writes=[rOSS], scale=-0.5)
            dve(lambda h, cs=cs: h.tensor_tensor(out=O[:, cs, :], in0=O[:, cs, :], in1=OSS[:].unsqueeze(2).to_broadcast([128, G, 64]), op=ALU.mult), ro + [rOSS], ro)
            dve(lambda h, cs=cs: h.tensor_tensor(out=O[:, cs, :], in0=O[:, cs, :], in1=GDN[:].unsqueeze(1).to_broadcast([128, G, 64]), op=ALU.mult), ro + [rGDN], ro)
            dve(lambda h, cs=cs: h.tensor_tensor(out=O[:, cs, :], in0=O[:, cs, :], in1=SZ[:, cs, :], op=ALU.mult), ro + [rCH[c] for c in range(g * G, (g + 1) * G)], ro)
        ov = o_dn.rearrange("(c t) (h d) -> h t c d", t=64, h=2)
        for hh in range(2):
            P.dma("sp", ov[hh], O[hh * 64:(hh + 1) * 64, :, :], reads=rO, writes=[rOUT[hh]])
        P.fence()


NE = 32


def emit_b(P, G, IN, l, NL, NC, xs, rxs, omall, romall, xo, rOUT):
    NT = NL + NC
    cvT = IN["cvT"]; adaw = IN["adaw2", l]; adab = IN["adab2", l]; adabT = IN["adab2T", l]
    g2T = IN["g2T", l]; wout = IN["wout", l]; rw = IN["rw", l]; rb = IN["rb", l]
    wgu = IN["wgu", l]; bguT = IN["bguT", l]; wd = IN["wd", l]; bd = IN["bd", l]; seli = IN["seli"]

    tiles = [(i * 128, 128, 0) for i in range(NL // 128)]
    if NC:
        tiles.append((NL, NC, 1))
    nlt = NL // 128
    passes = [list(range(0, nlt // 2)), list(range(nlt // 2, len(tiles)))]
    MAXT = max(len(p) for p in passes)

    with ExitStack() as st0:
        _sb = P.sb
        P_sb = lambda name, shape, dt=F32, stack=None: _sb("b_" + name, shape, dt, stack=(stack or st0))
        ID, rID, ONES, rONES, ONESB, rONESB, PS, rPS = G["ID"], G["rID"], G["ONES"], G["rONES"], G["ONESB"], G["rONESB"], G["PS"], G["rPS"]
        SELI = P_sb("SELI", [128, 4, 128], BF16); rSELI = Res()
        P.dma("pool", SELI[:], seli, writes=[rSELI])

        GT = P_sb("GT", [128, 2, 2, D]); rGT = Res()
        MODF = P_sb("MODF", [128, 16, 2]); rMODF = Res()
        SCALE2 = P_sb("SCALE2", [128, 8, 2]); rSCALE2 = Res()
        G2 = P_sb("G2", [128, 8]); rG2 = Res()
        P.dma("sp", G2[:], g2T, writes=[rG2])
        ABT = P_sb("ABT", [128, 16]); rABT = Res()
        P.dma("sp", ABT[:], adabT, writes=[rABT])
        RW = P_sb("RW", [128, 8, NE]); rRW = Res()
        P.dma("sp", RW[:], rw.rearrange("(k p) e -> p k e", p=128), writes=[rRW])
        RB = P_sb("RB", [1, NE]); rRB = Res()
        P.dma("sp", RB[:], rb, writes=[rRB])
        WO = P_sb("WO", [128, 8, D], BF16); rWO = Res()
        P.dma("pool", WO[:], wout.rearrange("(k p) f -> p k f", p=128), writes=[rWO])
        sub = ExitStack()
        ABR = P_sb("ABR", [1, 4096], stack=sub); rABR = Res()
        P.dma("sp", ABR[:], adab, writes=[rABR])
        CV = P_sb("CV", [128, 8, 2], stack=sub); rCV = Res()
        P.dma("sp", CV[:], cvT, writes=[rCV])
        SCV = P_sb("SCV", [128, 8, 2], stack=sub); rSCV = Res()
        P.act(SCV[:], CV[:], AF.Silu, reads=[rCV], writes=[rSCV])
        SCB = P_sb("SCB", [128, 8, 2, 128], stack=sub); rSCB = Res()
        P.op("dve", lambda h: h.tensor_tensor(out=SCB[:], in0=ONES[:].unsqueeze(1).unsqueeze(1).to_broadcast([128, 8, 2, 128]),
                                              in1=SCV[:].unsqueeze(3).to_broadcast([128, 8, 2, 128]), op=ALU.mult),
             reads=[rONES, rSCV], writes=[rSCB])

        AW = [P_sb("AW%d" % i, [128, 8, 512], stack=sub) for i in range(2)]; rAW = [Res(), Res()]
        for blk in range(8):
            aw, raw = AW[blk % 2], rAW[blk % 2]
            P.dma("sp", aw[:], adaw[:, blk * 512:(blk + 1) * 512].rearrange("(k p) f -> p k f", p=128), writes=[raw])
            if blk in (0, 1, 6, 7):
                which = 0 if blk < 2 else 1
                half = blk % 2
                for j in range(2):
                    ps, rps = PS[j], rPS[j]
                    for k in range(8):
                        P.mm(ps[:], SCB[:, k, j, :], aw[:, k, :], start=(k == 0), stop=False,
                             reads=[rSCB, raw], writes=[rps])
                    P.mm(ps[:], ONES[0:1, :], ABR[0:1, blk * 512:(blk + 1) * 512], start=False, stop=True,
                         reads=[rONES, rABR], writes=[rps])
                    P.act(GT[:, which, j, half * 512:(half + 1) * 512], ps[:], AF.Copy, reads=[rps], writes=[rGT])
            else:
                ps, rps = PS[2 + blk % 2], rPS[2 + blk % 2]
                for fcl in range(4):
                    fcg = (blk - 2) * 4 + fcl
                    for k in range(8):
                        P.mm(ps[:, fcl * 2:fcl * 2 + 2], aw[:, k, fcl * 128:(fcl + 1) * 128], SCV[:, k, :],
                             start=(k == 0), stop=(k == 7), reads=[rSCV, raw], writes=[rps])
                for fcl in range(4):
                    fcg = (blk - 2) * 4 + fcl
                    P.act(MODF[:, fcg, :], ps[:, fcl * 2:fcl * 2 + 2], AF.Identity, reads=[rps, rABT], writes=[rMODF],
                          bias=ABT[:, fcg:fcg + 1], scale=1.0)
        P.op("dve", lambda h: h.tensor_scalar(out=SCALE2[:], in0=MODF[:, 8:16, :], scalar1=1.0, scalar2=None, op0=ALU.add),
             reads=[rMODF], writes=[rSCALE2])
        P.op("dve", lambda h: h.tensor_tensor(out=SCALE2[:], in0=SCALE2[:], in1=G2[:].unsqueeze(2).to_broadcast([128, 8, 2]), op=ALU.mult),
             reads=[rSCALE2, rG2], writes=[rSCALE2])

        P.fence()
        sub.close()
        X1 = P_sb("X1", [128, MAXT, D]); rX1 = [Res() for _ in range(MAXT)]
        H2B = P_sb("H2B", [128, 8, MAXT * 128], BF16); rH2B = [Res() for _ in range(MAXT)]
        GATES = P_sb("GATES", [128, MAXT, NE]); rGATES = [Res() for _ in range(MAXT)]
        XT = [P_sb("XT%d" % i, [128, D]) for i in range(2)]; rXT = [Res(), Res()]
        OM = [P_sb("OM%d" % i, [128, 8, 128], BF16) for i in range(2)]; rOM = [Res(), Res()]
        OMX = P_sb("OMX", [128, 4, 4, 256], BF16); rOMX = Res()
        TMP = [P_sb("TMP%d" % i, [128, D]) for i in range(2)]; rTMP = [Res(), Res()]
        H2F = P_sb("H2F", [128, 8, 128]); rH2F = Res()
        SMALL = P_sb("SMALL", [128, 64]); rSM = Res()
        LG = P_sb("LG", [128, NE]); rLG = Res()
        EX = P_sb("EX", [128, NE]); rEX = Res()
        MK = P_sb("MK", [128, NE]); rMK = Res()
        WGU = P_sb("WGU", [128, 8, 2 * D], BF16); rWG = [Res() for _ in range(8)]; rWU = [Res() for _ in range(8)]
        WD = P_sb("WD", [128, 8, D], BF16); rWD = [Res() for _ in range(8)]
        BGU = [P_sb("BGU%d" % i, [128, 16]) for i in range(2)]; rBGU = [Res(), Res()]
        BD = [P_sb("BD%d" % i, [1, D], BF16) for i in range(2)]; rBD = [Res(), Res()]
        BGX = [P_sb("BGX%d" % i, [128, 16]) for i in range(2)]; rBGX = [Res(), Res()]
        SIGC = float(1.0 / (1.0 + np.exp(np.float64(-1.702 * 7.0))))
        ACTT = [P_sb("ACTT%d" % i, [128, 8, 512], BF16) for i in range(2)]; rACTT = [Res(), Res()]
        GP = [P_sb("GP%d" % i, [128, 512]) for i in range(2)]; rGP = [Res(), Res()]
        SG = [P_sb("SG%d" % i, [128, 512]) for i in range(2)]; rSG = [Res(), Res()]
        UP = [P_sb("UP%d" % i, [128, 512]) for i in range(2)]; rUP = [Res(), Res()]
        wcount = 0
        cnt = 0

        for pss in passes:
            for li, ti in enumerate(pss):
                r0, nr, j = tiles[ti]
                xt, rxt = XT[li % 2], rXT[li % 2]
                om, rom = OM[li % 2], rOM[li % 2]
                tmp, rtmp = TMP[li % 2], rTMP[li % 2]
                P.dma("sp", xt[:nr, :], xs[r0:r0 + nr, :], reads=rxs, writes=[rxt])
                for q in range(4):
                    grow = (256 + q * NL + r0) if j == 0 else (q * NC)
                    for jj in range(4):
                        P.dma("pool", OMX[:nr, q, jj, :], omall(jj, grow, nr), reads=romall, writes=[rOMX])
                for k in range(8):
                    ps, rps = PS[2 + k // 4], rPS[2 + k // 4]
                    for q in range(4):
                        P.mm(ps[:, (k % 4) * 128:(k % 4) * 128 + nr], OMX[:nr, q, k % 4, (k // 4) * 128:(k // 4 + 1) * 128], SELI[:nr, q, :nr],
                             start=(q == 0), stop=(q == 3), reads=[rOMX, rSELI], writes=[rps])
                for kk in range(2):
                    P.act(om[:, 4 * kk:4 * kk + 4, :nr], PS[2 + kk][:, :].rearrange("p (a t) -> p a t", a=4)[:, :, :nr], AF.Copy, reads=[rPS[2 + kk]], writes=[rom])
                for half in range(2):
                    ps, rps = PS[half], rPS[half]
                    for k in range(8):
                        P.mm(ps[:nr, :], om[:, k, :nr], WO[:, k, half * 512:(half + 1) * 512], start=(k == 0), stop=(k == 7),
                             reads=[rom, rWO], writes=[rps])
                    P.op("dve", lambda h, ps=ps, tmp=tmp, half=half, j=j, nr=nr: h.tensor_tensor(
                        out=tmp[:nr, half * 512:(half + 1) * 512], in0=ps[:nr, :], in1=GT[:nr, 0, j, half * 512:(half + 1) * 512], op=ALU.mult),
                        reads=[rps, rGT], writes=[rtmp])
                P.op("dve", lambda h, tmp=tmp, xt=xt, li=li, nr=nr: h.tensor_tensor(out=X1[:nr, li, :], in0=tmp[:nr, :], in1=xt[:nr, :], op=ALU.add),
                     reads=[rtmp, rxt], writes=[rX1[li]])
                P.act(tmp[:nr, :], X1[:nr, li, :], AF.Square, reads=[rX1[li]], writes=[rtmp, rSM], accum_out=SMALL[:nr, 0:1])
                P.act(SMALL[:nr, 1:2], SMALL[:nr, 0:1], AF.Sqrt, reads=[rSM], writes=[rSM], bias=1e-6, scale=1.0 / D)
                P.op("dve", lambda h, nr=nr: h.reciprocal(out=SMALL[:nr, 2:3], in_=SMALL[:nr, 1:2]), reads=[rSM], writes=[rSM])
                P.op("dve", lambda h, tmp=tmp, li=li, nr=nr: h.tensor_scalar(out=tmp[:nr, :], in0=X1[:nr, li, :], scalar1=SMALL[:nr, 2:3], scalar2=None, op0=ALU.mult),
                     reads=[rX1[li], rSM], writes=[rtmp])
                for k in range(8):
                    ps, rps = PS[2 + k // 4], rPS[2 + k // 4]
                    P.op("pe", lambda h, ps=ps, tmp=tmp, k=k, nr=nr: h.transpose(ps[:, (k % 4) * 128:(k % 4) * 128 + nr], tmp[:nr, k * 128:(k + 1) * 128], ID[:nr, :nr]),
                         reads=[rtmp, rID], writes=[rps])
                for k in range(8):
                    ps, rps = PS[2 + k // 4], rPS[2 + k // 4]
                    P.act(H2F[:, k, :nr], ps[:, (k % 4) * 128:(k % 4) * 128 + nr], AF.Identity, reads=[rps, rSCALE2, rMODF], writes=[rH2F],
                          scale=SCALE2[:, k, j:j + 1], bias=MODF[:, k, j:j + 1])
                P.op("dve", lambda h, li=li, nr=nr: h.tensor_copy(out=H2B[:, :, li * 128:li * 128 + nr], in_=H2F[:, :, :nr]),
                     reads=[rH2F], writes=[rH2B[li]])
                ps, rps = PS[4], rPS[4]
                for k in range(8):
                    P.mm(ps[:nr, 0:NE], H2F[:, k, :nr], RW[:, k, :], start=(k == 0), stop=False, reads=[rH2F, rRW], writes=[rps])
                P.mm(ps[:nr, 0:NE], ONES[0:1, :nr], RB[0:1, :], start=False, stop=True, reads=[rONES, rRB], writes=[rps])
                P.op("dve", lambda h, ps=ps, nr=nr: h.tensor_copy(out=LG[:nr, :], in_=ps[:nr, 0:NE]), reads=[rps], writes=[rLG])
                P.op("dve", lambda h, nr=nr: h.max(out=SMALL[:nr, 8:16], in_=LG[:nr, :]), reads=[rLG], writes=[rSM])
                P.op("dve", lambda h, nr=nr: h.tensor_scalar(out=SMALL[:nr, 16:17], in0=SMALL[:nr, 8:9], scalar1=-1.0, scalar2=None, op0=ALU.mult),
                     reads=[rSM], writes=[rSM])
                P.act(EX[:nr, :], LG[:nr, :], AF.Exp, reads=[rLG, rSM], writes=[rEX], bias=SMALL[:nr, 16:17], scale=1.0)
                P.op("dve", lambda h, nr=nr: h.tensor_scalar(out=MK[:nr, :], in0=LG[:nr, :], scalar1=SMALL[:nr, 11:12], scalar2=None, op0=ALU.is_ge),
                     reads=[rLG, rSM], writes=[rMK])
                P.op("dve", lambda h, nr=nr: h.tensor_tensor(out=EX[:nr, :], in0=EX[:nr, :], in1=MK[:nr, :], op=ALU.mult),
                     reads=[rEX, rMK], writes=[rEX])
                P.op("dve", lambda h, nr=nr: h.reduce_sum(out=SMALL[:nr, 17:18], in_=EX[:nr, :], axis=AX.X), reads=[rEX], writes=[rSM])
                P.op("dve", lambda h, nr=nr: h.reciprocal(out=SMALL[:nr, 18:19], in_=SMALL[:nr, 17:18]), reads=[rSM], writes=[rSM])
                P.op("dve", lambda h, li=li, nr=nr: h.tensor_scalar(out=GATES[:nr, li, :], in0=EX[:nr, :], scalar1=SMALL[:nr, 18:19], scalar2=None, op0=ALU.mult),
                     reads=[rEX, rSM], writes=[rGATES[li]])

            groups = []
            li = 0
            while li < len(pss):
                g = []
                while li < len(pss) and len(g) < 4 and tiles[pss[li]][1] == 128:
                    g.append(li); li += 1
                if not g:
                    g = [li]; li += 1
                groups.append(g)
            for e in range(IN.get("nex", NE)):
                wb = wcount % 2
                wcount += 1
                for fc in range(8):
                    P.dma("pool", WGU[:, :, fc * 128:(fc + 1) * 128], wgu[e, :, fc * 128:(fc + 1) * 128].rearrange("(k p) f -> p k f", p=128),
                          writes=[rWG[fc]])
                    P.dma("pool", WGU[:, :, D + fc * 128:D + (fc + 1) * 128], wgu[e, :, D + fc * 128:D + (fc + 1) * 128].rearrange("(k p) f -> p k f", p=128),
                          writes=[rWU[fc]])
                for fc in range(8):
                    P.dma("pool", WD[:, fc, :], wd[e, fc * 128:(fc + 1) * 128, :], writes=[rWD[fc]])
                P.dma("sp", BGU[wb][:], bguT[e], writes=[rBGU[wb]])
                P.op("dve", lambda h, wb=wb: h.tensor_scalar(out=BGX[wb][:, 0:8], in0=BGU[wb][:, 0:8], scalar1=1.702, scalar2=None, op0=ALU.mult),
                     reads=[rBGU[wb]], writes=[rBGX[wb]])
                P.op("dve", lambda h, wb=wb: h.tensor_scalar(out=BGX[wb][:, 8:16], in0=BGU[wb][:, 8:16], scalar1=1.0, scalar2=None, op0=ALU.add),
                     reads=[rBGU[wb]], writes=[rBGX[wb]])
                P.dma("pool", BD[wb][:], bd[e:e + 1, :], writes=[rBD[wb]])
                for g in groups:
                    ntok = sum(tiles[pss[l]][1] for l in g)
                    c0 = g[0] * 128
                    ab = cnt % 2
                    cnt += 1
                    actt, ractt = ACTT[ab], rACTT[ab]
                    rh = [rH2B[l] for l in g]
                    for fc in range(8):
                        pb = (fc % 2) * 2
                        psg, rpsg = PS[pb], rPS[pb]
                        psu, rpsu = PS[pb + 1], rPS[pb + 1]
                        for k in range(8):
                            P.mm(psg[:, :ntok], WGU[:, k, fc * 128:(fc + 1) * 128], H2B[:, k, c0:c0 + ntok], start=(k == 0), stop=(k == 7),
                                 reads=[rWG[fc]] + rh, writes=[rpsg])
                        for k in range(8):
                            P.mm(psu[:, :ntok], WGU[:, k, D + fc * 128:D + (fc + 1) * 128], H2B[:, k, c0:c0 + ntok], start=(k == 0), stop=(k == 7),
                                 reads=[rWU[fc]] + rh, writes=[rpsu])
                        tb = fc % 2
                        gp, sg, up = GP[tb], SG[tb], UP[tb]
                        P.act(sg[:, :ntok], psg[:, :ntok], AF.Sigmoid, reads=[rpsg, rBGX[wb]], writes=[rSG[tb]], scale=1.702, bias=BGX[wb][:, fc:fc + 1])
                        P.op("dve", lambda h, gp=gp, psg=psg, fc=fc, wb=wb, ntok=ntok: h.tensor_scalar(
                            out=gp[:, :ntok], in0=psg[:, :ntok], scalar1=BGU[wb][:, fc:fc + 1], scalar2=7.0, op0=ALU.add, op1=ALU.min),
                            reads=[rpsg, rBGU[wb]], writes=[rGP[tb]])
                        P.op("dve", lambda h, up=up, psu=psu, fc=fc, wb=wb, ntok=ntok: h.tensor_scalar(
                            out=up[:, :ntok], in0=psu[:, :ntok], scalar1=BGX[wb][:, 8 + fc:9 + fc], scalar2=8.0, op0=ALU.add, op1=ALU.min),
                            reads=[rpsu, rBGX[wb]], writes=[rUP[tb]])
                        P.op("dve", lambda h, gp=gp, sg=sg, ntok=ntok: h.scalar_tensor_tensor(out=gp[:, :ntok], in0=sg[:, :ntok], scalar=SIGC, in1=gp[:, :ntok], op0=ALU.min, op1=ALU.mult),
                             reads=[rGP[tb], rSG[tb]], writes=[rGP[tb]])
                        P.op("dve", lambda h, gp=gp, up=up, actt=actt, fc=fc, ntok=ntok: h.scalar_tensor_tensor(out=actt[:, fc, :ntok], in0=up[:, :ntok], scalar=-6.0, in1=gp[:, :ntok], op0=ALU.max, op1=ALU.mult),
                             reads=[rGP[tb], rUP[tb]], writes=[ractt])
                    for gi, l in enumerate(g):
                        r0, nr, j = tiles[pss[l]]
                        yb = 4 + (l % 2) * 2
                        for half in range(2):
                            ps, rps = PS[yb + half], rPS[yb + half]
                            for fc in range(8):
                                P.mm(ps[:nr, :], actt[:, fc, gi * 128:gi * 128 + nr], WD[:, fc, half * 512:(half + 1) * 512], start=(fc == 0), stop=False,
                                     reads=[ractt, rWD[fc]], writes=[rps])
                            P.mm(ps[:nr, :], ONESB[0:1, :nr], BD[wb][0:1, half * 512:(half + 1) * 512], start=False, stop=True,
                                 reads=[rONESB, rBD[wb]], writes=[rps])
                        tmp, rtmp = TMP[l % 2], rTMP[l % 2]
                        for half in range(2):
                            ps, rps = PS[yb + half], rPS[yb + half]
                            P.op("dve", lambda h, ps=ps, tmp=tmp, half=half, l=l, e=e, j=j, nr=nr: h.scalar_tensor_tensor(
                                out=tmp[:nr, half * 512:(half + 1) * 512], in0=ps[:nr, :], scalar=GATES[:nr, l, e:e + 1],
                                in1=GT[:nr, 1, j, half * 512:(half + 1) * 512], op0=ALU.mult, op1=ALU.mult),
                                reads=[rps, rGATES[l], rGT], writes=[rtmp])
                        P.op("dve", lambda h, tmp=tmp, l=l, nr=nr: h.tensor_tensor(out=X1[:nr, l, :], in0=X1[:nr, l, :], in1=tmp[:nr, :], op=ALU.add),
                             reads=[rtmp, rX1[l]], writes=[rX1[l]])
            for li, ti in enumerate(pss):
                r0, nr, j = tiles[ti]
                P.dma("sp", xo[r0:r0 + nr, :], X1[:nr, li, :], reads=[rX1[li]], writes=[rOUT[ti]])
        P.fence()


PER_LAYER = [("adaw1", [1024, 2048]), ("adab1T", [128, 16]), ("g1T", [128, 8]), ("wa", [1024, 384]), ("wz", [1024, 2, 68]),
             ("cw", [128, 3, 5]), ("nega", [128, 2]), ("dtb", [128, 2]), ("gdn", [128, 64]), ("ws", [1024, 256]),
             ("gqk", [128, 3, 64]), ("sinkb", [128, 2]),
             ("adaw2", [1024, 4096]), ("adab2", [1, 4096]), ("adab2T", [128, 16]), ("g2T", [128, 8]), ("wout", [1024, 1024]),
             ("rw", [1024, 32]), ("rb", [1, 32]), ("bguT", [32, 128, 16]), ("bd", [32, 1024])]
SHARED = [("ident", [128, 128]), ("cvT", [128, 8, 2]), ("blk1", [128, 128]), ("masks", [128, 6, 2, 64]), ("maskw", [128, 384]),
          ("ropet", [128, 64, 2, 2, 16]), ("seli", [128, 4, 128])]
NLOC = 2112


def build_fused(nex=32):
    nc = bass.Bass("TRN2", target_bir_lowering=False)
    dr = lambda name, shape: nc.dram_tensor(name, list(shape), F32, kind="ExternalInput").ap()
    IN = {}
    IN["xall"] = dr("xall", [8448, 1024]); IN["xs0"] = dr("xs0", [NLOC, 1024])
    for nm, shp in SHARED:
        IN[nm] = dr(nm, shp)
    for l in range(2):
        for nm, shp in PER_LAYER:
            IN[nm, l] = dr("%s_%d" % (nm, l), shp)
    for l in range(2):
        IN["wgu", l] = dr("wgu_%d" % l, [nex, 1024, 2048]); IN["wd", l] = dr("wd_%d" % l, [nex, 1024, 1024])
    IN["nex"] = nex
    xo = nc.dram_tensor("xo", [2048, 1024], F32, kind="ExternalOutput").ap()
    omloc = [nc.dram_tensor("omloc%d" % l, [8448, 256], F32).ap() for l in range(2)]
    OCH = 1024
    och = [(r, min(OCH, 8448 - r)) for r in range(0, 8448, OCH)]
    omall = [[nc.dram_tensor("omall%d_%d" % (l, k), [4 * n, 256], F32).ap() for k, (r, n) in enumerate(och)] for l in range(2)]
    xloc = nc.dram_tensor("xloc", [NLOC, 1024], F32).ap()
    XCH = 256
    xch = [(r, min(XCH, NLOC - r)) for r in range(0, NLOC, XCH)]
    xgat = [nc.dram_tensor("xgat_%d" % k, [4 * n, 1024], F32).ap() for k, (r, n) in enumerate(xch)]

    def om_rows(l, jj, grow, nr):
        k = grow // OCH
        n = och[k][1]
        off = jj * n + (grow - och[k][0])
        return omall[l][k][off:off + nr, :]

    def xg_rows(q, r, nr):
        k = r // XCH
        n = xch[k][1]
        off = q * n + (r - xch[k][0])
        return xgat[k][off:off + nr, :]
    groups = [[0, 1, 2, 3], [4, 5, 6, 7]]
    with ExitStack() as st:
        P = Prog(nc, st)
        G = emit_globals(P, IN)
        rxloc = [Res() for _ in range(17)]
        rxgat = Res()
        rcc = Res()
        rxo = [Res() for _ in range(16)]
        for l in range(2):
            last = (l == 1)
            if l == 0:
                xsrc = lambda n: [(0, 128, IN["xall"][n * 128:(n + 1) * 128, :])]
                rsrc = []
            else:
                def xsrc(n):
                    if n < 2:
                        a, b = 2 * n, 2 * n + 1
                        return [(0, 64, xg_rows(a, 2048, 64)), (64, 64, xg_rows(b, 2048, 64))]
                    i = n - 2
                    q, r = i // 16, (i % 16) * 128
                    return [(0, 128, xg_rows(q, r, 128))]
                rsrc = [rxgat]
            rdn = [Res(), Res()]
            rsw = [Res() for _ in range(66)]
            with ExitStack() as lst:
                C = emit_common(P, G, IN, l, lst)
                emit_dn(P, C, IN, l, last, xsrc, rsrc, omloc[l][:, 0:128], rdn)
                emit_swa(P, C, IN, l, last, xsrc, rsrc, omloc[l][:, 128:256], rsw)
                P.fence()
            romall = Res()
            if os.environ.get("NOCOLL") != "1":
                for k, (r, n) in enumerate(och):
                    P.coll("AllGather", ALU.bypass, groups, omloc[l][r:r + n, :], omall[l][k], reads=rdn + rsw, writes=[romall, rcc])
            if l == 0:
                emit_b(P, G, IN, 0, 2048, 64, IN["xs0"], [], lambda jj, grow, nr: om_rows(0, jj, grow, nr), [romall], xloc, rxloc)
                if os.environ.get("NOCOLL") not in ("1", "2"):
                    for k, (r, n) in enumerate(xch):
                        P.coll("AllGather", ALU.bypass, groups, xloc[r:r + n, :], xgat[k], reads=rxloc, writes=[rxgat, rcc])
            else:
                emit_b(P, G, IN, 1, 2048, 0, xloc, rxloc, lambda jj, grow, nr: om_rows(1, jj, grow, nr), [romall], xo, rxo)
        P.finish(rxo)
        print("fused instr counts", P.cnt, P.dcnt, "waits", P.n_wait, "sems", {k: len(v) for k, v in P.sem.items()})
    return nc


NEG = -1e30
def consts():
    f = np.float32
    i = np.arange(64)
    k_le_f = (i[:, None] <= i[None, :]).astype(f)
    k_ge_f = (i[:, None] >= i[None, :]).astype(f)
    m = np.zeros((64, 6, 2, 64), f)
    m[:, 0, 0] = k_le_f; m[:, 0, 1] = k_ge_f
    m[:, 1, 0] = np.where(i[None, :] >= i[:, None], 0, NEG)
    m[:, 1, 1] = np.where(i[None, :] <= i[:, None], 0, NEG)
    m[:, 2, 0] = np.where(i[None, :] <= i[:, None], 0, NEG)
    m[:, 2, 1] = np.where(i[None, :] >= i[:, None], 0, NEG)
    m[:, 3, 0] = np.where(i[None, :] > i[:, None], -1, 0)
    m[:, 3, 1] = np.where(i[None, :] < i[:, None], -1, 0)
    m[:, 4, 0] = np.where(i[None, :] < i[:, None], -1, 0)
    m[:, 4, 1] = np.where(i[None, :] > i[:, None], -1, 0)
    m[:, 5, 0] = np.eye(64); m[:, 5, 1] = np.eye(64)
    masks = np.concatenate([m, m], 0)
    blk1 = np.zeros((128, 128), f); blk1[:64, :64] = 1; blk1[64:, 64:] = 1
    qi = np.arange(128)[:, None]; kc = np.arange(384)[None, :]
    maskw = np.where((kc >= qi) & (kc <= qi + 256), 0, NEG).astype(f)
    t = np.arange(8192); row = (t // 64).astype(f); col = (t % 64).astype(f)
    inv = np.power(f(10000.0), -np.arange(16, dtype=f) / f(16)).astype(f)
    ar = row[:, None] * inv; ac = col[:, None] * inv
    rope = np.stack([np.stack([np.cos(ar), np.cos(ac)], 1), np.stack([np.sin(ar), np.sin(ac)], 1)], 1).astype(f)
    ropet = np.ascontiguousarray(rope.reshape(64, 128, 2, 2, 16).transpose(1, 0, 2, 3, 4))
    return dict(masks=masks, blk1=blk1, maskw=maskw, ropet=ropet, ident=np.eye(128, dtype=f))
def prep_a(inp, l, x, xc, K):
    f = np.float32
    w_in = inp["w_in"][l]; cwl = inp["dn_conv_w"][l]
    ada_w = np.ascontiguousarray(inp["ada_w"][l][:, 0:2048]); ada_b = inp["ada_b"][l][0:2048]
    base = dict(adaw=ada_w, adabT=np.ascontiguousarray(ada_b.reshape(16, 128).T), g1T=np.ascontiguousarray(inp["norm1_g"][l].reshape(8, 128).T), ident=K["ident"])
    dn, sw = [], []
    for c in range(8):
        b, j = c // 4, c % 4
        xall = np.ascontiguousarray(np.concatenate([xc[b], x[b]], 0))
        cv = np.stack([inp["c"][b], inp["c_ctx"]], -1)
        m = dict(base); m.update(xall=xall, cvT=np.ascontiguousarray(cv.reshape(8, 128, 2).transpose(1, 0, 2)))
        hd = [2 * j, 2 * j + 1]
        wa = np.concatenate([w_in[:, s * 512 + 128 * j: s * 512 + 128 * j + 128] for s in range(3)], 1)
        wz = np.stack([np.concatenate([w_in[:, 1536 + h * 64:1536 + (h + 1) * 64], w_in[:, [2048 + h, 2048 + 8 + h, 2064 + h, 2064 + 8 + h]]], 1) for h in hd], 1)
        cw = np.stack([cwl[:, s * 512 + 128 * j: s * 512 + 128 * j + 128].T for s in range(3)], 1)
        hp = np.repeat(np.array(hd), 64)
        nega = -np.exp(inp["dn_a_log"][l][:, hp]).T; dtb = inp["dn_dt_bias"][l][:, hp].T
        md = dict(m); md.update(wa=np.ascontiguousarray(wa), wz=np.ascontiguousarray(wz), cw=np.ascontiguousarray(cw), nega=np.ascontiguousarray(nega.astype(f)),
                                dtb=np.ascontiguousarray(dtb), gdn=np.ascontiguousarray(np.broadcast_to(inp["dn_out_g"][l], (128, 64))), blk1=K["blk1"], masks=K["masks"])
        dn.append(md)
        kv = j // 2
        ws = np.concatenate([w_in[:, 2080 + hd[0] * 64:2080 + hd[0] * 64 + 128], w_in[:, 2592 + kv * 64:2592 + (kv + 1) * 64], w_in[:, 2720 + kv * 64:2720 + (kv + 1) * 64]], 1)
        gqk = np.broadcast_to(np.stack([inp["q_norm_g"][l], inp["q_norm_g"][l], inp["k_norm_g"][l]], 0), (128, 3, 64))
        ms = dict(m); ms.update(ws=np.ascontiguousarray(ws), gqk=np.ascontiguousarray(gqk), ropet=K["ropet"],
                                sinkb=np.ascontiguousarray(np.broadcast_to(inp["sinks"][l][hd], (128, 2))), maskw=K["maskw"])
        sw.append(ms)
    return dn, sw
def gather_a(res_dn, res_sw):
    om_x = np.zeros((2, 8192, 1024), np.float32); om_c = np.zeros((2, 256, 1024), np.float32)
    for c in range(8):
        b, j = c // 4, c % 4
        od = res_dn[c]["o_dn"]; os_ = res_sw[c]["o_sw"]
        om_c[b, :, 128 * j:128 * j + 128] = od[:256]; om_x[b, :, 128 * j:128 * j + 128] = od[256:]
        om_c[b, :, 512 + 128 * j:512 + 128 * j + 128] = os_[:256]; om_x[b, :, 512 + 128 * j:512 + 128 * j + 128] = os_[256:]
    return om_x, om_c


def prep_b(inp, l, x, xc, om_x, om_c, last):
    f = np.float32
    ada_w = np.ascontiguousarray(inp["ada_w"][l][:, 2048:6144]); ada_b = inp["ada_b"][l][2048:6144]
    common = dict(
        adaw=ada_w, adab=np.ascontiguousarray(ada_b[None, :]),
        adabT=np.ascontiguousarray(ada_b[1024:3072].reshape(16, 128).T),
        g2T=np.ascontiguousarray(inp["norm2_g"][l].reshape(8, 128).T),
        wout=inp["w_out"][l], rw=inp["router_w"][l], rb=np.ascontiguousarray(inp["router_b"][l][None, :]),
        wgu=inp["w_gate_up"][l], bguT=np.ascontiguousarray(inp["b_gate_up"][l].reshape(32, 16, 128).transpose(0, 2, 1)),
        wd=inp["w_down"][l], bd=inp["b_down"][l], ident=np.eye(128, dtype=f))
    maps = []
    for c in range(8):
        b, q = c // 4, c % 4
        rows = [x[b, q * 2048:(q + 1) * 2048]]; oms = [om_x[b, q * 2048:(q + 1) * 2048]]
        if not last:
            rows.append(xc[b, q * 64:(q + 1) * 64]); oms.append(om_c[b, q * 64:(q + 1) * 64])
        xs = np.ascontiguousarray(np.concatenate(rows, 0)); om = np.concatenate(oms, 0)
        cv = np.stack([inp["c"][b], inp["c_ctx"]], -1)
        m = dict(common)
        m.update(xs=xs, omT=np.ascontiguousarray(om.T), cvT=np.ascontiguousarray(cv.reshape(8, 128, 2).transpose(1, 0, 2)))
        maps.append(m)
    return maps
def gather_b(results, last):
    x = np.zeros((2, 8192, 1024), np.float32); xc = np.zeros((2, 256, 1024), np.float32)
    for c in range(8):
        b, q = c // 4, c % 4
        xo = results[c]["xo"]
        x[b, q * 2048:(q + 1) * 2048] = xo[:2048]
        if not last:
            xc[b, q * 64:(q + 1) * 64] = xo[2048:]
    return x, xc


def prep_fused(inp, nex=32):
    f = np.float32
    K = consts()
    x = np.ascontiguousarray(inp["x"], dtype=f); xc = np.ascontiguousarray(inp["ctx"], dtype=f)
    A = [prep_a(inp, l, x, xc, K) for l in range(2)]
    zx = np.zeros((2, 8192, 1024), f); zc = np.zeros((2, 256, 1024), f)
    B = [prep_b(inp, l, zx, zc, zx, zc, False) for l in range(2)]
    wgu = [np.ascontiguousarray(inp["w_gate_up"][l][:nex], dtype=f) for l in range(2)]; wd = [np.ascontiguousarray(inp["w_down"][l][:nex], dtype=f) for l in range(2)]
    maps = []
    for c in range(8):
        b, q = c // 4, c % 4
        m = dict(xall=A[0][0][c]["xall"], cvT=A[0][0][c]["cvT"], ident=K["ident"], blk1=K["blk1"], masks=K["masks"], maskw=K["maskw"], ropet=K["ropet"])
        m["xs0"] = np.ascontiguousarray(np.concatenate([x[b, q * 2048:(q + 1) * 2048], xc[b, q * 64:(q + 1) * 64]], 0))
        seli = np.zeros((128, 4, 128), f); seli[:, q, :] = np.eye(128, dtype=f); m["seli"] = seli
        for l in range(2):
            dn, sw = A[l][0][c], A[l][1][c]; bb = B[l][c]
            for nm, src, key in [("adaw1", dn, "adaw"), ("adab1T", dn, "adabT"), ("g1T", dn, "g1T"), ("wa", dn, "wa"), ("wz", dn, "wz"), ("cw", dn, "cw"),
                                 ("nega", dn, "nega"), ("dtb", dn, "dtb"), ("gdn", dn, "gdn"), ("ws", sw, "ws"), ("gqk", sw, "gqk"), ("sinkb", sw, "sinkb"),
                                 ("adaw2", bb, "adaw"), ("adab2", bb, "adab"), ("adab2T", bb, "adabT"), ("g2T", bb, "g2T"), ("wout", bb, "wout"),
                                 ("rw", bb, "rw"), ("rb", bb, "rb"), ("bguT", bb, "bguT"), ("bd", bb, "bd")]:
                m["%s_%d" % (nm, l)] = np.ascontiguousarray(src[key], dtype=f)
        for l in range(2):
            m["wgu_%d" % l] = wgu[l]; m["wd_%d" % l] = wd[l]
        maps.append(m)
    return maps
def gather_fused(results):
    x = np.zeros((2, 8192, 1024), np.float32)
    for c in range(8):
        b, q = c // 4, c % 4
        x[b, q * 2048:(q + 1) * 2048] = results[c]["xo"]
    return x


_NC = None


def kernel(**inputs):
    global _NC
    inp = {k: np.asarray(v) for k, v in inputs.items()}
    if _NC is None:
        _NC = build_fused(32)
    maps = prep_fused(inp, 32)
    res = run_bass_kernel_spmd(_NC, maps, core_ids=list(range(8)))
    return gather_fused(res.results).astype(np.float32)
```

```python
import os
import numpy as np
from contextlib import ExitStack
import concourse.bass as bass
import concourse.mybir as mybir
from concourse.bass_utils import run_bass_kernel_spmd

F32 = mybir.dt.float32
BF16 = mybir.dt.bfloat16
AF = mybir.ActivationFunctionType
ALU = mybir.AluOpType
AX = mybir.AxisListType

SAME_ENGINE_SYNC = True
NRING = 8
EPOCH = 30000


class Res:
    __slots__ = ("name", "w", "r", "x")

    def __init__(self, name="", x=False):
        self.name = name
        self.w = None
        self.r = {}
        self.x = x


class Prog:
    def __init__(self, nc, stack):
        self.nc = nc
        self.e = {"pe": nc.tensor, "act": nc.scalar, "dve": nc.vector, "pool": nc.gpsimd, "sp": nc.sync}
        self.ops = {k: [] for k in self.e}
        self.cnt = {k: 0 for k in self.e}
        self.sem = {k: [stack.enter_context(nc.semaphore("s_" + k + "0"))] for k in self.e}
        self.ring = {q: [stack.enter_context(nc.semaphore("d_%s_%d" % (q, i))) for i in range(NRING)]
                     for q in ("sp", "act", "pool")}
        self.dcnt = {q: 0 for q in self.ring}
        self.waited = {k: {} for k in self.e}
        self.stack = stack
        self.n_wait = 0

    def sb(self, name, shape, dt=F32, stack=None):
        self.n_alloc = getattr(self, "n_alloc", 0) + 1
        return (stack or self.stack).enter_context(self.nc.sbuf_tensor("%s_%d" % (name, self.n_alloc), list(shape), dt))

    def fence(self):
        for E in self.e:
            for F in self.e:
                if F != E and self.cnt[F] > 0:
                    self._wait(E, ("c", F, self.cnt[F]))
            for q in self.ring:
                n = self.dcnt[q]
                for k in range(max(0, n - NRING), n):
                    self._wait(E, ("d", q, k))

    def ps(self, name, shape, dt=F32):
        return self.stack.enter_context(self.nc.psum_tensor(name, list(shape), dt))

    def _semval(self, ev):
        if ev[0] == "c":
            ep, v = divmod(ev[2] - 1, EPOCH)
            return ("c", ev[1]), self.sem[ev[1]][ep], (ep, v + 1)
        if ev[0] == "x":
            return ("x", ev[1]), self.ccsems[ev[1]], (0, 1)
        _, q, n = ev
        slot = n % NRING
        return ("d", q, slot), self.ring[q][slot], (0, 16 * (n // NRING + 1))

    def _wait(self, eng, ev):
        if ev[0] == "c" and ev[1] == eng:
            if eng == "pe" or not SAME_ENGINE_SYNC:
                return
        key, sem, val = self._semval(ev)
        if self.waited[eng].get(key, (0, 0)) >= val:
            return
        self.waited[eng][key] = val
        self.n_wait += 1
        self.ops[eng].append(lambda h, sem=sem, val=val[1]: h.wait_ge(sem, val))

    def _deps(self, eng, reads, writes):
        deps = []
        for r in reads:
            if r.w is not None:
                deps.append(r.w)
            if r.x:
                for k, ev in r.r.items():
                    if not (k[0] == "c" and k[1] == eng):
                        deps.append(ev)
        for w in writes:
            if w.w is not None:
                deps.append(w.w)
            deps.extend(w.r.values())
        for ev in deps:
            self._wait(eng, ev)

    def _record(self, ev, reads, writes):
        if ev[0] == "c":
            key = ("c", ev[1])
        elif ev[0] == "x":
            key = ("x", ev[1])
        else:
            key = ("d", ev[1], ev[2] % NRING)
        for r in reads:
            r.r[key] = ev
        for w in writes:
            w.w = ev
            w.r = {}

    def op(self, eng, fn, reads=(), writes=()):
        self._deps(eng, reads, writes)
        self.cnt[eng] += 1
        ev = ("c", eng, self.cnt[eng])
        ep = (self.cnt[eng] - 1) // EPOCH
        if ep >= len(self.sem[eng]):
            self.sem[eng].append(self.stack.enter_context(self.nc.semaphore("s_%s%d" % (eng, ep))))
        sem = self.sem[eng][ep]
        self.ops[eng].append(lambda h, fn=fn, sem=sem: fn(h).then_inc(sem, 1))
        self._record(ev, reads, writes)

    def dma(self, q, out, in_, reads=(), writes=(), **kw):
        self._deps(q, reads, writes)
        n = self.dcnt[q]
        self.dcnt[q] += 1
        if n >= NRING:
            self._wait(q, ("d", q, n - NRING))
        sem = self.ring[q][n % NRING]
        self.ops[q].append(lambda h, out=out, in_=in_, sem=sem, kw=kw: h.dma_start(out=out, in_=in_, **kw).then_inc(sem, 16))
        ev = ("d", q, n)
        self._record(ev, reads, writes)
        return ev

    def coll(self, kind, op, groups, in_ap, out_ap, reads=(), writes=()):
        q = "pool"
        self._deps(q, reads, writes)
        if not hasattr(self, "ccsems"):
            self.ccsems = []
        sem = self.stack.enter_context(self.nc.semaphore("cc%d" % len(self.ccsems)))
        self.ccsems.append(sem)
        self.ops[q].append(lambda h, sem=sem: h.collective_compute(kind, op, replica_groups=groups, ins=[in_ap.opt()], outs=[out_ap.opt()]).then_inc(sem))
        ev = ("x", len(self.ccsems) - 1, 0)
        self._record(ev, reads, writes)
        return ev

    def finish(self, final_res):
        for r in final_res:
            if r.w is not None:
                self._wait("sp", r.w)
        for q in self.ring:
            n = self.dcnt[q]
            for k in range(max(0, n - NRING), n):
                self._wait("sp", ("d", q, k))
        with self.nc.Block() as block:
            @block.tensor
            def _(h):
                for f in self.ops["pe"]:
                    f(h)

            @block.scalar
            def _(h):
                for f in self.ops["act"]:
                    f(h)

            @block.vector
            def _(h):
                for f in self.ops["dve"]:
                    f(h)

            @block.gpsimd
            def _(h):
                for f in self.ops["pool"]:
                    f(h)

            @block.sync
            def _(h):
                for f in self.ops["sp"]:
                    f(h)

    def mm(self, out, lhsT, rhs, start=True, stop=True, reads=(), writes=(), **kw):
        self.op("pe", lambda h: h.matmul(out, lhsT, rhs, start=start, stop=stop, **kw), reads, writes)

    def act(self, out, in_, func, reads=(), writes=(), **kw):
        self.op("act", lambda h: h.activation(out=out, in_=in_, func=func, **kw), reads, writes)


D = 1024
NB = 66
NCH = 132


def emit_globals(P, IN):
    G = {}
    ID = P.sb("ID", [128, 128]); rID = Res(); P.dma("sp", ID[:], IN["ident"], writes=[rID])
    ONES = P.sb("ONES", [128, 128]); rONES = Res()
    P.op("dve", lambda h: h.memset(ONES[:], 1.0), writes=[rONES])
    ONESB = P.sb("ONESB", [1, 128], BF16); rONESB = Res()
    P.op("dve", lambda h: h.memset(ONESB[:], 1.0), writes=[rONESB])
    PS = [P.ps("PS%d" % i, [128, 512]) for i in range(8)]; rPS = [Res("ps%d" % i, x=True) for i in range(8)]
    G.update(ID=ID, rID=rID, ONES=ONES, rONES=rONES, ONESB=ONESB, rONESB=rONESB, PS=PS, rPS=rPS)
    return G


def emit_common(P, G, IN, l, stk):
    cvT = IN["cvT"]; adaw = IN["adaw1", l]; adabT = IN["adab1T", l]; g1T = IN["g1T", l]
    C = dict(G)
    PS, rPS = G["PS"], G["rPS"]
    MODF = P.sb("MODF", [128, 16, 2], stack=stk); rMODF = Res()
    SCALE1 = P.sb("SCALE1", [128, 8, 2], stack=stk); rSCALE1 = Res()
    G1 = P.sb("G1", [128, 8], stack=stk); rG1 = Res(); P.dma("sp", G1[:], g1T, writes=[rG1])
    ABT = P.sb("ABT", [128, 16], stack=stk); rABT = Res(); P.dma("sp", ABT[:], adabT, writes=[rABT])
    sub = ExitStack()
    CV = P.sb("CV", [128, 8, 2], stack=sub); rCV = Res(); P.dma("sp", CV[:], cvT, writes=[rCV])
    SCV = P.sb("SCV", [128, 8, 2], stack=sub); rSCV = Res()
    P.act(SCV[:], CV[:], AF.Silu, reads=[rCV], writes=[rSCV])
    AW = [P.sb("AW%d" % i, [128, 8, 512], stack=sub) for i in range(2)]; rAW = [Res(), Res()]
    for blk in range(4):
        aw, raw = AW[blk % 2], rAW[blk % 2]
        P.dma("sp", aw[:], adaw[:, blk * 512:(blk + 1) * 512].rearrange("(k p) f -> p k f", p=128), writes=[raw])
        ps, rps = PS[blk % 2], rPS[blk % 2]
        for fcl in range(4):
            for k in range(8):
                P.mm(ps[:, fcl * 2:fcl * 2 + 2], aw[:, k, fcl * 128:(fcl + 1) * 128], SCV[:, k, :],
                     start=(k == 0), stop=(k == 7), reads=[rSCV, raw], writes=[rps])
        for fcl in range(4):
            fcg = blk * 4 + fcl
            P.act(MODF[:, fcg, :], ps[:, fcl * 2:fcl * 2 + 2], AF.Identity, reads=[rps, rABT], writes=[rMODF],
                  bias=ABT[:, fcg:fcg + 1], scale=1.0)
    P.op("dve", lambda h: h.tensor_scalar(out=SCALE1[:], in0=MODF[:, 8:16, :], scalar1=1.0, scalar2=None, op0=ALU.add),
         reads=[rMODF], writes=[rSCALE1])
    P.op("dve", lambda h: h.tensor_tensor(out=SCALE1[:], in0=SCALE1[:], in1=G1[:].unsqueeze(2).to_broadcast([128, 8, 2]), op=ALU.mult),
         reads=[rSCALE1, rG1], writes=[rSCALE1])
    P.fence()
    sub.close()
    C.update(MODF=MODF, rMODF=rMODF, SCALE1=SCALE1, rSCALE1=rSCALE1)
    return C


def emit_frontend(P, C, xsrc, rsrc, consume, blocks, stk, tbanks=(0, 1)):
    PS, rPS = C["PS"], C["rPS"]
    XT = [P.sb("XT%d" % i, [128, D], stack=stk) for i in range(2)]; rXT = [Res(), Res()]
    XN = P.sb("XN", [128, D], stack=stk); rXN = Res()
    HX = [P.sb("HX%d" % i, [128, 8, 128], BF16, stack=stk) for i in range(2)]; rHX = [Res(), Res()]
    SM = P.sb("FSM", [128, 8], stack=stk); rSM = Res()
    for idx, n in enumerate(blocks):
        j = 1 if n < 2 else 0
        xt, rxt = XT[idx % 2], rXT[idx % 2]
        hx, rhx = HX[idx % 2], rHX[idx % 2]
        for (p0, np_, src) in xsrc(n):
            P.dma("sp", xt[p0:p0 + np_, :], src, reads=rsrc, writes=[rxt])
        P.op("dve", lambda h: h.memset(SM[:, 0:1], 0.0), writes=[rSM])
        P.act(XN[:], xt[:], AF.Square, reads=[rxt], writes=[rXN, rSM], accum_out=SM[:, 0:1])
        P.act(SM[:, 1:2], SM[:, 0:1], AF.Ln, reads=[rSM], writes=[rSM], bias=1e-6, scale=1.0 / D)
        P.act(SM[:, 2:3], SM[:, 1:2], AF.Exp, reads=[rSM], writes=[rSM], scale=-0.5)
        P.op("dve", lambda h, xt=xt: h.tensor_scalar(out=XN[:], in0=xt[:], scalar1=SM[:, 2:3], scalar2=None, op0=ALU.mult),
             reads=[rxt, rSM], writes=[rXN])
        for half in range(2):
            ps, rps = PS[tbanks[half]], rPS[tbanks[half]]
            for k in range(4 * half, 4 * half + 4):
                P.op("pe", lambda h, ps=ps, k=k: h.transpose(ps[:, (k % 4) * 128:(k % 4 + 1) * 128], XN[:, k * 128:(k + 1) * 128], C["ID"][:]),
                     reads=[rXN, C["rID"]], writes=[rps])
            for k in range(4 * half, 4 * half + 4):
                P.act(hx[:, k, :], ps[:, (k % 4) * 128:(k % 4 + 1) * 128], AF.Identity, reads=[rps, C["rSCALE1"], C["rMODF"]], writes=[rhx],
                      scale=C["SCALE1"][:, k, j:j + 1], bias=C["MODF"][:, k, j:j + 1])
        consume(n, hx, rhx)


def emit_swa(P, C, IN, l, last, xsrc, rsrc, o_sw, rOUT):
    ws = IN["ws", l]; gqk = IN["gqk", l]; ropet = IN["ropet"]; sinkb = IN["sinkb", l]; maskw = IN["maskw"]
    with ExitStack() as st0:
        _sb = P.sb
        P_sb = lambda name, shape, dt=F32: _sb("sw_" + name, shape, dt, stack=st0)
        PS, rPS, ID, rID = C["PS"], C["rPS"], C["ID"], C["rID"]
        WS = P_sb("WS", [128, 8, 256], BF16); rWS = Res()
        P.dma("pool", WS[:], ws.rearrange("(k p) f -> p k f", p=128), writes=[rWS])
        GQK = P_sb("GQK", [128, 3, 64]); rGQK = Res(); P.dma("sp", GQK[:], gqk, writes=[rGQK])
        ROPE = P_sb("ROPE", [128, 64, 2, 2, 16]); rROPE = Res(); P.dma("sp", ROPE[:], ropet, writes=[rROPE])
        SINK = P_sb("SINK", [128, 2]); rSINK = Res(); P.dma("sp", SINK[:], sinkb, writes=[rSINK])
        MASKW = P_sb("MASKW", [128, 384]); rMASKW = Res(); P.dma("sp", MASKW[:], maskw, writes=[rMASKW])
        SQT = P_sb("SQT", [128, NB * 128], BF16); rSQT = [Res() for _ in range(NB)]
        SKT = P_sb("SKT", [128, NB * 128], BF16); rSKT = [Res() for _ in range(NB)]
        SV = P_sb("SV", [128, NB, 64], BF16); rSV = [Res() for _ in range(NB)]
        QK = P_sb("QK", [128, 3, 64]); rQK = Res()
        QKR = P_sb("QKR", [128, 4, 64]); rQKR = Res()
        SQ = P_sb("SQ", [128, 3, 64]); rSQ = Res()
        T1 = P_sb("T1", [128, 3, 2, 16]); rT1 = Res()
        T2 = P_sb("T2", [128, 3, 2, 16]); rT2 = Res()
        SM = P_sb("SM", [128, 16]); rSM = Res()
        S = P_sb("S", [128, 640]); rS = Res()
        E = P_sb("E", [128, 640]); rE = Res()
        ET = P_sb("ET", [128, 5, 128], BF16); rET = Res()
        OSW = [P_sb("OSW%d" % i, [128, 128]) for i in range(2)]; rOSW = [Res(), Res()]

        HL = []
        for h in range(2):
            Ld = dict(S=P_sb("S%d" % h, [128, 640]), rS=Res(), E=P_sb("E%d" % h, [128, 640]), rE=Res(),
                      ET=P_sb("ET%d" % h, [128, 5, 128], BF16), rET=Res(), SM=P_sb("SMh%d" % h, [128, 8]), rSM=Res(),
                      b0=PS[4 + 2 * h], r0=rPS[4 + 2 * h], b1=PS[5 + 2 * h], r1=rPS[5 + 2 * h])
            HL.append(Ld)

        def att_gen(n, h, osw, rosw):
            Ld = HL[h]
            S_, rS_, E_, rE_, ET_, rET_, SM_, rSM_ = Ld["S"], Ld["rS"], Ld["E"], Ld["rE"], Ld["ET"], Ld["rET"], Ld["SM"], Ld["rSM"]
            b0, r0, b1, r1 = Ld["b0"], Ld["r0"], Ld["b1"], Ld["r1"]
            if n >= 2:
                lo, hi = max(2, n - 1), min(NB - 1, n + 1)
                nl = (hi - lo + 1) * 128
                m0 = (lo - (n - 1)) * 128
            else:
                lo, hi, nl, m0 = 0, -1, 0, 0
            ntot = nl + 256
            kblocks = list(range(lo, hi + 1)) + [0, 1]
            hs = slice(h * 64, (h + 1) * 64)
            if nl:
                P.mm(b0[:, 0:nl], SQT[hs, n * 128:(n + 1) * 128], SKT[hs, lo * 128:(hi + 1) * 128],
                     reads=[rSQT[n]] + [rSKT[b] for b in range(lo, hi + 1)], writes=[r0])
            P.mm(b1[:, 0:256], SQT[hs, n * 128:(n + 1) * 128], SKT[hs, 0:256], reads=[rSQT[n], rSKT[0], rSKT[1]], writes=[r1])
            yield
            if nl:
                P.op("dve", lambda hh: hh.scalar_tensor_tensor(out=S_[:, 0:nl], in0=b0[:, 0:nl], scalar=0.125,
                                                              in1=MASKW[:, m0:m0 + nl], op0=ALU.mult, op1=ALU.add),
                     reads=[r0, rMASKW], writes=[rS_])
            P.act(S_[:, nl:ntot], b1[:, 0:256], AF.Copy, reads=[r1], writes=[rS_], scale=0.125)
            yield
            P.op("dve", lambda hh: hh.reduce_max(out=SM_[:, 0:1], in_=S_[:, 0:ntot], axis=AX.X), reads=[rS_], writes=[rSM_])
            yield
            P.op("dve", lambda hh: hh.tensor_tensor(out=SM_[:, 1:2], in0=SM_[:, 0:1], in1=SINK[:, h:h + 1], op=ALU.max),
                 reads=[rSM_, rSINK], writes=[rSM_])
            yield
            P.op("dve", lambda hh: hh.tensor_scalar(out=SM_[:, 2:3], in0=SM_[:, 1:2], scalar1=-1.0, scalar2=None, op0=ALU.mult),
                 reads=[rSM_], writes=[rSM_])
            P.op("dve", lambda hh: hh.memset(SM_[:, 3:4], 0.0), writes=[rSM_])
            yield
            P.act(E_[:, 0:ntot], S_[:, 0:ntot], AF.Exp, reads=[rS_, rSM_], writes=[rE_, rSM_], bias=SM_[:, 2:3], scale=1.0, accum_out=SM_[:, 3:4])
            P.act(SM_[:, 4:5], SINK[:, h:h + 1], AF.Exp, reads=[rSINK, rSM_], writes=[rSM_], bias=SM_[:, 2:3], scale=1.0)
            yield
            P.op("dve", lambda hh: hh.tensor_tensor(out=SM_[:, 5:6], in0=SM_[:, 3:4], in1=SM_[:, 4:5], op=ALU.add), reads=[rSM_], writes=[rSM_])
            nk = ntot // 128
            n4 = min(nk, 4)
            for c in range(n4):
                P.op("pe", lambda hh, c=c: hh.transpose(b1[:, c * 128:(c + 1) * 128], E_[:, c * 128:(c + 1) * 128], ID[:]),
                     reads=[rE_, rID], writes=[r1])
            if nk > 4:
                P.op("pe", lambda hh: hh.transpose(b0[:, 384:512], E_[:, 512:640], ID[:]), reads=[rE_, rID], writes=[r0])
            yield
            P.op("dve", lambda hh: hh.reciprocal(out=SM_[:, 6:7], in_=SM_[:, 5:6]), reads=[rSM_], writes=[rSM_])
            P.act(ET_[:, 0:n4, :], b1[:, 0:n4 * 128], AF.Copy, reads=[r1], writes=[rET_])
            if nk > 4:
                P.act(ET_[:, 4, :], b0[:, 384:512], AF.Copy, reads=[r0], writes=[rET_])
            yield
            for c in range(nk):
                kb = kblocks[c]
                P.mm(b1[:, 0:64], ET_[:, c, :], SV[:, kb, :], start=(c == 0), stop=(c == nk - 1), reads=[rET_, rSV[kb]], writes=[r1])
            yield
            P.op("dve", lambda hh: hh.tensor_scalar(out=osw[:, h * 64:(h + 1) * 64], in0=b1[:, 0:64], scalar1=SM_[:, 6:7], scalar2=None, op0=ALU.mult),
                 reads=[r1, rSM_], writes=[rosw])
            yield

        BL = []
        for li in range(2):
            F = dict(XT=P_sb("XT%d" % li, [128, D]), rXT=Res(), XN=P_sb("XN%d" % li, [128, D]), rXN=Res(),
                     HX=P_sb("HX%d" % li, [128, 8, 128], BF16), rHX=Res(), SM=P_sb("BSM%d" % li, [128, 16]), rSM=Res(),
                     QK=P_sb("QK%d" % li, [128, 3, 64]), rQK=Res(), QKR=P_sb("QKR%d" % li, [128, 4, 64]), rQKR=Res(),
                     SQ=P_sb("SQ%d" % li, [128, 3, 64]), rSQ=Res(), T1=P_sb("T1%d" % li, [128, 3, 2, 16]), rT1=Res(),
                     T2=P_sb("T2%d" % li, [128, 3, 2, 16]), rT2=Res(),
                     bT=PS[2 * li], rT=rPS[2 * li], bA=PS[2 * li + 1], rA=rPS[2 * li + 1])
            BL.append(F)

        def blk_gen(n, F):
            j = 1 if n < 2 else 0
            xt, rxt, XN_, rXN_, hx, rhx, SMb, rSMb = F["XT"], F["rXT"], F["XN"], F["rXN"], F["HX"], F["rHX"], F["SM"], F["rSM"]
            QK_, rQK_, QKR_, rQKR_, SQ_, rSQ_, T1_, rT1_, T2_, rT2_ = F["QK"], F["rQK"], F["QKR"], F["rQKR"], F["SQ"], F["rSQ"], F["T1"], F["rT1"], F["T2"], F["rT2"]
            for (p0, np_, src) in xsrc(n):
                P.dma("sp", xt[p0:p0 + np_, :], src, reads=rsrc, writes=[rxt])
            P.op("dve", lambda h: h.memset(SMb[:, 0:1], 0.0), writes=[rSMb])
            yield
            P.act(XN_[:], xt[:], AF.Square, reads=[rxt], writes=[rXN_, rSMb], accum_out=SMb[:, 0:1])
            yield
            P.act(SMb[:, 1:2], SMb[:, 0:1], AF.Ln, reads=[rSMb], writes=[rSMb], bias=1e-6, scale=1.0 / D)
            yield
            P.act(SMb[:, 2:3], SMb[:, 1:2], AF.Exp, reads=[rSMb], writes=[rSMb], scale=-0.5)
            yield
            P.op("dve", lambda h: h.tensor_scalar(out=XN_[:], in0=xt[:], scalar1=SMb[:, 2:3], scalar2=None, op0=ALU.mult),
                 reads=[rxt, rSMb], writes=[rXN_])
            yield
            ps, rps = F["bT"], F["rT"]
            for half in range(2):
                for k in range(4 * half, 4 * half + 4):
                    P.op("pe", lambda h, k=k, ps=ps: h.transpose(ps[:, (k % 4) * 128:(k % 4 + 1) * 128], XN_[:, k * 128:(k + 1) * 128], ID[:]),
                         reads=[rXN_, rID], writes=[rps])
                yield
                for k in range(4 * half, 4 * half + 4):
                    P.act(hx[:, k, :], ps[:, (k % 4) * 128:(k % 4 + 1) * 128], AF.Identity, reads=[rps, C["rSCALE1"], C["rMODF"]], writes=[rhx],
                          scale=C["SCALE1"][:, k, j:j + 1], bias=C["MODF"][:, k, j:j + 1])
                yield
            pa, rpa = F["bA"], F["rA"]
            for k in range(8):
                P.mm(pa[:, 0:256], hx[:, k, :], WS[:, k, :], start=(k == 0), stop=(k == 7), reads=[rhx, rWS], writes=[rpa])
            yield
            P.act(QK_[:], pa[:, 0:192], AF.Copy, reads=[rpa], writes=[rQK_])
            P.act(SV[:, n, :], pa[:, 192:256], AF.Copy, reads=[rpa], writes=[rSV[n]])
            yield
            P.op("dve", lambda h: h.tensor_tensor(out=SQ_[:], in0=QK_[:], in1=QK_[:], op=ALU.mult), reads=[rQK_], writes=[rSQ_])
            yield
            P.op("dve", lambda h: h.reduce_sum(out=SMb[:, 8:11], in_=SQ_[:], axis=AX.X), reads=[rSQ_], writes=[rSMb])
            yield
            P.act(SMb[:, 11:14], SMb[:, 8:11], AF.Ln, reads=[rSMb], writes=[rSMb], bias=1e-6, scale=1.0 / 64)
            yield
            P.act(SMb[:, 8:11], SMb[:, 11:14], AF.Exp, reads=[rSMb], writes=[rSMb], scale=-0.5)
            yield
            P.op("dve", lambda h: h.tensor_tensor(out=QK_[:], in0=QK_[:], in1=SMb[:, 8:11].unsqueeze(2).to_broadcast([128, 3, 64]), op=ALU.mult),
                 reads=[rQK_, rSMb], writes=[rQK_])
            yield
            P.op("dve", lambda h: h.tensor_tensor(out=QK_[:], in0=QK_[:], in1=GQK[:], op=ALU.mult), reads=[rQK_, rGQK], writes=[rQK_])
            yield
            if n >= 2:
                bi = n - 2
                q5 = QK_[:].rearrange("p s (a t f) -> p s a t f", a=2, t=2)
                o5 = QKR_[:, 0:3, :].rearrange("p s (a t f) -> p s a t f", a=2, t=2)
                X1, X2 = q5[:, :, :, 0, :], q5[:, :, :, 1, :]
                Cc = ROPE[:, bi, 0, :, :].unsqueeze(1).to_broadcast([128, 3, 2, 16])
                Sn = ROPE[:, bi, 1, :, :].unsqueeze(1).to_broadcast([128, 3, 2, 16])
                tt = lambda out, a, b, op, reads, writes: P.op("dve", lambda h: h.tensor_tensor(out=out, in0=a, in1=b, op=op), reads=reads, writes=writes)
                tt(T1_[:], X1, Cc, ALU.mult, [rQK_, rROPE], [rT1_])
                tt(T2_[:], X2, Sn, ALU.mult, [rQK_, rROPE], [rT2_])
                yield
                tt(o5[:, :, :, 0, :], T1_[:], T2_[:], ALU.subtract, [rT1_, rT2_], [rQKR_])
                yield
                tt(T1_[:], X2, Cc, ALU.mult, [rQK_, rROPE], [rT1_])
                tt(T2_[:], X1, Sn, ALU.mult, [rQK_, rROPE], [rT2_])
                yield
                tt(o5[:, :, :, 1, :], T1_[:], T2_[:], ALU.add, [rT1_, rT2_], [rQKR_])
                yield
            else:
                P.op("dve", lambda h: h.tensor_copy(out=QKR_[:, 0:3, :], in_=QK_[:]), reads=[rQK_], writes=[rQKR_])
                yield
            P.op("dve", lambda h: h.tensor_copy(out=QKR_[:, 3, :], in_=QKR_[:, 2, :]), reads=[rQKR_], writes=[rQKR_])
            yield
            P.op("pe", lambda h: h.transpose(pa[:, 256:384], QKR_[:, 0:2, :].rearrange("p a f -> p (a f)"), ID[:]), reads=[rQKR_, rID], writes=[rpa])
            P.op("pe", lambda h: h.transpose(pa[:, 384:512], QKR_[:, 2:4, :].rearrange("p a f -> p (a f)"), ID[:]), reads=[rQKR_, rID], writes=[rpa])
            yield
            P.act(SQT[:, n * 128:(n + 1) * 128], pa[:, 256:384], AF.Copy, reads=[rpa], writes=[rSQT[n]])
            P.act(SKT[:, n * 128:(n + 1) * 128], pa[:, 384:512], AF.Copy, reads=[rpa], writes=[rSKT[n]])
            yield

        qblocks = ([] if last else [0, 1]) + list(range(2, NB))
        done_blk = set()
        blk_active = {}
        blk_lane = {}
        free_bl = [0, 1]
        att_active = None
        nxt = 0
        qi = 0
        while nxt < NB or blk_active or att_active is not None or qi < len(qblocks):
            while free_bl and nxt < NB:
                li_ = free_bl.pop(0)
                blk_lane[nxt] = li_
                blk_active[nxt] = blk_gen(nxt, BL[li_]); nxt += 1
            if att_active is None and qi < len(qblocks):
                m = qblocks[qi]
                need = [b for b in (0, 1, m - 1, m, m + 1) if 0 <= b < NB and (b < 2 or b >= 2)]
                if m < 2:
                    need = [0, 1]
                if all(b in done_blk for b in need):
                    osw, rosw = OSW[m % 2], rOSW[m % 2]
                    att_active = (m, [att_gen(m, 0, osw, rosw), att_gen(m, 1, osw, rosw)], [True, True])
                    qi += 1
            progressed = False
            for nb_ in list(blk_active):
                try:
                    next(blk_active[nb_]); progressed = True
                except StopIteration:
                    del blk_active[nb_]; done_blk.add(nb_); progressed = True
                    free_bl.append(blk_lane.pop(nb_))
            if att_active is not None:
                m, gs, alive = att_active
                for i in range(2):
                    if alive[i]:
                        try:
                            next(gs[i]); progressed = True
                        except StopIteration:
                            alive[i] = False; progressed = True
                if not any(alive):
                    P.dma("sp", o_sw[m * 128:(m + 1) * 128, :], OSW[m % 2][:], reads=[rOSW[m % 2]], writes=[rOUT[m]])
                    att_active = None
            assert progressed or att_active is None
        P.fence()


def emit_dn(P, C, IN, l, last, xsrc, rsrc, o_dn, rOUT):
    wa = IN["wa", l]; wz = IN["wz", l]; cw = IN["cw", l]; nega = IN["nega", l]; dtb = IN["dtb", l]; gdn = IN["gdn", l]
    blk1 = IN["blk1"]; masks = IN["masks"]
    NS = NCH + 1
    with ExitStack() as st0:
        _sb = P.sb
        P_sb = lambda name, shape, dt=F32: _sb("dn_" + name, shape, dt, stack=st0)
        PS, rPS, ID, rID, ONES, rONES = C["PS"], C["rPS"], C["ID"], C["rID"], C["ONES"], C["rONES"]
        WA = P_sb("WA", [128, 8, 384], BF16); rWA = Res(); P.dma("pool", WA[:], wa.rearrange("(k p) f -> p k f", p=128), writes=[rWA])
        WZ = P_sb("WZ", [128, 8, 2, 68], BF16); rWZ = Res(); P.dma("pool", WZ[:], wz.rearrange("(k p) h f -> p k h f", p=128), writes=[rWZ])
        CW = P_sb("CW", [128, 3, 5]); rCW = Res(); P.dma("sp", CW[:], cw, writes=[rCW])
        NEGA = P_sb("NEGA", [128, 2]); rNEGA = Res(); P.dma("sp", NEGA[:], nega, writes=[rNEGA])
        DTB = P_sb("DTB", [128, 2]); rDTB = Res(); P.dma("sp", DTB[:], dtb, writes=[rDTB])
        GDN = P_sb("GDN", [128, 64]); rGDN = Res(); P.dma("sp", GDN[:], gdn, writes=[rGDN])
        BLK = P_sb("BLK", [128, 128]); rBLK = Res(); P.dma("sp", BLK[:], blk1, writes=[rBLK])
        MSK = P_sb("MSK", [128, 6, 2, 64]); rMSK = Res(); P.dma("sp", MSK[:], masks, writes=[rMSK])
        TRI, NEGMT, NEGM, NSTT, NST, ID2 = [MSK[:, i, :, :] for i in range(6)]
        ONES3 = P_sb("ONES3", [128, 2, 64]); rONES3 = Res()
        P.op("dve", lambda h: h.memset(ONES3[:], 1.0), writes=[rONES3])
        QT = P_sb("QT", [128, NCH * 64], BF16); KT = P_sb("KT", [128, NCH * 64], BF16)
        KTM = P_sb("KTM", [128, NCH, 64], BF16); VTM = P_sb("VTM", [128, NCH, 64], BF16)
        SZ = P_sb("SZ", [128, NCH, 64], BF16)
        Gs = P_sb("Gs", [128, NS, 2]); Bs = P_sb("Bs", [128, NS, 2])
        O = P_sb("O", [128, NCH, 64])
        rCH = [Res() for _ in range(NCH)]
        rO = [Res() for _ in range(NCH)]
        P.op("dve", lambda h: h.memset(O[:], 0.0), writes=rO)
        NLF = int(os.environ.get("DNLANES", "4"))
        NCB = NLF + 2
        fst = ExitStack()
        P_sbf = lambda name, shape, dt=F32: _sb("dnf_" + name, shape, dt, stack=fst)
        CBL = [P_sbf("CBL%d" % i, [128, 3, 132]) for i in range(NCB)]; rCBL = [Res() for _ in range(NCB)]
        for i in range(NCB):
            P.op("dve", lambda h, i=i: h.memset(CBL[i][:], 0.0), writes=[rCBL[i]])

        def step_of(c, d):
            if d == 0:
                return c
            return 4 - c if c < 4 else 136 - c

        FL = []
        for li in range(NLF):
            F = dict(XT=P_sbf("XT%d" % li, [128, D]), rXT=Res(), XN=P_sbf("XN%d" % li, [128, D]), rXN=Res(),
                     HX=P_sbf("HX%d" % li, [128, 8, 128], BF16), rHX=Res(), SM=P_sbf("FSM%d" % li, [128, 8]), rSM=Res(),
                     CVb=P_sbf("CVb%d" % li, [128, 3, 128]), rCVb=Res(), SQ2=P_sbf("SQ2%d" % li, [128, 2, 128]), rSQ2=Res(),
                     RS2=P_sbf("RS2%d" % li, [128, 2, 128]), rRS2=Res(), KN=P_sbf("KN%d" % li, [128, 128]), rKN=Res(),
                     GT_=P_sbf("GT_%d" % li, [128, 2, 2, 8]), rGT_=Res(), EXb=P_sbf("EXb%d" % li, [128, 3, 128]), rEXb=Res(),
                     EZ=P_sbf("EZ%d" % li, [128, 2, 64]), rEZ=Res(),
                     bT=PS[2 * li], rT=rPS[2 * li], bA=PS[2 * li + 1], rA=rPS[2 * li + 1], bB=PS[2 * li + 1], rB=rPS[2 * li + 1],
                     bZ=PS[2 * li], rZ=rPS[2 * li])
            FL.append(F)

        def conv_gen(m, F):
            CVb, rCVb, SQ2, rSQ2, RS2, rRS2, KN, rKN = F["CVb"], F["rCVb"], F["SQ2"], F["rSQ2"], F["RS2"], F["rRS2"], F["KN"], F["rKN"]
            CB, rCB = CBL[m % NCB], rCBL[m % NCB]
            rcv = [Res(), Res(), Res()]
            for s_ in range(3):
                P.op("dve", lambda h, s_=s_: h.tensor_scalar(out=CVb[:, s_, :], in0=CB[:, s_, 0:128], scalar1=CW[:, s_, 0:1], scalar2=None, op0=ALU.mult),
                     reads=[rCB, rCW], writes=[rcv[s_], rCVb])
            yield
            for tap in range(1, 5):
                for s_ in range(3):
                    P.op("dve", lambda h, s_=s_, tap=tap: h.scalar_tensor_tensor(out=CVb[:, s_, :], in0=CB[:, s_, tap:tap + 128], scalar=CW[:, s_, tap:tap + 1],
                                                                              in1=CVb[:, s_, :], op0=ALU.mult, op1=ALU.add),
                         reads=[rCB, rCW, rcv[s_]], writes=[rcv[s_]] + ([rCVb] if tap == 4 else []))
                yield
            EXb, rEXb = F["EXb"], F["rEXb"]
            P.act(EXb[:], CVb[:], AF.Exp, reads=[rCVb], writes=[rEXb], scale=-1.0)
            yield
            P.op("dve", lambda h: h.tensor_scalar(out=EXb[:], in0=EXb[:], scalar1=1.0, scalar2=None, op0=ALU.add), reads=[rEXb], writes=[rEXb])
            yield
            P.op("dve", lambda h: h.reciprocal(out=EXb[:], in_=EXb[:]), reads=[rEXb], writes=[rEXb])
            yield
            P.op("dve", lambda h: h.tensor_tensor(out=CVb[:], in0=CVb[:], in1=EXb[:], op=ALU.mult), reads=[rCVb, rEXb], writes=[rCVb])
            yield
            P.op("dve", lambda h: h.tensor_tensor(out=SQ2[:], in0=CVb[:, 0:2, :], in1=CVb[:, 0:2, :], op=ALU.mult), reads=[rCVb], writes=[rSQ2])
            yield
            ps, rps = F["bB"], F["rB"]
            P.mm(ps[:, 0:256], BLK[:], SQ2[:].rearrange("p a t -> p (a t)"), reads=[rBLK, rSQ2], writes=[rps])
            yield
            P.act(RS2[:].rearrange("p a t -> p (a t)"), ps[:, 0:256], AF.Ln, reads=[rps], writes=[rRS2], bias=1e-6, scale=1.0)
            yield
            P.act(RS2[:], RS2[:], AF.Exp, reads=[rRS2], writes=[rRS2], scale=-0.5)
            yield
            rc = [rCH[2 * m], rCH[2 * m + 1]]
            P.op("dve", lambda h: h.scalar_tensor_tensor(out=QT[:, m * 128:(m + 1) * 128], in0=CVb[:, 0, :], scalar=0.125, in1=RS2[:, 0, :], op0=ALU.mult, op1=ALU.mult),
                 reads=[rCVb, rRS2], writes=rc)
            P.op("dve", lambda h: h.tensor_tensor(out=KN[:], in0=CVb[:, 1, :], in1=RS2[:, 1, :], op=ALU.mult), reads=[rCVb, rRS2], writes=[rKN])
            yield
            P.op("dve", lambda h: h.tensor_copy(out=KT[:, m * 128:(m + 1) * 128], in_=KN[:]), reads=[rKN], writes=rc)
            for which in range(2):
                for cc in range(2):
                    for hh in range(2):
                        hs = slice(hh * 64, (hh + 1) * 64)
                        srcap = KN[hs, cc * 64:(cc + 1) * 64] if which == 0 else CVb[hs, 2, cc * 64:(cc + 1) * 64]
                        P.op("pe", lambda h, hs=hs, cc=cc, which=which, srcap=srcap: h.matmul(ps[hs, 256 + which * 128 + cc * 64: 256 + which * 128 + (cc + 1) * 64], srcap, ID[hs, hs], start=True, stop=True),
                             reads=[rKN, rCVb, rID], writes=[rps])
            yield
            P.act(KTM[:, 2 * m:2 * m + 2, :], ps[:, 256:384].rearrange("p (c f) -> p c f", c=2), AF.Copy, reads=[rps], writes=rc)
            P.act(VTM[:, 2 * m:2 * m + 2, :], ps[:, 384:512].rearrange("p (c f) -> p c f", c=2), AF.Copy, reads=[rps], writes=rc)
            yield

        def blk_gen(n, F):
            j = 1 if n < 2 else 0
            xt, rxt, XN, rXN, hx, rhx, SM, rSM = F["XT"], F["rXT"], F["XN"], F["rXN"], F["HX"], F["rHX"], F["SM"], F["rSM"]
            for (p0, np_, src) in xsrc(n):
                P.dma("sp", xt[p0:p0 + np_, :], src, reads=rsrc, writes=[rxt])
            P.op("dve", lambda h: h.memset(SM[:, 0:1], 0.0), writes=[rSM])
            yield
            P.act(XN[:], xt[:], AF.Square, reads=[rxt], writes=[rXN, rSM], accum_out=SM[:, 0:1])
            yield
            P.act(SM[:, 1:2], SM[:, 0:1], AF.Ln, reads=[rSM], writes=[rSM], bias=1e-6, scale=1.0 / D)
            yield
            P.act(SM[:, 2:3], SM[:, 1:2], AF.Exp, reads=[rSM], writes=[rSM], scale=-0.5)
            yield
            P.op("dve", lambda h: h.tensor_scalar(out=XN[:], in0=xt[:], scalar1=SM[:, 2:3], scalar2=None, op0=ALU.mult),
                 reads=[rxt, rSM], writes=[rXN])
            yield
            ps, rps = F["bT"], F["rT"]
            for half in range(2):
                for k in range(4 * half, 4 * half + 4):
                    P.op("pe", lambda h, k=k, ps=ps: h.transpose(ps[:, (k % 4) * 128:(k % 4 + 1) * 128], XN[:, k * 128:(k + 1) * 128], ID[:]),
                         reads=[rXN, rID], writes=[rps])
                yield
                for k in range(4 * half, 4 * half + 4):
                    P.act(hx[:, k, :], ps[:, (k % 4) * 128:(k % 4 + 1) * 128], AF.Identity, reads=[rps, C["rSCALE1"], C["rMODF"]], writes=[rhx],
                          scale=C["SCALE1"][:, k, j:j + 1], bias=C["MODF"][:, k, j:j + 1])
                yield
            ps, rps = F["bA"], F["rA"]
            for s_ in range(3):
                for k in range(8):
                    P.mm(ps[:, s_ * 128:(s_ + 1) * 128], WA[:, k, s_ * 128:(s_ + 1) * 128], hx[:, k, :], start=(k == 0), stop=(k == 7), reads=[rWA, rhx], writes=[rps])
            yield
            CB, rCB = CBL[n % NCB], rCBL[n % NCB]
            first = n in (0, 2)
            lastb = n in (1, NB - 1)
            P.act(CB[:, :, 2:130], ps[:, 0:384].rearrange("p (s t) -> p s t", s=3), AF.Copy, reads=[rps], writes=[rCB])
            if first:
                P.op("dve", lambda h: h.memset(CB[:, :, 0:2], 0.0), writes=[rCB])
            else:
                Pv, rPv = CBL[(n - 1) % NCB], rCBL[(n - 1) % NCB]
                P.op("dve", lambda h: h.tensor_copy(out=CB[:, :, 0:2], in_=Pv[:, :, 128:130]), reads=[rPv], writes=[rCB])
                P.op("dve", lambda h: h.tensor_copy(out=Pv[:, :, 130:132], in_=CB[:, :, 2:4]), reads=[rCB], writes=[rPv])
            if lastb:
                P.op("dve", lambda h: h.memset(CB[:, :, 130:132], 0.0), writes=[rCB])
            yield
            ps, rps = F["bZ"], F["rZ"]
            for cc in range(2):
                for hh in range(2):
                    hs = slice(hh * 64, (hh + 1) * 64)
                    for k in range(8):
                        P.mm(ps[hs, cc * 68:(cc + 1) * 68], hx[:, k, cc * 64:(cc + 1) * 64], WZ[:, k, hh, :], start=(k == 0), stop=(k == 7),
                             reads=[rhx, rWZ], writes=[rps])
            yield
            rc = [rCH[2 * n], rCH[2 * n + 1]]
            pz = ps[:, 0:136].rearrange("p (c f) -> p c f", c=2)
            EZ, rEZ = F["EZ"], F["rEZ"]
            P.act(EZ[:], pz[:, :, 0:64], AF.Exp, reads=[rps], writes=[rEZ], scale=-1.0)
            P.op("dve", lambda h: h.tensor_scalar(out=EZ[:], in0=EZ[:], scalar1=1.0, scalar2=None, op0=ALU.add), reads=[rEZ], writes=[rEZ])
            yield
            P.op("dve", lambda h: h.reciprocal(out=EZ[:], in_=EZ[:]), reads=[rEZ], writes=[rEZ])
            yield
            P.op("dve", lambda h: h.tensor_tensor(out=SZ[:, 2 * n:2 * n + 2, :], in0=pz[:, :, 0:64], in1=EZ[:], op=ALU.mult), reads=[rps, rEZ], writes=rc)
            GT_, rGT_ = F["GT_"], F["rGT_"]
            xa = GT_[:, :, :, 0]; ab = GT_[:, :, :, 1]; ee = GT_[:, :, :, 2]; ll = GT_[:, :, :, 3]; rr = GT_[:, :, :, 4]; gg = GT_[:, :, :, 5]; bb = GT_[:, :, :, 6]
            P.op("dve", lambda h: h.tensor_tensor(out=xa, in0=pz[:, :, 64:66], in1=DTB[:].unsqueeze(1).to_broadcast([128, 2, 2]), op=ALU.add), reads=[rps, rDTB], writes=[rGT_])
            yield
            P.act(bb, pz[:, :, 66:68], AF.Exp, reads=[rps], writes=[rGT_], scale=-1.0)
            P.act(ab, xa, AF.Abs, reads=[rGT_], writes=[rGT_])
            yield
            P.op("dve", lambda h: h.tensor_scalar(out=bb, in0=bb, scalar1=1.0, scalar2=None, op0=ALU.add), reads=[rGT_], writes=[rGT_])
            P.op("dve", lambda h: h.reciprocal(out=bb, in_=bb), reads=[rGT_], writes=[rGT_])
            yield
            P.act(ee, ab, AF.Exp, reads=[rGT_], writes=[rGT_], scale=-1.0)
            yield
            P.act(ll, ee, AF.Ln, reads=[rGT_], writes=[rGT_], bias=1.0, scale=1.0)
            P.op("dve", lambda h: h.tensor_scalar(out=rr, in0=xa, scalar1=0.0, scalar2=None, op0=ALU.max), reads=[rGT_], writes=[rGT_])
            yield
            P.op("dve", lambda h: h.tensor_tensor(out=rr, in0=rr, in1=ll, op=ALU.add), reads=[rGT_], writes=[rGT_])
            yield
            P.op("dve", lambda h: h.tensor_tensor(out=gg, in0=rr, in1=NEGA[:].unsqueeze(1).to_broadcast([128, 2, 2]), op=ALU.mult), reads=[rGT_, rNEGA], writes=[rGT_])
            yield
            for cc in range(2):
                c = 2 * n + cc
                for d in range(2):
                    s2 = step_of(c, d)
                    P.op("dve", lambda h, cc=cc, d=d, s2=s2: h.tensor_copy(out=Gs[:, s2, d:d + 1], in_=GT_[:, cc, d, 5:6]), reads=[rGT_], writes=[rCH[c]])
                    P.op("dve", lambda h, cc=cc, d=d, s2=s2: h.tensor_copy(out=Bs[:, s2, d:d + 1], in_=GT_[:, cc, d, 6:7]), reads=[rGT_], writes=[rCH[c]])
                yield
            if not first:
                yield from conv_gen(n - 1, F)
            if lastb:
                yield from conv_gen(n, F)

        active = []
        free_lanes = list(range(NLF))
        nxt = 0
        while nxt < NB or active:
            while free_lanes and nxt < NB:
                li = free_lanes.pop(0)
                active.append((blk_gen(nxt, FL[li]), li)); nxt += 1
            for item in list(active):
                try:
                    next(item[0])
                except StopIteration:
                    active.remove(item)
                    free_lanes.append(item[1])

        P.fence()
        fst.close()
        Sst = [(P_sb("S0", [128, 2, 64]), Res()), (P_sb("S1", [128, 2, 64]), Res())]
        for (t, r) in Sst:
            P.op("dve", lambda h, t=t: h.memset(t[:], 0.0), writes=[r])
        HS = [slice(0, 64), slice(64, 128)]
        dve = lambda fn, reads, writes: P.op("dve", fn, reads, writes)

        def make_lane(li):
            L = {}
            for nm in ("GBC", "BBC", "EB", "DT1", "TA", "DECT", "DEC", "DECS", "DECTS", "VB", "KBE", "KD", "QGT", "AQM", "NWT", "VN", "SGL", "C0", "C1"):
                L[nm] = (P_sb("%s_%d" % (nm, li), [128, 2, 64]), Res())
            L["W0"] = (P_sb("W0_%d" % li, [128, 2, 128]), Res()); L["W1"] = (P_sb("W1_%d" % li, [128, 2, 128]), Res())
            L["SC"] = (P_sb("SC_%d" % li, [128, 8]), Res())
            a, b, c = PS[3 * li], PS[3 * li + 1], PS[3 * li + 2]
            v = lambda bank, lo, n: bank[:, lo:lo + 2 * n].rearrange("p (d f) -> p d f", d=2)
            ra, rb, rc = rPS[3 * li], rPS[3 * li + 1], rPS[3 * li + 2]
            L["ps1"] = (v(a, 0, 64), ra); L["ps4"] = (v(a, 128, 64), ra); L["ps2"] = (v(a, 256, 64), ra); L["ps3"] = (v(a, 384, 64), ra)
            L["psI"] = (v(b, 0, 128), rb); L["psC"] = (v(b, 256, 64), rb); L["pcol"] = (b[:, 384:386], rb)
            L["ps5"] = (v(c, 0, 64), rc); L["pswT"] = (v(c, 128, 64), rc); L["ps6"] = (v(c, 256, 64), rc); L["ps7"] = (v(c, 384, 64), rc)
            return L

        lanes = [make_lane(0), make_lane(1)]

        def step_gen(s, L):
            GBC, rGBC = L["GBC"]; BBC, rBBC = L["BBC"]; EB, rEB = L["EB"]; DT1, rDT1 = L["DT1"]; TA, rTA = L["TA"]
            DECT, rDECT = L["DECT"]; DEC, rDEC = L["DEC"]; DECS, rDECS = L["DECS"]; DECTS, rDECTS = L["DECTS"]
            VB, rVB = L["VB"]; KBE, rKBE = L["KBE"]; KD, rKD = L["KD"]; QGT, rQGT = L["QGT"]; AQM, rAQM = L["AQM"]
            NWT, rNWT = L["NWT"]; VN, rVN = L["VN"]; SGL, rSGL = L["SGL"]; SC, rSC = L["SC"]
            Cb = [L["C0"], L["C1"]]; Wb = [L["W0"], L["W1"]]
            ps1, r1 = L["ps1"]; ps4, r4 = L["ps4"]; ps2, r2 = L["ps2"]; ps3, r3 = L["ps3"]
            psI, rI = L["psI"]; psC, rC = L["psC"]; pcol, rcol = L["pcol"]
            ps5, r5 = L["ps5"]; pswT, rwT = L["pswT"]; ps6, r6 = L["ps6"]; ps7, r7 = L["ps7"]
            dirs = [d for d in range(2) if (d == 0 and s <= NCH - 1) or (d == 1 and s >= 1)]
            ch = {0: s, 1: (4 - s if s <= 4 else 136 - s)}
            d0, d1 = dirs[0], dirs[-1] + 1
            ds = slice(d0, d1)
            nd = d1 - d0
            rch = [rCH[ch[d]] for d in dirs]
            Scur, rScur = Sst[s % 2]; Snew, rSnew = Sst[(s + 1) % 2]
            tok = {d: slice(ch[d] * 64, ch[d] * 64 + 64) for d in dirs}
            dve(lambda h: h.tensor_tensor(out=GBC[:, ds, :], in0=ONES3[:, ds, :], in1=Gs[:, s, ds].unsqueeze(2).to_broadcast([128, nd, 64]), op=ALU.mult), [rONES3] + rch, [rGBC])
            dve(lambda h: h.tensor_tensor(out=BBC[:, ds, :], in0=ONES3[:, ds, :], in1=Bs[:, s, ds].unsqueeze(2).to_broadcast([128, nd, 64]), op=ALU.mult), [rONES3] + rch, [rBBC])
            yield
            for d in dirs:
                for hs in HS:
                    P.mm(ps1[hs, d, :], GBC[hs, d, :], TRI[hs, d, :], reads=[rGBC, rMSK], writes=[r1])
            for d in dirs:
                for hs in HS:
                    P.mm(pcol[hs, d:d + 1], TRI[hs, d, :], Gs[hs, s, d:d + 1], reads=[rMSK] + rch, writes=[rcol])
            for d in dirs:
                for hs in HS:
                    P.mm(ps4[hs, d, :], BBC[hs, d, :], ID2[hs, d, :], reads=[rBBC, rMSK], writes=[r4])
            for d in dirs:
                for hs in HS:
                    P.mm(ps2[hs, d, :], KT[hs, tok[d]], KT[hs, tok[d]], reads=rch, writes=[r2])
            for d in dirs:
                for hs in HS:
                    P.mm(ps3[hs, d, :], KT[hs, tok[d]], QT[hs, tok[d]], reads=rch, writes=[r3])
            yield
            dve(lambda h: h.tensor_copy(out=SC[:, 0:2][:, ds], in_=pcol[:, ds]), [rcol], [rSC])
            P.act(EB[:, ds, :], ps1[:, ds, :], AF.Exp, reads=[r1], writes=[rEB])
            yield
            P.act(SC[:, 2:4][:, ds], SC[:, 0:2][:, ds], AF.Exp, reads=[rSC], writes=[rSC])
            dve(lambda h: h.tensor_tensor(out=DT1[:, ds, :], in0=ps1[:, ds, :], in1=SC[:, 0:2][:, ds].unsqueeze(2).to_broadcast([128, nd, 64]), op=ALU.subtract), [r1, rSC], [rDT1])
            yield
            dve(lambda h: h.tensor_tensor(out=TA[:, ds, :], in0=DT1[:, ds, :], in1=NEGMT[:, ds, :], op=ALU.add), [rDT1, rMSK], [rTA])
            yield
            P.act(DECT[:, ds, :], TA[:, ds, :], AF.Exp, reads=[rTA], writes=[rDECT])
            yield
            dve(lambda h: h.scalar_tensor_tensor(out=TA[:, ds, :], in0=DT1[:, ds, :], scalar=-1.0, in1=NEGM[:, ds, :], op0=ALU.mult, op1=ALU.add), [rDT1, rMSK, rDECT], [rTA])
            yield
            P.act(DEC[:, ds, :], TA[:, ds, :], AF.Exp, reads=[rTA], writes=[rDEC])
            for d in dirs:
                lastc = 63 if d == 0 else 0
                dve(lambda h, d=d, lastc=lastc: h.tensor_tensor(out=SC[:, 4 + d:5 + d], in0=ps1[:, d, lastc:lastc + 1], in1=SC[:, d:d + 1], op=ALU.subtract), [r1, rSC], [rSC])
            yield
            P.act(SC[:, 4:6][:, ds], SC[:, 4:6][:, ds], AF.Exp, reads=[rSC], writes=[rSC])
            C0, rC0 = Cb[0]; W0, rW0 = Wb[0]
            dve(lambda h: h.tensor_tensor(out=DECS[:, ds, :], in0=DEC[:, ds, :], in1=NST[:, ds, :], op=ALU.mult), [rDEC, rMSK], [rDECS])
            yield
            for d in dirs:
                dve(lambda h, d=d: h.scalar_tensor_tensor(out=C0[:, d, :], in0=ps2[:, d, :], scalar=Bs[:, s, d:d + 1], in1=DECS[:, d, :], op0=ALU.mult, op1=ALU.mult),
                    [r2, rDECS] + rch, [rC0])
            yield
            dve(lambda h: h.tensor_tensor(out=DECTS[:, ds, :], in0=DECT[:, ds, :], in1=NSTT[:, ds, :], op=ALU.mult), [rDECT, rMSK], [rDECTS])
            yield
            dve(lambda h: h.tensor_tensor(out=DECTS[:, ds, :], in0=ps2[:, ds, :], in1=DECTS[:, ds, :], op=ALU.mult), [r2, rDECTS], [rDECTS])
            yield
            dve(lambda h: h.tensor_tensor(out=W0[:, ds, 0:64], in0=ps4[:, ds, :], in1=DECTS[:, ds, :], op=ALU.mult), [r4, rDECTS], [rW0])
            dve(lambda h: h.tensor_copy(out=W0[:, ds, 64:128], in_=ID2[:, ds, :]), [rMSK], [rW0])
            yield
            for m in range(6):
                (Wc, rWc), (Wn, rWn) = Wb[m % 2], Wb[(m + 1) % 2]
                (Cc, rCc), (Cn, rCn) = Cb[m % 2], Cb[(m + 1) % 2]
                for d in dirs:
                    for hs in HS:
                        P.mm(psI[hs, d, :], Cc[hs, d, :], Wc[hs, d, :], reads=[rCc, rWc], writes=[rI])
                if m < 5:
                    for d in dirs:
                        for hs in HS:
                            P.mm(psC[hs, d, :], Wc[hs, d, 0:64], Cc[hs, d, :], reads=[rCc, rWc], writes=[rC])
                yield
                dve(lambda h, Wn=Wn, Wc=Wc: h.tensor_tensor(out=Wn[:, ds, 64:128], in0=Wc[:, ds, 64:128], in1=psI[:, ds, 64:128], op=ALU.add), [rWc, rI], [rWn])
                if m < 5:
                    P.act(Wn[:, ds, 0:64], psI[:, ds, 0:64], AF.Copy, reads=[rI], writes=[rWn])
                    P.act(Cn[:, ds, :], psC[:, ds, :], AF.Copy, reads=[rC], writes=[rCn])
                yield
                if m == 2:
                    yield "HALF"
            TITt, rTIT = Wb[0]
            TIT = TITt[:, :, 64:128]
            for d in dirs:
                c = ch[d]
                dve(lambda h, d=d, c=c: h.tensor_scalar(out=VB[:, d, :], in0=VTM[:, c, :], scalar1=Bs[:, s, d:d + 1], scalar2=None, op0=ALU.mult), rch, [rVB])
                dve(lambda h, d=d, c=c: h.tensor_scalar(out=KBE[:, d, :], in0=KTM[:, c, :], scalar1=Bs[:, s, d:d + 1], scalar2=SC[:, 2 + d:3 + d], op0=ALU.mult, op1=ALU.mult), rch + [rSC], [rKBE])
                yield
                dve(lambda h, d=d, c=c: h.tensor_scalar(out=KD[:, d, :], in0=KTM[:, c, :], scalar1=SC[:, 4 + d:5 + d], scalar2=None, op0=ALU.mult), rch + [rSC], [rKD])
                dve(lambda h, d=d: h.tensor_tensor(out=QGT[:, d, :], in0=QT[:, tok[d]], in1=EB[:, d, :], op=ALU.mult), rch + [rEB], [rQGT])
                yield
            dve(lambda h: h.tensor_tensor(out=AQM[:, ds, :], in0=ps3[:, ds, :], in1=DECT[:, ds, :], op=ALU.mult), [r3, rDECT], [rAQM])
            for d in dirs:
                for hs in HS:
                    P.mm(pswT[hs, d, :], KBE[hs, d, :], TIT[hs, d, :], reads=[rKBE, rTIT], writes=[rwT])
            yield
            dve(lambda h: h.tensor_scalar(out=NWT[:, ds, :], in0=pswT[:, ds, :], scalar1=-1.0, scalar2=None, op0=ALU.mult), [rwT], [rNWT])
            yield
            for d in dirs:
                for hs in HS:
                    P.mm(ps5[hs, d, :], TIT[hs, d, :], VB[hs, d, :], start=True, stop=False, reads=[rTIT, rVB], writes=[r5])
                    P.mm(ps5[hs, d, :], NWT[hs, d, :], Scur[hs, d, :], start=False, stop=True, reads=[rNWT, rScur], writes=[r5])
            yield
            dve(lambda h: h.tensor_copy(out=VN[:, ds, :], in_=ps5[:, ds, :]), [r5], [rVN])
            yield
            for d in dirs:
                for hs in HS:
                    P.mm(ps6[hs, d, :], QGT[hs, d, :], Scur[hs, d, :], start=True, stop=False, reads=[rQGT, rScur], writes=[r6])
                    P.mm(ps6[hs, d, :], AQM[hs, d, :], VN[hs, d, :], start=False, stop=True, reads=[rAQM, rVN], writes=[r6])
            for d in dirs:
                for hs in HS:
                    P.mm(ps7[hs, d, :], KD[hs, d, :], VN[hs, d, :], reads=[rKD, rVN], writes=[r7])
            yield
            for d in range(2):
                if d in dirs:
                    lastc = 63 if d == 0 else 0
                    dve(lambda h, d=d, lastc=lastc: h.tensor_scalar(out=SGL[:, d, :], in0=Scur[:, d, :], scalar1=EB[:, d, lastc:lastc + 1], scalar2=None, op0=ALU.mult), [rScur, rEB], [rSGL])
                    dve(lambda h, d=d: h.tensor_tensor(out=Snew[:, d, :], in0=SGL[:, d, :], in1=ps7[:, d, :], op=ALU.add), [rSGL, r7], [rSnew])
                else:
                    dve(lambda h, d=d: h.tensor_copy(out=Snew[:, d, :], in_=Scur[:, d, :]), [rScur], [rSnew])
            yield
            for d in dirs:
                c = ch[d]
                dve(lambda h, d=d, c=c: h.tensor_tensor(out=O[:, c, :], in0=O[:, c, :], in1=ps6[:, d, :], op=ALU.add), [r6, rO[c]], [rO[c]])
            yield

        def drive(older, newer):
            a_done = older is None
            b_done = newer is None
            while not (a_done and b_done):
                if not a_done:
                    try:
                        next(older)
                    except StopIteration:
                        a_done = True
                if not b_done:
                    try:
                        if next(newer) == "HALF":
                            b_done = True
                    except StopIteration:
                        b_done = True

        prev = None
        for s in range(NS):
            g = step_gen(s, lanes[s % 2])
            drive(prev, g)
            prev = g
        drive(prev, None)

        P.fence()
        G = 33
        OSQ = P_sb("OSQ", [128, G, 64]); rOSQ = Res()
        OSS = P_sb("OSS", [128, G]); rOSS = Res()
        for g in range(NCH // G):
            cs = slice(g * G, (g + 1) * G)
            ro = [rO[c] for c in range(g * G, (g + 1) * G)]
            dve(lambda h, cs=cs: h.tensor_tensor(out=OSQ[:], in0=O[:, cs, :], in1=O[:, cs, :], op=ALU.mult), ro, [rOSQ])
            dve(lambda h: h.reduce_sum(out=OSS[:], in_=OSQ[:], axis=AX.X), [rOSQ], [rOSS])
            P.act(OSS[:], OSS[:], AF.Ln, reads=[rOSS], writes=[rOSS], bias=1e-6, scale=1.0 / 64)
            P.act(OSS[:], OSS[:], AF.Exp, reads=[rOSS], writes=[rOSS], scale=-0.5)
            dve(lambda h, cs=cs: h.tensor_tensor(out=O[:, cs, :], in0=O[:, cs, :], in1=OSS[:].unsqueeze(2).to_broadcast([128, G, 64]), op=ALU.mult), ro + [rOSS], ro)
            dve(lambda h, cs=cs: h.tensor_tensor(out=O[:, cs, :], in0=O[:, cs, :], in1=GDN[:].unsqueeze(1).to_broadcast([128, G, 64]), op=ALU.mult), ro + [rGDN], ro)
            dve(lambda h, cs=cs: h.tensor_tensor(out=O[:, cs, :], in0=O[:, cs, :], in1=SZ[:, cs, :], op=ALU.mult), ro + [rCH[c] for c in range(g * G, (g + 1) * G)], ro)
        ov = o_dn.rearrange("(c t) (h d) -> h t c d", t=64, h=2)
        for hh in range(2):
            P.dma("sp", ov[hh], O[hh * 64:(hh + 1) * 64, :, :], reads=rO, writes=[rOUT[hh]])
        P.fence()


NE = 32


def emit_b(P, G, IN, l, NL, NC, xs, rxs, omall, romall, xo, rOUT):
    NT = NL + NC
    cvT = IN["cvT"]; adaw = IN["adaw2", l]; adab = IN["adab2", l]; adabT = IN["adab2T", l]
    g2T = IN["g2T", l]; wout = IN["wout", l]; rw = IN["rw", l]; rb = IN["rb", l]
    wgu = IN["wgu", l]; bguT = IN["bguT", l]; wd = IN["wd", l]; bd = IN["bd", l]; seli = IN["seli"]

    tiles = [(i * 128, 128, 0) for i in range(NL // 128)]
    if NC:
        tiles.append((NL, NC, 1))
    nlt = NL // 128
    passes = [list(range(0, nlt // 2)), list(range(nlt // 2, len(tiles)))]
    MAXT = max(len(p) for p in passes)

    with ExitStack() as st0:
        _sb = P.sb
        P_sb = lambda name, shape, dt=F32, stack=None: _sb("b_" + name, shape, dt, stack=(stack or st0))
        ID, rID, ONES, rONES, ONESB, rONESB, PS, rPS = G["ID"], G["rID"], G["ONES"], G["rONES"], G["ONESB"], G["rONESB"], G["PS"], G["rPS"]
        SELI = P_sb("SELI", [128, 4, 128], BF16); rSELI = Res()
        P.dma("pool", SELI[:], seli, writes=[rSELI])

        GT = P_sb("GT", [128, 2, 2, D]); rGT = Res()
        MODF = P_sb("MODF", [128, 16, 2]); rMODF = Res()
        SCALE2 = P_sb("SCALE2", [128, 8, 2]); rSCALE2 = Res()
        G2 = P_sb("G2", [128, 8]); rG2 = Res()
        P.dma("sp", G2[:], g2T, writes=[rG2])
        ABT = P_sb("ABT", [128, 16]); rABT = Res()
        P.dma("sp", ABT[:], adabT, writes=[rABT])
        RW = P_sb("RW", [128, 8, NE]); rRW = Res()
        P.dma("sp", RW[:], rw.rearrange("(k p) e -> p k e", p=128), writes=[rRW])
        RB = P_sb("RB", [1, NE]); rRB = Res()
        P.dma("sp", RB[:], rb, writes=[rRB])
        WO = P_sb("WO", [128, 8, D], BF16); rWO = Res()
        P.dma("pool", WO[:], wout.rearrange("(k p) f -> p k f", p=128), writes=[rWO])
        sub = ExitStack()
        ABR = P_sb("ABR", [1, 4096], stack=sub); rABR = Res()
        P.dma("sp", ABR[:], adab, writes=[rABR])
        CV = P_sb("CV", [128, 8, 2], stack=sub); rCV = Res()
        P.dma("sp", CV[:], cvT, writes=[rCV])
        SCV = P_sb("SCV", [128, 8, 2], stack=sub); rSCV = Res()
        P.act(SCV[:], CV[:], AF.Silu, reads=[rCV], writes=[rSCV])
        SCB = P_sb("SCB", [128, 8, 2, 128], stack=sub); rSCB = Res()
        P.op("dve", lambda h: h.tensor_tensor(out=SCB[:], in0=ONES[:].unsqueeze(1).unsqueeze(1).to_broadcast([128, 8, 2, 128]),
                                              in1=SCV[:].unsqueeze(3).to_broadcast([128, 8, 2, 128]), op=ALU.mult),
             reads=[rONES, rSCV], writes=[rSCB])

        AW = [P_sb("AW%d" % i, [128, 8, 512], stack=sub) for i in range(2)]; rAW = [Res(), Res()]
        for blk in range(8):
            aw, raw = AW[blk % 2], rAW[blk % 2]
            P.dma("sp", aw[:], adaw[:, blk * 512:(blk + 1) * 512].rearrange("(k p) f -> p k f", p=128), writes=[raw])
            if blk in (0, 1, 6, 7):
                which = 0 if blk < 2 else 1
                half = blk % 2
                for j in range(2):
                    ps, rps = PS[j], rPS[j]
                    for k in range(8):
                        P.mm(ps[:], SCB[:, k, j, :], aw[:, k, :], start=(k == 0), stop=False,
                             reads=[rSCB, raw], writes=[rps])
                    P.mm(ps[:], ONES[0:1, :], ABR[0:1, blk * 512:(blk + 1) * 512], start=False, stop=True,
                         reads=[rONES, rABR], writes=[rps])
                    P.act(GT[:, which, j, half * 512:(half + 1) * 512], ps[:], AF.Copy, reads=[rps], writes=[rGT])
            else:
                ps, rps = PS[2 + blk % 2], rPS[2 + blk % 2]
                for fcl in range(4):
                    fcg = (blk - 2) * 4 + fcl
                    for k in range(8):
                        P.mm(ps[:, fcl * 2:fcl * 2 + 2], aw[:, k, fcl * 128:(fcl + 1) * 128], SCV[:, k, :],
                             start=(k == 0), stop=(k == 7), reads=[rSCV, raw], writes=[rps])
                for fcl in range(4):
                    fcg = (blk - 2) * 4 + fcl
                    P.act(MODF[:, fcg, :], ps[:, fcl * 2:fcl * 2 + 2], AF.Identity, reads=[rps, rABT], writes=[rMODF],
                          bias=ABT[:, fcg:fcg + 1], scale=1.0)
        P.op("dve", lambda h: h.tensor_scalar(out=SCALE2[:], in0=MODF[:, 8:16, :], scalar1=1.0, scalar2=None, op0=ALU.add),
             reads=[rMODF], writes=[rSCALE2])
        P.op("dve", lambda h: h.tensor_tensor(out=SCALE2[:], in0=SCALE2[:], in1=G2[:].unsqueeze(2).to_broadcast([128, 8, 2]), op=ALU.mult),
             reads=[rSCALE2, rG2], writes=[rSCALE2])

        P.fence()
        sub.close()
        X1 = P_sb("X1", [128, MAXT, D]); rX1 = [Res() for _ in range(MAXT)]
        H2B = P_sb("H2B", [128, 8, MAXT * 128], BF16); rH2B = [Res() for _ in range(MAXT)]
        GATES = P_sb("GATES", [128, MAXT, NE]); rGATES = [Res() for _ in range(MAXT)]
        XT = [P_sb("XT%d" % i, [128, D]) for i in range(2)]; rXT = [Res(), Res()]
        OM = [P_sb("OM%d" % i, [128, 8, 128], BF16) for i in range(2)]; rOM = [Res(), Res()]
        OMX = P_sb("OMX", [128, 4, 4, 256], BF16); rOMX = Res()
        TMP = [P_sb("TMP%d" % i, [128, D]) for i in range(2)]; rTMP = [Res(), Res()]
        H2F = P_sb("H2F", [128, 8, 128]); rH2F = Res()
        SMALL = P_sb("SMALL", [128, 64]); rSM = Res()
        LG = P_sb("LG", [128, NE]); rLG = Res()
        EX = P_sb("EX", [128, NE]); rEX = Res()
        MK = P_sb("MK", [128, NE]); rMK = Res()
        WGU = P_sb("WGU", [128, 8, 2 * D], BF16); rWG = [Res() for _ in range(8)]; rWU = [Res() for _ in range(8)]
        WD = P_sb("WD", [128, 8, D], BF16); rWD = [Res() for _ in range(8)]
        BGU = [P_sb("BGU%d" % i, [128, 16]) for i in range(2)]; rBGU = [Res(), Res()]
        BD = [P_sb("BD%d" % i, [1, D], BF16) for i in range(2)]; rBD = [Res(), Res()]
        BGX = [P_sb("BGX%d" % i, [128, 16]) for i in range(2)]; rBGX = [Res(), Res()]
        SIGC = float(1.0 / (1.0 + np.exp(np.float64(-1.702 * 7.0))))
        ACTT = [P_sb("ACTT%d" % i, [128, 8, 512], BF16) for i in range(2)]; rACTT = [Res(), Res()]
        GP = [P_sb("GP%d" % i, [128, 512]) for i in range(2)]; rGP = [Res(), Res()]
        SG = [P_sb("SG%d" % i, [128, 512]) for i in range(2)]; rSG = [Res(), Res()]
        UP = [P_sb("UP%d" % i, [128, 512]) for i in range(2)]; rUP = [Res(), Res()]
        wcount = 0
        cnt = 0

        for pss in passes:
            for li, ti in enumerate(pss):
                r0, nr, j = tiles[ti]
                xt, rxt = XT[li % 2], rXT[li % 2]
                om, rom = OM[li % 2], rOM[li % 2]
                tmp, rtmp = TMP[li % 2], rTMP[li % 2]
                P.dma("sp", xt[:nr, :], xs[r0:r0 + nr, :], reads=rxs, writes=[rxt])
                for q in range(4):
                    grow = (256 + q * NL + r0) if j == 0 else (q * NC)
                    for jj in range(4):
                        P.dma("pool", OMX[:nr, q, jj, :], omall(jj, grow, nr), reads=romall, writes=[rOMX])
                for k in range(8):
                    ps, rps = PS[2 + k // 4], rPS[2 + k // 4]
                    for q in range(4):
                        P.mm(ps[:, (k % 4) * 128:(k % 4) * 128 + nr], OMX[:nr, q, k % 4, (k // 4) * 128:(k // 4 + 1) * 128], SELI[:nr, q, :nr],
                             start=(q == 0), stop=(q == 3), reads=[rOMX, rSELI], writes=[rps])
                for kk in range(2):
                    P.act(om[:, 4 * kk:4 * kk + 4, :nr], PS[2 + kk][:, :].rearrange("p (a t) -> p a t", a=4)[:, :, :nr], AF.Copy, reads=[rPS[2 + kk]], writes=[rom])
                for half in range(2):
                    ps, rps = PS[half], rPS[half]
                    for k in range(8):
                        P.mm(ps[:nr, :], om[:, k, :nr], WO[:, k, half * 512:(half + 1) * 512], start=(k == 0), stop=(k == 7),
                             reads=[rom, rWO], writes=[rps])
                    P.op("dve", lambda h, ps=ps, tmp=tmp, half=half, j=j, nr=nr: h.tensor_tensor(
                        out=tmp[:nr, half * 512:(half + 1) * 512], in0=ps[:nr, :], in1=GT[:nr, 0, j, half * 512:(half + 1) * 512], op=ALU.mult),
                        reads=[rps, rGT], writes=[rtmp])
                P.op("dve", lambda h, tmp=tmp, xt=xt, li=li, nr=nr: h.tensor_tensor(out=X1[:nr, li, :], in0=tmp[:nr, :], in1=xt[:nr, :], op=ALU.add),
                     reads=[rtmp, rxt], writes=[rX1[li]])
                P.act(tmp[:nr, :], X1[:nr, li, :], AF.Square, reads=[rX1[li]], writes=[rtmp, rSM], accum_out=SMALL[:nr, 0:1])
                P.act(SMALL[:nr, 1:2], SMALL[:nr, 0:1], AF.Sqrt, reads=[rSM], writes=[rSM], bias=1e-6, scale=1.0 / D)
                P.op("dve", lambda h, nr=nr: h.reciprocal(out=SMALL[:nr, 2:3], in_=SMALL[:nr, 1:2]), reads=[rSM], writes=[rSM])
                P.op("dve", lambda h, tmp=tmp, li=li, nr=nr: h.tensor_scalar(out=tmp[:nr, :], in0=X1[:nr, li, :], scalar1=SMALL[:nr, 2:3], scalar2=None, op0=ALU.mult),
                     reads=[rX1[li], rSM], writes=[rtmp])
                for k in range(8):
                    ps, rps = PS[2 + k // 4], rPS[2 + k // 4]
                    P.op("pe", lambda h, ps=ps, tmp=tmp, k=k, nr=nr: h.transpose(ps[:, (k % 4) * 128:(k % 4) * 128 + nr], tmp[:nr, k * 128:(k + 1) * 128], ID[:nr, :nr]),
                         reads=[rtmp, rID], writes=[rps])
                for k in range(8):
                    ps, rps = PS[2 + k // 4], rPS[2 + k // 4]
                    P.act(H2F[:, k, :nr], ps[:, (k % 4) * 128:(k % 4) * 128 + nr], AF.Identity, reads=[rps, rSCALE2, rMODF], writes=[rH2F],
                          scale=SCALE2[:, k, j:j + 1], bias=MODF[:, k, j:j + 1])
                P.op("dve", lambda h, li=li, nr=nr: h.tensor_copy(out=H2B[:, :, li * 128:li * 128 + nr], in_=H2F[:, :, :nr]),
                     reads=[rH2F], writes=[rH2B[li]])
                ps, rps = PS[4], rPS[4]
                for k in range(8):
                    P.mm(ps[:nr, 0:NE], H2F[:, k, :nr], RW[:, k, :], start=(k == 0), stop=False, reads=[rH2F, rRW], writes=[rps])
                P.mm(ps[:nr, 0:NE], ONES[0:1, :nr], RB[0:1, :], start=False, stop=True, reads=[rONES, rRB], writes=[rps])
                P.op("dve", lambda h, ps=ps, nr=nr: h.tensor_copy(out=LG[:nr, :], in_=ps[:nr, 0:NE]), reads=[rps], writes=[rLG])
                P.op("dve", lambda h, nr=nr: h.max(out=SMALL[:nr, 8:16], in_=LG[:nr, :]), reads=[rLG], writes=[rSM])
                P.op("dve", lambda h, nr=nr: h.tensor_scalar(out=SMALL[:nr, 16:17], in0=SMALL[:nr, 8:9], scalar1=-1.0, scalar2=None, op0=ALU.mult),
                     reads=[rSM], writes=[rSM])
                P.act(EX[:nr, :], LG[:nr, :], AF.Exp, reads=[rLG, rSM], writes=[rEX], bias=SMALL[:nr, 16:17], scale=1.0)
                P.op("dve", lambda h, nr=nr: h.tensor_scalar(out=MK[:nr, :], in0=LG[:nr, :], scalar1=SMALL[:nr, 11:12], scalar2=None, op0=ALU.is_ge),
                     reads=[rLG, rSM], writes=[rMK])
                P.op("dve", lambda h, nr=nr: h.tensor_tensor(out=EX[:nr, :], in0=EX[:nr, :], in1=MK[:nr, :], op=ALU.mult),
                     reads=[rEX, rMK], writes=[rEX])
                P.op("dve", lambda h, nr=nr: h.reduce_sum(out=SMALL[:nr, 17:18], in_=EX[:nr, :], axis=AX.X), reads=[rEX], writes=[rSM])
                P.op("dve", lambda h, nr=nr: h.reciprocal(out=SMALL[:nr, 18:19], in_=SMALL[:nr, 17:18]), reads=[rSM], writes=[rSM])
                P.op("dve", lambda h, li=li, nr=nr: h.tensor_scalar(out=GATES[:nr, li, :], in0=EX[:nr, :], scalar1=SMALL[:nr, 18:19], scalar2=None, op0=ALU.mult),
                     reads=[rEX, rSM], writes=[rGATES[li]])

            groups = []
            li = 0
            while li < len(pss):
                g = []
                while li < len(pss) and len(g) < 4 and tiles[pss[li]][1] == 128:
                    g.append(li); li += 1
                if not g:
                    g = [li]; li += 1
                groups.append(g)
            for e in range(IN.get("nex", NE)):
                wb = wcount % 2
                wcount += 1
                for fc in range(8):
                    P.dma("pool", WGU[:, :, fc * 128:(fc + 1) * 128], wgu[e, :, fc * 128:(fc + 1) * 128].rearrange("(k p) f -> p k f", p=128),
                          writes=[rWG[fc]])
                    P.dma("pool", WGU[:, :, D + fc * 128:D + (fc + 1) * 128], wgu[e, :, D + fc * 128:D + (fc + 1) * 128].rearrange("(k p) f -> p k f", p=128),
                          writes=[rWU[fc]])
                for fc in range(8):
                    P.dma("pool", WD[:, fc, :], wd[e, fc * 128:(fc + 1) * 128, :], writes=[rWD[fc]])
                P.dma("sp", BGU[wb][:], bguT[e], writes=[rBGU[wb]])
                P.op("dve", lambda h, wb=wb: h.tensor_scalar(out=BGX[wb][:, 0:8], in0=BGU[wb][:, 0:8], scalar1=1.702, scalar2=None, op0=ALU.mult),
                     reads=[rBGU[wb]], writes=[rBGX[wb]])
                P.op("dve", lambda h, wb=wb: h.tensor_scalar(out=BGX[wb][:, 8:16], in0=BGU[wb][:, 8:16], scalar1=1.0, scalar2=None, op0=ALU.add),
                     reads=[rBGU[wb]], writes=[rBGX[wb]])
                P.dma("pool", BD[wb][:], bd[e:e + 1, :], writes=[rBD[wb]])
                for g in groups:
                    ntok = sum(tiles[pss[l]][1] for l in g)
                    c0 = g[0] * 128
                    ab = cnt % 2
                    cnt += 1
                    actt, ractt = ACTT[ab], rACTT[ab]
                    rh = [rH2B[l] for l in g]
                    for fc in range(8):
                        pb = (fc % 2) * 2
                        psg, rpsg = PS[pb], rPS[pb]
                        psu, rpsu = PS[pb + 1], rPS[pb + 1]
                        for k in range(8):
                            P.mm(psg[:, :ntok], WGU[:, k, fc * 128:(fc + 1) * 128], H2B[:, k, c0:c0 + ntok], start=(k == 0), stop=(k == 7),
                                 reads=[rWG[fc]] + rh, writes=[rpsg])
                        for k in range(8):
                            P.mm(psu[:, :ntok], WGU[:, k, D + fc * 128:D + (fc + 1) * 128], H2B[:, k, c0:c0 + ntok], start=(k == 0), stop=(k == 7),
                                 reads=[rWU[fc]] + rh, writes=[rpsu])
                        tb = fc % 2
                        gp, sg, up = GP[tb], SG[tb], UP[tb]
                        P.act(sg[:, :ntok], psg[:, :ntok], AF.Sigmoid, reads=[rpsg, rBGX[wb]], writes=[rSG[tb]], scale=1.702, bias=BGX[wb][:, fc:fc + 1])
                        P.op("dve", lambda h, gp=gp, psg=psg, fc=fc, wb=wb, ntok=ntok: h.tensor_scalar(
                            out=gp[:, :ntok], in0=psg[:, :ntok], scalar1=BGU[wb][:, fc:fc + 1], scalar2=7.0, op0=ALU.add, op1=ALU.min),
                            reads=[rpsg, rBGU[wb]], writes=[rGP[tb]])
                        P.op("dve", lambda h, up=up, psu=psu, fc=fc, wb=wb, ntok=ntok: h.tensor_scalar(
                            out=up[:, :ntok], in0=psu[:, :ntok], scalar1=BGX[wb][:, 8 + fc:9 + fc], scalar2=8.0, op0=ALU.add, op1=ALU.min),
                            reads=[rpsu, rBGX[wb]], writes=[rUP[tb]])
                        P.op("dve", lambda h, gp=gp, sg=sg, ntok=ntok: h.scalar_tensor_tensor(out=gp[:, :ntok], in0=sg[:, :ntok], scalar=SIGC, in1=gp[:, :ntok], op0=ALU.min, op1=ALU.mult),
                             reads=[rGP[tb], rSG[tb]], writes=[rGP[tb]])
                        P.op("dve", lambda h, gp=gp, up=up, actt=actt, fc=fc, ntok=ntok: h.scalar_tensor_tensor(out=actt[:, fc, :ntok], in0=up[:, :ntok], scalar=-6.0, in1=gp[:, :ntok], op0=ALU.max, op1=ALU.mult),
                             reads=[rGP[tb], rUP[tb]], writes=[ractt])
                    for gi, l in enumerate(g):
                        r0, nr, j = tiles[pss[l]]
                        yb = 4 + (l % 2) * 2
                        for half in range(2):
                            ps, rps = PS[yb + half], rPS[yb + half]
                            for fc in range(8):
                                P.mm(ps[:nr, :], actt[:, fc, gi * 128:gi * 128 + nr], WD[:, fc, half * 512:(half + 1) * 512], start=(fc == 0), stop=False,
                                     reads=[ractt, rWD[fc]], writes=[rps])
                            P.mm(ps[:nr, :], ONESB[0:1, :nr], BD[wb][0:1, half * 512:(half + 1) * 512], start=False, stop=True,
                                 reads=[rONESB, rBD[wb]], writes=[rps])
                        tmp, rtmp = TMP[l % 2], rTMP[l % 2]
                        for half in range(2):
                            ps, rps = PS[yb + half], rPS[yb + half]
                            P.op("dve", lambda h, ps=ps, tmp=tmp, half=half, l=l, e=e, j=j, nr=nr: h.scalar_tensor_tensor(
                                out=tmp[:nr, half * 512:(half + 1) * 512], in0=ps[:nr, :], scalar=GATES[:nr, l, e:e + 1],
                                in1=GT[:nr, 1, j, half * 512:(half + 1) * 512], op0=ALU.mult, op1=ALU.mult),
                                reads=[rps, rGATES[l], rGT], writes=[rtmp])
                        P.op("dve", lambda h, tmp=tmp, l=l, nr=nr: h.tensor_tensor(out=X1[:nr, l, :], in0=X1[:nr, l, :], in1=tmp[:nr, :], op=ALU.add),
                             reads=[rtmp, rX1[l]], writes=[rX1[l]])
            for li, ti in enumerate(pss):
                r0, nr, j = tiles[ti]
                P.dma("sp", xo[r0:r0 + nr, :], X1[:nr, li, :], reads=[rX1[li]], writes=[rOUT[ti]])
        P.fence()


PER_LAYER = [("adaw1", [1024, 2048]), ("adab1T", [128, 16]), ("g1T", [128, 8]), ("wa", [1024, 384]), ("wz", [1024, 2, 68]),
             ("cw", [128, 3, 5]), ("nega", [128, 2]), ("dtb", [128, 2]), ("gdn", [128, 64]), ("ws", [1024, 256]),
             ("gqk", [128, 3, 64]), ("sinkb", [128, 2]),
             ("adaw2", [1024, 4096]), ("adab2", [1, 4096]), ("adab2T", [128, 16]), ("g2T", [128, 8]), ("wout", [1024, 1024]),
             ("rw", [1024, 32]), ("rb", [1, 32]), ("bguT", [32, 128, 16]), ("bd", [32, 1024])]
SHARED = [("ident", [128, 128]), ("cvT", [128, 8, 2]), ("blk1", [128, 128]), ("masks", [128, 6, 2, 64]), ("maskw", [128, 384]),
          ("ropet", [128, 64, 2, 2, 16]), ("seli", [128, 4, 128])]
NLOC = 2112


def build_fused(nex=32):
    nc = bass.Bass("TRN2", target_bir_lowering=False)
    dr = lambda name, shape: nc.dram_tensor(name, list(shape), F32, kind="ExternalInput").ap()
    IN = {}
    IN["xall"] = dr("xall", [8448, 1024]); IN["xs0"] = dr("xs0", [NLOC, 1024])
    for nm, shp in SHARED:
        IN[nm] = dr(nm, shp)
    for l in range(2):
        for nm, shp in PER_LAYER:
            IN[nm, l] = dr("%s_%d" % (nm, l), shp)
    for l in range(2):
        IN["wgu", l] = dr("wgu_%d" % l, [nex, 1024, 2048]); IN["wd", l] = dr("wd_%d" % l, [nex, 1024, 1024])
    IN["nex"] = nex
    xo = nc.dram_tensor("xo", [2048, 1024], F32, kind="ExternalOutput").ap()
    omloc = [nc.dram_tensor("omloc%d" % l, [8448, 256], F32).ap() for l in range(2)]
    OCH = 1024
    och = [(r, min(OCH, 8448 - r)) for r in range(0, 8448, OCH)]
    omall = [[nc.dram_tensor("omall%d_%d" % (l, k), [4 * n, 256], F32).ap() for k, (r, n) in enumerate(och)] for l in range(2)]
    xloc = nc.dram_tensor("xloc", [NLOC, 1024], F32).ap()
    XCH = 256
    xch = [(r, min(XCH, NLOC - r)) for r in range(0, NLOC, XCH)]
    xgat = [nc.dram_tensor("xgat_%d" % k, [4 * n, 1024], F32).ap() for k, (r, n) in enumerate(xch)]

    def om_rows(l, jj, grow, nr):
        k = grow // OCH
        n = och[k][1]
        off = jj * n + (grow - och[k][0])
        return omall[l][k][off:off + nr, :]

    def xg_rows(q, r, nr):
        k = r // XCH
        n = xch[k][1]
        off = q * n + (r - xch[k][0])
        return xgat[k][off:off + nr, :]
    groups = [[0, 1, 2, 3], [4, 5, 6, 7]]
    with ExitStack() as st:
        P = Prog(nc, st)
        G = emit_globals(P, IN)
        rxloc = [Res() for _ in range(17)]
        rxgat = Res()
        rcc = Res()
        rxo = [Res() for _ in range(16)]
        for l in range(2):
            last = (l == 1)
            if l == 0:
                xsrc = lambda n: [(0, 128, IN["xall"][n * 128:(n + 1) * 128, :])]
                rsrc = []
            else:
                def xsrc(n):
                    if n < 2:
                        a, b = 2 * n, 2 * n + 1
                        return [(0, 64, xg_rows(a, 2048, 64)), (64, 64, xg_rows(b, 2048, 64))]
                    i = n - 2
                    q, r = i // 16, (i % 16) * 128
                    return [(0, 128, xg_rows(q, r, 128))]
                rsrc = [rxgat]
            rdn = [Res(), Res()]
            rsw = [Res() for _ in range(66)]
            with ExitStack() as lst:
                C = emit_common(P, G, IN, l, lst)
                emit_dn(P, C, IN, l, last, xsrc, rsrc, omloc[l][:, 0:128], rdn)
                emit_swa(P, C, IN, l, last, xsrc, rsrc, omloc[l][:, 128:256], rsw)
                P.fence()
            romall = Res()
            if os.environ.get("NOCOLL") != "1":
                for k, (r, n) in enumerate(och):
                    P.coll("AllGather", ALU.bypass, groups, omloc[l][r:r + n, :], omall[l][k], reads=rdn + rsw, writes=[romall, rcc])
            if l == 0:
                emit_b(P, G, IN, 0, 2048, 64, IN["xs0"], [], lambda jj, grow, nr: om_rows(0, jj, grow, nr), [romall], xloc, rxloc)
                if os.environ.get("NOCOLL") not in ("1", "2"):
                    for k, (r, n) in enumerate(xch):
                        P.coll("AllGather", ALU.bypass, groups, xloc[r:r + n, :], xgat[k], reads=rxloc, writes=[rxgat, rcc])
            else:
                emit_b(P, G, IN, 1, 2048, 0, xloc, rxloc, lambda jj, grow, nr: om_rows(1, jj, grow, nr), [romall], xo, rxo)
        P.finish(rxo)
        print("fused instr counts", P.cnt, P.dcnt, "waits", P.n_wait, "sems", {k: len(v) for k, v in P.sem.items()})
    return nc


NEG = -1e30
def consts():
    f = np.float32
    i = np.arange(64)
    k_le_f = (i[:, None] <= i[None, :]).astype(f)
    k_ge_f = (i[:, None] >= i[None, :]).astype(f)
    m = np.zeros((64, 6, 2, 64), f)
    m[:, 0, 0] = k_le_f; m[:, 0, 1] = k_ge_f
    m[:, 1, 0] = np.where(i[None, :] >= i[:, None], 0, NEG)
    m[:, 1, 1] = np.where(i[None, :] <= i[:, None], 0, NEG)
    m[:, 2, 0] = np.where(i[None, :] <= i[:, None], 0, NEG)
    m[:, 2, 1] = np.where(i[None, :] >= i[:, None], 0, NEG)
    m[:, 3, 0] = np.where(i[None, :] > i[:, None], -1, 0)
    m[:, 3, 1] = np.where(i[None, :] < i[:, None], -1, 0)
    m[:, 4, 0] = np.where(i[None, :] < i[:, None], -1, 0)
    m[:, 4, 1] = np.where(i[None, :] > i[:, None], -1, 0)
    m[:, 5, 0] = np.eye(64); m[:, 5, 1] = np.eye(64)
    masks = np.concatenate([m, m], 0)
    blk1 = np.zeros((128, 128), f); blk1[:64, :64] = 1; blk1[64:, 64:] = 1
    qi = np.arange(128)[:, None]; kc = np.arange(384)[None, :]
    maskw = np.where((kc >= qi) & (kc <= qi + 256), 0, NEG).astype(f)
    t = np.arange(8192); row = (t // 64).astype(f); col = (t % 64).astype(f)
    inv = np.power(f(10000.0), -np.arange(16, dtype=f) / f(16)).astype(f)
    ar = row[:, None] * inv; ac = col[:, None] * inv
    rope = np.stack([np.stack([np.cos(ar), np.cos(ac)], 1), np.stack([np.sin(ar), np.sin(ac)], 1)], 1).astype(f)
    ropet = np.ascontiguousarray(rope.reshape(64, 128, 2, 2, 16).transpose(1, 0, 2, 3, 4))
    return dict(masks=masks, blk1=blk1, maskw=maskw, ropet=ropet, ident=np.eye(128, dtype=f))
def prep_a(inp, l, x, xc, K):
    f = np.float32
    w_in = inp["w_in"][l]; cwl = inp["dn_conv_w"][l]
    ada_w = np.ascontiguousarray(inp["ada_w"][l][:, 0:2048]); ada_b = inp["ada_b"][l][0:2048]
    base = dict(adaw=ada_w, adabT=np.ascontiguousarray(ada_b.reshape(16, 128).T), g1T=np.ascontiguousarray(inp["norm1_g"][l].reshape(8, 128).T), ident=K["ident"])
    dn, sw = [], []
    for c in range(8):
        b, j = c // 4, c % 4
        xall = np.ascontiguousarray(np.concatenate([xc[b], x[b]], 0))
        cv = np.stack([inp["c"][b], inp["c_ctx"]], -1)
        m = dict(base); m.update(xall=xall, cvT=np.ascontiguousarray(cv.reshape(8, 128, 2).transpose(1, 0, 2)))
        hd = [2 * j, 2 * j + 1]
        wa = np.concatenate([w_in[:, s * 512 + 128 * j: s * 512 + 128 * j + 128] for s in range(3)], 1)
        wz = np.stack([np.concatenate([w_in[:, 1536 + h * 64:1536 + (h + 1) * 64], w_in[:, [2048 + h, 2048 + 8 + h, 2064 + h, 2064 + 8 + h]]], 1) for h in hd], 1)
        cw = np.stack([cwl[:, s * 512 + 128 * j: s * 512 + 128 * j + 128].T for s in range(3)], 1)
        hp = np.repeat(np.array(hd), 64)
        nega = -np.exp(inp["dn_a_log"][l][:, hp]).T; dtb = inp["dn_dt_bias"][l][:, hp].T
        md = dict(m); md.update(wa=np.ascontiguousarray(wa), wz=np.ascontiguousarray(wz), cw=np.ascontiguousarray(cw), nega=np.ascontiguousarray(nega.astype(f)),
                                dtb=np.ascontiguousarray(dtb), gdn=np.ascontiguousarray(np.broadcast_to(inp["dn_out_g"][l], (128, 64))), blk1=K["blk1"], masks=K["masks"])
        dn.append(md)
        kv = j // 2
        ws = np.concatenate([w_in[:, 2080 + hd[0] * 64:2080 + hd[0] * 64 + 128], w_in[:, 2592 + kv * 64:2592 + (kv + 1) * 64], w_in[:, 2720 + kv * 64:2720 + (kv + 1) * 64]], 1)
        gqk = np.broadcast_to(np.stack([inp["q_norm_g"][l], inp["q_norm_g"][l], inp["k_norm_g"][l]], 0), (128, 3, 64))
        ms = dict(m); ms.update(ws=np.ascontiguousarray(ws), gqk=np.ascontiguousarray(gqk), ropet=K["ropet"],
                                sinkb=np.ascontiguousarray(np.broadcast_to(inp["sinks"][l][hd], (128, 2))), maskw=K["maskw"])
        sw.append(ms)
    return dn, sw
def gather_a(res_dn, res_sw):
    om_x = np.zeros((2, 8192, 1024), np.float32); om_c = np.zeros((2, 256, 1024), np.float32)
    for c in range(8):
        b, j = c // 4, c % 4
        od = res_dn[c]["o_dn"]; os_ = res_sw[c]["o_sw"]
        om_c[b, :, 128 * j:128 * j + 128] = od[:256]; om_x[b, :, 128 * j:128 * j + 128] = od[256:]
        om_c[b, :, 512 + 128 * j:512 + 128 * j + 128] = os_[:256]; om_x[b, :, 512 + 128 * j:512 + 128 * j + 128] = os_[256:]
    return om_x, om_c


def prep_b(inp, l, x, xc, om_x, om_c, last):
    f = np.float32
    ada_w = np.ascontiguousarray(inp["ada_w"][l][:, 2048:6144]); ada_b = inp["ada_b"][l][2048:6144]
    common = dict(
        adaw=ada_w, adab=np.ascontiguousarray(ada_b[None, :]),
        adabT=np.ascontiguousarray(ada_b[1024:3072].reshape(16, 128).T),
        g2T=np.ascontiguousarray(inp["norm2_g"][l].reshape(8, 128).T),
        wout=inp["w_out"][l], rw=inp["router_w"][l], rb=np.ascontiguousarray(inp["router_b"][l][None, :]),
        wgu=inp["w_gate_up"][l], bguT=np.ascontiguousarray(inp["b_gate_up"][l].reshape(32, 16, 128).transpose(0, 2, 1)),
        wd=inp["w_down"][l], bd=inp["b_down"][l], ident=np.eye(128, dtype=f))
    maps = []
    for c in range(8):
        b, q = c // 4, c % 4
        rows = [x[b, q * 2048:(q + 1) * 2048]]; oms = [om_x[b, q * 2048:(q + 1) * 2048]]
        if not last:
            rows.append(xc[b, q * 64:(q + 1) * 64]); oms.append(om_c[b, q * 64:(q + 1) * 64])
        xs = np.ascontiguousarray(np.concatenate(rows, 0)); om = np.concatenate(oms, 0)
        cv = np.stack([inp["c"][b], inp["c_ctx"]], -1)
        m = dict(common)
        m.update(xs=xs, omT=np.ascontiguousarray(om.T), cvT=np.ascontiguousarray(cv.reshape(8, 128, 2).transpose(1, 0, 2)))
        maps.append(m)
    return maps
def gather_b(results, last):
    x = np.zeros((2, 8192, 1024), np.float32); xc = np.zeros((2, 256, 1024), np.float32)
    for c in range(8):
        b, q = c // 4, c % 4
        xo = results[c]["xo"]
        x[b, q * 2048:(q + 1) * 2048] = xo[:2048]
        if not last:
            xc[b, q * 64:(q + 1) * 64] = xo[2048:]
    return x, xc


def prep_fused(inp, nex=32):
    f = np.float32
    K = consts()
    x = np.ascontiguousarray(inp["x"], dtype=f); xc = np.ascontiguousarray(inp["ctx"], dtype=f)
    A = [prep_a(inp, l, x, xc, K) for l in range(2)]
    zx = np.zeros((2, 8192, 1024), f); zc = np.zeros((2, 256, 1024), f)
    B = [prep_b(inp, l, zx, zc, zx, zc, False) for l in range(2)]
    wgu = [np.ascontiguousarray(inp["w_gate_up"][l][:nex], dtype=f) for l in range(2)]; wd = [np.ascontiguousarray(inp["w_down"][l][:nex], dtype=f) for l in range(2)]
    maps = []
    for c in range(8):
        b, q = c // 4, c % 4
        m = dict(xall=A[0][0][c]["xall"], cvT=A[0][0][c]["cvT"], ident=K["ident"], blk1=K["blk1"], masks=K["masks"], maskw=K["maskw"], ropet=K["ropet"])
        m["xs0"] = np.ascontiguousarray(np.concatenate([x[b, q * 2048:(q + 1) * 2048], xc[b, q * 64:(q + 1) * 64]], 0))
        seli = np.zeros((128, 4, 128), f); seli[:, q, :] = np.eye(128, dtype=f); m["seli"] = seli
        for l in range(2):
            dn, sw = A[l][0][c], A[l][1][c]; bb = B[l][c]
            for nm, src, key in [("adaw1", dn, "adaw"), ("adab1T", dn, "adabT"), ("g1T", dn, "g1T"), ("wa", dn, "wa"), ("wz", dn, "wz"), ("cw", dn, "cw"),
                                 ("nega", dn, "nega"), ("dtb", dn, "dtb"), ("gdn", dn, "gdn"), ("ws", sw, "ws"), ("gqk", sw, "gqk"), ("sinkb", sw, "sinkb"),
                                 ("adaw2", bb, "adaw"), ("adab2", bb, "adab"), ("adab2T", bb, "adabT"), ("g2T", bb, "g2T"), ("wout", bb, "wout"),
                                 ("rw", bb, "rw"), ("rb", bb, "rb"), ("bguT", bb, "bguT"), ("bd", bb, "bd")]:
                m["%s_%d" % (nm, l)] = np.ascontiguousarray(src[key], dtype=f)
        for l in range(2):
            m["wgu_%d" % l] = wgu[l]; m["wd_%d" % l] = wd[l]
        maps.append(m)
    return maps
def gather_fused(results):
    x = np.zeros((2, 8192, 1024), np.float32)
    for c in range(8):
        b, q = c // 4, c % 4
        x[b, q * 2048:(q + 1) * 2048] = results[c]["xo"]
    return x


_NC = None


def kernel(**inputs):
    global _NC
    inp = {k: np.asarray(v) for k, v in inputs.items()}
    if _NC is None:
        _NC = build_fused(32)
    maps = prep_fused(inp, 32)
    res = run_bass_kernel_spmd(_NC, maps, core_ids=list(range(8)))
    return gather_fused(res.results).astype(np.float32)
```

```python
import os
import numpy as np
from contextlib import ExitStack
import concourse.bass as bass
import concourse.mybir as mybir
from concourse.bass_utils import run_bass_kernel_spmd

F32 = mybir.dt.float32
BF16 = mybir.dt.bfloat16
AF = mybir.ActivationFunctionType
ALU = mybir.AluOpType
AX = mybir.AxisListType

SAME_ENGINE_SYNC = True
NRING = 8
EPOCH = 30000


class Res:
    __slots__ = ("name", "w", "r", "x")

    def __init__(self, name="", x=False):
        self.name = name
        self.w = None
        self.r = {}
        self.x = x


class Prog:
    def __init__(self, nc, stack):
        self.nc = nc
        self.e = {"pe": nc.tensor, "act": nc.scalar, "dve": nc.vector, "pool": nc.gpsimd, "sp": nc.sync}
        self.ops = {k: [] for k in self.e}
        self.cnt = {k: 0 for k in self.e}
        self.sem = {k: [stack.enter_context(nc.semaphore("s_" + k + "0"))] for k in self.e}
        self.ring = {q: [stack.enter_context(nc.semaphore("d_%s_%d" % (q, i))) for i in range(NRING)]
                     for q in ("sp", "act", "pool")}
        self.dcnt = {q: 0 for q in self.ring}
        self.waited = {k: {} for k in self.e}
        self.stack = stack
        self.n_wait = 0

    def sb(self, name, shape, dt=F32, stack=None):
        self.n_alloc = getattr(self, "n_alloc", 0) + 1
        return (stack or self.stack).enter_context(self.nc.sbuf_tensor("%s_%d" % (name, self.n_alloc), list(shape), dt))

    def fence(self):
        for E in self.e:
            for F in self.e:
                if F != E and self.cnt[F] > 0:
                    self._wait(E, ("c", F, self.cnt[F]))
            for q in self.ring:
                n = self.dcnt[q]
                for k in range(max(0, n - NRING), n):
                    self._wait(E, ("d", q, k))

    def ps(self, name, shape, dt=F32):
        return self.stack.enter_context(self.nc.psum_tensor(name, list(shape), dt))

    def _semval(self, ev):
        if ev[0] == "c":
            ep, v = divmod(ev[2] - 1, EPOCH)
            return ("c", ev[1]), self.sem[ev[1]][ep], (ep, v + 1)
        if ev[0] == "x":
            return ("x", ev[1]), self.ccsems[ev[1]], (0, 1)
        _, q, n = ev
        slot = n % NRING
        return ("d", q, slot), self.ring[q][slot], (0, 16 * (n // NRING + 1))

    def _wait(self, eng, ev):
        if ev[0] == "c" and ev[1] == eng:
            if eng == "pe" or not SAME_ENGINE_SYNC:
                return
        key, sem, val = self._semval(ev)
        if self.waited[eng].get(key, (0, 0)) >= val:
            return
        self.waited[eng][key] = val
        self.n_wait += 1
        self.ops[eng].append(lambda h, sem=sem, val=val[1]: h.wait_ge(sem, val))

    def _deps(self, eng, reads, writes):
        deps = []
        for r in reads:
            if r.w is not None:
                deps.append(r.w)
            if r.x:
                for k, ev in r.r.items():
                    if not (k[0] == "c" and k[1] == eng):
                        deps.append(ev)
        for w in writes:
            if w.w is not None:
                deps.append(w.w)
            deps.extend(w.r.values())
        for ev in deps:
            self._wait(eng, ev)

    def _record(self, ev, reads, writes):
        if ev[0] == "c":
            key = ("c", ev[1])
        elif ev[0] == "x":
            key = ("x", ev[1])
        else:
            key = ("d", ev[1], ev[2] % NRING)
        for r in reads:
            r.r[key] = ev
        for w in writes:
            w.w = ev
            w.r = {}

    def op(self, eng, fn, reads=(), writes=()):
        self._deps(eng, reads, writes)
        self.cnt[eng] += 1
        ev = ("c", eng, self.cnt[eng])
        ep = (self.cnt[eng] - 1) // EPOCH
        if ep >= len(self.sem[eng]):
            self.sem[eng].append(self.stack.enter_context(self.nc.semaphore("s_%s%d" % (eng, ep))))
        sem = self.sem[eng][ep]
        self.ops[eng].append(lambda h, fn=fn, sem=sem: fn(h).then_inc(sem, 1))
        self._record(ev, reads, writes)

    def dma(self, q, out, in_, reads=(), writes=(), **kw):
        self._deps(q, reads, writes)
        n = self.dcnt[q]
        self.dcnt[q] += 1
        if n >= NRING:
            self._wait(q, ("d", q, n - NRING))
        sem = self.ring[q][n % NRING]
        self.ops[q].append(lambda h, out=out, in_=in_, sem=sem, kw=kw: h.dma_start(out=out, in_=in_, **kw).then_inc(sem, 16))
        ev = ("d", q, n)
        self._record(ev, reads, writes)
        return ev

    def coll(self, kind, op, groups, in_ap, out_ap, reads=(), writes=()):
        q = "pool"
        self._deps(q, reads, writes)
        if not hasattr(self, "ccsems"):
            self.ccsems = []
        sem = self.stack.enter_context(self.nc.semaphore("cc%d" % len(self.ccsems)))
        self.ccsems.append(sem)
        self.ops[q].append(lambda h, sem=sem: h.collective_compute(kind, op, replica_groups=groups, ins=[in_ap.opt()], outs=[out_ap.opt()]).then_inc(sem))
        ev = ("x", len(self.ccsems) - 1, 0)
        self._record(ev, reads, writes)
        return ev

    def finish(self, final_res):
        for r in final_res:
            if r.w is not None:
                self._wait("sp", r.w)
        for q in self.ring:
            n = self.dcnt[q]
            for k in range(max(0, n - NRING), n):
                self._wait("sp", ("d", q, k))
        with self.nc.Block() as block:
            @block.tensor
            def _(h):
                for f in self.ops["pe"]:
                    f(h)

            @block.scalar
            def _(h):
                for f in self.ops["act"]:
                    f(h)

            @block.vector
            def _(h):
                for f in self.ops["dve"]:
                    f(h)

            @block.gpsimd
            def _(h):
                for f in self.ops["pool"]:
                    f(h)

            @block.sync
            def _(h):
                for f in self.ops["sp"]:
                    f(h)

    def mm(self, out, lhsT, rhs, start=True, stop=True, reads=(), writes=(), **kw):
        self.op("pe", lambda h: h.matmul(out, lhsT, rhs, start=start, stop=stop, **kw), reads, writes)

    def act(self, out, in_, func, reads=(), writes=(), **kw):
        self.op("act", lambda h: h.activation(out=out, in_=in_, func=func, **kw), reads, writes)


D = 1024
NB = 66
NCH = 132


def emit_globals(P, IN):
    G = {}
    ID = P.sb("ID", [128, 128]); rID = Res(); P.dma("sp", ID[:], IN["ident"], writes=[rID])
    ONES = P.sb("ONES", [128, 128]); rONES = Res()
    P.op("dve", lambda h: h.memset(ONES[:], 1.0), writes=[rONES])
    ONESB = P.sb("ONESB", [1, 128], BF16); rONESB = Res()
    P.op("dve", lambda h: h.memset(ONESB[:], 1.0), writes=[rONESB])
    PS = [P.ps("PS%d" % i, [128, 512]) for i in range(8)]; rPS = [Res("ps%d" % i, x=True) for i in range(8)]
    G.update(ID=ID, rID=rID, ONES=ONES, rONES=rONES, ONESB=ONESB, rONESB=rONESB, PS=PS, rPS=rPS)
    return G


def emit_common(P, G, IN, l, stk):
    cvT = IN["cvT"]; adaw = IN["adaw1", l]; adabT = IN["adab1T", l]; g1T = IN["g1T", l]
    C = dict(G)
    PS, rPS = G["PS"], G["rPS"]
    MODF = P.sb("MODF", [128, 16, 2], stack=stk); rMODF = Res()
    SCALE1 = P.sb("SCALE1", [128, 8, 2], stack=stk); rSCALE1 = Res()
    G1 = P.sb("G1", [128, 8], stack=stk); rG1 = Res(); P.dma("sp", G1[:], g1T, writes=[rG1])
    ABT = P.sb("ABT", [128, 16], stack=stk); rABT = Res(); P.dma("sp", ABT[:], adabT, writes=[rABT])
    sub = ExitStack()
    CV = P.sb("CV", [128, 8, 2], stack=sub); rCV = Res(); P.dma("sp", CV[:], cvT, writes=[rCV])
    SCV = P.sb("SCV", [128, 8, 2], stack=sub); rSCV = Res()
    P.act(SCV[:], CV[:], AF.Silu, reads=[rCV], writes=[rSCV])
    AW = [P.sb("AW%d" % i, [128, 8, 512], stack=sub) for i in range(2)]; rAW = [Res(), Res()]
    for blk in range(4):
        aw, raw = AW[blk % 2], rAW[blk % 2]
        P.dma("sp", aw[:], adaw[:, blk * 512:(blk + 1) * 512].rearrange("(k p) f -> p k f", p=128), writes=[raw])
        ps, rps = PS[blk % 2], rPS[blk % 2]
        for fcl in range(4):
            for k in range(8):
                P.mm(ps[:, fcl * 2:fcl * 2 + 2], aw[:, k, fcl * 128:(fcl + 1) * 128], SCV[:, k, :],
                     start=(k == 0), stop=(k == 7), reads=[rSCV, raw], writes=[rps])
        for fcl in range(4):
            fcg = blk * 4 + fcl
            P.act(MODF[:, fcg, :], ps[:, fcl * 2:fcl * 2 + 2], AF.Identity, reads=[rps, rABT], writes=[rMODF],
                  bias=ABT[:, fcg:fcg + 1], scale=1.0)
    P.op("dve", lambda h: h.tensor_scalar(out=SCALE1[:], in0=MODF[:, 8:16, :], scalar1=1.0, scalar2=None, op0=ALU.add),
         reads=[rMODF], writes=[rSCALE1])
    P.op("dve", lambda h: h.tensor_tensor(out=SCALE1[:], in0=SCALE1[:], in1=G1[:].unsqueeze(2).to_broadcast([128, 8, 2]), op=ALU.mult),
         reads=[rSCALE1, rG1], writes=[rSCALE1])
    P.fence()
    sub.close()
    C.update(MODF=MODF, rMODF=rMODF, SCALE1=SCALE1, rSCALE1=rSCALE1)
    return C


def emit_frontend(P, C, xsrc, rsrc, consume, blocks, stk, tbanks=(0, 1)):
    PS, rPS = C["PS"], C["rPS"]
    XT = [P.sb("XT%d" % i, [128, D], stack=stk) for i in range(2)]; rXT = [Res(), Res()]
    XN = P.sb("XN", [128, D], stack=stk); rXN = Res()
    HX = [P.sb("HX%d" % i, [128, 8, 128], BF16, stack=stk) for i in range(2)]; rHX = [Res(), Res()]
    SM = P.sb("FSM", [128, 8], stack=stk); rSM = Res()
    for idx, n in enumerate(blocks):
        j = 1 if n < 2 else 0
        xt, rxt = XT[idx % 2], rXT[idx % 2]
        hx, rhx = HX[idx % 2], rHX[idx % 2]
        for (p0, np_, src) in xsrc(n):
            P.dma("sp", xt[p0:p0 + np_, :], src, reads=rsrc, writes=[rxt])
        P.op("dve", lambda h: h.memset(SM[:, 0:1], 0.0), writes=[rSM])
        P.act(XN[:], xt[:], AF.Square, reads=[rxt], writes=[rXN, rSM], accum_out=SM[:, 0:1])
        P.act(SM[:, 1:2], SM[:, 0:1], AF.Ln, reads=[rSM], writes=[rSM], bias=1e-6, scale=1.0 / D)
        P.act(SM[:, 2:3], SM[:, 1:2], AF.Exp, reads=[rSM], writes=[rSM], scale=-0.5)
        P.op("dve", lambda h, xt=xt: h.tensor_scalar(out=XN[:], in0=xt[:], scalar1=SM[:, 2:3], scalar2=None, op0=ALU.mult),
             reads=[rxt, rSM], writes=[rXN])
        for half in range(2):
            ps, rps = PS[tbanks[half]], rPS[tbanks[half]]
            for k in range(4 * half, 4 * half + 4):
                P.op("pe", lambda h, ps=ps, k=k: h.transpose(ps[:, (k % 4) * 128:(k % 4 + 1) * 128], XN[:, k * 128:(k + 1) * 128], C["ID"][:]),
                     reads=[rXN, C["rID"]], writes=[rps])
            for k in range(4 * half, 4 * half + 4):
                P.act(hx[:, k, :], ps[:, (k % 4) * 128:(k % 4 + 1) * 128], AF.Identity, reads=[rps, C["rSCALE1"], C["rMODF"]], writes=[rhx],
                      scale=C["SCALE1"][:, k, j:j + 1], bias=C["MODF"][:, k, j:j + 1])
        consume(n, hx, rhx)


def emit_swa(P, C, IN, l, last, xsrc, rsrc, o_sw, rOUT):
    ws = IN["ws", l]; gqk = IN["gqk", l]; ropet = IN["ropet"]; sinkb = IN["sinkb", l]; maskw = IN["maskw"]
    with ExitStack() as st0:
        _sb = P.sb
        P_sb = lambda name, shape, dt=F32: _sb("sw_" + name, shape, dt, stack=st0)
        PS, rPS, ID, rID = C["PS"], C["rPS"], C["ID"], C["rID"]
        WS = P_sb("WS", [128, 8, 256], BF16); rWS = Res()
        P.dma("pool", WS[:], ws.rearrange("(k p) f -> p k f", p=128), writes=[rWS])
        GQK = P_sb("GQK", [128, 3, 64]); rGQK = Res(); P.dma("sp", GQK[:], gqk, writes=[rGQK])
        ROPE = P_sb("ROPE", [128, 64, 2, 2, 16]); rROPE = Res(); P.dma("sp", ROPE[:], ropet, writes=[rROPE])
        SINK = P_sb("SINK", [128, 2]); rSINK = Res(); P.dma("sp", SINK[:], sinkb, writes=[rSINK])
        MASKW = P_sb("MASKW", [128, 384]); rMASKW = Res(); P.dma("sp", MASKW[:], maskw, writes=[rMASKW])
        SQT = P_sb("SQT", [128, NB * 128], BF16); rSQT = [Res() for _ in range(NB)]
        SKT = P_sb("SKT", [128, NB * 128], BF16); rSKT = [Res() for _ in range(NB)]
        SV = P_sb("SV", [128, NB, 64], BF16); rSV = [Res() for _ in range(NB)]
        QK = P_sb("QK", [128, 3, 64]); rQK = Res()
        QKR = P_sb("QKR", [128, 4, 64]); rQKR = Res()
        SQ = P_sb("SQ", [128, 3, 64]); rSQ = Res()
        T1 = P_sb("T1", [128, 3, 2, 16]); rT1 = Res()
        T2 = P_sb("T2", [128, 3, 2, 16]); rT2 = Res()
        SM = P_sb("SM", [128, 16]); rSM = Res()
        S = P_sb("S", [128, 640]); rS = Res()
        E = P_sb("E", [128, 640]); rE = Res()
        ET = P_sb("ET", [128, 5, 128], BF16); rET = Res()
        OSW = [P_sb("OSW%d" % i, [128, 128]) for i in range(2)]; rOSW = [Res(), Res()]

        HL = []
        for h in range(2):
            Ld = dict(S=P_sb("S%d" % h, [128, 640]), rS=Res(), E=P_sb("E%d" % h, [128, 640]), rE=Res(),
                      ET=P_sb("ET%d" % h, [128, 5, 128], BF16), rET=Res(), SM=P_sb("SMh%d" % h, [128, 8]), rSM=Res(),
                      b0=PS[4 + 2 * h], r0=rPS[4 + 2 * h], b1=PS[5 + 2 * h], r1=rPS[5 + 2 * h])
            HL.append(Ld)

        def att_gen(n, h, osw, rosw):
            Ld = HL[h]
            S_, rS_, E_, rE_, ET_, rET_, SM_, rSM_ = Ld["S"], Ld["rS"], Ld["E"], Ld["rE"], Ld["ET"], Ld["rET"], Ld["SM"], Ld["rSM"]
            b0, r0, b1, r1 = Ld["b0"], Ld["r0"], Ld["b1"], Ld["r1"]
            if n >= 2:
                lo, hi = max(2, n - 1), min(NB - 1, n + 1)
                nl = (hi - lo + 1) * 128
                m0 = (lo - (n - 1)) * 128
            else:
                lo, hi, nl, m0 = 0, -1, 0, 0
            ntot = nl + 256
            kblocks = list(range(lo, hi + 1)) + [0, 1]
            hs = slice(h * 64, (h + 1) * 64)
            if nl:
                P.mm(b0[:, 0:nl], SQT[hs, n * 128:(n + 1) * 128], SKT[hs, lo * 128:(hi + 1) * 128],
                     reads=[rSQT[n]] + [rSKT[b] for b in range(lo, hi + 1)], writes=[r0])
            P.mm(b1[:, 0:256], SQT[hs, n * 128:(n + 1) * 128], SKT[hs, 0:256], reads=[rSQT[n], rSKT[0], rSKT[1]], writes=[r1])
            yield
            if nl:
                P.op("dve", lambda hh: hh.scalar_tensor_tensor(out=S_[:, 0:nl], in0=b0[:, 0:nl], scalar=0.125,
                                                              in1=MASKW[:, m0:m0 + nl], op0=ALU.mult, op1=ALU.add),
                     reads=[r0, rMASKW], writes=[rS_])
            P.act(S_[:, nl:ntot], b1[:, 0:256], AF.Copy, reads=[r1], writes=[rS_], scale=0.125)
            yield
            P.op("dve", lambda hh: hh.reduce_max(out=SM_[:, 0:1], in_=S_[:, 0:ntot], axis=AX.X), reads=[rS_], writes=[rSM_])
            yield
            P.op("dve", lambda hh: hh.tensor_tensor(out=SM_[:, 1:2], in0=SM_[:, 0:1], in1=SINK[:, h:h + 1], op=ALU.max),
                 reads=[rSM_, rSINK], writes=[rSM_])
            yield
            P.op("dve", lambda hh: hh.tensor_scalar(out=SM_[:, 2:3], in0=SM_[:, 1:2], scalar1=-1.0, scalar2=None, op0=ALU.mult),
                 reads=[rSM_], writes=[rSM_])
            P.op("dve", lambda hh: hh.memset(SM_[:, 3:4], 0.0), writes=[rSM_])
            yield
            P.act(E_[:, 0:ntot], S_[:, 0:ntot], AF.Exp, reads=[rS_, rSM_], writes=[rE_, rSM_], bias=SM_[:, 2:3], scale=1.0, accum_out=SM_[:, 3:4])
            P.act(SM_[:, 4:5], SINK[:, h:h + 1], AF.Exp, reads=[rSINK, rSM_], writes=[rSM_], bias=SM_[:, 2:3], scale=1.0)
            yield
            P.op("dve", lambda hh: hh.tensor_tensor(out=SM_[:, 5:6], in0=SM_[:, 3:4], in1=SM_[:, 4:5], op=ALU.add), reads=[rSM_], writes=[rSM_])
            nk = ntot // 128
            n4 = min(nk, 4)
            for c in range(n4):
                P.op("pe", lambda hh, c=c: hh.transpose(b1[:, c * 128:(c + 1) * 128], E_[:, c * 128:(c + 1) * 128], ID[:]),
                     reads=[rE_, rID], writes=[r1])
            if nk > 4:
                P.op("pe", lambda hh: hh.transpose(b0[:, 384:512], E_[:, 512:640], ID[:]), reads=[rE_, rID], writes=[r0])
            yield
            P.op("dve", lambda hh: hh.reciprocal(out=SM_[:, 6:7], in_=SM_[:, 5:6]), reads=[rSM_], writes=[rSM_])
            P.act(ET_[:, 0:n4, :], b1[:, 0:n4 * 128], AF.Copy, reads=[r1], writes=[rET_])
            if nk > 4:
                P.act(ET_[:, 4, :], b0[:, 384:512], AF.Copy, reads=[r0], writes=[rET_])
            yield
            for c in range(nk):
                kb = kblocks[c]
                P.mm(b1[:, 0:64], ET_[:, c, :], SV[:, kb, :], start=(c == 0), stop=(c == nk - 1), reads=[rET_, rSV[kb]], writes=[r1])
            yield
            P.op("dve", lambda hh: hh.tensor_scalar(out=osw[:, h * 64:(h + 1) * 64], in0=b1[:, 0:64], scalar1=SM_[:, 6:7], scalar2=None, op0=ALU.mult),
                 reads=[r1, rSM_], writes=[rosw])
            yield

        BL = []
        for li in range(2):
            F = dict(XT=P_sb("XT%d" % li, [128, D]), rXT=Res(), XN=P_sb("XN%d" % li, [128, D]), rXN=Res(),
                     HX=P_sb("HX%d" % li, [128, 8, 128], BF16), rHX=Res(), SM=P_sb("BSM%d" % li, [128, 16]), rSM=Res(),
                     QK=P_sb("QK%d" % li, [128, 3, 64]), rQK=Res(), QKR=P_sb("QKR%d" % li, [128, 4, 64]), rQKR=Res(),
                     SQ=P_sb("SQ%d" % li, [128, 3, 64]), rSQ=Res(), T1=P_sb("T1%d" % li, [128, 3, 2, 16]), rT1=Res(),
                     T2=P_sb("T2%d" % li, [128, 3, 2, 16]), rT2=Res(),
                     bT=PS[2 * li], rT=rPS[2 * li], bA=PS[2 * li + 1], rA=rPS[2 * li + 1])
            BL.append(F)

        def blk_gen(n, F):
            j = 1 if n < 2 else 0
            xt, rxt, XN_, rXN_, hx, rhx, SMb, rSMb = F["XT"], F["rXT"], F["XN"], F["rXN"], F["HX"], F["rHX"], F["SM"], F["rSM"]
            QK_, rQK_, QKR_, rQKR_, SQ_, rSQ_, T1_, rT1_, T2_, rT2_ = F["QK"], F["rQK"], F["QKR"], F["rQKR"], F["SQ"], F["rSQ"], F["T1"], F["rT1"], F["T2"], F["rT2"]
            for (p0, np_, src) in xsrc(n):
                P.dma("sp", xt[p0:p0 + np_, :], src, reads=rsrc, writes=[rxt])
            P.op("dve", lambda h: h.memset(SMb[:, 0:1], 0.0), writes=[rSMb])
            yield
            P.act(XN_[:], xt[:], AF.Square, reads=[rxt], writes=[rXN_, rSMb], accum_out=SMb[:, 0:1])
            yield
            P.act(SMb[:, 1:2], SMb[:, 0:1], AF.Ln, reads=[rSMb], writes=[rSMb], bias=1e-6, scale=1.0 / D)
            yield
            P.act(SMb[:, 2:3], SMb[:, 1:2], AF.Exp, reads=[rSMb], writes=[rSMb], scale=-0.5)
            yield
            P.op("dve", lambda h: h.tensor_scalar(out=XN_[:], in0=xt[:], scalar1=SMb[:, 2:3], scalar2=None, op0=ALU.mult),
                 reads=[rxt, rSMb], writes=[rXN_])
            yield
            ps, rps = F["bT"], F["rT"]
            for half in range(2):
                for k in range(4 * half, 4 * half + 4):
                    P.op("pe", lambda h, k=k, ps=ps: h.transpose(ps[:, (k % 4) * 128:(k % 4 + 1) * 128], XN_[:, k * 128:(k + 1) * 128], ID[:]),
                         reads=[rXN_, rID], writes=[rps])
                yield
                for k in range(4 * half, 4 * half + 4):
                    P.act(hx[:, k, :], ps[:, (k % 4) * 128:(k % 4 + 1) * 128], AF.Identity, reads=[rps, C["rSCALE1"], C["rMODF"]], writes=[rhx],
                          scale=C["SCALE1"][:, k, j:j + 1], bias=C["MODF"][:, k, j:j + 1])
                yield
            pa, rpa = F["bA"], F["rA"]
            for k in range(8):
                P.mm(pa[:, 0:256], hx[:, k, :], WS[:, k, :], start=(k == 0), stop=(k == 7), reads=[rhx, rWS], writes=[rpa])
            yield
            P.act(QK_[:], pa[:, 0:192], AF.Copy, reads=[rpa], writes=[rQK_])
            P.act(SV[:, n, :], pa[:, 192:256], AF.Copy, reads=[rpa], writes=[rSV[n]])
            yield
            P.op("dve", lambda h: h.tensor_tensor(out=SQ_[:], in0=QK_[:], in1=QK_[:], op=ALU.mult), reads=[rQK_], writes=[rSQ_])
            yield
            P.op("dve", lambda h: h.reduce_sum(out=SMb[:, 8:11], in_=SQ_[:], axis=AX.X), reads=[rSQ_], writes=[rSMb])
            yield
            P.act(SMb[:, 11:14], SMb[:, 8:11], AF.Ln, reads=[rSMb], writes=[rSMb], bias=1e-6, scale=1.0 / 64)
            yield
            P.act(SMb[:, 8:11], SMb[:, 11:14], AF.Exp, reads=[rSMb], writes=[rSMb], scale=-0.5)
            yield
            P.op("dve", lambda h: h.tensor_tensor(out=QK_[:], in0=QK_[:], in1=SMb[:, 8:11].unsqueeze(2).to_broadcast([128, 3, 64]), op=ALU.mult),
                 reads=[rQK_, rSMb], writes=[rQK_])
            yield
            P.op("dve", lambda h: h.tensor_tensor(out=QK_[:], in0=QK_[:], in1=GQK[:], op=ALU.mult), reads=[rQK_, rGQK], writes=[rQK_])
            yield
            if n >= 2:
                bi = n - 2
                q5 = QK_[:].rearrange("p s (a t f) -> p s a t f", a=2, t=2)
                o5 = QKR_[:, 0:3, :].rearrange("p s (a t f) -> p s a t f", a=2, t=2)
                X1, X2 = q5[:, :, :, 0, :], q5[:, :, :, 1, :]
                Cc = ROPE[:, bi, 0, :, :].unsqueeze(1).to_broadcast([128, 3, 2, 16])
                Sn = ROPE[:, bi, 1, :, :].unsqueeze(1).to_broadcast([128, 3, 2, 16])
                tt = lambda out, a, b, op, reads, writes: P.op("dve", lambda h: h.tensor_tensor(out=out, in0=a, in1=b, op=op), reads=reads, writes=writes)
                tt(T1_[:], X1, Cc, ALU.mult, [rQK_, rROPE], [rT1_])
                tt(T2_[:], X2, Sn, ALU.mult, [rQK_, rROPE], [rT2_])
                yield
                tt(o5[:, :, :, 0, :], T1_[:], T2_[:], ALU.subtract, [rT1_, rT2_], [rQKR_])
                yield
                tt(T1_[:], X2, Cc, ALU.mult, [rQK_, rROPE], [rT1_])
                tt(T2_[:], X1, Sn, ALU.mult, [rQK_, rROPE], [rT2_])
                yield
                tt(o5[:, :, :, 1, :], T1_[:], T2_[:], ALU.add, [rT1_, rT2_], [rQKR_])
                yield
            else:
                P.op("dve", lambda h: h.tensor_copy(out=QKR_[:, 0:3, :], in_=QK_[:]), reads=[rQK_], writes=[rQKR_])
                yield
            P.op("dve", lambda h: h.tensor_copy(out=QKR_[:, 3, :], in_=QKR_[:, 2, :]), reads=[rQKR_], writes=[rQKR_])
            yield
            P.op("pe", lambda h: h.transpose(pa[:, 256:384], QKR_[:, 0:2, :].rearrange("p a f -> p (a f)"), ID[:]), reads=[rQKR_, rID], writes=[rpa])
            P.op("pe", lambda h: h.transpose(pa[:, 384:512], QKR_[:, 2:4, :].rearrange("p a f -> p (a f)"), ID[:]), reads=[rQKR_, rID], writes=[rpa])
            yield
            P.act(SQT[:, n * 128:(n + 1) * 128], pa[:, 256:384], AF.Copy, reads=[rpa], writes=[rSQT[n]])
            P.act(SKT[:, n * 128:(n + 1) * 128], pa[:, 384:512], AF.Copy, reads=[rpa], writes=[rSKT[n]])
            yield

        qblocks = ([] if last else [0, 1]) + list(range(2, NB))
        done_blk = set()
        blk_active = {}
        blk_lane = {}
        free_bl = [0, 1]
        att_active = None
        nxt = 0
        qi = 0
        while nxt < NB or blk_active or att_active is not None or qi < len(qblocks):
            while free_bl and nxt < NB:
                li_ = free_bl.pop(0)
                blk_lane[nxt] = li_
                blk_active[nxt] = blk_gen(nxt, BL[li_]); nxt += 1
            if att_active is None and qi < len(qblocks):
                m = qblocks[qi]
                need = [b for b in (0, 1, m - 1, m, m + 1) if 0 <= b < NB and (b < 2 or b >= 2)]
                if m < 2:
                    need = [0, 1]
                if all(b in done_blk for b in need):
                    osw, rosw = OSW[m % 2], rOSW[m % 2]
                    att_active = (m, [att_gen(m, 0, osw, rosw), att_gen(m, 1, osw, rosw)], [True, True])
                    qi += 1
            progressed = False
            for nb_ in list(blk_active):
                try:
                    next(blk_active[nb_]); progressed = True
                except StopIteration:
                    del blk_active[nb_]; done_blk.add(nb_); progressed = True
                    free_bl.append(blk_lane.pop(nb_))
            if att_active is not None:
                m, gs, alive = att_active
                for i in range(2):
                    if alive[i]:
                        try:
                            next(gs[i]); progressed = True
                        except StopIteration:
                            alive[i] = False; progressed = True
                if not any(alive):
                    P.dma("sp", o_sw[m * 128:(m + 1) * 128, :], OSW[m % 2][:], reads=[rOSW[m % 2]], writes=[rOUT[m]])
                    att_active = None
            assert progressed or att_active is None
        P.fence()


def emit_dn(P, C, IN, l, last, xsrc, rsrc, o_dn, rOUT):
    wa = IN["wa", l]; wz = IN["wz", l]; cw = IN["cw", l]; nega = IN["nega", l]; dtb = IN["dtb", l]; gdn = IN["gdn", l]
    blk1 = IN["blk1"]; masks = IN["masks"]
    NS = NCH + 1
    with ExitStack() as st0:
        _sb = P.sb
        P_sb = lambda name, shape, dt=F32: _sb("dn_" + name, shape, dt, stack=st0)
        PS, rPS, ID, rID, ONES, rONES = C["PS"], C["rPS"], C["ID"], C["rID"], C["ONES"], C["rONES"]
        WA = P_sb("WA", [128, 8, 384], BF16); rWA = Res(); P.dma("pool", WA[:], wa.rearrange("(k p) f -> p k f", p=128), writes=[rWA])
        WZ = P_sb("WZ", [128, 8, 2, 68], BF16); rWZ = Res(); P.dma("pool", WZ[:], wz.rearrange("(k p) h f -> p k h f", p=128), writes=[rWZ])
        CW = P_sb("CW", [128, 3, 5]); rCW = Res(); P.dma("sp", CW[:], cw, writes=[rCW])
        NEGA = P_sb("NEGA", [128, 2]); rNEGA = Res(); P.dma("sp", NEGA[:], nega, writes=[rNEGA])
        DTB = P_sb("DTB", [128, 2]); rDTB = Res(); P.dma("sp", DTB[:], dtb, writes=[rDTB])
        GDN = P_sb("GDN", [128, 64]); rGDN = Res(); P.dma("sp", GDN[:], gdn, writes=[rGDN])
        BLK = P_sb("BLK", [128, 128]); rBLK = Res(); P.dma("sp", BLK[:], blk1, writes=[rBLK])
        MSK = P_sb("MSK", [128, 6, 2, 64]); rMSK = Res(); P.dma("sp", MSK[:], masks, writes=[rMSK])
        TRI, NEGMT, NEGM, NSTT, NST, ID2 = [MSK[:, i, :, :] for i in range(6)]
        ONES3 = P_sb("ONES3", [128, 2, 64]); rONES3 = Res()
        P.op("dve", lambda h: h.memset(ONES3[:], 1.0), writes=[rONES3])
        QT = P_sb("QT", [128, NCH * 64], BF16); KT = P_sb("KT", [128, NCH * 64], BF16)
        KTM = P_sb("KTM", [128, NCH, 64], BF16); VTM = P_sb("VTM", [128, NCH, 64], BF16)
        SZ = P_sb("SZ", [128, NCH, 64], BF16)
        Gs = P_sb("Gs", [128, NS, 2]); Bs = P_sb("Bs", [128, NS, 2])
        O = P_sb("O", [128, NCH, 64])
        rCH = [Res() for _ in range(NCH)]
        rO = [Res() for _ in range(NCH)]
        P.op("dve", lambda h: h.memset(O[:], 0.0), writes=rO)
        NLF = int(os.environ.get("DNLANES", "4"))
        NCB = NLF + 2
        fst = ExitStack()
        P_sbf = lambda name, shape, dt=F32: _sb("dnf_" + name, shape, dt, stack=fst)
        CBL = [P_sbf("CBL%d" % i, [128, 3, 132]) for i in range(NCB)]; rCBL = [Res() for _ in range(NCB)]
        for i in range(NCB):
            P.op("dve", lambda h, i=i: h.memset(CBL[i][:], 0.0), writes=[rCBL[i]])

        def step_of(c, d):
            if d == 0:
                return c
            return 4 - c if c < 4 else 136 - c

        FL = []
        for li in range(NLF):
            F = dict(XT=P_sbf("XT%d" % li, [128, D]), rXT=Res(), XN=P_sbf("XN%d" % li, [128, D]), rXN=Res(),
                     HX=P_sbf("HX%d" % li, [128, 8, 128], BF16), rHX=Res(), SM=P_sbf("FSM%d" % li, [128, 8]), rSM=Res(),
                     CVb=P_sbf("CVb%d" % li, [128, 3, 128]), rCVb=Res(), SQ2=P_sbf("SQ2%d" % li, [128, 2, 128]), rSQ2=Res(),
                     RS2=P_sbf("RS2%d" % li, [128, 2, 128]), rRS2=Res(), KN=P_sbf("KN%d" % li, [128, 128]), rKN=Res(),
                     GT_=P_sbf("GT_%d" % li, [128, 2, 2, 8]), rGT_=Res(), EXb=P_sbf("EXb%d" % li, [128, 3, 128]), rEXb=Res(),
                     EZ=P_sbf("EZ%d" % li, [128, 2, 64]), rEZ=Res(),
                     bT=PS[2 * li], rT=rPS[2 * li], bA=PS[2 * li + 1], rA=rPS[2 * li + 1], bB=PS[2 * li + 1], rB=rPS[2 * li + 1],
                     bZ=PS[2 * li], rZ=rPS[2 * li])
            FL.append(F)

        def conv_gen(m, F):
            CVb, rCVb, SQ2, rSQ2, RS2, rRS2, KN, rKN = F["CVb"], F["rCVb"], F["SQ2"], F["rSQ2"], F["RS2"], F["rRS2"], F["KN"], F["rKN"]
            CB, rCB = CBL[m % NCB], rCBL[m % NCB]
            rcv = [Res(), Res(), Res()]
            for s_ in range(3):
                P.op("dve", lambda h, s_=s_: h.tensor_scalar(out=CVb[:, s_, :], in0=CB[:, s_, 0:128], scalar1=CW[:, s_, 0:1], scalar2=None, op0=ALU.mult),
                     reads=[rCB, rCW], writes=[rcv[s_], rCVb])
            yield
            for tap in range(1, 5):
                for s_ in range(3):
                    P.op("dve", lambda h, s_=s_, tap=tap: h.scalar_tensor_tensor(out=CVb[:, s_, :], in0=CB[:, s_, tap:tap + 128], scalar=CW[:, s_, tap:tap + 1],
                                                                              in1=CVb[:, s_, :], op0=ALU.mult, op1=ALU.add),
                         reads=[rCB, rCW, rcv[s_]], writes=[rcv[s_]] + ([rCVb] if tap == 4 else []))
                yield
            EXb, rEXb = F["EXb"], F["rEXb"]
            P.act(EXb[:], CVb[:], AF.Exp, reads=[rCVb], writes=[rEXb], scale=-1.0)
            yield
            P.op("dve", lambda h: h.tensor_scalar(out=EXb[:], in0=EXb[:], scalar1=1.0, scalar2=None, op0=ALU.add), reads=[rEXb], writes=[rEXb])
            yield
            P.op("dve", lambda h: h.reciprocal(out=EXb[:], in_=EXb[:]), reads=[rEXb], writes=[rEXb])
            yield
            P.op("dve", lambda h: h.tensor_tensor(out=CVb[:], in0=CVb[:], in1=EXb[:], op=ALU.mult), reads=[rCVb, rEXb], writes=[rCVb])
            yield
            P.op("dve", lambda h: h.tensor_tensor(out=SQ2[:], in0=CVb[:, 0:2, :], in1=CVb[:, 0:2, :], op=ALU.mult), reads=[rCVb], writes=[rSQ2])
            yield
            ps, rps = F["bB"], F["rB"]
            P.mm(ps[:, 0:256], BLK[:], SQ2[:].rearrange("p a t -> p (a t)"), reads=[rBLK, rSQ2], writes=[rps])
            yield
            P.act(RS2[:].rearrange("p a t -> p (a t)"), ps[:, 0:256], AF.Ln, reads=[rps], writes=[rRS2], bias=1e-6, scale=1.0)
            yield
            P.act(RS2[:], RS2[:], AF.Exp, reads=[rRS2], writes=[rRS2], scale=-0.5)
            yield
            rc = [rCH[2 * m], rCH[2 * m + 1]]
            P.op("dve", lambda h: h.scalar_tensor_tensor(out=QT[:, m * 128:(m + 1) * 128], in0=CVb[:, 0, :], scalar=0.125, in1=RS2[:, 0, :], op0=ALU.mult, op1=ALU.mult),
                 reads=[rCVb, rRS2], writes=rc)
            P.op("dve", lambda h: h.tensor_tensor(out=KN[:], in0=CVb[:, 1, :], in1=RS2[:, 1, :], op=ALU.mult), reads=[rCVb, rRS2], writes=[rKN])
            yield
            P.op("dve", lambda h: h.tensor_copy(out=KT[:, m * 128:(m + 1) * 128], in_=KN[:]), reads=[rKN], writes=rc)
            for which in range(2):
                for cc in range(2):
                    for hh in range(2):
                        hs = slice(hh * 64, (hh + 1) * 64)
                        srcap = KN[hs, cc * 64:(cc + 1) * 64] if which == 0 else CVb[hs, 2, cc * 64:(cc + 1) * 64]
                        P.op("pe", lambda h, hs=hs, cc=cc, which=which, srcap=srcap: h.matmul(ps[hs, 256 + which * 128 + cc * 64: 256 + which * 128 + (cc + 1) * 64], srcap, ID[hs, hs], start=True, stop=True),
                             reads=[rKN, rCVb, rID], writes=[rps])
            yield
            P.act(KTM[:, 2 * m:2 * m + 2, :], ps[:, 256:384].rearrange("p (c f) -> p c f", c=2), AF.Copy, reads=[rps], writes=rc)
            P.act(VTM[:, 2 * m:2 * m + 2, :], ps[:, 384:512].rearrange("p (c f) -> p c f", c=2), AF.Copy, reads=[rps], writes=rc)
            yield

        def blk_gen(n, F):
            j = 1 if n < 2 else 0
            xt, rxt, XN, rXN, hx, rhx, SM, rSM = F["XT"], F["rXT"], F["XN"], F["rXN"], F["HX"], F["rHX"], F["SM"], F["rSM"]
            for (p0, np_, src) in xsrc(n):
                P.dma("sp", xt[p0:p0 + np_, :], src, reads=rsrc, writes=[rxt])
            P.op("dve", lambda h: h.memset(SM[:, 0:1], 0.0), writes=[rSM])
            yield
            P.act(XN[:], xt[:], AF.Square, reads=[rxt], writes=[rXN, rSM], accum_out=SM[:, 0:1])
            yield
            P.act(SM[:, 1:2], SM[:, 0:1], AF.Ln, reads=[rSM], writes=[rSM], bias=1e-6, scale=1.0 / D)
            yield
            P.act(SM[:, 2:3], SM[:, 1:2], AF.Exp, reads=[rSM], writes=[rSM], scale=-0.5)
            yield
            P.op("dve", lambda h: h.tensor_scalar(out=XN[:], in0=xt[:], scalar1=SM[:, 2:3], scalar2=None, op0=ALU.mult),
                 reads=[rxt, rSM], writes=[rXN])
            yield
            ps, rps = F["bT"], F["rT"]
            for half in range(2):
                for k in range(4 * half, 4 * half + 4):
                    P.op("pe", lambda h, k=k, ps=ps: h.transpose(ps[:, (k % 4) * 128:(k % 4 + 1) * 128], XN[:, k * 128:(k + 1) * 128], ID[:]),
                         reads=[rXN, rID], writes=[rps])
                yield
                for k in range(4 * half, 4 * half + 4):
                    P.act(hx[:, k, :], ps[:, (k % 4) * 128:(k % 4 + 1) * 128], AF.Identity, reads=[rps, C["rSCALE1"], C["rMODF"]], writes=[rhx],
                          scale=C["SCALE1"][:, k, j:j + 1], bias=C["MODF"][:, k, j:j + 1])
                yield
            ps, rps = F["bA"], F["rA"]
            for s_ in range(3):
                for k in range(8):
                    P.mm(ps[:, s_ * 128:(s_ + 1) * 128], WA[:, k, s_ * 128:(s_ + 1) * 128], hx[:, k, :], start=(k == 0), stop=(k == 7), reads=[rWA, rhx], writes=[rps])
            yield
            CB, rCB = CBL[n % NCB], rCBL[n % NCB]
            first = n in (0, 2)
            lastb = n in (1, NB - 1)
            P.act(CB[:, :, 2:130], ps[:, 0:384].rearrange("p (s t) -> p s t", s=3), AF.Copy, reads=[rps], writes=[rCB])
            if first:
                P.op("dve", lambda h: h.memset(CB[:, :, 0:2], 0.0), writes=[rCB])
            else:
                Pv, rPv = CBL[(n - 1) % NCB], rCBL[(n - 1) % NCB]
                P.op("dve", lambda h: h.tensor_copy(out=CB[:, :, 0:2], in_=Pv[:, :, 128:130]), reads=[rPv], writes=[rCB])
                P.op("dve", lambda h: h.tensor_copy(out=Pv[:, :, 130:132], in_=CB[:, :, 2:4]), reads=[rCB], writes=[rPv])
            if lastb:
                P.op("dve", lambda h: h.memset(CB[:, :, 130:132], 0.0), writes=[rCB])
            yield
            ps, rps = F["bZ"], F["rZ"]
            for cc in range(2):
                for hh in range(2):
                    hs = slice(hh * 64, (hh + 1) * 64)
                    for k in range(8):
                        P.mm(ps[hs, cc * 68:(cc + 1) * 68], hx[:, k, cc * 64:(cc + 1) * 64], WZ[:, k, hh, :], start=(k == 0), stop=(k == 7),
                             reads=[rhx, rWZ], writes=[rps])
            yield
            rc = [rCH[2 * n], rCH[2 * n + 1]]
            pz = ps[:, 0:136].rearrange("p (c f) -> p c f", c=2)
            EZ, rEZ = F["EZ"], F["rEZ"]
            P.act(EZ[:], pz[:, :, 0:64], AF.Exp, reads=[rps], writes=[rEZ], scale=-1.0)
            P.op("dve", lambda h: h.tensor_scalar(out=EZ[:], in0=EZ[:], scalar1=1.0, scalar2=None, op0=ALU.add), reads=[rEZ], writes=[rEZ])
            yield
            P.op("dve", lambda h: h.reciprocal(out=EZ[:], in_=EZ[:]), reads=[rEZ], writes=[rEZ])
            yield
            P.op("dve", lambda h: h.tensor_tensor(out=SZ[:, 2 * n:2 * n + 2, :], in0=pz[:, :, 0:64], in1=EZ[:], op=ALU.mult), reads=[rps, rEZ], writes=rc)
            GT_, rGT_ = F["GT_"], F["rGT_"]
            xa = GT_[:, :, :, 0]; ab = GT_[:, :, :, 1]; ee = GT_[:, :, :, 2]; ll = GT_[:, :, :, 3]; rr = GT_[:, :, :, 4]; gg = GT_[:, :, :, 5]; bb = GT_[:, :, :, 6]
            P.op("dve", lambda h: h.tensor_tensor(out=xa, in0=pz[:, :, 64:66], in1=DTB[:].unsqueeze(1).to_broadcast([128, 2, 2]), op=ALU.add), reads=[rps, rDTB], writes=[rGT_])
            yield
            P.act(bb, pz[:, :, 66:68], AF.Exp, reads=[rps], writes=[rGT_], scale=-1.0)
            P.act(ab, xa, AF.Abs, reads=[rGT_], writes=[rGT_])
            yield
            P.op("dve", lambda h: h.tensor_scalar(out=bb, in0=bb, scalar1=1.0, scalar2=None, op0=ALU.add), reads=[rGT_], writes=[rGT_])
            P.op("dve", lambda h: h.reciprocal(out=bb, in_=bb), reads=[rGT_], writes=[rGT_])
            yield
            P.act(ee, ab, AF.Exp, reads=[rGT_], writes=[rGT_], scale=-1.0)
            yield
            P.act(ll, ee, AF.Ln, reads=[rGT_], writes=[rGT_], bias=1.0, scale=1.0)
            P.op("dve", lambda h: h.tensor_scalar(out=rr, in0=xa, scalar1=0.0, scalar2=None, op0=ALU.max), reads=[rGT_], writes=[rGT_])
            yield
            P.op("dve", lambda h: h.tensor_tensor(out=rr, in0=rr, in1=ll, op=ALU.add), reads=[rGT_], writes=[rGT_])
            yield
            P.op("dve", lambda h: h.tensor_tensor(out=gg, in0=rr, in1=NEGA[:].unsqueeze(1).to_broadcast([128, 2, 2]), op=ALU.mult), reads=[rGT_, rNEGA], writes=[rGT_])
            yield
            for cc in range(2):
                c = 2 * n + cc
                for d in range(2):
                    s2 = step_of(c, d)
                    P.op("dve", lambda h, cc=cc, d=d, s2=s2: h.tensor_copy(out=Gs[:, s2, d:d + 1], in_=GT_[:, cc, d, 5:6]), reads=[rGT_], writes=[rCH[c]])
                    P.op("dve", lambda h, cc=cc, d=d, s2=s2: h.tensor_copy(out=Bs[:, s2, d:d + 1], in_=GT_[:, cc, d, 6:7]), reads=[rGT_], writes=[rCH[c]])
                yield
            if not first:
                yield from conv_gen(n - 1, F)
            if lastb:
                yield from conv_gen(n, F)

        active = []
        free_lanes = list(range(NLF))
        nxt = 0
        while nxt < NB or active:
            while free_lanes and nxt < NB:
                li = free_lanes.pop(0)
                active.append((blk_gen(nxt, FL[li]), li)); nxt += 1
            for item in list(active):
                try:
                    next(item[0])
                except StopIteration:
                    active.remove(item)
                    free_lanes.append(item[1])

        P.fence()
        fst.close()
        Sst = [(P_sb("S0", [128, 2, 64]), Res()), (P_sb("S1", [128, 2, 64]), Res())]
        for (t, r) in Sst:
            P.op("dve", lambda h, t=t: h.memset(t[:], 0.0), writes=[r])
        HS = [slice(0, 64), slice(64, 128)]
        dve = lambda fn, reads, writes: P.op("dve", fn, reads, writes)

        def make_lane(li):
            L = {}
            for nm in ("GBC", "BBC", "EB", "DT1", "TA", "DECT", "DEC", "DECS", "DECTS", "VB", "KBE", "KD", "QGT", "AQM", "NWT", "VN", "SGL", "C0", "C1"):
                L[nm] = (P_sb("%s_%d" % (nm, li), [128, 2, 64]), Res())
            L["W0"] = (P_sb("W0_%d" % li, [128, 2, 128]), Res()); L["W1"] = (P_sb("W1_%d" % li, [128, 2, 128]), Res())
            L["SC"] = (P_sb("SC_%d" % li, [128, 8]), Res())
            a, b = PS[2 * li], PS[2 * li + 1]
            v = lambda bank, lo, n: bank[:, lo:lo + 2 * n].rearrange("p (d f) -> p d f", d=2)
            ra, rb = rPS[2 * li], rPS[2 * li + 1]
            L["ps1"] = (v(a, 0, 64), ra); L["ps4"] = (v(a, 128, 64), ra); L["ps2"] = (v(a, 256, 64), ra); L["ps3"] = (v(a, 384, 64), ra)
            L["psI"] = (v(b, 0, 128), rb); L["psC"] = (v(b, 256, 64), rb); L["pcol"] = (b[:, 384:386], rb)
            L["ps5"] = (v(b, 0, 64), rb); L["pswT"] = (v(b, 128, 64), rb); L["ps6"] = (v(b, 256, 64), rb); L["ps7"] = (v(b, 384, 64), rb)
            return L

        NLS = int(os.environ.get("SCLANES", "4"))
        lanes = [make_lane(i) for i in range(NLS)]

        def step_gen(s, L):
            GBC, rGBC = L["GBC"]; BBC, rBBC = L["BBC"]; EB, rEB = L["EB"]; DT1, rDT1 = L["DT1"]; TA, rTA = L["TA"]
            DECT, rDECT = L["DECT"]; DEC, rDEC = L["DEC"]; DECS, rDECS = L["DECS"]; DECTS, rDECTS = L["DECTS"]
            VB, rVB = L["VB"]; KBE, rKBE = L["KBE"]; KD, rKD = L["KD"]; QGT, rQGT = L["QGT"]; AQM, rAQM = L["AQM"]
            NWT, rNWT = L["NWT"]; VN, rVN = L["VN"]; SGL, rSGL = L["SGL"]; SC, rSC = L["SC"]
            Cb = [L["C0"], L["C1"]]; Wb = [L["W0"], L["W1"]]
            ps1, r1 = L["ps1"]; ps4, r4 = L["ps4"]; ps2, r2 = L["ps2"]; ps3, r3 = L["ps3"]
            psI, rI = L["psI"]; psC, rC = L["psC"]; pcol, rcol = L["pcol"]
            ps5, r5 = L["ps5"]; pswT, rwT = L["pswT"]; ps6, r6 = L["ps6"]; ps7, r7 = L["ps7"]
            dirs = [d for d in range(2) if (d == 0 and s <= NCH - 1) or (d == 1 and s >= 1)]
            ch = {0: s, 1: (4 - s if s <= 4 else 136 - s)}
            d0, d1 = dirs[0], dirs[-1] + 1
            ds = slice(d0, d1)
            nd = d1 - d0
            rch = [rCH[ch[d]] for d in dirs]
            Scur, rScur = Sst[s % 2]; Snew, rSnew = Sst[(s + 1) % 2]
            tok = {d: slice(ch[d] * 64, ch[d] * 64 + 64) for d in dirs}
            dve(lambda h: h.tensor_tensor(out=GBC[:, ds, :], in0=ONES3[:, ds, :], in1=Gs[:, s, ds].unsqueeze(2).to_broadcast([128, nd, 64]), op=ALU.mult), [rONES3] + rch, [rGBC])
            dve(lambda h: h.tensor_tensor(out=BBC[:, ds, :], in0=ONES3[:, ds, :], in1=Bs[:, s, ds].unsqueeze(2).to_broadcast([128, nd, 64]), op=ALU.mult), [rONES3] + rch, [rBBC])
            yield
            for d in dirs:
                for hs in HS:
                    P.mm(ps1[hs, d, :], GBC[hs, d, :], TRI[hs, d, :], reads=[rGBC, rMSK], writes=[r1])
            for d in dirs:
                for hs in HS:
                    P.mm(pcol[hs, d:d + 1], TRI[hs, d, :], Gs[hs, s, d:d + 1], reads=[rMSK] + rch, writes=[rcol])
            for d in dirs:
                for hs in HS:
                    P.mm(ps4[hs, d, :], BBC[hs, d, :], ID2[hs, d, :], reads=[rBBC, rMSK], writes=[r4])
            for d in dirs:
                for hs in HS:
                    P.mm(ps2[hs, d, :], KT[hs, tok[d]], KT[hs, tok[d]], reads=rch, writes=[r2])
            for d in dirs:
                for hs in HS:
                    P.mm(ps3[hs, d, :], KT[hs, tok[d]], QT[hs, tok[d]], reads=rch, writes=[r3])
            yield
            dve(lambda h: h.tensor_copy(out=SC[:, 0:2][:, ds], in_=pcol[:, ds]), [rcol], [rSC])
            P.act(EB[:, ds, :], ps1[:, ds, :], AF.Exp, reads=[r1], writes=[rEB])
            yield
            P.act(SC[:, 2:4][:, ds], SC[:, 0:2][:, ds], AF.Exp, reads=[rSC], writes=[rSC])
            dve(lambda h: h.tensor_tensor(out=DT1[:, ds, :], in0=ps1[:, ds, :], in1=SC[:, 0:2][:, ds].unsqueeze(2).to_broadcast([128, nd, 64]), op=ALU.subtract), [r1, rSC], [rDT1])
            yield
            dve(lambda h: h.tensor_tensor(out=TA[:, ds, :], in0=DT1[:, ds, :], in1=NEGMT[:, ds, :], op=ALU.add), [rDT1, rMSK], [rTA])
            yield
            P.act(DECT[:, ds, :], TA[:, ds, :], AF.Exp, reads=[rTA], writes=[rDECT])
            yield
            dve(lambda h: h.scalar_tensor_tensor(out=TA[:, ds, :], in0=DT1[:, ds, :], scalar=-1.0, in1=NEGM[:, ds, :], op0=ALU.mult, op1=ALU.add), [rDT1, rMSK, rDECT], [rTA])
            yield
            P.act(DEC[:, ds, :], TA[:, ds, :], AF.Exp, reads=[rTA], writes=[rDEC])
            for d in dirs:
                lastc = 63 if d == 0 else 0
                dve(lambda h, d=d, lastc=lastc: h.tensor_tensor(out=SC[:, 4 + d:5 + d], in0=ps1[:, d, lastc:lastc + 1], in1=SC[:, d:d + 1], op=ALU.subtract), [r1, rSC], [rSC])
            yield
            P.act(SC[:, 4:6][:, ds], SC[:, 4:6][:, ds], AF.Exp, reads=[rSC], writes=[rSC])
            C0, rC0 = Cb[0]; W0, rW0 = Wb[0]
            dve(lambda h: h.tensor_tensor(out=DECS[:, ds, :], in0=DEC[:, ds, :], in1=NST[:, ds, :], op=ALU.mult), [rDEC, rMSK], [rDECS])
            yield
            for d in dirs:
                dve(lambda h, d=d: h.scalar_tensor_tensor(out=C0[:, d, :], in0=ps2[:, d, :], scalar=Bs[:, s, d:d + 1], in1=DECS[:, d, :], op0=ALU.mult, op1=ALU.mult),
                    [r2, rDECS] + rch, [rC0])
            yield
            dve(lambda h: h.tensor_tensor(out=DECTS[:, ds, :], in0=DECT[:, ds, :], in1=NSTT[:, ds, :], op=ALU.mult), [rDECT, rMSK], [rDECTS])
            yield
            dve(lambda h: h.tensor_tensor(out=DECTS[:, ds, :], in0=ps2[:, ds, :], in1=DECTS[:, ds, :], op=ALU.mult), [r2, rDECTS], [rDECTS])
            yield
            dve(lambda h: h.tensor_tensor(out=W0[:, ds, 0:64], in0=ps4[:, ds, :], in1=DECTS[:, ds, :], op=ALU.mult), [r4, rDECTS], [rW0])
            dve(lambda h: h.tensor_copy(out=W0[:, ds, 64:128], in_=ID2[:, ds, :]), [rMSK], [rW0])
            yield
            for m in range(6):
                (Wc, rWc), (Wn, rWn) = Wb[m % 2], Wb[(m + 1) % 2]
                (Cc, rCc), (Cn, rCn) = Cb[m % 2], Cb[(m + 1) % 2]
                for d in dirs:
                    for hs in HS:
                        P.mm(psI[hs, d, :], Cc[hs, d, :], Wc[hs, d, :], reads=[rCc, rWc], writes=[rI])
                if m < 5:
                    for d in dirs:
                        for hs in HS:
                            P.mm(psC[hs, d, :], Wc[hs, d, 0:64], Cc[hs, d, :], reads=[rCc, rWc], writes=[rC])
                yield
                dve(lambda h, Wn=Wn, Wc=Wc: h.tensor_tensor(out=Wn[:, ds, 64:128], in0=Wc[:, ds, 64:128], in1=psI[:, ds, 64:128], op=ALU.add), [rWc, rI], [rWn])
                if m < 5:
                    P.act(Wn[:, ds, 0:64], psI[:, ds, 0:64], AF.Copy, reads=[rI], writes=[rWn])
                    P.act(Cn[:, ds, :], psC[:, ds, :], AF.Copy, reads=[rC], writes=[rCn])
                yield
            TITt, rTIT = Wb[0]
            TIT = TITt[:, :, 64:128]
            for d in dirs:
                c = ch[d]
                dve(lambda h, d=d, c=c: h.tensor_scalar(out=VB[:, d, :], in0=VTM[:, c, :], scalar1=Bs[:, s, d:d + 1], scalar2=None, op0=ALU.mult), rch, [rVB])
                dve(lambda h, d=d, c=c: h.tensor_scalar(out=KBE[:, d, :], in0=KTM[:, c, :], scalar1=Bs[:, s, d:d + 1], scalar2=SC[:, 2 + d:3 + d], op0=ALU.mult, op1=ALU.mult), rch + [rSC], [rKBE])
                yield
                dve(lambda h, d=d, c=c: h.tensor_scalar(out=KD[:, d, :], in0=KTM[:, c, :], scalar1=SC[:, 4 + d:5 + d], scalar2=None, op0=ALU.mult), rch + [rSC], [rKD])
                dve(lambda h, d=d: h.tensor_tensor(out=QGT[:, d, :], in0=QT[:, tok[d]], in1=EB[:, d, :], op=ALU.mult), rch + [rEB], [rQGT])
                yield
            dve(lambda h: h.tensor_tensor(out=AQM[:, ds, :], in0=ps3[:, ds, :], in1=DECT[:, ds, :], op=ALU.mult), [r3, rDECT], [rAQM])
            for d in dirs:
                for hs in HS:
                    P.mm(pswT[hs, d, :], KBE[hs, d, :], TIT[hs, d, :], reads=[rKBE, rTIT], writes=[rwT])
            yield
            dve(lambda h: h.tensor_scalar(out=NWT[:, ds, :], in0=pswT[:, ds, :], scalar1=-1.0, scalar2=None, op0=ALU.mult), [rwT], [rNWT])
            yield
            yield "REC"
            for d in dirs:
                for hs in HS:
                    P.mm(ps5[hs, d, :], TIT[hs, d, :], VB[hs, d, :], start=True, stop=False, reads=[rTIT, rVB], writes=[r5])
                    P.mm(ps5[hs, d, :], NWT[hs, d, :], Scur[hs, d, :], start=False, stop=True, reads=[rNWT, rScur], writes=[r5])
            yield
            dve(lambda h: h.tensor_copy(out=VN[:, ds, :], in_=ps5[:, ds, :]), [r5], [rVN])
            yield
            for d in dirs:
                for hs in HS:
                    P.mm(ps6[hs, d, :], QGT[hs, d, :], Scur[hs, d, :], start=True, stop=False, reads=[rQGT, rScur], writes=[r6])
                    P.mm(ps6[hs, d, :], AQM[hs, d, :], VN[hs, d, :], start=False, stop=True, reads=[rAQM, rVN], writes=[r6])
            for d in dirs:
                for hs in HS:
                    P.mm(ps7[hs, d, :], KD[hs, d, :], VN[hs, d, :], reads=[rKD, rVN], writes=[r7])
            yield
            for d in range(2):
                if d in dirs:
                    lastc = 63 if d == 0 else 0
                    dve(lambda h, d=d, lastc=lastc: h.tensor_scalar(out=SGL[:, d, :], in0=Scur[:, d, :], scalar1=EB[:, d, lastc:lastc + 1], scalar2=None, op0=ALU.mult), [rScur, rEB], [rSGL])
                    dve(lambda h, d=d: h.tensor_tensor(out=Snew[:, d, :], in0=SGL[:, d, :], in1=ps7[:, d, :], op=ALU.add), [rSGL, r7], [rSnew])
                else:
                    dve(lambda h, d=d: h.tensor_copy(out=Snew[:, d, :], in_=Scur[:, d, :]), [rScur], [rSnew])
            yield
            for d in dirs:
                c = ch[d]
                dve(lambda h, d=d, c=c: h.tensor_tensor(out=O[:, c, :], in0=O[:, c, :], in1=ps6[:, d, :], op=ALU.add), [r6, rO[c]], [rO[c]])
            yield

        STAG = int(os.environ.get("SCSTAG", "3"))
        active = []
        free_l = list(range(NLS))
        finished = set([-1])
        nxt = 0
        since = STAG
        while nxt < NS or active:
            if nxt < NS and free_l and since >= STAG:
                li = free_l.pop(0)
                active.append([step_gen(nxt, lanes[li]), li, nxt, False]); nxt += 1
                since = 0
            since += 1
            for item in list(active):
                if item[3]:
                    if (item[2] - 1) not in finished:
                        continue
                    item[3] = False
                try:
                    if next(item[0]) == "REC":
                        item[3] = True
                except StopIteration:
                    active.remove(item)
                    free_l.append(item[1])
                    finished.add(item[2])

        P.fence()
        G = 33
        OSQ = P_sb("OSQ", [128, G, 64]); rOSQ = Res()
        OSS = P_sb("OSS", [128, G]); rOSS = Res()
        for g in range(NCH // G):
            cs = slice(g * G, (g + 1) * G)
            ro = [rO[c] for c in range(g * G, (g + 1) * G)]
            dve(lambda h, cs=cs: h.tensor_tensor(out=OSQ[:], in0=O[:, cs, :], in1=O[:, cs, :], op=ALU.mult), ro, [rOSQ])
            dve(lambda h: h.reduce_sum(out=OSS[:], in_=OSQ[:], axis=AX.X), [rOSQ], [rOSS])
            P.act(OSS[:], OSS[:], AF.Ln, reads=[rOSS], writes=[rOSS], bias=1e-6, scale=1.0 / 64)
            P.act(OSS[:], OSS[:], AF.Exp, reads=[rOSS], writes=[rOSS], scale=-0.5)
            dve(lambda h, cs=cs: h.tensor_tensor(out=O[:, cs, :], in0=O[:, cs, :], in1=OSS[:].unsqueeze(2).to_broadcast([128, G, 64]), op=ALU.mult), ro + [rOSS], ro)
            dve(lambda h, cs=cs: h.tensor_tensor(out=O[:, cs, :], in0=O[:, cs, :], in1=GDN[:].unsqueeze(1).to_broadcast([128, G, 64]), op=ALU.mult), ro + [rGDN], ro)
            dve(lambda h, cs=cs: h.tensor_tensor(out=O[:, cs, :], in0=O[:, cs, :], in1=SZ[:, cs, :], op=ALU.mult), ro + [rCH[c] for c in range(g * G, (g + 1) * G)], ro)
        ov = o_dn.rearrange("(c t) (h d) -> h t c d", t=64, h=2)
        for hh in range(2):
            P.dma("sp", ov[hh], O[hh * 64:(hh + 1) * 64, :, :], reads=rO, writes=[rOUT[hh]])
        P.fence()


NE = 32


def emit_b(P, G, IN, l, NL, NC, xs, rxs, omall, romall, xo, rOUT):
    NT = NL + NC
    cvT = IN["cvT"]; adaw = IN["adaw2", l]; adab = IN["adab2", l]; adabT = IN["adab2T", l]
    g2T = IN["g2T", l]; wout = IN["wout", l]; rw = IN["rw", l]; rb = IN["rb", l]
    wgu = IN["wgu", l]; bguT = IN["bguT", l]; wd = IN["wd", l]; bd = IN["bd", l]; seli = IN["seli"]

    tiles = [(i * 128, 128, 0) for i in range(NL // 128)]
    if NC:
        tiles.append((NL, NC, 1))
    nlt = NL // 128
    passes = [list(range(0, nlt // 2)), list(range(nlt // 2, len(tiles)))]
    MAXT = max(len(p) for p in passes)

    with ExitStack() as st0:
        _sb = P.sb
        P_sb = lambda name, shape, dt=F32, stack=None: _sb("b_" + name, shape, dt, stack=(stack or st0))
        ID, rID, ONES, rONES, ONESB, rONESB, PS, rPS = G["ID"], G["rID"], G["ONES"], G["rONES"], G["ONESB"], G["rONESB"], G["PS"], G["rPS"]
        SELI = P_sb("SELI", [128, 4, 128], BF16); rSELI = Res()
        P.dma("pool", SELI[:], seli, writes=[rSELI])

        GT = P_sb("GT", [128, 2, 2, D]); rGT = Res()
        MODF = P_sb("MODF", [128, 16, 2]); rMODF = Res()
        SCALE2 = P_sb("SCALE2", [128, 8, 2]); rSCALE2 = Res()
        G2 = P_sb("G2", [128, 8]); rG2 = Res()
        P.dma("sp", G2[:], g2T, writes=[rG2])
        ABT = P_sb("ABT", [128, 16]); rABT = Res()
        P.dma("sp", ABT[:], adabT, writes=[rABT])
        RW = P_sb("RW", [128, 8, NE]); rRW = Res()
        P.dma("sp", RW[:], rw.rearrange("(k p) e -> p k e", p=128), writes=[rRW])
        RB = P_sb("RB", [1, NE]); rRB = Res()
        P.dma("sp", RB[:], rb, writes=[rRB])
        WO = P_sb("WO", [128, 8, D], BF16); rWO = Res()
        P.dma("pool", WO[:], wout.rearrange("(k p) f -> p k f", p=128), writes=[rWO])
        sub = ExitStack()
        ABR = P_sb("ABR", [1, 4096], stack=sub); rABR = Res()
        P.dma("sp", ABR[:], adab, writes=[rABR])
        CV = P_sb("CV", [128, 8, 2], stack=sub); rCV = Res()
        P.dma("sp", CV[:], cvT, writes=[rCV])
        SCV = P_sb("SCV", [128, 8, 2], stack=sub); rSCV = Res()
        P.act(SCV[:], CV[:], AF.Silu, reads=[rCV], writes=[rSCV])
        SCB = P_sb("SCB", [128, 8, 2, 128], stack=sub); rSCB = Res()
        P.op("dve", lambda h: h.tensor_tensor(out=SCB[:], in0=ONES[:].unsqueeze(1).unsqueeze(1).to_broadcast([128, 8, 2, 128]),
                                              in1=SCV[:].unsqueeze(3).to_broadcast([128, 8, 2, 128]), op=ALU.mult),
             reads=[rONES, rSCV], writes=[rSCB])

        AW = [P_sb("AW%d" % i, [128, 8, 512], stack=sub) for i in range(2)]; rAW = [Res(), Res()]
        for blk in range(8):
            aw, raw = AW[blk % 2], rAW[blk % 2]
            P.dma("sp", aw[:], adaw[:, blk * 512:(blk + 1) * 512].rearrange("(k p) f -> p k f", p=128), writes=[raw])
            if blk in (0, 1, 6, 7):
                which = 0 if blk < 2 else 1
                half = blk % 2
                for j in range(2):
                    ps, rps = PS[j], rPS[j]
                    for k in range(8):
                        P.mm(ps[:], SCB[:, k, j, :], aw[:, k, :], start=(k == 0), stop=False,
                             reads=[rSCB, raw], writes=[rps])
                    P.mm(ps[:], ONES[0:1, :], ABR[0:1, blk * 512:(blk + 1) * 512], start=False, stop=True,
                         reads=[rONES, rABR], writes=[rps])
                    P.act(GT[:, which, j, half * 512:(half + 1) * 512], ps[:], AF.Copy, reads=[rps], writes=[rGT])
            else:
                ps, rps = PS[2 + blk % 2], rPS[2 + blk % 2]
                for fcl in range(4):
                    fcg = (blk - 2) * 4 + fcl
                    for k in range(8):
                        P.mm(ps[:, fcl * 2:fcl * 2 + 2], aw[:, k, fcl * 128:(fcl + 1) * 128], SCV[:, k, :],
                             start=(k == 0), stop=(k == 7), reads=[rSCV, raw], writes=[rps])
                for fcl in range(4):
                    fcg = (blk - 2) * 4 + fcl
                    P.act(MODF[:, fcg, :], ps[:, fcl * 2:fcl * 2 + 2], AF.Identity, reads=[rps, rABT], writes=[rMODF],
                          bias=ABT[:, fcg:fcg + 1], scale=1.0)
        P.op("dve", lambda h: h.tensor_scalar(out=SCALE2[:], in0=MODF[:, 8:16, :], scalar1=1.0, scalar2=None, op0=ALU.add),
             reads=[rMODF], writes=[rSCALE2])
        P.op("dve", lambda h: h.tensor_tensor(out=SCALE2[:], in0=SCALE2[:], in1=G2[:].unsqueeze(2).to_broadcast([128, 8, 2]), op=ALU.mult),
             reads=[rSCALE2, rG2], writes=[rSCALE2])

        P.fence()
        sub.close()
        X1 = P_sb("X1", [128, MAXT, D]); rX1 = [Res() for _ in range(MAXT)]
        H2B = P_sb("H2B", [128, 8, MAXT * 128], BF16); rH2B = [Res() for _ in range(MAXT)]
        GATES = P_sb("GATES", [128, MAXT, NE]); rGATES = [Res() for _ in range(MAXT)]
        XT = [P_sb("XT%d" % i, [128, D]) for i in range(2)]; rXT = [Res(), Res()]
        OM = [P_sb("OM%d" % i, [128, 8, 128], BF16) for i in range(2)]; rOM = [Res(), Res()]
        OMX = P_sb("OMX", [128, 4, 4, 256], BF16); rOMX = Res()
        TMP = [P_sb("TMP%d" % i, [128, D]) for i in range(2)]; rTMP = [Res(), Res()]
        H2F = P_sb("H2F", [128, 8, 128]); rH2F = Res()
        SMALL = P_sb("SMALL", [128, 64]); rSM = Res()
        LG = P_sb("LG", [128, NE]); rLG = Res()
        EX = P_sb("EX", [128, NE]); rEX = Res()
        MK = P_sb("MK", [128, NE]); rMK = Res()
        WGU = P_sb("WGU", [128, 8, 2 * D], BF16); rWG = [Res() for _ in range(8)]; rWU = [Res() for _ in range(8)]
        WD = P_sb("WD", [128, 8, D], BF16); rWD = [Res() for _ in range(8)]
        BGU = [P_sb("BGU%d" % i, [128, 16]) for i in range(2)]; rBGU = [Res(), Res()]
        BD = [P_sb("BD%d" % i, [1, D], BF16) for i in range(2)]; rBD = [Res(), Res()]
        BGX = [P_sb("BGX%d" % i, [128, 16]) for i in range(2)]; rBGX = [Res(), Res()]
        SIGC = float(1.0 / (1.0 + np.exp(np.float64(-1.702 * 7.0))))
        ACTT = [P_sb("ACTT%d" % i, [128, 8, 512], BF16) for i in range(2)]; rACTT = [Res(), Res()]
        GP = [P_sb("GP%d" % i, [128, 512]) for i in range(2)]; rGP = [Res(), Res()]
        SG = [P_sb("SG%d" % i, [128, 512]) for i in range(2)]; rSG = [Res(), Res()]
        UP = [P_sb("UP%d" % i, [128, 512]) for i in range(2)]; rUP = [Res(), Res()]
        wcount = 0
        cnt = 0

        for pss in passes:
            for li, ti in enumerate(pss):
                r0, nr, j = tiles[ti]
                xt, rxt = XT[li % 2], rXT[li % 2]
                om, rom = OM[li % 2], rOM[li % 2]
                tmp, rtmp = TMP[li % 2], rTMP[li % 2]
                P.dma("sp", xt[:nr, :], xs[r0:r0 + nr, :], reads=rxs, writes=[rxt])
                for q in range(4):
                    grow = (256 + q * NL + r0) if j == 0 else (q * NC)
                    for jj in range(4):
                        P.dma("pool", OMX[:nr, q, jj, :], omall(jj, grow, nr), reads=romall, writes=[rOMX])
                for k in range(8):
                    ps, rps = PS[2 + k // 4], rPS[2 + k // 4]
                    for q in range(4):
                        P.mm(ps[:, (k % 4) * 128:(k % 4) * 128 + nr], OMX[:nr, q, k % 4, (k // 4) * 128:(k // 4 + 1) * 128], SELI[:nr, q, :nr],
                             start=(q == 0), stop=(q == 3), reads=[rOMX, rSELI], writes=[rps])
                for kk in range(2):
                    P.act(om[:, 4 * kk:4 * kk + 4, :nr], PS[2 + kk][:, :].rearrange("p (a t) -> p a t", a=4)[:, :, :nr], AF.Copy, reads=[rPS[2 + kk]], writes=[rom])
                for half in range(2):
                    ps, rps = PS[half], rPS[half]
                    for k in range(8):
                        P.mm(ps[:nr, :], om[:, k, :nr], WO[:, k, half * 512:(half + 1) * 512], start=(k == 0), stop=(k == 7),
                             reads=[rom, rWO], writes=[rps])
                    P.op("dve", lambda h, ps=ps, tmp=tmp, half=half, j=j, nr=nr: h.tensor_tensor(
                        out=tmp[:nr, half * 512:(half + 1) * 512], in0=ps[:nr, :], in1=GT[:nr, 0, j, half * 512:(half + 1) * 512], op=ALU.mult),
                        reads=[rps, rGT], writes=[rtmp])
                P.op("dve", lambda h, tmp=tmp, xt=xt, li=li, nr=nr: h.tensor_tensor(out=X1[:nr, li, :], in0=tmp[:nr, :], in1=xt[:nr, :], op=ALU.add),
                     reads=[rtmp, rxt], writes=[rX1[li]])
                P.act(tmp[:nr, :], X1[:nr, li, :], AF.Square, reads=[rX1[li]], writes=[rtmp, rSM], accum_out=SMALL[:nr, 0:1])
                P.act(SMALL[:nr, 1:2], SMALL[:nr, 0:1], AF.Sqrt, reads=[rSM], writes=[rSM], bias=1e-6, scale=1.0 / D)
                P.op("dve", lambda h, nr=nr: h.reciprocal(out=SMALL[:nr, 2:3], in_=SMALL[:nr, 1:2]), reads=[rSM], writes=[rSM])
                P.op("dve", lambda h, tmp=tmp, li=li, nr=nr: h.tensor_scalar(out=tmp[:nr, :], in0=X1[:nr, li, :], scalar1=SMALL[:nr, 2:3], scalar2=None, op0=ALU.mult),
                     reads=[rX1[li], rSM], writes=[rtmp])
                for k in range(8):
                    ps, rps = PS[2 + k // 4], rPS[2 + k // 4]
                    P.op("pe", lambda h, ps=ps, tmp=tmp, k=k, nr=nr: h.transpose(ps[:, (k % 4) * 128:(k % 4) * 128 + nr], tmp[:nr, k * 128:(k + 1) * 128], ID[:nr, :nr]),
                         reads=[rtmp, rID], writes=[rps])
                for k in range(8):
                    ps, rps = PS[2 + k // 4], rPS[2 + k // 4]
                    P.act(H2F[:, k, :nr], ps[:, (k % 4) * 128:(k % 4) * 128 + nr], AF.Identity, reads=[rps, rSCALE2, rMODF], writes=[rH2F],
                          scale=SCALE2[:, k, j:j + 1], bias=MODF[:, k, j:j + 1])
                P.op("dve", lambda h, li=li, nr=nr: h.tensor_copy(out=H2B[:, :, li * 128:li * 128 + nr], in_=H2F[:, :, :nr]),
                     reads=[rH2F], writes=[rH2B[li]])
                ps, rps = PS[4], rPS[4]
                for k in range(8):
                    P.mm(ps[:nr, 0:NE], H2F[:, k, :nr], RW[:, k, :], start=(k == 0), stop=False, reads=[rH2F, rRW], writes=[rps])
                P.mm(ps[:nr, 0:NE], ONES[0:1, :nr], RB[0:1, :], start=False, stop=True, reads=[rONES, rRB], writes=[rps])
                P.op("dve", lambda h, ps=ps, nr=nr: h.tensor_copy(out=LG[:nr, :], in_=ps[:nr, 0:NE]), reads=[rps], writes=[rLG])
                P.op("dve", lambda h, nr=nr: h.max(out=SMALL[:nr, 8:16], in_=LG[:nr, :]), reads=[rLG], writes=[rSM])
                P.op("dve", lambda h, nr=nr: h.tensor_scalar(out=SMALL[:nr, 16:17], in0=SMALL[:nr, 8:9], scalar1=-1.0, scalar2=None, op0=ALU.mult),
                     reads=[rSM], writes=[rSM])
                P.act(EX[:nr, :], LG[:nr, :], AF.Exp, reads=[rLG, rSM], writes=[rEX], bias=SMALL[:nr, 16:17], scale=1.0)
                P.op("dve", lambda h, nr=nr: h.tensor_scalar(out=MK[:nr, :], in0=LG[:nr, :], scalar1=SMALL[:nr, 11:12], scalar2=None, op0=ALU.is_ge),
                     reads=[rLG, rSM], writes=[rMK])
                P.op("dve", lambda h, nr=nr: h.tensor_tensor(out=EX[:nr, :], in0=EX[:nr, :], in1=MK[:nr, :], op=ALU.mult),
                     reads=[rEX, rMK], writes=[rEX])
                P.op("dve", lambda h, nr=nr: h.reduce_sum(out=SMALL[:nr, 17:18], in_=EX[:nr, :], axis=AX.X), reads=[rEX], writes=[rSM])
                P.op("dve", lambda h, nr=nr: h.reciprocal(out=SMALL[:nr, 18:19], in_=SMALL[:nr, 17:18]), reads=[rSM], writes=[rSM])
                P.op("dve", lambda h, li=li, nr=nr: h.tensor_scalar(out=GATES[:nr, li, :], in0=EX[:nr, :], scalar1=SMALL[:nr, 18:19], scalar2=None, op0=ALU.mult),
                     reads=[rEX, rSM], writes=[rGATES[li]])

            groups = []
            li = 0
            while li < len(pss):
                g = []
                while li < len(pss) and len(g) < 4 and tiles[pss[li]][1] == 128:
                    g.append(li); li += 1
                if not g:
                    g = [li]; li += 1
                groups.append(g)
            for e in range(IN.get("nex", NE)):
                wb = wcount % 2
                wcount += 1
                for fc in range(8):
                    P.dma("pool", WGU[:, :, fc * 128:(fc + 1) * 128], wgu[e, :, fc * 128:(fc + 1) * 128].rearrange("(k p) f -> p k f", p=128),
                          writes=[rWG[fc]])
                    P.dma("pool", WGU[:, :, D + fc * 128:D + (fc + 1) * 128], wgu[e, :, D + fc * 128:D + (fc + 1) * 128].rearrange("(k p) f -> p k f", p=128),
                          writes=[rWU[fc]])
                for fc in range(8):
                    P.dma("pool", WD[:, fc, :], wd[e, fc * 128:(fc + 1) * 128, :], writes=[rWD[fc]])
                P.dma("sp", BGU[wb][:], bguT[e], writes=[rBGU[wb]])
                P.op("dve", lambda h, wb=wb: h.tensor_scalar(out=BGX[wb][:, 0:8], in0=BGU[wb][:, 0:8], scalar1=1.702, scalar2=None, op0=ALU.mult),
                     reads=[rBGU[wb]], writes=[rBGX[wb]])
                P.op("dve", lambda h, wb=wb: h.tensor_scalar(out=BGX[wb][:, 8:16], in0=BGU[wb][:, 8:16], scalar1=1.0, scalar2=None, op0=ALU.add),
                     reads=[rBGU[wb]], writes=[rBGX[wb]])
                P.dma("pool", BD[wb][:], bd[e:e + 1, :], writes=[rBD[wb]])
                for g in groups:
                    ntok = sum(tiles[pss[l]][1] for l in g)
                    c0 = g[0] * 128
                    ab = cnt % 2
                    cnt += 1
                    actt, ractt = ACTT[ab], rACTT[ab]
                    rh = [rH2B[l] for l in g]
                    for fc in range(8):
                        pb = (fc % 2) * 2
                        psg, rpsg = PS[pb], rPS[pb]
                        psu, rpsu = PS[pb + 1], rPS[pb + 1]
                        for k in range(8):
                            P.mm(psg[:, :ntok], WGU[:, k, fc * 128:(fc + 1) * 128], H2B[:, k, c0:c0 + ntok], start=(k == 0), stop=(k == 7),
                                 reads=[rWG[fc]] + rh, writes=[rpsg])
                        for k in range(8):
                            P.mm(psu[:, :ntok], WGU[:, k, D + fc * 128:D + (fc + 1) * 128], H2B[:, k, c0:c0 + ntok], start=(k == 0), stop=(k == 7),
                                 reads=[rWU[fc]] + rh, writes=[rpsu])
                        tb = fc % 2
                        gp, sg, up = GP[tb], SG[tb], UP[tb]
                        P.act(sg[:, :ntok], psg[:, :ntok], AF.Sigmoid, reads=[rpsg, rBGX[wb]], writes=[rSG[tb]], scale=1.702, bias=BGX[wb][:, fc:fc + 1])
                        P.op("dve", lambda h, gp=gp, psg=psg, fc=fc, wb=wb, ntok=ntok: h.tensor_scalar(
                            out=gp[:, :ntok], in0=psg[:, :ntok], scalar1=BGU[wb][:, fc:fc + 1], scalar2=7.0, op0=ALU.add, op1=ALU.min),
                            reads=[rpsg, rBGU[wb]], writes=[rGP[tb]])
                        P.op("dve", lambda h, up=up, psu=psu, fc=fc, wb=wb, ntok=ntok: h.tensor_scalar(
                            out=up[:, :ntok], in0=psu[:, :ntok], scalar1=BGX[wb][:, 8 + fc:9 + fc], scalar2=8.0, op0=ALU.add, op1=ALU.min),
                            reads=[rpsu, rBGX[wb]], writes=[rUP[tb]])
                        P.op("dve", lambda h, gp=gp, sg=sg, ntok=ntok: h.scalar_tensor_tensor(out=gp[:, :ntok], in0=sg[:, :ntok], scalar=SIGC, in1=gp[:, :ntok], op0=ALU.min, op1=ALU.mult),
                             reads=[rGP[tb], rSG[tb]], writes=[rGP[tb]])
                        P.op("dve", lambda h, gp=gp, up=up, actt=actt, fc=fc, ntok=ntok: h.scalar_tensor_tensor(out=actt[:, fc, :ntok], in0=up[:, :ntok], scalar=-6.0, in1=gp[:, :ntok], op0=ALU.max, op1=ALU.mult),
                             reads=[rGP[tb], rUP[tb]], writes=[ractt])
                    for gi, l in enumerate(g):
                        r0, nr, j = tiles[pss[l]]
                        yb = 4 + (l % 2) * 2
                        for half in range(2):
                            ps, rps = PS[yb + half], rPS[yb + half]
                            for fc in range(8):
                                P.mm(ps[:nr, :], actt[:, fc, gi * 128:gi * 128 + nr], WD[:, fc, half * 512:(half + 1) * 512], start=(fc == 0), stop=False,
                                     reads=[ractt, rWD[fc]], writes=[rps])
                            P.mm(ps[:nr, :], ONESB[0:1, :nr], BD[wb][0:1, half * 512:(half + 1) * 512], start=False, stop=True,
                                 reads=[rONESB, rBD[wb]], writes=[rps])
                        tmp, rtmp = TMP[l % 2], rTMP[l % 2]
                        for half in range(2):
                            ps, rps = PS[yb + half], rPS[yb + half]
                            P.op("dve", lambda h, ps=ps, tmp=tmp, half=half, l=l, e=e, j=j, nr=nr: h.scalar_tensor_tensor(
                                out=tmp[:nr, half * 512:(half + 1) * 512], in0=ps[:nr, :], scalar=GATES[:nr, l, e:e + 1],
                                in1=GT[:nr, 1, j, half * 512:(half + 1) * 512], op0=ALU.mult, op1=ALU.mult),
                                reads=[rps, rGATES[l], rGT], writes=[rtmp])
                        P.op("dve", lambda h, tmp=tmp, l=l, nr=nr: h.tensor_tensor(out=X1[:nr, l, :], in0=X1[:nr, l, :], in1=tmp[:nr, :], op=ALU.add),
                             reads=[rtmp, rX1[l]], writes=[rX1[l]])
            for li, ti in enumerate(pss):
                r0, nr, j = tiles[ti]
                P.dma("sp", xo[r0:r0 + nr, :], X1[:nr, li, :], reads=[rX1[li]], writes=[rOUT[ti]])
        P.fence()


PER_LAYER = [("adaw1", [1024, 2048]), ("adab1T", [128, 16]), ("g1T", [128, 8]), ("wa", [1024, 384]), ("wz", [1024, 2, 68]),
             ("cw", [128, 3, 5]), ("nega", [128, 2]), ("dtb", [128, 2]), ("gdn", [128, 64]), ("ws", [1024, 256]),
             ("gqk", [128, 3, 64]), ("sinkb", [128, 2]),
             ("adaw2", [1024, 4096]), ("adab2", [1, 4096]), ("adab2T", [128, 16]), ("g2T", [128, 8]), ("wout", [1024, 1024]),
             ("rw", [1024, 32]), ("rb", [1, 32]), ("bguT", [32, 128, 16]), ("bd", [32, 1024])]
SHARED = [("ident", [128, 128]), ("cvT", [128, 8, 2]), ("blk1", [128, 128]), ("masks", [128, 6, 2, 64]), ("maskw", [128, 384]),
          ("ropet", [128, 64, 2, 2, 16]), ("seli", [128, 4, 128])]
NLOC = 2112


def build_fused(nex=32):
    nc = bass.Bass("TRN2", target_bir_lowering=False)
    dr = lambda name, shape: nc.dram_tensor(name, list(shape), F32, kind="ExternalInput").ap()
    IN = {}
    IN["xall"] = dr("xall", [8448, 1024]); IN["xs0"] = dr("xs0", [NLOC, 1024])
    for nm, shp in SHARED:
        IN[nm] = dr(nm, shp)
    for l in range(2):
        for nm, shp in PER_LAYER:
            IN[nm, l] = dr("%s_%d" % (nm, l), shp)
    for l in range(2):
        IN["wgu", l] = dr("wgu_%d" % l, [nex, 1024, 2048]); IN["wd", l] = dr("wd_%d" % l, [nex, 1024, 1024])
    IN["nex"] = nex
    xo = nc.dram_tensor("xo", [2048, 1024], F32, kind="ExternalOutput").ap()
    omloc = [nc.dram_tensor("omloc%d" % l, [8448, 256], F32).ap() for l in range(2)]
    OCH = 1024
    och = [(r, min(OCH, 8448 - r)) for r in range(0, 8448, OCH)]
    omall = [[nc.dram_tensor("omall%d_%d" % (l, k), [4 * n, 256], F32).ap() for k, (r, n) in enumerate(och)] for l in range(2)]
    xloc = nc.dram_tensor("xloc", [NLOC, 1024], F32).ap()
    XCH = 256
    xch = [(r, min(XCH, NLOC - r)) for r in range(0, NLOC, XCH)]
    xgat = [nc.dram_tensor("xgat_%d" % k, [4 * n, 1024], F32).ap() for k, (r, n) in enumerate(xch)]

    def om_rows(l, jj, grow, nr):
        k = grow // OCH
        n = och[k][1]
        off = jj * n + (grow - och[k][0])
        return omall[l][k][off:off + nr, :]

    def xg_rows(q, r, nr):
        k = r // XCH
        n = xch[k][1]
        off = q * n + (r - xch[k][0])
        return xgat[k][off:off + nr, :]
    groups = [[0, 1, 2, 3], [4, 5, 6, 7]]
    with ExitStack() as st:
        P = Prog(nc, st)
        G = emit_globals(P, IN)
        rxloc = [Res() for _ in range(17)]
        rxgat = Res()
        rcc = Res()
        rxo = [Res() for _ in range(16)]
        for l in range(2):
            last = (l == 1)
            if l == 0:
                xsrc = lambda n: [(0, 128, IN["xall"][n * 128:(n + 1) * 128, :])]
                rsrc = []
            else:
                def xsrc(n):
                    if n < 2:
                        a, b = 2 * n, 2 * n + 1
                        return [(0, 64, xg_rows(a, 2048, 64)), (64, 64, xg_rows(b, 2048, 64))]
                    i = n - 2
                    q, r = i // 16, (i % 16) * 128
                    return [(0, 128, xg_rows(q, r, 128))]
                rsrc = [rxgat]
            rdn = [Res(), Res()]
            rsw = [Res() for _ in range(66)]
            with ExitStack() as lst:
                C = emit_common(P, G, IN, l, lst)
                emit_dn(P, C, IN, l, last, xsrc, rsrc, omloc[l][:, 0:128], rdn)
                emit_swa(P, C, IN, l, last, xsrc, rsrc, omloc[l][:, 128:256], rsw)
                P.fence()
            romall = Res()
            if os.environ.get("NOCOLL") != "1":
                for k, (r, n) in enumerate(och):
                    P.coll("AllGather", ALU.bypass, groups, omloc[l][r:r + n, :], omall[l][k], reads=rdn + rsw, writes=[romall, rcc])
            if l == 0:
                emit_b(P, G, IN, 0, 2048, 64, IN["xs0"], [], lambda jj, grow, nr: om_rows(0, jj, grow, nr), [romall], xloc, rxloc)
                if os.environ.get("NOCOLL") not in ("1", "2"):
                    for k, (r, n) in enumerate(xch):
                        P.coll("AllGather", ALU.bypass, groups, xloc[r:r + n, :], xgat[k], reads=rxloc, writes=[rxgat, rcc])
            else:
                emit_b(P, G, IN, 1, 2048, 0, xloc, rxloc, lambda jj, grow, nr: om_rows(1, jj, grow, nr), [romall], xo, rxo)
        P.finish(rxo)
        print("fused instr counts", P.cnt, P.dcnt, "waits", P.n_wait, "sems", {k: len(v) for k, v in P.sem.items()})
    return nc


NEG = -1e30
def consts():
    f = np.float32
    i = np.arange(64)
    k_le_f = (i[:, None] <= i[None, :]).astype(f)
    k_ge_f = (i[:, None] >= i[None, :]).astype(f)
    m = np.zeros((64, 6, 2, 64), f)
    m[:, 0, 0] = k_le_f; m[:, 0, 1] = k_ge_f
    m[:, 1, 0] = np.where(i[None, :] >= i[:, None], 0, NEG)
    m[:, 1, 1] = np.where(i[None, :] <= i[:, None], 0, NEG)
    m[:, 2, 0] = np.where(i[None, :] <= i[:, None], 0, NEG)
    m[:, 2, 1] = np.where(i[None, :] >= i[:, None], 0, NEG)
    m[:, 3, 0] = np.where(i[None, :] > i[:, None], -1, 0)
    m[:, 3, 1] = np.where(i[None, :] < i[:, None], -1, 0)
    m[:, 4, 0] = np.where(i[None, :] < i[:, None], -1, 0)
    m[:, 4, 1] = np.where(i[None, :] > i[:, None], -1, 0)
    m[:, 5, 0] = np.eye(64); m[:, 5, 1] = np.eye(64)
    masks = np.concatenate([m, m], 0)
    blk1 = np.zeros((128, 128), f); blk1[:64, :64] = 1; blk1[64:, 64:] = 1
    qi = np.arange(128)[:, None]; kc = np.arange(384)[None, :]
    maskw = np.where((kc >= qi) & (kc <= qi + 256), 0, NEG).astype(f)
    t = np.arange(8192); row = (t // 64).astype(f); col = (t % 64).astype(f)
    inv = np.power(f(10000.0), -np.arange(16, dtype=f) / f(16)).astype(f)
    ar = row[:, None] * inv; ac = col[:, None] * inv
    rope = np.stack([np.stack([np.cos(ar), np.cos(ac)], 1), np.stack([np.sin(ar), np.sin(ac)], 1)], 1).astype(f)
    ropet = np.ascontiguousarray(rope.reshape(64, 128, 2, 2, 16).transpose(1, 0, 2, 3, 4))
    return dict(masks=masks, blk1=blk1, maskw=maskw, ropet=ropet, ident=np.eye(128, dtype=f))
def prep_a(inp, l, x, xc, K):
    f = np.float32
    w_in = inp["w_in"][l]; cwl = inp["dn_conv_w"][l]
    ada_w = np.ascontiguousarray(inp["ada_w"][l][:, 0:2048]); ada_b = inp["ada_b"][l][0:2048]
    base = dict(adaw=ada_w, adabT=np.ascontiguousarray(ada_b.reshape(16, 128).T), g1T=np.ascontiguousarray(inp["norm1_g"][l].reshape(8, 128).T), ident=K["ident"])
    dn, sw = [], []
    for c in range(8):
        b, j = c // 4, c % 4
        xall = np.ascontiguousarray(np.concatenate([xc[b], x[b]], 0))
        cv = np.stack([inp["c"][b], inp["c_ctx"]], -1)
        m = dict(base); m.update(xall=xall, cvT=np.ascontiguousarray(cv.reshape(8, 128, 2).transpose(1, 0, 2)))
        hd = [2 * j, 2 * j + 1]
        wa = np.concatenate([w_in[:, s * 512 + 128 * j: s * 512 + 128 * j + 128] for s in range(3)], 1)
        wz = np.stack([np.concatenate([w_in[:, 1536 + h * 64:1536 + (h + 1) * 64], w_in[:, [2048 + h, 2048 + 8 + h, 2064 + h, 2064 + 8 + h]]], 1) for h in hd], 1)
        cw = np.stack([cwl[:, s * 512 + 128 * j: s * 512 + 128 * j + 128].T for s in range(3)], 1)
        hp = np.repeat(np.array(hd), 64)
        nega = -np.exp(inp["dn_a_log"][l][:, hp]).T; dtb = inp["dn_dt_bias"][l][:, hp].T
        md = dict(m); md.update(wa=np.ascontiguousarray(wa), wz=np.ascontiguousarray(wz), cw=np.ascontiguousarray(cw), nega=np.ascontiguousarray(nega.astype(f)),
                                dtb=np.ascontiguousarray(dtb), gdn=np.ascontiguousarray(np.broadcast_to(inp["dn_out_g"][l], (128, 64))), blk1=K["blk1"], masks=K["masks"])
        dn.append(md)
        kv = j // 2
        ws = np.concatenate([w_in[:, 2080 + hd[0] * 64:2080 + hd[0] * 64 + 128], w_in[:, 2592 + kv * 64:2592 + (kv + 1) * 64], w_in[:, 2720 + kv * 64:2720 + (kv + 1) * 64]], 1)
        gqk = np.broadcast_to(np.stack([inp["q_norm_g"][l], inp["q_norm_g"][l], inp["k_norm_g"][l]], 0), (128, 3, 64))
        ms = dict(m); ms.update(ws=np.ascontiguousarray(ws), gqk=np.ascontiguousarray(gqk), ropet=K["ropet"],
                                sinkb=np.ascontiguousarray(np.broadcast_to(inp["sinks"][l][hd], (128, 2))), maskw=K["maskw"])
        sw.append(ms)
    return dn, sw
def gather_a(res_dn, res_sw):
    om_x = np.zeros((2, 8192, 1024), np.float32); om_c = np.zeros((2, 256, 1024), np.float32)
    for c in range(8):
        b, j = c // 4, c % 4
        od = res_dn[c]["o_dn"]; os_ = res_sw[c]["o_sw"]
        om_c[b, :, 128 * j:128 * j + 128] = od[:256]; om_x[b, :, 128 * j:128 * j + 128] = od[256:]
        om_c[b, :, 512 + 128 * j:512 + 128 * j + 128] = os_[:256]; om_x[b, :, 512 + 128 * j:512 + 128 * j + 128] = os_[256:]
    return om_x, om_c


def prep_b(inp, l, x, xc, om_x, om_c, last):
    f = np.float32
    ada_w = np.ascontiguousarray(inp["ada_w"][l][:, 2048:6144]); ada_b = inp["ada_b"][l][2048:6144]
    common = dict(
        adaw=ada_w, adab=np.ascontiguousarray(ada_b[None, :]),
        adabT=np.ascontiguousarray(ada_b[1024:3072].reshape(16, 128).T),
        g2T=np.ascontiguousarray(inp["norm2_g"][l].reshape(8, 128).T),
        wout=inp["w_out"][l], rw=inp["router_w"][l], rb=np.ascontiguousarray(inp["router_b"][l][None, :]),
        wgu=inp["w_gate_up"][l], bguT=np.ascontiguousarray(inp["b_gate_up"][l].reshape(32, 16, 128).transpose(0, 2, 1)),
        wd=inp["w_down"][l], bd=inp["b_down"][l], ident=np.eye(128, dtype=f))
    maps = []
    for c in range(8):
        b, q = c // 4, c % 4
        rows = [x[b, q * 2048:(q + 1) * 2048]]; oms = [om_x[b, q * 2048:(q + 1) * 2048]]
        if not last:
            rows.append(xc[b, q * 64:(q + 1) * 64]); oms.append(om_c[b, q * 64:(q + 1) * 64])
        xs = np.ascontiguousarray(np.concatenate(rows, 0)); om = np.concatenate(oms, 0)
        cv = np.stack([inp["c"][b], inp["c_ctx"]], -1)
        m = dict(common)
        m.update(xs=xs, omT=np.ascontiguousarray(om.T), cvT=np.ascontiguousarray(cv.reshape(8, 128, 2).transpose(1, 0, 2)))
        maps.append(m)
    return maps
def gather_b(results, last):
    x = np.zeros((2, 8192, 1024), np.float32); xc = np.zeros((2, 256, 1024), np.float32)
    for c in range(8):
        b, q = c // 4, c % 4
        xo = results[c]["xo"]
        x[b, q * 2048:(q + 1) * 2048] = xo[:2048]
        if not last:
            xc[b, q * 64:(q + 1) * 64] = xo[2048:]
    return x, xc


def prep_fused(inp, nex=32):
    f = np.float32
    K = consts()
    x = np.ascontiguousarray(inp["x"], dtype=f); xc = np.ascontiguousarray(inp["ctx"], dtype=f)
    A = [prep_a(inp, l, x, xc, K) for l in range(2)]
    zx = np.zeros((2, 8192, 1024), f); zc = np.zeros((2, 256, 1024), f)
    B = [prep_b(inp, l, zx, zc, zx, zc, False) for l in range(2)]
    wgu = [np.ascontiguousarray(inp["w_gate_up"][l][:nex], dtype=f) for l in range(2)]; wd = [np.ascontiguousarray(inp["w_down"][l][:nex], dtype=f) for l in range(2)]
    maps = []
    for c in range(8):
        b, q = c // 4, c % 4
        m = dict(xall=A[0][0][c]["xall"], cvT=A[0][0][c]["cvT"], ident=K["ident"], blk1=K["blk1"], masks=K["masks"], maskw=K["maskw"], ropet=K["ropet"])
        m["xs0"] = np.ascontiguousarray(np.concatenate([x[b, q * 2048:(q + 1) * 2048], xc[b, q * 64:(q + 1) * 64]], 0))
        seli = np.zeros((128, 4, 128), f); seli[:, q, :] = np.eye(128, dtype=f); m["seli"] = seli
        for l in range(2):
            dn, sw = A[l][0][c], A[l][1][c]; bb = B[l][c]
            for nm, src, key in [("adaw1", dn, "adaw"), ("adab1T", dn, "adabT"), ("g1T", dn, "g1T"), ("wa", dn, "wa"), ("wz", dn, "wz"), ("cw", dn, "cw"),
                                 ("nega", dn, "nega"), ("dtb", dn, "dtb"), ("gdn", dn, "gdn"), ("ws", sw, "ws"), ("gqk", sw, "gqk"), ("sinkb", sw, "sinkb"),
                                 ("adaw2", bb, "adaw"), ("adab2", bb, "adab"), ("adab2T", bb, "adabT"), ("g2T", bb, "g2T"), ("wout", bb, "wout"),
                                 ("rw", bb, "rw"), ("rb", bb, "rb"), ("bguT", bb, "bguT"), ("bd", bb, "bd")]:
                m["%s_%d" % (nm, l)] = np.ascontiguousarray(src[key], dtype=f)
        for l in range(2):
            m["wgu_%d" % l] = wgu[l]; m["wd_%d" % l] = wd[l]
        maps.append(m)
    return maps
def gather_fused(results):
    x = np.zeros((2, 8192, 1024), np.float32)
    for c in range(8):
        b, q = c // 4, c % 4
        x[b, q * 2048:(q + 1) * 2048] = results[c]["xo"]
    return x


_NC = None


def kernel(**inputs):
    global _NC
    inp = {k: np.asarray(v) for k, v in inputs.items()}
    if _NC is None:
        _NC = build_fused(32)
    maps = prep_fused(inp, 32)
    res = run_bass_kernel_spmd(_NC, maps, core_ids=list(range(8)))
    return gather_fused(res.results).astype(np.float32)
```

```python
import os
import numpy as np
from contextlib import ExitStack
import concourse.bass as bass
import concourse.mybir as mybir
from concourse.bass_utils import run_bass_kernel_spmd

F32 = mybir.dt.float32
BF16 = mybir.dt.bfloat16
AF = mybir.ActivationFunctionType
ALU = mybir.AluOpType
AX = mybir.AxisListType

SAME_ENGINE_SYNC = True
NRING = 8
EPOCH = 30000


class Res:
    __slots__ = ("name", "w", "r", "x")

    def __init__(self, name="", x=False):
        self.name = name
        self.w = None
        self.r = {}
        self.x = x


class Prog:
    def __init__(self, nc, stack):
        self.nc = nc
        self.e = {"pe": nc.tensor, "act": nc.scalar, "dve": nc.vector, "pool": nc.gpsimd, "sp": nc.sync}
        self.ops = {k: [] for k in self.e}
        self.cnt = {k: 0 for k in self.e}
        self.sem = {k: [stack.enter_context(nc.semaphore("s_" + k + "0"))] for k in self.e}
        self.ring = {q: [stack.enter_context(nc.semaphore("d_%s_%d" % (q, i))) for i in range(NRING)]
                     for q in ("sp", "act", "pool")}
        self.dcnt = {q: 0 for q in self.ring}
        self.waited = {k: {} for k in self.e}
        self.stack = stack
        self.n_wait = 0

    def sb(self, name, shape, dt=F32, stack=None):
        self.n_alloc = getattr(self, "n_alloc", 0) + 1
        return (stack or self.stack).enter_context(self.nc.sbuf_tensor("%s_%d" % (name, self.n_alloc), list(shape), dt))

    def fence(self):
        for E in self.e:
            for F in self.e:
                if F != E and self.cnt[F] > 0:
                    self._wait(E, ("c", F, self.cnt[F]))
            for q in self.ring:
                n = self.dcnt[q]
                for k in range(max(0, n - NRING), n):
                    self._wait(E, ("d", q, k))

    def ps(self, name, shape, dt=F32):
        return self.stack.enter_context(self.nc.psum_tensor(name, list(shape), dt))

    def _semval(self, ev):
        if ev[0] == "c":
            ep, v = divmod(ev[2] - 1, EPOCH)
            return ("c", ev[1]), self.sem[ev[1]][ep], (ep, v + 1)
        if ev[0] == "x":
            return ("x", ev[1]), self.ccsems[ev[1]], (0, 1)
        _, q, n = ev
        slot = n % NRING
        return ("d", q, slot), self.ring[q][slot], (0, 16 * (n // NRING + 1))

    def _wait(self, eng, ev):
        if ev[0] == "c" and ev[1] == eng:
            if eng == "pe" or not SAME_ENGINE_SYNC:
                return
        key, sem, val = self._semval(ev)
        if self.waited[eng].get(key, (0, 0)) >= val:
            return
        self.waited[eng][key] = val
        self.n_wait += 1
        self.ops[eng].append(lambda h, sem=sem, val=val[1]: h.wait_ge(sem, val))

    def _deps(self, eng, reads, writes):
        deps = []
        for r in reads:
            if r.w is not None:
                deps.append(r.w)
            if r.x:
                for k, ev in r.r.items():
                    if not (k[0] == "c" and k[1] == eng):
                        deps.append(ev)
        for w in writes:
            if w.w is not None:
                deps.append(w.w)
            deps.extend(w.r.values())
        for ev in deps:
            self._wait(eng, ev)

    def _record(self, ev, reads, writes):
        if ev[0] == "c":
            key = ("c", ev[1])
        elif ev[0] == "x":
            key = ("x", ev[1])
        else:
            key = ("d", ev[1], ev[2] % NRING)
        for r in reads:
            r.r[key] = ev
        for w in writes:
            w.w = ev
            w.r = {}

    def op(self, eng, fn, reads=(), writes=()):
        self._deps(eng, reads, writes)
        self.cnt[eng] += 1
        ev = ("c", eng, self.cnt[eng])
        ep = (self.cnt[eng] - 1) // EPOCH
        if ep >= len(self.sem[eng]):
            self.sem[eng].append(self.stack.enter_context(self.nc.semaphore("s_%s%d" % (eng, ep))))
        sem = self.sem[eng][ep]
        self.ops[eng].append(lambda h, fn=fn, sem=sem: fn(h).then_inc(sem, 1))
        self._record(ev, reads, writes)

    def dma(self, q, out, in_, reads=(), writes=(), **kw):
        self._deps(q, reads, writes)
        n = self.dcnt[q]
        self.dcnt[q] += 1
        if n >= NRING:
            self._wait(q, ("d", q, n - NRING))
        sem = self.ring[q][n % NRING]
        self.ops[q].append(lambda h, out=out, in_=in_, sem=sem, kw=kw: h.dma_start(out=out, in_=in_, **kw).then_inc(sem, 16))
        ev = ("d", q, n)
        self._record(ev, reads, writes)
        return ev

    def coll(self, kind, op, groups, in_ap, out_ap, reads=(), writes=()):
        q = "pool"
        self._deps(q, reads, writes)
        if not hasattr(self, "ccsems"):
            self.ccsems = []
        sem = self.stack.enter_context(self.nc.semaphore("cc%d" % len(self.ccsems)))
        self.ccsems.append(sem)
        self.ops[q].append(lambda h, sem=sem: h.collective_compute(kind, op, replica_groups=groups, ins=[in_ap.opt()], outs=[out_ap.opt()]).then_inc(sem))
        ev = ("x", len(self.ccsems) - 1, 0)
        self._record(ev, reads, writes)
        return ev

    def finish(self, final_res):
        for r in final_res:
            if r.w is not None:
                self._wait("sp", r.w)
        for q in self.ring:
            n = self.dcnt[q]
            for k in range(max(0, n - NRING), n):
                self._wait("sp", ("d", q, k))
        with self.nc.Block() as block:
            @block.tensor
            def _(h):
                for f in self.ops["pe"]:
                    f(h)

            @block.scalar
            def _(h):
                for f in self.ops["act"]:
                    f(h)

            @block.vector
            def _(h):
                for f in self.ops["dve"]:
                    f(h)

            @block.gpsimd
            def _(h):
                for f in self.ops["pool"]:
                    f(h)

            @block.sync
            def _(h):
                for f in self.ops["sp"]:
                    f(h)

    def mm(self, out, lhsT, rhs, start=True, stop=True, reads=(), writes=(), **kw):
        self.op("pe", lambda h: h.matmul(out, lhsT, rhs, start=start, stop=stop, **kw), reads, writes)

    def act(self, out, in_, func, reads=(), writes=(), **kw):
        self.op("act", lambda h: h.activation(out=out, in_=in_, func=func, **kw), reads, writes)


D = 1024
NB = 66
NCH = 132


def emit_globals(P, IN):
    G = {}
    ID = P.sb("ID", [128, 128]); rID = Res(); P.dma("sp", ID[:], IN["ident"], writes=[rID])
    ONES = P.sb("ONES", [128, 128]); rONES = Res()
    P.op("dve", lambda h: h.memset(ONES[:], 1.0), writes=[rONES])
    ONESB = P.sb("ONESB", [1, 128], BF16); rONESB = Res()
    P.op("dve", lambda h: h.memset(ONESB[:], 1.0), writes=[rONESB])
    PS = [P.ps("PS%d" % i, [128, 512]) for i in range(8)]; rPS = [Res("ps%d" % i, x=True) for i in range(8)]
    G.update(ID=ID, rID=rID, ONES=ONES, rONES=rONES, ONESB=ONESB, rONESB=rONESB, PS=PS, rPS=rPS)
    return G


def emit_common(P, G, IN, l, stk):
    cvT = IN["cvT"]; adaw = IN["adaw1", l]; adabT = IN["adab1T", l]; g1T = IN["g1T", l]
    C = dict(G)
    PS, rPS = G["PS"], G["rPS"]
    MODF = P.sb("MODF", [128, 16, 2], stack=stk); rMODF = Res()
    SCALE1 = P.sb("SCALE1", [128, 8, 2], stack=stk); rSCALE1 = Res()
    G1 = P.sb("G1", [128, 8], stack=stk); rG1 = Res(); P.dma("sp", G1[:], g1T, writes=[rG1])
    ABT = P.sb("ABT", [128, 16], stack=stk); rABT = Res(); P.dma("sp", ABT[:], adabT, writes=[rABT])
    sub = ExitStack()
    CV = P.sb("CV", [128, 8, 2], stack=sub); rCV = Res(); P.dma("sp", CV[:], cvT, writes=[rCV])
    SCV = P.sb("SCV", [128, 8, 2], stack=sub); rSCV = Res()
    P.act(SCV[:], CV[:], AF.Silu, reads=[rCV], writes=[rSCV])
    AW = [P.sb("AW%d" % i, [128, 8, 512], stack=sub) for i in range(2)]; rAW = [Res(), Res()]
    for blk in range(4):
        aw, raw = AW[blk % 2], rAW[blk % 2]
        P.dma("sp", aw[:], adaw[:, blk * 512:(blk + 1) * 512].rearrange("(k p) f -> p k f", p=128), writes=[raw])
        ps, rps = PS[blk % 2], rPS[blk % 2]
        for fcl in range(4):
            for k in range(8):
                P.mm(ps[:, fcl * 2:fcl * 2 + 2], aw[:, k, fcl * 128:(fcl + 1) * 128], SCV[:, k, :],
                     start=(k == 0), stop=(k == 7), reads=[rSCV, raw], writes=[rps])
        for fcl in range(4):
            fcg = blk * 4 + fcl
            P.act(MODF[:, fcg, :], ps[:, fcl * 2:fcl * 2 + 2], AF.Identity, reads=[rps, rABT], writes=[rMODF],
                  bias=ABT[:, fcg:fcg + 1], scale=1.0)
    P.op("dve", lambda h: h.tensor_scalar(out=SCALE1[:], in0=MODF[:, 8:16, :], scalar1=1.0, scalar2=None, op0=ALU.add),
         reads=[rMODF], writes=[rSCALE1])
    P.op("dve", lambda h: h.tensor_tensor(out=SCALE1[:], in0=SCALE1[:], in1=G1[:].unsqueeze(2).to_broadcast([128, 8, 2]), op=ALU.mult),
         reads=[rSCALE1, rG1], writes=[rSCALE1])
    P.fence()
    sub.close()
    C.update(MODF=MODF, rMODF=rMODF, SCALE1=SCALE1, rSCALE1=rSCALE1)
    return C


def emit_frontend(P, C, xsrc, rsrc, consume, blocks, stk, tbanks=(0, 1)):
    PS, rPS = C["PS"], C["rPS"]
    XT = [P.sb("XT%d" % i, [128, D], stack=stk) for i in range(2)]; rXT = [Res(), Res()]
    XN = P.sb("XN", [128, D], stack=stk); rXN = Res()
    HX = [P.sb("HX%d" % i, [128, 8, 128], BF16, stack=stk) for i in range(2)]; rHX = [Res(), Res()]
    SM = P.sb("FSM", [128, 8], stack=stk); rSM = Res()
    for idx, n in enumerate(blocks):
        j = 1 if n < 2 else 0
        xt, rxt = XT[idx % 2], rXT[idx % 2]
        hx, rhx = HX[idx % 2], rHX[idx % 2]
        for (p0, np_, src) in xsrc(n):
            P.dma("sp", xt[p0:p0 + np_, :], src, reads=rsrc, writes=[rxt])
        P.op("dve", lambda h: h.memset(SM[:, 0:1], 0.0), writes=[rSM])
        P.act(XN[:], xt[:], AF.Square, reads=[rxt], writes=[rXN, rSM], accum_out=SM[:, 0:1])
        P.act(SM[:, 1:2], SM[:, 0:1], AF.Ln, reads=[rSM], writes=[rSM], bias=1e-6, scale=1.0 / D)
        P.act(SM[:, 2:3], SM[:, 1:2], AF.Exp, reads=[rSM], writes=[rSM], scale=-0.5)
        P.op("dve", lambda h, xt=xt: h.tensor_scalar(out=XN[:], in0=xt[:], scalar1=SM[:, 2:3], scalar2=None, op0=ALU.mult),
             reads=[rxt, rSM], writes=[rXN])
        for half in range(2):
            ps, rps = PS[tbanks[half]], rPS[tbanks[half]]
            for k in range(4 * half, 4 * half + 4):
                P.op("pe", lambda h, ps=ps, k=k: h.transpose(ps[:, (k % 4) * 128:(k % 4 + 1) * 128], XN[:, k * 128:(k + 1) * 128], C["ID"][:]),
                     reads=[rXN, C["rID"]], writes=[rps])
            for k in range(4 * half, 4 * half + 4):
                P.act(hx[:, k, :], ps[:, (k % 4) * 128:(k % 4 + 1) * 128], AF.Identity, reads=[rps, C["rSCALE1"], C["rMODF"]], writes=[rhx],
                      scale=C["SCALE1"][:, k, j:j + 1], bias=C["MODF"][:, k, j:j + 1])
        consume(n, hx, rhx)


def emit_swa(P, C, IN, l, last, xsrc, rsrc, o_sw, rOUT):
    ws = IN["ws", l]; gqk = IN["gqk", l]; ropet = IN["ropet"]; sinkb = IN["sinkb", l]; maskw = IN["maskw"]
    with ExitStack() as st0:
        _sb = P.sb
        P_sb = lambda name, shape, dt=F32: _sb("sw_" + name, shape, dt, stack=st0)
        PS, rPS, ID, rID = C["PS"], C["rPS"], C["ID"], C["rID"]
        WS = P_sb("WS", [128, 8, 256], BF16); rWS = Res()
        P.dma("pool", WS[:], ws.rearrange("(k p) f -> p k f", p=128), writes=[rWS])
        GQK = P_sb("GQK", [128, 3, 64]); rGQK = Res(); P.dma("sp", GQK[:], gqk, writes=[rGQK])
        ROPE = P_sb("ROPE", [128, 64, 2, 2, 16]); rROPE = Res(); P.dma("sp", ROPE[:], ropet, writes=[rROPE])
        SINK = P_sb("SINK", [128, 2]); rSINK = Res(); P.dma("sp", SINK[:], sinkb, writes=[rSINK])
        MASKW = P_sb("MASKW", [128, 384]); rMASKW = Res(); P.dma("sp", MASKW[:], maskw, writes=[rMASKW])
        SQT = P_sb("SQT", [128, NB * 128], BF16); rSQT = [Res() for _ in range(NB)]
        SKT = P_sb("SKT", [128, NB * 128], BF16); rSKT = [Res() for _ in range(NB)]
        SV = P_sb("SV", [128, NB, 64], BF16); rSV = [Res() for _ in range(NB)]
        QK = P_sb("QK", [128, 3, 64]); rQK = Res()
        QKR = P_sb("QKR", [128, 4, 64]); rQKR = Res()
        SQ = P_sb("SQ", [128, 3, 64]); rSQ = Res()
        T1 = P_sb("T1", [128, 3, 2, 16]); rT1 = Res()
        T2 = P_sb("T2", [128, 3, 2, 16]); rT2 = Res()
        SM = P_sb("SM", [128, 16]); rSM = Res()
        S = P_sb("S", [128, 640]); rS = Res()
        E = P_sb("E", [128, 640]); rE = Res()
        ET = P_sb("ET", [128, 5, 128], BF16); rET = Res()
        OSW = [P_sb("OSW%d" % i, [128, 128]) for i in range(2)]; rOSW = [Res(), Res()]

        HL = []
        for h in range(2):
            Ld = dict(S=P_sb("S%d" % h, [128, 640]), rS=Res(), E=P_sb("E%d" % h, [128, 640]), rE=Res(),
                      ET=P_sb("ET%d" % h, [128, 5, 128], BF16), rET=Res(), SM=P_sb("SMh%d" % h, [128, 8]), rSM=Res(),
                      b0=PS[4 + 2 * h], r0=rPS[4 + 2 * h], b1=PS[5 + 2 * h], r1=rPS[5 + 2 * h])
            HL.append(Ld)

        def att_gen(n, h, osw, rosw):
            Ld = HL[h]
            S_, rS_, E_, rE_, ET_, rET_, SM_, rSM_ = Ld["S"], Ld["rS"], Ld["E"], Ld["rE"], Ld["ET"], Ld["rET"], Ld["SM"], Ld["rSM"]
            b0, r0, b1, r1 = Ld["b0"], Ld["r0"], Ld["b1"], Ld["r1"]
            if n >= 2:
                lo, hi = max(2, n - 1), min(NB - 1, n + 1)
                nl = (hi - lo + 1) * 128
                m0 = (lo - (n - 1)) * 128
            else:
                lo, hi, nl, m0 = 0, -1, 0, 0
            ntot = nl + 256
            kblocks = list(range(lo, hi + 1)) + [0, 1]
            hs = slice(h * 64, (h + 1) * 64)
            if nl:
                P.mm(b0[:, 0:nl], SQT[hs, n * 128:(n + 1) * 128], SKT[hs, lo * 128:(hi + 1) * 128],
                     reads=[rSQT[n]] + [rSKT[b] for b in range(lo, hi + 1)], writes=[r0])
            P.mm(b1[:, 0:256], SQT[hs, n * 128:(n + 1) * 128], SKT[hs, 0:256], reads=[rSQT[n], rSKT[0], rSKT[1]], writes=[r1])
            yield
            if nl:
                P.op("dve", lambda hh: hh.scalar_tensor_tensor(out=S_[:, 0:nl], in0=b0[:, 0:nl], scalar=0.125,
                                                              in1=MASKW[:, m0:m0 + nl], op0=ALU.mult, op1=ALU.add),
                     reads=[r0, rMASKW], writes=[rS_])
            P.act(S_[:, nl:ntot], b1[:, 0:256], AF.Copy, reads=[r1], writes=[rS_], scale=0.125)
            yield
            P.op("dve", lambda hh: hh.reduce_max(out=SM_[:, 0:1], in_=S_[:, 0:ntot], axis=AX.X), reads=[rS_], writes=[rSM_])
            yield
            P.op("dve", lambda hh: hh.tensor_tensor(out=SM_[:, 1:2], in0=SM_[:, 0:1], in1=SINK[:, h:h + 1], op=ALU.max),
                 reads=[rSM_, rSINK], writes=[rSM_])
            yield
            P.op("dve", lambda hh: hh.tensor_scalar(out=SM_[:, 2:3], in0=SM_[:, 1:2], scalar1=-1.0, scalar2=None, op0=ALU.mult),
                 reads=[rSM_], writes=[rSM_])
            P.op("dve", lambda hh: hh.memset(SM_[:, 3:4], 0.0), writes=[rSM_])
            yield
            P.act(E_[:, 0:ntot], S_[:, 0:ntot], AF.Exp, reads=[rS_, rSM_], writes=[rE_, rSM_], bias=SM_[:, 2:3], scale=1.0, accum_out=SM_[:, 3:4])
            P.act(SM_[:, 4:5], SINK[:, h:h + 1], AF.Exp, reads=[rSINK, rSM_], writes=[rSM_], bias=SM_[:, 2:3], scale=1.0)
            yield
            P.op("dve", lambda hh: hh.tensor_tensor(out=SM_[:, 5:6], in0=SM_[:, 3:4], in1=SM_[:, 4:5], op=ALU.add), reads=[rSM_], writes=[rSM_])
            nk = ntot // 128
            n4 = min(nk, 4)
            for c in range(n4):
                P.op("pe", lambda hh, c=c: hh.transpose(b1[:, c * 128:(c + 1) * 128], E_[:, c * 128:(c + 1) * 128], ID[:]),
                     reads=[rE_, rID], writes=[r1])
            if nk > 4:
                P.op("pe", lambda hh: hh.transpose(b0[:, 384:512], E_[:, 512:640], ID[:]), reads=[rE_, rID], writes=[r0])
            yield
            P.op("dve", lambda hh: hh.reciprocal(out=SM_[:, 6:7], in_=SM_[:, 5:6]), reads=[rSM_], writes=[rSM_])
            P.act(ET_[:, 0:n4, :], b1[:, 0:n4 * 128], AF.Copy, reads=[r1], writes=[rET_])
            if nk > 4:
                P.act(ET_[:, 4, :], b0[:, 384:512], AF.Copy, reads=[r0], writes=[rET_])
            yield
            for c in range(nk):
                kb = kblocks[c]
                P.mm(b1[:, 0:64], ET_[:, c, :], SV[:, kb, :], start=(c == 0), stop=(c == nk - 1), reads=[rET_, rSV[kb]], writes=[r1])
            yield
            P.op("dve", lambda hh: hh.tensor_scalar(out=osw[:, h * 64:(h + 1) * 64], in0=b1[:, 0:64], scalar1=SM_[:, 6:7], scalar2=None, op0=ALU.mult),
                 reads=[r1, rSM_], writes=[rosw])
            yield

        BL = []
        for li in range(2):
            F = dict(XT=P_sb("XT%d" % li, [128, D]), rXT=Res(), XN=P_sb("XN%d" % li, [128, D]), rXN=Res(),
                     HX=P_sb("HX%d" % li, [128, 8, 128], BF16), rHX=Res(), SM=P_sb("BSM%d" % li, [128, 16]), rSM=Res(),
                     QK=P_sb("QK%d" % li, [128, 3, 64]), rQK=Res(), QKR=P_sb("QKR%d" % li, [128, 4, 64]), rQKR=Res(),
                     SQ=P_sb("SQ%d" % li, [128, 3, 64]), rSQ=Res(), T1=P_sb("T1%d" % li, [128, 3, 2, 16]), rT1=Res(),
                     T2=P_sb("T2%d" % li, [128, 3, 2, 16]), rT2=Res(),
                     bT=PS[2 * li], rT=rPS[2 * li], bA=PS[2 * li + 1], rA=rPS[2 * li + 1])
            BL.append(F)

        def blk_gen(n, F):
            j = 1 if n < 2 else 0
            xt, rxt, XN_, rXN_, hx, rhx, SMb, rSMb = F["XT"], F["rXT"], F["XN"], F["rXN"], F["HX"], F["rHX"], F["SM"], F["rSM"]
            QK_, rQK_, QKR_, rQKR_, SQ_, rSQ_, T1_, rT1_, T2_, rT2_ = F["QK"], F["rQK"], F["QKR"], F["rQKR"], F["SQ"], F["rSQ"], F["T1"], F["rT1"], F["T2"], F["rT2"]
            for (p0, np_, src) in xsrc(n):
                P.dma("sp", xt[p0:p0 + np_, :], src, reads=rsrc, writes=[rxt])
            P.op("dve", lambda h: h.memset(SMb[:, 0:1], 0.0), writes=[rSMb])
            yield
            P.act(XN_[:], xt[:], AF.Square, reads=[rxt], writes=[rXN_, rSMb], accum_out=SMb[:, 0:1])
            yield
            P.act(SMb[:, 1:2], SMb[:, 0:1], AF.Ln, reads=[rSMb], writes=[rSMb], bias=1e-6, scale=1.0 / D)
            yield
            P.act(SMb[:, 2:3], SMb[:, 1:2], AF.Exp, reads=[rSMb], writes=[rSMb], scale=-0.5)
            yield
            P.op("dve", lambda h: h.tensor_scalar(out=XN_[:], in0=xt[:], scalar1=SMb[:, 2:3], scalar2=None, op0=ALU.mult),
                 reads=[rxt, rSMb], writes=[rXN_])
            yield
            ps, rps = F["bT"], F["rT"]
            for half in range(2):
                for k in range(4 * half, 4 * half + 4):
                    P.op("pe", lambda h, k=k, ps=ps: h.transpose(ps[:, (k % 4) * 128:(k % 4 + 1) * 128], XN_[:, k * 128:(k + 1) * 128], ID[:]),
                         reads=[rXN_, rID], writes=[rps])
                yield
                for k in range(4 * half, 4 * half + 4):
                    P.act(hx[:, k, :], ps[:, (k % 4) * 128:(k % 4 + 1) * 128], AF.Identity, reads=[rps, C["rSCALE1"], C["rMODF"]], writes=[rhx],
                          scale=C["SCALE1"][:, k, j:j + 1], bias=C["MODF"][:, k, j:j + 1])
                yield
            pa, rpa = F["bA"], F["rA"]
            for k in range(8):
                P.mm(pa[:, 0:256], hx[:, k, :], WS[:, k, :], start=(k == 0), stop=(k == 7), reads=[rhx, rWS], writes=[rpa])
            yield
            P.act(QK_[:], pa[:, 0:192], AF.Copy, reads=[rpa], writes=[rQK_])
            P.act(SV[:, n, :], pa[:, 192:256], AF.Copy, reads=[rpa], writes=[rSV[n]])
            yield
            P.op("dve", lambda h: h.tensor_tensor(out=SQ_[:], in0=QK_[:], in1=QK_[:], op=ALU.mult), reads=[rQK_], writes=[rSQ_])
            yield
            P.op("dve", lambda h: h.reduce_sum(out=SMb[:, 8:11], in_=SQ_[:], axis=AX.X), reads=[rSQ_], writes=[rSMb])
            yield
            P.act(SMb[:, 11:14], SMb[:, 8:11], AF.Ln, reads=[rSMb], writes=[rSMb], bias=1e-6, scale=1.0 / 64)
            yield
            P.act(SMb[:, 8:11], SMb[:, 11:14], AF.Exp, reads=[rSMb], writes=[rSMb], scale=-0.5)
            yield
            P.op("dve", lambda h: h.tensor_tensor(out=QK_[:], in0=QK_[:], in1=SMb[:, 8:11].unsqueeze(2).to_broadcast([128, 3, 64]), op=ALU.mult),
                 reads=[rQK_, rSMb], writes=[rQK_])
            yield
            P.op("dve", lambda h: h.tensor_tensor(out=QK_[:], in0=QK_[:], in1=GQK[:], op=ALU.mult), reads=[rQK_, rGQK], writes=[rQK_])
            yield
            if n >= 2:
                bi = n - 2
                q5 = QK_[:].rearrange("p s (a t f) -> p s a t f", a=2, t=2)
                o5 = QKR_[:, 0:3, :].rearrange("p s (a t f) -> p s a t f", a=2, t=2)
                X1, X2 = q5[:, :, :, 0, :], q5[:, :, :, 1, :]
                Cc = ROPE[:, bi, 0, :, :].unsqueeze(1).to_broadcast([128, 3, 2, 16])
                Sn = ROPE[:, bi, 1, :, :].unsqueeze(1).to_broadcast([128, 3, 2, 16])
                tt = lambda out, a, b, op, reads, writes: P.op("dve", lambda h: h.tensor_tensor(out=out, in0=a, in1=b, op=op), reads=reads, writes=writes)
                tt(T1_[:], X1, Cc, ALU.mult, [rQK_, rROPE], [rT1_])
                tt(T2_[:], X2, Sn, ALU.mult, [rQK_, rROPE], [rT2_])
                yield
                tt(o5[:, :, :, 0, :], T1_[:], T2_[:], ALU.subtract, [rT1_, rT2_], [rQKR_])
                yield
                tt(T1_[:], X2, Cc, ALU.mult, [rQK_, rROPE], [rT1_])
                tt(T2_[:], X1, Sn, ALU.mult, [rQK_, rROPE], [rT2_])
                yield
                tt(o5[:, :, :, 1, :], T1_[:], T2_[:], ALU.add, [rT1_, rT2_], [rQKR_])
                yield
            else:
                P.op("dve", lambda h: h.tensor_copy(out=QKR_[:, 0:3, :], in_=QK_[:]), reads=[rQK_], writes=[rQKR_])
                yield
            P.op("dve", lambda h: h.tensor_copy(out=QKR_[:, 3, :], in_=QKR_[:, 2, :]), reads=[rQKR_], writes=[rQKR_])
            yield
            P.op("pe", lambda h: h.transpose(pa[:, 256:384], QKR_[:, 0:2, :].rearrange("p a f -> p (a f)"), ID[:]), reads=[rQKR_, rID], writes=[rpa])
            P.op("pe", lambda h: h.transpose(pa[:, 384:512], QKR_[:, 2:4, :].rearrange("p a f -> p (a f)"), ID[:]), reads=[rQKR_, rID], writes=[rpa])
            yield
            P.act(SQT[:, n * 128:(n + 1) * 128], pa[:, 256:384], AF.Copy, reads=[rpa], writes=[rSQT[n]])
            P.act(SKT[:, n * 128:(n + 1) * 128], pa[:, 384:512], AF.Copy, reads=[rpa], writes=[rSKT[n]])
            yield

        qblocks = ([] if last else [0, 1]) + list(range(2, NB))
        done_blk = set()
        blk_active = {}
        blk_lane = {}
        free_bl = [0, 1]
        att_active = None
        nxt = 0
        qi = 0
        while nxt < NB or blk_active or att_active is not None or qi < len(qblocks):
            while free_bl and nxt < NB:
                li_ = free_bl.pop(0)
                blk_lane[nxt] = li_
                blk_active[nxt] = blk_gen(nxt, BL[li_]); nxt += 1
            if att_active is None and qi < len(qblocks):
                m = qblocks[qi]
                need = [b for b in (0, 1, m - 1, m, m + 1) if 0 <= b < NB and (b < 2 or b >= 2)]
                if m < 2:
                    need = [0, 1]
                if all(b in done_blk for b in need):
                    osw, rosw = OSW[m % 2], rOSW[m % 2]
                    att_active = (m, [att_gen(m, 0, osw, rosw), att_gen(m, 1, osw, rosw)], [True, True])
                    qi += 1
            progressed = False
            for nb_ in list(blk_active):
                try:
                    next(blk_active[nb_]); progressed = True
                except StopIteration:
                    del blk_active[nb_]; done_blk.add(nb_); progressed = True
                    free_bl.append(blk_lane.pop(nb_))
            if att_active is not None:
                m, gs, alive = att_active
                for i in range(2):
                    if alive[i]:
                        try:
                            next(gs[i]); progressed = True
                        except StopIteration:
                            alive[i] = False; progressed = True
                if not any(alive):
                    P.dma("sp", o_sw[m * 128:(m + 1) * 128, :], OSW[m % 2][:], reads=[rOSW[m % 2]], writes=[rOUT[m]])
                    att_active = None
            assert progressed or att_active is None
        P.fence()


def emit_dn(P, C, IN, l, last, xsrc, rsrc, o_dn, rOUT):
    wa = IN["wa", l]; wz = IN["wz", l]; cw = IN["cw", l]; nega = IN["nega", l]; dtb = IN["dtb", l]; gdn = IN["gdn", l]
    blk1 = IN["blk1"]; masks = IN["masks"]
    NS = NCH + 1
    with ExitStack() as st0:
        _sb = P.sb
        P_sb = lambda name, shape, dt=F32: _sb("dn_" + name, shape, dt, stack=st0)
        PS, rPS, ID, rID, ONES, rONES = C["PS"], C["rPS"], C["ID"], C["rID"], C["ONES"], C["rONES"]
        WA = P_sb("WA", [128, 8, 384], BF16); rWA = Res(); P.dma("pool", WA[:], wa.rearrange("(k p) f -> p k f", p=128), writes=[rWA])
        WZ = P_sb("WZ", [128, 8, 2, 68], BF16); rWZ = Res(); P.dma("pool", WZ[:], wz.rearrange("(k p) h f -> p k h f", p=128), writes=[rWZ])
        CW = P_sb("CW", [128, 3, 5]); rCW = Res(); P.dma("sp", CW[:], cw, writes=[rCW])
        NEGA = P_sb("NEGA", [128, 2]); rNEGA = Res(); P.dma("sp", NEGA[:], nega, writes=[rNEGA])
        DTB = P_sb("DTB", [128, 2]); rDTB = Res(); P.dma("sp", DTB[:], dtb, writes=[rDTB])
        GDN = P_sb("GDN", [128, 64]); rGDN = Res(); P.dma("sp", GDN[:], gdn, writes=[rGDN])
        BLK = P_sb("BLK", [128, 128]); rBLK = Res(); P.dma("sp", BLK[:], blk1, writes=[rBLK])
        MSK = P_sb("MSK", [128, 6, 2, 64]); rMSK = Res(); P.dma("sp", MSK[:], masks, writes=[rMSK])
        TRI, NEGMT, NEGM, NSTT, NST, ID2 = [MSK[:, i, :, :] for i in range(6)]
        ONES3 = P_sb("ONES3", [128, 2, 64]); rONES3 = Res()
        P.op("dve", lambda h: h.memset(ONES3[:], 1.0), writes=[rONES3])
        QT = P_sb("QT", [128, NCH * 64], BF16); KT = P_sb("KT", [128, NCH * 64], BF16)
        KTM = P_sb("KTM", [128, NCH, 64], BF16); VTM = P_sb("VTM", [128, NCH, 64], BF16)
        SZ = P_sb("SZ", [128, NCH, 64], BF16)
        Gs = P_sb("Gs", [128, NS, 2]); Bs = P_sb("Bs", [128, NS, 2])
        O = P_sb("O", [128, NCH, 64])
        rCH = [Res() for _ in range(NCH)]
        rO = [Res() for _ in range(NCH)]
        P.op("dve", lambda h: h.memset(O[:], 0.0), writes=rO)
        NLF = 4
        NCB = NLF + 2
        fst = ExitStack()
        P_sbf = lambda name, shape, dt=F32: _sb("dnf_" + name, shape, dt, stack=fst)
        CBL = [P_sbf("CBL%d" % i, [128, 3, 132]) for i in range(NCB)]; rCBL = [Res() for _ in range(NCB)]
        for i in range(NCB):
            P.op("dve", lambda h, i=i: h.memset(CBL[i][:], 0.0), writes=[rCBL[i]])

        def step_of(c, d):
            if d == 0:
                return c
            return 4 - c if c < 4 else 136 - c

        FL = []
        for li in range(NLF):
            F = dict(XT=P_sbf("XT%d" % li, [128, D]), rXT=Res(), XN=P_sbf("XN%d" % li, [128, D]), rXN=Res(),
                     HX=P_sbf("HX%d" % li, [128, 8, 128], BF16), rHX=Res(), SM=P_sbf("FSM%d" % li, [128, 8]), rSM=Res(),
                     CVb=P_sbf("CVb%d" % li, [128, 3, 128]), rCVb=Res(), SQ2=P_sbf("SQ2%d" % li, [128, 2, 128]), rSQ2=Res(),
                     RS2=P_sbf("RS2%d" % li, [128, 2, 128]), rRS2=Res(), KN=P_sbf("KN%d" % li, [128, 128]), rKN=Res(),
                     GT_=P_sbf("GT_%d" % li, [128, 2, 2, 8]), rGT_=Res(), EXb=P_sbf("EXb%d" % li, [128, 3, 128]), rEXb=Res(),
                     EZ=P_sbf("EZ%d" % li, [128, 2, 64]), rEZ=Res(),
                     bT=PS[2 * li], rT=rPS[2 * li], bA=PS[2 * li + 1], rA=rPS[2 * li + 1], bB=PS[2 * li + 1], rB=rPS[2 * li + 1],
                     bZ=PS[2 * li], rZ=rPS[2 * li])
            FL.append(F)

        def conv_gen(m, F):
            CVb, rCVb, SQ2, rSQ2, RS2, rRS2, KN, rKN = F["CVb"], F["rCVb"], F["SQ2"], F["rSQ2"], F["RS2"], F["rRS2"], F["KN"], F["rKN"]
            CB, rCB = CBL[m % NCB], rCBL[m % NCB]
            rcv = [Res(), Res(), Res()]
            for s_ in range(3):
                P.op("dve", lambda h, s_=s_: h.tensor_scalar(out=CVb[:, s_, :], in0=CB[:, s_, 0:128], scalar1=CW[:, s_, 0:1], scalar2=None, op0=ALU.mult),
                     reads=[rCB, rCW], writes=[rcv[s_], rCVb])
            yield
            for tap in range(1, 5):
                for s_ in range(3):
                    P.op("dve", lambda h, s_=s_, tap=tap: h.scalar_tensor_tensor(out=CVb[:, s_, :], in0=CB[:, s_, tap:tap + 128], scalar=CW[:, s_, tap:tap + 1],
                                                                              in1=CVb[:, s_, :], op0=ALU.mult, op1=ALU.add),
                         reads=[rCB, rCW, rcv[s_]], writes=[rcv[s_]] + ([rCVb] if tap == 4 else []))
                yield
            EXb, rEXb = F["EXb"], F["rEXb"]
            P.act(EXb[:], CVb[:], AF.Exp, reads=[rCVb], writes=[rEXb], scale=-1.0)
            yield
            P.op("dve", lambda h: h.tensor_scalar(out=EXb[:], in0=EXb[:], scalar1=1.0, scalar2=None, op0=ALU.add), reads=[rEXb], writes=[rEXb])
            yield
            P.op("dve", lambda h: h.reciprocal(out=EXb[:], in_=EXb[:]), reads=[rEXb], writes=[rEXb])
            yield
            P.op("dve", lambda h: h.tensor_tensor(out=CVb[:], in0=CVb[:], in1=EXb[:], op=ALU.mult), reads=[rCVb, rEXb], writes=[rCVb])
            yield
            P.op("dve", lambda h: h.tensor_tensor(out=SQ2[:], in0=CVb[:, 0:2, :], in1=CVb[:, 0:2, :], op=ALU.mult), reads=[rCVb], writes=[rSQ2])
            yield
            ps, rps = F["bB"], F["rB"]
            P.mm(ps[:, 0:256], BLK[:], SQ2[:].rearrange("p a t -> p (a t)"), reads=[rBLK, rSQ2], writes=[rps])
            yield
            P.act(RS2[:].rearrange("p a t -> p (a t)"), ps[:, 0:256], AF.Ln, reads=[rps], writes=[rRS2], bias=1e-6, scale=1.0)
            yield
            P.act(RS2[:], RS2[:], AF.Exp, reads=[rRS2], writes=[rRS2], scale=-0.5)
            yield
            rc = [rCH[2 * m], rCH[2 * m + 1]]
            P.op("dve", lambda h: h.scalar_tensor_tensor(out=QT[:, m * 128:(m + 1) * 128], in0=CVb[:, 0, :], scalar=0.125, in1=RS2[:, 0, :], op0=ALU.mult, op1=ALU.mult),
                 reads=[rCVb, rRS2], writes=rc)
            P.op("dve", lambda h: h.tensor_tensor(out=KN[:], in0=CVb[:, 1, :], in1=RS2[:, 1, :], op=ALU.mult), reads=[rCVb, rRS2], writes=[rKN])
            yield
            P.op("dve", lambda h: h.tensor_copy(out=KT[:, m * 128:(m + 1) * 128], in_=KN[:]), reads=[rKN], writes=rc)
            for which in range(2):
                for cc in range(2):
                    for hh in range(2):
                        hs = slice(hh * 64, (hh + 1) * 64)
                        srcap = KN[hs, cc * 64:(cc + 1) * 64] if which == 0 else CVb[hs, 2, cc * 64:(cc + 1) * 64]
                        P.op("pe", lambda h, hs=hs, cc=cc, which=which, srcap=srcap: h.matmul(ps[hs, 256 + which * 128 + cc * 64: 256 + which * 128 + (cc + 1) * 64], srcap, ID[hs, hs], start=True, stop=True),
                             reads=[rKN, rCVb, rID], writes=[rps])
            yield
            P.act(KTM[:, 2 * m:2 * m + 2, :], ps[:, 256:384].rearrange("p (c f) -> p c f", c=2), AF.Copy, reads=[rps], writes=rc)
            P.act(VTM[:, 2 * m:2 * m + 2, :], ps[:, 384:512].rearrange("p (c f) -> p c f", c=2), AF.Copy, reads=[rps], writes=rc)
            yield

        def blk_gen(n, F):
            j = 1 if n < 2 else 0
            xt, rxt, XN, rXN, hx, rhx, SM, rSM = F["XT"], F["rXT"], F["XN"], F["rXN"], F["HX"], F["rHX"], F["SM"], F["rSM"]
            for (p0, np_, src) in xsrc(n):
                P.dma("sp", xt[p0:p0 + np_, :], src, reads=rsrc, writes=[rxt])
            P.op("dve", lambda h: h.memset(SM[:, 0:1], 0.0), writes=[rSM])
            yield
            P.act(XN[:], xt[:], AF.Square, reads=[rxt], writes=[rXN, rSM], accum_out=SM[:, 0:1])
            yield
            P.act(SM[:, 1:2], SM[:, 0:1], AF.Ln, reads=[rSM], writes=[rSM], bias=1e-6, scale=1.0 / D)
            yield
            P.act(SM[:, 2:3], SM[:, 1:2], AF.Exp, reads=[rSM], writes=[rSM], scale=-0.5)
            yield
            P.op("dve", lambda h: h.tensor_scalar(out=XN[:], in0=xt[:], scalar1=SM[:, 2:3], scalar2=None, op0=ALU.mult),
                 reads=[rxt, rSM], writes=[rXN])
            yield
            ps, rps = F["bT"], F["rT"]
            for half in range(2):
                for k in range(4 * half, 4 * half + 4):
                    P.op("pe", lambda h, k=k, ps=ps: h.transpose(ps[:, (k % 4) * 128:(k % 4 + 1) * 128], XN[:, k * 128:(k + 1) * 128], ID[:]),
                         reads=[rXN, rID], writes=[rps])
                yield
                for k in range(4 * half, 4 * half + 4):
                    P.act(hx[:, k, :], ps[:, (k % 4) * 128:(k % 4 + 1) * 128], AF.Identity, reads=[rps, C["rSCALE1"], C["rMODF"]], writes=[rhx],
                          scale=C["SCALE1"][:, k, j:j + 1], bias=C["MODF"][:, k, j:j + 1])
                yield
            ps, rps = F["bA"], F["rA"]
            for s_ in range(3):
                for k in range(8):
                    P.mm(ps[:, s_ * 128:(s_ + 1) * 128], WA[:, k, s_ * 128:(s_ + 1) * 128], hx[:, k, :], start=(k == 0), stop=(k == 7), reads=[rWA, rhx], writes=[rps])
            yield
            CB, rCB = CBL[n % NCB], rCBL[n % NCB]
            first = n in (0, 2)
            lastb = n in (1, NB - 1)
            P.act(CB[:, :, 2:130], ps[:, 0:384].rearrange("p (s t) -> p s t", s=3), AF.Copy, reads=[rps], writes=[rCB])
            if first:
                P.op("dve", lambda h: h.memset(CB[:, :, 0:2], 0.0), writes=[rCB])
            else:
                Pv, rPv = CBL[(n - 1) % NCB], rCBL[(n - 1) % NCB]
                P.op("dve", lambda h: h.tensor_copy(out=CB[:, :, 0:2], in_=Pv[:, :, 128:130]), reads=[rPv], writes=[rCB])
                P.op("dve", lambda h: h.tensor_copy(out=Pv[:, :, 130:132], in_=CB[:, :, 2:4]), reads=[rCB], writes=[rPv])
            if lastb:
                P.op("dve", lambda h: h.memset(CB[:, :, 130:132], 0.0), writes=[rCB])
            yield
            ps, rps = F["bZ"], F["rZ"]
            for cc in range(2):
                for hh in range(2):
                    hs = slice(hh * 64, (hh + 1) * 64)
                    for k in range(8):
                        P.mm(ps[hs, cc * 68:(cc + 1) * 68], hx[:, k, cc * 64:(cc + 1) * 64], WZ[:, k, hh, :], start=(k == 0), stop=(k == 7),
                             reads=[rhx, rWZ], writes=[rps])
            yield
            rc = [rCH[2 * n], rCH[2 * n + 1]]
            pz = ps[:, 0:136].rearrange("p (c f) -> p c f", c=2)
            EZ, rEZ = F["EZ"], F["rEZ"]
            P.act(EZ[:], pz[:, :, 0:64], AF.Exp, reads=[rps], writes=[rEZ], scale=-1.0)
            P.op("dve", lambda h: h.tensor_scalar(out=EZ[:], in0=EZ[:], scalar1=1.0, scalar2=None, op0=ALU.add), reads=[rEZ], writes=[rEZ])
            yield
            P.op("dve", lambda h: h.reciprocal(out=EZ[:], in_=EZ[:]), reads=[rEZ], writes=[rEZ])
            yield
            P.op("dve", lambda h: h.tensor_tensor(out=SZ[:, 2 * n:2 * n + 2, :], in0=pz[:, :, 0:64], in1=EZ[:], op=ALU.mult), reads=[rps, rEZ], writes=rc)
            GT_, rGT_ = F["GT_"], F["rGT_"]
            xa = GT_[:, :, :, 0]; ab = GT_[:, :, :, 1]; ee = GT_[:, :, :, 2]; ll = GT_[:, :, :, 3]; rr = GT_[:, :, :, 4]; gg = GT_[:, :, :, 5]; bb = GT_[:, :, :, 6]
            P.op("dve", lambda h: h.tensor_tensor(out=xa, in0=pz[:, :, 64:66], in1=DTB[:].unsqueeze(1).to_broadcast([128, 2, 2]), op=ALU.add), reads=[rps, rDTB], writes=[rGT_])
            yield
            P.act(bb, pz[:, :, 66:68], AF.Exp, reads=[rps], writes=[rGT_], scale=-1.0)
            P.act(ab, xa, AF.Abs, reads=[rGT_], writes=[rGT_])
            yield
            P.op("dve", lambda h: h.tensor_scalar(out=bb, in0=bb, scalar1=1.0, scalar2=None, op0=ALU.add), reads=[rGT_], writes=[rGT_])
            P.op("dve", lambda h: h.reciprocal(out=bb, in_=bb), reads=[rGT_], writes=[rGT_])
            yield
            P.act(ee, ab, AF.Exp, reads=[rGT_], writes=[rGT_], scale=-1.0)
            yield
            P.act(ll, ee, AF.Ln, reads=[rGT_], writes=[rGT_], bias=1.0, scale=1.0)
            P.op("dve", lambda h: h.tensor_scalar(out=rr, in0=xa, scalar1=0.0, scalar2=None, op0=ALU.max), reads=[rGT_], writes=[rGT_])
            yield
            P.op("dve", lambda h: h.tensor_tensor(out=rr, in0=rr, in1=ll, op=ALU.add), reads=[rGT_], writes=[rGT_])
            yield
            P.op("dve", lambda h: h.tensor_tensor(out=gg, in0=rr, in1=NEGA[:].unsqueeze(1).to_broadcast([128, 2, 2]), op=ALU.mult), reads=[rGT_, rNEGA], writes=[rGT_])
            yield
            for cc in range(2):
                c = 2 * n + cc
                for d in range(2):
                    s2 = step_of(c, d)
                    P.op("dve", lambda h, cc=cc, d=d, s2=s2: h.tensor_copy(out=Gs[:, s2, d:d + 1], in_=GT_[:, cc, d, 5:6]), reads=[rGT_], writes=[rCH[c]])
                    P.op("dve", lambda h, cc=cc, d=d, s2=s2: h.tensor_copy(out=Bs[:, s2, d:d + 1], in_=GT_[:, cc, d, 6:7]), reads=[rGT_], writes=[rCH[c]])
                yield
            if not first:
                yield from conv_gen(n - 1, F)
            if lastb:
                yield from conv_gen(n, F)

        active = []
        free_lanes = list(range(NLF))
        nxt = 0
        while nxt < NB or active:
            while free_lanes and nxt < NB:
                li = free_lanes.pop(0)
                active.append((blk_gen(nxt, FL[li]), li)); nxt += 1
            for item in list(active):
                try:
                    next(item[0])
                except StopIteration:
                    active.remove(item)
                    free_lanes.append(item[1])

        P.fence()
        fst.close()
        Sst = [(P_sb("S0", [128, 2, 64]), Res()), (P_sb("S1", [128, 2, 64]), Res())]
        for (t, r) in Sst:
            P.op("dve", lambda h, t=t: h.memset(t[:], 0.0), writes=[r])
        HS = [slice(0, 64), slice(64, 128)]
        dve = lambda fn, reads, writes: P.op("dve", fn, reads, writes)

        def make_lane(li):
            L = {}
            for nm in ("GBC", "BBC", "EB", "DT1", "TA", "DECT", "DEC", "DECS", "DECTS", "VB", "KBE", "KD", "QGT", "AQM", "NWT", "VN", "SGL", "C0", "C1"):
                L[nm] = (P_sb("%s_%d" % (nm, li), [128, 2, 64]), Res())
            L["W0"] = (P_sb("W0_%d" % li, [128, 2, 128]), Res()); L["W1"] = (P_sb("W1_%d" % li, [128, 2, 128]), Res())
            L["SC"] = (P_sb("SC_%d" % li, [128, 8]), Res())
            a, b = PS[2 * li], PS[2 * li + 1]
            v = lambda bank, lo, n: bank[:, lo:lo + 2 * n].rearrange("p (d f) -> p d f", d=2)
            ra, rb = rPS[2 * li], rPS[2 * li + 1]
            L["ps1"] = (v(a, 0, 64), ra); L["ps4"] = (v(a, 128, 64), ra); L["ps2"] = (v(a, 256, 64), ra); L["ps3"] = (v(a, 384, 64), ra)
            L["psI"] = (v(b, 0, 128), rb); L["psC"] = (v(b, 256, 64), rb); L["pcol"] = (b[:, 384:386], rb)
            L["ps5"] = (v(b, 0, 64), rb); L["pswT"] = (v(b, 128, 64), rb); L["ps6"] = (v(b, 256, 64), rb); L["ps7"] = (v(b, 384, 64), rb)
            return L

        NLS = 4
        lanes = [make_lane(i) for i in range(NLS)]

        def step_gen(s, L):
            GBC, rGBC = L["GBC"]; BBC, rBBC = L["BBC"]; EB, rEB = L["EB"]; DT1, rDT1 = L["DT1"]; TA, rTA = L["TA"]
            DECT, rDECT = L["DECT"]; DEC, rDEC = L["DEC"]; DECS, rDECS = L["DECS"]; DECTS, rDECTS = L["DECTS"]
            VB, rVB = L["VB"]; KBE, rKBE = L["KBE"]; KD, rKD = L["KD"]; QGT, rQGT = L["QGT"]; AQM, rAQM = L["AQM"]
            NWT, rNWT = L["NWT"]; VN, rVN = L["VN"]; SGL, rSGL = L["SGL"]; SC, rSC = L["SC"]
            Cb = [L["C0"], L["C1"]]; Wb = [L["W0"], L["W1"]]
            ps1, r1 = L["ps1"]; ps4, r4 = L["ps4"]; ps2, r2 = L["ps2"]; ps3, r3 = L["ps3"]
            psI, rI = L["psI"]; psC, rC = L["psC"]; pcol, rcol = L["pcol"]
            ps5, r5 = L["ps5"]; pswT, rwT = L["pswT"]; ps6, r6 = L["ps6"]; ps7, r7 = L["ps7"]
            dirs = [d for d in range(2) if (d == 0 and s <= NCH - 1) or (d == 1 and s >= 1)]
            ch = {0: s, 1: (4 - s if s <= 4 else 136 - s)}
            d0, d1 = dirs[0], dirs[-1] + 1
            ds = slice(d0, d1)
            nd = d1 - d0
            rch = [rCH[ch[d]] for d in dirs]
            Scur, rScur = Sst[s % 2]; Snew, rSnew = Sst[(s + 1) % 2]
            tok = {d: slice(ch[d] * 64, ch[d] * 64 + 64) for d in dirs}
            dve(lambda h: h.tensor_tensor(out=GBC[:, ds, :], in0=ONES3[:, ds, :], in1=Gs[:, s, ds].unsqueeze(2).to_broadcast([128, nd, 64]), op=ALU.mult), [rONES3] + rch, [rGBC])
            dve(lambda h: h.tensor_tensor(out=BBC[:, ds, :], in0=ONES3[:, ds, :], in1=Bs[:, s, ds].unsqueeze(2).to_broadcast([128, nd, 64]), op=ALU.mult), [rONES3] + rch, [rBBC])
            yield
            for d in dirs:
                for hs in HS:
                    P.mm(ps1[hs, d, :], GBC[hs, d, :], TRI[hs, d, :], reads=[rGBC, rMSK], writes=[r1])
            for d in dirs:
                for hs in HS:
                    P.mm(pcol[hs, d:d + 1], TRI[hs, d, :], Gs[hs, s, d:d + 1], reads=[rMSK] + rch, writes=[rcol])
            for d in dirs:
                for hs in HS:
                    P.mm(ps4[hs, d, :], BBC[hs, d, :], ID2[hs, d, :], reads=[rBBC, rMSK], writes=[r4])
            for d in dirs:
                for hs in HS:
                    P.mm(ps2[hs, d, :], KT[hs, tok[d]], KT[hs, tok[d]], reads=rch, writes=[r2])
            for d in dirs:
                for hs in HS:
                    P.mm(ps3[hs, d, :], KT[hs, tok[d]], QT[hs, tok[d]], reads=rch, writes=[r3])
            yield
            dve(lambda h: h.tensor_copy(out=SC[:, 0:2][:, ds], in_=pcol[:, ds]), [rcol], [rSC])
            P.act(EB[:, ds, :], ps1[:, ds, :], AF.Exp, reads=[r1], writes=[rEB])
            yield
            P.act(SC[:, 2:4][:, ds], SC[:, 0:2][:, ds], AF.Exp, reads=[rSC], writes=[rSC])
            dve(lambda h: h.tensor_tensor(out=DT1[:, ds, :], in0=ps1[:, ds, :], in1=SC[:, 0:2][:, ds].unsqueeze(2).to_broadcast([128, nd, 64]), op=ALU.subtract), [r1, rSC], [rDT1])
            yield
            dve(lambda h: h.tensor_tensor(out=TA[:, ds, :], in0=DT1[:, ds, :], in1=NEGMT[:, ds, :], op=ALU.add), [rDT1, rMSK], [rTA])
            yield
            P.act(DECT[:, ds, :], TA[:, ds, :], AF.Exp, reads=[rTA], writes=[rDECT])
            yield
            dve(lambda h: h.scalar_tensor_tensor(out=TA[:, ds, :], in0=DT1[:, ds, :], scalar=-1.0, in1=NEGM[:, ds, :], op0=ALU.mult, op1=ALU.add), [rDT1, rMSK, rDECT], [rTA])
            yield
            P.act(DEC[:, ds, :], TA[:, ds, :], AF.Exp, reads=[rTA], writes=[rDEC])
            for d in dirs:
                lastc = 63 if d == 0 else 0
                dve(lambda h, d=d, lastc=lastc: h.tensor_tensor(out=SC[:, 4 + d:5 + d], in0=ps1[:, d, lastc:lastc + 1], in1=SC[:, d:d + 1], op=ALU.subtract), [r1, rSC], [rSC])
            yield
            P.act(SC[:, 4:6][:, ds], SC[:, 4:6][:, ds], AF.Exp, reads=[rSC], writes=[rSC])
            C0, rC0 = Cb[0]; W0, rW0 = Wb[0]
            dve(lambda h: h.tensor_tensor(out=DECS[:, ds, :], in0=DEC[:, ds, :], in1=NST[:, ds, :], op=ALU.mult), [rDEC, rMSK], [rDECS])
            yield
            for d in dirs:
                dve(lambda h, d=d: h.scalar_tensor_tensor(out=C0[:, d, :], in0=ps2[:, d, :], scalar=Bs[:, s, d:d + 1], in1=DECS[:, d, :], op0=ALU.mult, op1=ALU.mult),
                    [r2, rDECS] + rch, [rC0])
            yield
            dve(lambda h: h.tensor_tensor(out=DECTS[:, ds, :], in0=DECT[:, ds, :], in1=NSTT[:, ds, :], op=ALU.mult), [rDECT, rMSK], [rDECTS])
            yield
            dve(lambda h: h.tensor_tensor(out=DECTS[:, ds, :], in0=ps2[:, ds, :], in1=DECTS[:, ds, :], op=ALU.mult), [r2, rDECTS], [rDECTS])
            yield
            dve(lambda h: h.tensor_tensor(out=W0[:, ds, 0:64], in0=ps4[:, ds, :], in1=DECTS[:, ds, :], op=ALU.mult), [r4, rDECTS], [rW0])
            dve(lambda h: h.tensor_copy(out=W0[:, ds, 64:128], in_=ID2[:, ds, :]), [rMSK], [rW0])
            yield
            for m in range(6):
                (Wc, rWc), (Wn, rWn) = Wb[m % 2], Wb[(m + 1) % 2]
                (Cc, rCc), (Cn, rCn) = Cb[m % 2], Cb[(m + 1) % 2]
                for d in dirs:
                    for hs in HS:
                        P.mm(psI[hs, d, :], Cc[hs, d, :], Wc[hs, d, :], reads=[rCc, rWc], writes=[rI])
                if m < 5:
                    for d in dirs:
                        for hs in HS:
                            P.mm(psC[hs, d, :], Wc[hs, d, 0:64], Cc[hs, d, :], reads=[rCc, rWc], writes=[rC])
                yield
                dve(lambda h, Wn=Wn, Wc=Wc: h.tensor_tensor(out=Wn[:, ds, 64:128], in0=Wc[:, ds, 64:128], in1=psI[:, ds, 64:128], op=ALU.add), [rWc, rI], [rWn])
                if m < 5:
                    P.act(Wn[:, ds, 0:64], psI[:, ds, 0:64], AF.Copy, reads=[rI], writes=[rWn])
                    P.act(Cn[:, ds, :], psC[:, ds, :], AF.Copy, reads=[rC], writes=[rCn])
                yield
            TITt, rTIT = Wb[0]
            TIT = TITt[:, :, 64:128]
            for d in dirs:
                c = ch[d]
                dve(lambda h, d=d, c=c: h.tensor_scalar(out=VB[:, d, :], in0=VTM[:, c, :], scalar1=Bs[:, s, d:d + 1], scalar2=None, op0=ALU.mult), rch, [rVB])
                dve(lambda h, d=d, c=c: h.tensor_scalar(out=KBE[:, d, :], in0=KTM[:, c, :], scalar1=Bs[:, s, d:d + 1], scalar2=SC[:, 2 + d:3 + d], op0=ALU.mult, op1=ALU.mult), rch + [rSC], [rKBE])
                yield
                dve(lambda h, d=d, c=c: h.tensor_scalar(out=KD[:, d, :], in0=KTM[:, c, :], scalar1=SC[:, 4 + d:5 + d], scalar2=None, op0=ALU.mult), rch + [rSC], [rKD])
                dve(lambda h, d=d: h.tensor_tensor(out=QGT[:, d, :], in0=QT[:, tok[d]], in1=EB[:, d, :], op=ALU.mult), rch + [rEB], [rQGT])
                yield
            dve(lambda h: h.tensor_tensor(out=AQM[:, ds, :], in0=ps3[:, ds, :], in1=DECT[:, ds, :], op=ALU.mult), [r3, rDECT], [rAQM])
            for d in dirs:
                for hs in HS:
                    P.mm(pswT[hs, d, :], KBE[hs, d, :], TIT[hs, d, :], reads=[rKBE, rTIT], writes=[rwT])
            yield
            dve(lambda h: h.tensor_scalar(out=NWT[:, ds, :], in0=pswT[:, ds, :], scalar1=-1.0, scalar2=None, op0=ALU.mult), [rwT], [rNWT])
            yield
            yield "REC"
            for d in dirs:
                for hs in HS:
                    P.mm(ps5[hs, d, :], TIT[hs, d, :], VB[hs, d, :], start=True, stop=False, reads=[rTIT, rVB], writes=[r5])
                    P.mm(ps5[hs, d, :], NWT[hs, d, :], Scur[hs, d, :], start=False, stop=True, reads=[rNWT, rScur], writes=[r5])
            yield
            dve(lambda h: h.tensor_copy(out=VN[:, ds, :], in_=ps5[:, ds, :]), [r5], [rVN])
            yield
            for d in dirs:
                for hs in HS:
                    P.mm(ps6[hs, d, :], QGT[hs, d, :], Scur[hs, d, :], start=True, stop=False, reads=[rQGT, rScur], writes=[r6])
                    P.mm(ps6[hs, d, :], AQM[hs, d, :], VN[hs, d, :], start=False, stop=True, reads=[rAQM, rVN], writes=[r6])
            for d in dirs:
                for hs in HS:
                    P.mm(ps7[hs, d, :], KD[hs, d, :], VN[hs, d, :], reads=[rKD, rVN], writes=[r7])
            yield
            for d in range(2):
                if d in dirs:
                    lastc = 63 if d == 0 else 0
                    dve(lambda h, d=d, lastc=lastc: h.tensor_scalar(out=SGL[:, d, :], in0=Scur[:, d, :], scalar1=EB[:, d, lastc:lastc + 1], scalar2=None, op0=ALU.mult), [rScur, rEB], [rSGL])
                    dve(lambda h, d=d: h.tensor_tensor(out=Snew[:, d, :], in0=SGL[:, d, :], in1=ps7[:, d, :], op=ALU.add), [rSGL, r7], [rSnew])
                else:
                    dve(lambda h, d=d: h.tensor_copy(out=Snew[:, d, :], in_=Scur[:, d, :]), [rScur], [rSnew])
            yield
            for d in dirs:
                c = ch[d]
                dve(lambda h, d=d, c=c: h.tensor_tensor(out=O[:, c, :], in0=O[:, c, :], in1=ps6[:, d, :], op=ALU.add), [r6, rO[c]], [rO[c]])
            yield

        STAG = 3
        active = []
        free_l = list(range(NLS))
        finished = set([-1])
        nxt = 0
        since = STAG
        while nxt < NS or active:
            if nxt < NS and free_l and since >= STAG:
                li = free_l.pop(0)
                active.append([step_gen(nxt, lanes[li]), li, nxt, False]); nxt += 1
                since = 0
            since += 1
            for item in list(active):
                if item[3]:
                    if (item[2] - 1) not in finished:
                        continue
                    item[3] = False
                try:
                    if next(item[0]) == "REC":
                        item[3] = True
                except StopIteration:
                    active.remove(item)
                    free_l.append(item[1])
                    finished.add(item[2])

        P.fence()
        G = 33
        OSQ = P_sb("OSQ", [128, G, 64]); rOSQ = Res()
        OSS = P_sb("OSS", [128, G]); rOSS = Res()
        for g in range(NCH // G):
            cs = slice(g * G, (g + 1) * G)
            ro = [rO[c] for c in range(g * G, (g + 1) * G)]
            dve(lambda h, cs=cs: h.tensor_tensor(out=OSQ[:], in0=O[:, cs, :], in1=O[:, cs, :], op=ALU.mult), ro, [rOSQ])
            dve(lambda h: h.reduce_sum(out=OSS[:], in_=OSQ[:], axis=AX.X), [rOSQ], [rOSS])
            P.act(OSS[:], OSS[:], AF.Ln, reads=[rOSS], writes=[rOSS], bias=1e-6, scale=1.0 / 64)
            P.act(OSS[:], OSS[:], AF.Exp, reads=[rOSS], writes=[rOSS], scale=-0.5)
            dve(lambda h, cs=cs: h.tensor_tensor(out=O[:, cs, :], in0=O[:, cs, :], in1=OSS[:].unsqueeze(2).to_broadcast([128, G, 64]), op=ALU.mult), ro + [rOSS], ro)
            dve(lambda h, cs=cs: h.tensor_tensor(out=O[:, cs, :], in0=O[:, cs, :], in1=GDN[:].unsqueeze(1).to_broadcast([128, G, 64]), op=ALU.mult), ro + [rGDN], ro)
            dve(lambda h, cs=cs: h.tensor_tensor(out=O[:, cs, :], in0=O[:, cs, :], in1=SZ[:, cs, :], op=ALU.mult), ro + [rCH[c] for c in range(g * G, (g + 1) * G)], ro)
        ov = o_dn.rearrange("(c t) (h d) -> h t c d", t=64, h=2)
        for hh in range(2):
            P.dma("sp", ov[hh], O[hh * 64:(hh + 1) * 64, :, :], reads=rO, writes=[rOUT[hh]])
        P.fence()


NE = 32


def emit_b(P, G, IN, l, NL, NC, xs, rxs, omall, romall, xo, rOUT):
    NT = NL + NC
    cvT = IN["cvT"]; adaw = IN["adaw2", l]; adab = IN["adab2", l]; adabT = IN["adab2T", l]
    g2T = IN["g2T", l]; wout = IN["wout", l]; rw = IN["rw", l]; rb = IN["rb", l]
    wgu = IN["wgu", l]; bguT = IN["bguT", l]; wd = IN["wd", l]; bd = IN["bd", l]; seli = IN["seli"]

    tiles = [(i * 128, 128, 0) for i in range(NL // 128)]
    if NC:
        tiles.append((NL, NC, 1))
    nlt = NL // 128
    passes = [list(range(0, nlt // 2)), list(range(nlt // 2, len(tiles)))]
    MAXT = max(len(p) for p in passes)

    with ExitStack() as st0:
        _sb = P.sb
        P_sb = lambda name, shape, dt=F32, stack=None: _sb("b_" + name, shape, dt, stack=(stack or st0))
        ID, rID, ONES, rONES, ONESB, rONESB, PS, rPS = G["ID"], G["rID"], G["ONES"], G["rONES"], G["ONESB"], G["rONESB"], G["PS"], G["rPS"]
        SELI = P_sb("SELI", [128, 4, 128], BF16); rSELI = Res()
        P.dma("pool", SELI[:], seli, writes=[rSELI])

        GT = P_sb("GT", [128, 2, 2, D]); rGT = Res()
        MODF = P_sb("MODF", [128, 16, 2]); rMODF = Res()
        SCALE2 = P_sb("SCALE2", [128, 8, 2]); rSCALE2 = Res()
        G2 = P_sb("G2", [128, 8]); rG2 = Res()
        P.dma("sp", G2[:], g2T, writes=[rG2])
        ABT = P_sb("ABT", [128, 16]); rABT = Res()
        P.dma("sp", ABT[:], adabT, writes=[rABT])
        RW = P_sb("RW", [128, 8, NE]); rRW = Res()
        P.dma("sp", RW[:], rw.rearrange("(k p) e -> p k e", p=128), writes=[rRW])
        RB = P_sb("RB", [1, NE]); rRB = Res()
        P.dma("sp", RB[:], rb, writes=[rRB])
        WO = P_sb("WO", [128, 8, D], BF16); rWO = Res()
        P.dma("pool", WO[:], wout.rearrange("(k p) f -> p k f", p=128), writes=[rWO])
        sub = ExitStack()
        ABR = P_sb("ABR", [1, 4096], stack=sub); rABR = Res()
        P.dma("sp", ABR[:], adab, writes=[rABR])
        CV = P_sb("CV", [128, 8, 2], stack=sub); rCV = Res()
        P.dma("sp", CV[:], cvT, writes=[rCV])
        SCV = P_sb("SCV", [128, 8, 2], stack=sub); rSCV = Res()
        P.act(SCV[:], CV[:], AF.Silu, reads=[rCV], writes=[rSCV])
        SCB = P_sb("SCB", [128, 8, 2, 128], stack=sub); rSCB = Res()
        P.op("dve", lambda h: h.tensor_tensor(out=SCB[:], in0=ONES[:].unsqueeze(1).unsqueeze(1).to_broadcast([128, 8, 2, 128]),
                                              in1=SCV[:].unsqueeze(3).to_broadcast([128, 8, 2, 128]), op=ALU.mult),
             reads=[rONES, rSCV], writes=[rSCB])

        AW = [P_sb("AW%d" % i, [128, 8, 512], stack=sub) for i in range(2)]; rAW = [Res(), Res()]
        for blk in range(8):
            aw, raw = AW[blk % 2], rAW[blk % 2]
            P.dma("sp", aw[:], adaw[:, blk * 512:(blk + 1) * 512].rearrange("(k p) f -> p k f", p=128), writes=[raw])
            if blk in (0, 1, 6, 7):
                which = 0 if blk < 2 else 1
                half = blk % 2
                for j in range(2):
                    ps, rps = PS[j], rPS[j]
                    for k in range(8):
                        P.mm(ps[:], SCB[:, k, j, :], aw[:, k, :], start=(k == 0), stop=False,
                             reads=[rSCB, raw], writes=[rps])
                    P.mm(ps[:], ONES[0:1, :], ABR[0:1, blk * 512:(blk + 1) * 512], start=False, stop=True,
                         reads=[rONES, rABR], writes=[rps])
                    P.act(GT[:, which, j, half * 512:(half + 1) * 512], ps[:], AF.Copy, reads=[rps], writes=[rGT])
            else:
                ps, rps = PS[2 + blk % 2], rPS[2 + blk % 2]
                for fcl in range(4):
                    fcg = (blk - 2) * 4 + fcl
                    for k in range(8):
                        P.mm(ps[:, fcl * 2:fcl * 2 + 2], aw[:, k, fcl * 128:(fcl + 1) * 128], SCV[:, k, :],
                             start=(k == 0), stop=(k == 7), reads=[rSCV, raw], writes=[rps])
                for fcl in range(4):
                    fcg = (blk - 2) * 4 + fcl
                    P.act(MODF[:, fcg, :], ps[:, fcl * 2:fcl * 2 + 2], AF.Identity, reads=[rps, rABT], writes=[rMODF],
                          bias=ABT[:, fcg:fcg + 1], scale=1.0)
        P.op("dve", lambda h: h.tensor_scalar(out=SCALE2[:], in0=MODF[:, 8:16, :], scalar1=1.0, scalar2=None, op0=ALU.add),
             reads=[rMODF], writes=[rSCALE2])
        P.op("dve", lambda h: h.tensor_tensor(out=SCALE2[:], in0=SCALE2[:], in1=G2[:].unsqueeze(2).to_broadcast([128, 8, 2]), op=ALU.mult),
             reads=[rSCALE2, rG2], writes=[rSCALE2])

        P.fence()
        sub.close()
        X1 = P_sb("X1", [128, MAXT, D]); rX1 = [Res() for _ in range(MAXT)]
        H2B = P_sb("H2B", [128, 8, MAXT * 128], BF16); rH2B = [Res() for _ in range(MAXT)]
        GATES = P_sb("GATES", [128, MAXT, NE]); rGATES = [Res() for _ in range(MAXT)]
        XT = [P_sb("XT%d" % i, [128, D]) for i in range(2)]; rXT = [Res(), Res()]
        OM = [P_sb("OM%d" % i, [128, 8, 128], BF16) for i in range(2)]; rOM = [Res(), Res()]
        OMX = P_sb("OMX", [128, 4, 4, 256], BF16); rOMX = Res()
        TMP = [P_sb("TMP%d" % i, [128, D]) for i in range(2)]; rTMP = [Res(), Res()]
        H2F = P_sb("H2F", [128, 8, 128]); rH2F = Res()
        SMALL = P_sb("SMALL", [128, 64]); rSM = Res()
        LG = P_sb("LG", [128, NE]); rLG = Res()
        EX = P_sb("EX", [128, NE]); rEX = Res()
        MK = P_sb("MK", [128, NE]); rMK = Res()
        WGU = P_sb("WGU", [128, 8, 2 * D], BF16); rWG = [Res() for _ in range(8)]; rWU = [Res() for _ in range(8)]
        WD = P_sb("WD", [128, 8, D], BF16); rWD = [Res() for _ in range(8)]
        BGU = [P_sb("BGU%d" % i, [128, 16]) for i in range(2)]; rBGU = [Res(), Res()]
        BD = [P_sb("BD%d" % i, [1, D], BF16) for i in range(2)]; rBD = [Res(), Res()]
        BGX = [P_sb("BGX%d" % i, [128, 16]) for i in range(2)]; rBGX = [Res(), Res()]
        SIGC = float(1.0 / (1.0 + np.exp(np.float64(-1.702 * 7.0))))
        ACTT = [P_sb("ACTT%d" % i, [128, 8, 512], BF16) for i in range(2)]; rACTT = [Res(), Res()]
        GP = [P_sb("GP%d" % i, [128, 512]) for i in range(2)]; rGP = [Res(), Res()]
        SG = [P_sb("SG%d" % i, [128, 512]) for i in range(2)]; rSG = [Res(), Res()]
        UP = [P_sb("UP%d" % i, [128, 512]) for i in range(2)]; rUP = [Res(), Res()]
        wcount = 0
        cnt = 0

        for pss in passes:
            for li, ti in enumerate(pss):
                r0, nr, j = tiles[ti]
                xt, rxt = XT[li % 2], rXT[li % 2]
                om, rom = OM[li % 2], rOM[li % 2]
                tmp, rtmp = TMP[li % 2], rTMP[li % 2]
                P.dma("sp", xt[:nr, :], xs[r0:r0 + nr, :], reads=rxs, writes=[rxt])
                for q in range(4):
                    grow = (256 + q * NL + r0) if j == 0 else (q * NC)
                    for jj in range(4):
                        P.dma("pool", OMX[:nr, q, jj, :], omall(jj, grow, nr), reads=romall, writes=[rOMX])
                for k in range(8):
                    ps, rps = PS[2 + k // 4], rPS[2 + k // 4]
                    for q in range(4):
                        P.mm(ps[:, (k % 4) * 128:(k % 4) * 128 + nr], OMX[:nr, q, k % 4, (k // 4) * 128:(k // 4 + 1) * 128], SELI[:nr, q, :nr],
                             start=(q == 0), stop=(q == 3), reads=[rOMX, rSELI], writes=[rps])
                for kk in range(2):
                    P.act(om[:, 4 * kk:4 * kk + 4, :nr], PS[2 + kk][:, :].rearrange("p (a t) -> p a t", a=4)[:, :, :nr], AF.Copy, reads=[rPS[2 + kk]], writes=[rom])
                for half in range(2):
                    ps, rps = PS[half], rPS[half]
                    for k in range(8):
                        P.mm(ps[:nr, :], om[:, k, :nr], WO[:, k, half * 512:(half + 1) * 512], start=(k == 0), stop=(k == 7),
                             reads=[rom, rWO], writes=[rps])
                    P.op("dve", lambda h, ps=ps, tmp=tmp, half=half, j=j, nr=nr: h.tensor_tensor(
                        out=tmp[:nr, half * 512:(half + 1) * 512], in0=ps[:nr, :], in1=GT[:nr, 0, j, half * 512:(half + 1) * 512], op=ALU.mult),
                        reads=[rps, rGT], writes=[rtmp])
                P.op("dve", lambda h, tmp=tmp, xt=xt, li=li, nr=nr: h.tensor_tensor(out=X1[:nr, li, :], in0=tmp[:nr, :], in1=xt[:nr, :], op=ALU.add),
                     reads=[rtmp, rxt], writes=[rX1[li]])
                P.act(tmp[:nr, :], X1[:nr, li, :], AF.Square, reads=[rX1[li]], writes=[rtmp, rSM], accum_out=SMALL[:nr, 0:1])
                P.act(SMALL[:nr, 1:2], SMALL[:nr, 0:1], AF.Sqrt, reads=[rSM], writes=[rSM], bias=1e-6, scale=1.0 / D)
                P.op("dve", lambda h, nr=nr: h.reciprocal(out=SMALL[:nr, 2:3], in_=SMALL[:nr, 1:2]), reads=[rSM], writes=[rSM])
                P.op("dve", lambda h, tmp=tmp, li=li, nr=nr: h.tensor_scalar(out=tmp[:nr, :], in0=X1[:nr, li, :], scalar1=SMALL[:nr, 2:3], scalar2=None, op0=ALU.mult),
                     reads=[rX1[li], rSM], writes=[rtmp])
                for k in range(8):
                    ps, rps = PS[2 + k // 4], rPS[2 + k // 4]
                    P.op("pe", lambda h, ps=ps, tmp=tmp, k=k, nr=nr: h.transpose(ps[:, (k % 4) * 128:(k % 4) * 128 + nr], tmp[:nr, k * 128:(k + 1) * 128], ID[:nr, :nr]),
                         reads=[rtmp, rID], writes=[rps])
                for k in range(8):
                    ps, rps = PS[2 + k // 4], rPS[2 + k // 4]
                    P.act(H2F[:, k, :nr], ps[:, (k % 4) * 128:(k % 4) * 128 + nr], AF.Identity, reads=[rps, rSCALE2, rMODF], writes=[rH2F],
                          scale=SCALE2[:, k, j:j + 1], bias=MODF[:, k, j:j + 1])
                P.op("dve", lambda h, li=li, nr=nr: h.tensor_copy(out=H2B[:, :, li * 128:li * 128 + nr], in_=H2F[:, :, :nr]),
                     reads=[rH2F], writes=[rH2B[li]])
                ps, rps = PS[4], rPS[4]
                for k in range(8):
                    P.mm(ps[:nr, 0:NE], H2F[:, k, :nr], RW[:, k, :], start=(k == 0), stop=False, reads=[rH2F, rRW], writes=[rps])
                P.mm(ps[:nr, 0:NE], ONES[0:1, :nr], RB[0:1, :], start=False, stop=True, reads=[rONES, rRB], writes=[rps])
                P.op("dve", lambda h, ps=ps, nr=nr: h.tensor_copy(out=LG[:nr, :], in_=ps[:nr, 0:NE]), reads=[rps], writes=[rLG])
                P.op("dve", lambda h, nr=nr: h.max(out=SMALL[:nr, 8:16], in_=LG[:nr, :]), reads=[rLG], writes=[rSM])
                P.op("dve", lambda h, nr=nr: h.tensor_scalar(out=SMALL[:nr, 16:17], in0=SMALL[:nr, 8:9], scalar1=-1.0, scalar2=None, op0=ALU.mult),
                     reads=[rSM], writes=[rSM])
                P.act(EX[:nr, :], LG[:nr, :], AF.Exp, reads=[rLG, rSM], writes=[rEX], bias=SMALL[:nr, 16:17], scale=1.0)
                P.op("dve", lambda h, nr=nr: h.tensor_scalar(out=MK[:nr, :], in0=LG[:nr, :], scalar1=SMALL[:nr, 11:12], scalar2=None, op0=ALU.is_ge),
                     reads=[rLG, rSM], writes=[rMK])
                P.op("dve", lambda h, nr=nr: h.tensor_tensor(out=EX[:nr, :], in0=EX[:nr, :], in1=MK[:nr, :], op=ALU.mult),
                     reads=[rEX, rMK], writes=[rEX])
                P.op("dve", lambda h, nr=nr: h.reduce_sum(out=SMALL[:nr, 17:18], in_=EX[:nr, :], axis=AX.X), reads=[rEX], writes=[rSM])
                P.op("dve", lambda h, nr=nr: h.reciprocal(out=SMALL[:nr, 18:19], in_=SMALL[:nr, 17:18]), reads=[rSM], writes=[rSM])
                P.op("dve", lambda h, li=li, nr=nr: h.tensor_scalar(out=GATES[:nr, li, :], in0=EX[:nr, :], scalar1=SMALL[:nr, 18:19], scalar2=None, op0=ALU.mult),
                     reads=[rEX, rSM], writes=[rGATES[li]])

            groups = []
            li = 0
            while li < len(pss):
                g = []
                while li < len(pss) and len(g) < 4 and tiles[pss[li]][1] == 128:
                    g.append(li); li += 1
                if not g:
                    g = [li]; li += 1
                groups.append(g)
            for e in range(IN.get("nex", NE)):
                wb = wcount % 2
                wcount += 1
                for fc in range(8):
                    P.dma("pool", WGU[:, :, fc * 128:(fc + 1) * 128], wgu[e, :, fc * 128:(fc + 1) * 128].rearrange("(k p) f -> p k f", p=128),
                          writes=[rWG[fc]])
                    P.dma("pool", WGU[:, :, D + fc * 128:D + (fc + 1) * 128], wgu[e, :, D + fc * 128:D + (fc + 1) * 128].rearrange("(k p) f -> p k f", p=128),
                          writes=[rWU[fc]])
                for fc in range(8):
                    P.dma("pool", WD[:, fc, :], wd[e, fc * 128:(fc + 1) * 128, :], writes=[rWD[fc]])
                P.dma("sp", BGU[wb][:], bguT[e], writes=[rBGU[wb]])
                P.op("dve", lambda h, wb=wb: h.tensor_scalar(out=BGX[wb][:, 0:8], in0=BGU[wb][:, 0:8], scalar1=1.702, scalar2=None, op0=ALU.mult),
                     reads=[rBGU[wb]], writes=[rBGX[wb]])
                P.op("dve", lambda h, wb=wb: h.tensor_scalar(out=BGX[wb][:, 8:16], in0=BGU[wb][:, 8:16], scalar1=1.0, scalar2=None, op0=ALU.add),
                     reads=[rBGU[wb]], writes=[rBGX[wb]])
                P.dma("pool", BD[wb][:], bd[e:e + 1, :], writes=[rBD[wb]])
                for g in groups:
                    ntok = sum(tiles[pss[l]][1] for l in g)
                    c0 = g[0] * 128
                    ab = cnt % 2
                    cnt += 1
                    actt, ractt = ACTT[ab], rACTT[ab]
                    rh = [rH2B[l] for l in g]
                    for fc in range(8):
                        pb = (fc % 2) * 2
                        psg, rpsg = PS[pb], rPS[pb]
                        psu, rpsu = PS[pb + 1], rPS[pb + 1]
                        for k in range(8):
                            P.mm(psg[:, :ntok], WGU[:, k, fc * 128:(fc + 1) * 128], H2B[:, k, c0:c0 + ntok], start=(k == 0), stop=(k == 7),
                                 reads=[rWG[fc]] + rh, writes=[rpsg])
                        for k in range(8):
                            P.mm(psu[:, :ntok], WGU[:, k, D + fc * 128:D + (fc + 1) * 128], H2B[:, k, c0:c0 + ntok], start=(k == 0), stop=(k == 7),
                                 reads=[rWU[fc]] + rh, writes=[rpsu])
                        tb = fc % 2
                        gp, sg, up = GP[tb], SG[tb], UP[tb]
                        P.act(sg[:, :ntok], psg[:, :ntok], AF.Sigmoid, reads=[rpsg, rBGX[wb]], writes=[rSG[tb]], scale=1.702, bias=BGX[wb][:, fc:fc + 1])
                        P.op("dve", lambda h, gp=gp, psg=psg, fc=fc, wb=wb, ntok=ntok: h.tensor_scalar(
                            out=gp[:, :ntok], in0=psg[:, :ntok], scalar1=BGU[wb][:, fc:fc + 1], scalar2=7.0, op0=ALU.add, op1=ALU.min),
                            reads=[rpsg, rBGU[wb]], writes=[rGP[tb]])
                        P.op("dve", lambda h, up=up, psu=psu, fc=fc, wb=wb, ntok=ntok: h.tensor_scalar(
                            out=up[:, :ntok], in0=psu[:, :ntok], scalar1=BGX[wb][:, 8 + fc:9 + fc], scalar2=8.0, op0=ALU.add, op1=ALU.min),
                            reads=[rpsu, rBGX[wb]], writes=[rUP[tb]])
                        P.op("dve", lambda h, gp=gp, sg=sg, ntok=ntok: h.scalar_tensor_tensor(out=gp[:, :ntok], in0=sg[:, :ntok], scalar=SIGC, in1=gp[:, :ntok], op0=ALU.min, op1=ALU.mult),
                             reads=[rGP[tb], rSG[tb]], writes=[rGP[tb]])
                        P.op("dve", lambda h, gp=gp, up=up, actt=actt, fc=fc, ntok=ntok: h.scalar_tensor_tensor(out=actt[:, fc, :ntok], in0=up[:, :ntok], scalar=-6.0, in1=gp[:, :ntok], op0=ALU.max, op1=ALU.mult),
                             reads=[rGP[tb], rUP[tb]], writes=[ractt])
                    for gi, l in enumerate(g):
                        r0, nr, j = tiles[pss[l]]
                        yb = 4 + (l % 2) * 2
                        for half in range(2):
                            ps, rps = PS[yb + half], rPS[yb + half]
                            for fc in range(8):
                                P.mm(ps[:nr, :], actt[:, fc, gi * 128:gi * 128 + nr], WD[:, fc, half * 512:(half + 1) * 512], start=(fc == 0), stop=False,
                                     reads=[ractt, rWD[fc]], writes=[rps])
                            P.mm(ps[:nr, :], ONESB[0:1, :nr], BD[wb][0:1, half * 512:(half + 1) * 512], start=False, stop=True,
                                 reads=[rONESB, rBD[wb]], writes=[rps])
                        tmp, rtmp = TMP[l % 2], rTMP[l % 2]
                        for half in range(2):
                            ps, rps = PS[yb + half], rPS[yb + half]
                            P.op("dve", lambda h, ps=ps, tmp=tmp, half=half, l=l, e=e, j=j, nr=nr: h.scalar_tensor_tensor(
                                out=tmp[:nr, half * 512:(half + 1) * 512], in0=ps[:nr, :], scalar=GATES[:nr, l, e:e + 1],
                                in1=GT[:nr, 1, j, half * 512:(half + 1) * 512], op0=ALU.mult, op1=ALU.mult),
                                reads=[rps, rGATES[l], rGT], writes=[rtmp])
                        P.op("dve", lambda h, tmp=tmp, l=l, nr=nr: h.tensor_tensor(out=X1[:nr, l, :], in0=X1[:nr, l, :], in1=tmp[:nr, :], op=ALU.add),
                             reads=[rtmp, rX1[l]], writes=[rX1[l]])
            for li, ti in enumerate(pss):
                r0, nr, j = tiles[ti]
                P.dma("sp", xo[r0:r0 + nr, :], X1[:nr, li, :], reads=[rX1[li]], writes=[rOUT[ti]])
        P.fence()


PER_LAYER = [("adaw1", [1024, 2048]), ("adab1T", [128, 16]), ("g1T", [128, 8]), ("wa", [1024, 384]), ("wz", [1024, 2, 68]),
             ("cw", [128, 3, 5]), ("nega", [128, 2]), ("dtb", [128, 2]), ("gdn", [128, 64]), ("ws", [1024, 256]),
             ("gqk", [128, 3, 64]), ("sinkb", [128, 2]),
             ("adaw2", [1024, 4096]), ("adab2", [1, 4096]), ("adab2T", [128, 16]), ("g2T", [128, 8]), ("wout", [1024, 1024]),
             ("rw", [1024, 32]), ("rb", [1, 32]), ("bguT", [32, 128, 16]), ("bd", [32, 1024])]
SHARED = [("ident", [128, 128]), ("cvT", [128, 8, 2]), ("blk1", [128, 128]), ("masks", [128, 6, 2, 64]), ("maskw", [128, 384]),
          ("ropet", [128, 64, 2, 2, 16]), ("seli", [128, 4, 128])]
NLOC = 2112


def build_fused(nex=32):
    nc = bass.Bass("TRN2", target_bir_lowering=False)
    dr = lambda name, shape: nc.dram_tensor(name, list(shape), F32, kind="ExternalInput").ap()
    IN = {}
    IN["xall"] = dr("xall", [8448, 1024]); IN["xs0"] = dr("xs0", [NLOC, 1024])
    for nm, shp in SHARED:
        IN[nm] = dr(nm, shp)
    for l in range(2):
        for nm, shp in PER_LAYER:
            IN[nm, l] = dr("%s_%d" % (nm, l), shp)
    for l in range(2):
        IN["wgu", l] = dr("wgu_%d" % l, [nex, 1024, 2048]); IN["wd", l] = dr("wd_%d" % l, [nex, 1024, 1024])
    IN["nex"] = nex
    xo = nc.dram_tensor("xo", [2048, 1024], F32, kind="ExternalOutput").ap()
    omloc = [nc.dram_tensor("omloc%d" % l, [8448, 256], F32).ap() for l in range(2)]
    OCH = 1024
    och = [(r, min(OCH, 8448 - r)) for r in range(0, 8448, OCH)]
    omall = [[nc.dram_tensor("omall%d_%d" % (l, k), [4 * n, 256], F32).ap() for k, (r, n) in enumerate(och)] for l in range(2)]
    xloc = nc.dram_tensor("xloc", [NLOC, 1024], F32).ap()
    XCH = 256
    xch = [(r, min(XCH, NLOC - r)) for r in range(0, NLOC, XCH)]
    xgat = [nc.dram_tensor("xgat_%d" % k, [4 * n, 1024], F32).ap() for k, (r, n) in enumerate(xch)]

    def om_rows(l, jj, grow, nr):
        k = grow // OCH
        n = och[k][1]
        off = jj * n + (grow - och[k][0])
        return omall[l][k][off:off + nr, :]

    def xg_rows(q, r, nr):
        k = r // XCH
        n = xch[k][1]
        off = q * n + (r - xch[k][0])
        return xgat[k][off:off + nr, :]
    groups = [[0, 1, 2, 3], [4, 5, 6, 7]]
    with ExitStack() as st:
        P = Prog(nc, st)
        G = emit_globals(P, IN)
        rxloc = [Res() for _ in range(17)]
        rxgat = Res()
        rcc = Res()
        rxo = [Res() for _ in range(16)]
        for l in range(2):
            last = (l == 1)
            if l == 0:
                xsrc = lambda n: [(0, 128, IN["xall"][n * 128:(n + 1) * 128, :])]
                rsrc = []
            else:
                def xsrc(n):
                    if n < 2:
                        a, b = 2 * n, 2 * n + 1
                        return [(0, 64, xg_rows(a, 2048, 64)), (64, 64, xg_rows(b, 2048, 64))]
                    i = n - 2
                    q, r = i // 16, (i % 16) * 128
                    return [(0, 128, xg_rows(q, r, 128))]
                rsrc = [rxgat]
            rdn = [Res(), Res()]
            rsw = [Res() for _ in range(66)]
            with ExitStack() as lst:
                C = emit_common(P, G, IN, l, lst)
                emit_dn(P, C, IN, l, last, xsrc, rsrc, omloc[l][:, 0:128], rdn)
                emit_swa(P, C, IN, l, last, xsrc, rsrc, omloc[l][:, 128:256], rsw)
                P.fence()
            romall = Res()
            if True:
                for k, (r, n) in enumerate(och):
                    P.coll("AllGather", ALU.bypass, groups, omloc[l][r:r + n, :], omall[l][k], reads=rdn + rsw, writes=[romall, rcc])
            if l == 0:
                emit_b(P, G, IN, 0, 2048, 64, IN["xs0"], [], lambda jj, grow, nr: om_rows(0, jj, grow, nr), [romall], xloc, rxloc)
                if True:
                    for k, (r, n) in enumerate(xch):
                        P.coll("AllGather", ALU.bypass, groups, xloc[r:r + n, :], xgat[k], reads=rxloc, writes=[rxgat, rcc])
            else:
                emit_b(P, G, IN, 1, 2048, 0, xloc, rxloc, lambda jj, grow, nr: om_rows(1, jj, grow, nr), [romall], xo, rxo)
        P.finish(rxo)
        print("fused instr counts", P.cnt, P.dcnt, "waits", P.n_wait, "sems", {k: len(v) for k, v in P.sem.items()})
    return nc


NEG = -1e30
def consts():
    f = np.float32
    i = np.arange(64)
    k_le_f = (i[:, None] <= i[None, :]).astype(f)
    k_ge_f = (i[:, None] >= i[None, :]).astype(f)
    m = np.zeros((64, 6, 2, 64), f)
    m[:, 0, 0] = k_le_f; m[:, 0, 1] = k_ge_f
    m[:, 1, 0] = np.where(i[None, :] >= i[:, None], 0, NEG)
    m[:, 1, 1] = np.where(i[None, :] <= i[:, None], 0, NEG)
    m[:, 2, 0] = np.where(i[None, :] <= i[:, None], 0, NEG)
    m[:, 2, 1] = np.where(i[None, :] >= i[:, None], 0, NEG)
    m[:, 3, 0] = np.where(i[None, :] > i[:, None], -1, 0)
    m[:, 3, 1] = np.where(i[None, :] < i[:, None], -1, 0)
    m[:, 4, 0] = np.where(i[None, :] < i[:, None], -1, 0)
    m[:, 4, 1] = np.where(i[None, :] > i[:, None], -1, 0)
    m[:, 5, 0] = np.eye(64); m[:, 5, 1] = np.eye(64)
    masks = np.concatenate([m, m], 0)
    blk1 = np.zeros((128, 128), f); blk1[:64, :64] = 1; blk1[64:, 64:] = 1
    qi = np.arange(128)[:, None]; kc = np.arange(384)[None, :]
    maskw = np.where((kc >= qi) & (kc <= qi + 256), 0, NEG).astype(f)
    t = np.arange(8192); row = (t // 64).astype(f); col = (t % 64).astype(f)
    inv = np.power(f(10000.0), -np.arange(16, dtype=f) / f(16)).astype(f)
    ar = row[:, None] * inv; ac = col[:, None] * inv
    rope = np.stack([np.stack([np.cos(ar), np.cos(ac)], 1), np.stack([np.sin(ar), np.sin(ac)], 1)], 1).astype(f)
    ropet = np.ascontiguousarray(rope.reshape(64, 128, 2, 2, 16).transpose(1, 0, 2, 3, 4))
    return dict(masks=masks, blk1=blk1, maskw=maskw, ropet=ropet, ident=np.eye(128, dtype=f))
def prep_a(inp, l, x, xc, K):
    f = np.float32
    w_in = inp["w_in"][l]; cwl = inp["dn_conv_w"][l]
    ada_w = np.ascontiguousarray(inp["ada_w"][l][:, 0:2048]); ada_b = inp["ada_b"][l][0:2048]
    base = dict(adaw=ada_w, adabT=np.ascontiguousarray(ada_b.reshape(16, 128).T), g1T=np.ascontiguousarray(inp["norm1_g"][l].reshape(8, 128).T), ident=K["ident"])
    dn, sw = [], []
    for c in range(8):
        b, j = c // 4, c % 4
        xall = np.ascontiguousarray(np.concatenate([xc[b], x[b]], 0))
        cv = np.stack([inp["c"][b], inp["c_ctx"]], -1)
        m = dict(base); m.update(xall=xall, cvT=np.ascontiguousarray(cv.reshape(8, 128, 2).transpose(1, 0, 2)))
        hd = [2 * j, 2 * j + 1]
        wa = np.concatenate([w_in[:, s * 512 + 128 * j: s * 512 + 128 * j + 128] for s in range(3)], 1)
        wz = np.stack([np.concatenate([w_in[:, 1536 + h * 64:1536 + (h + 1) * 64], w_in[:, [2048 + h, 2048 + 8 + h, 2064 + h, 2064 + 8 + h]]], 1) for h in hd], 1)
        cw = np.stack([cwl[:, s * 512 + 128 * j: s * 512 + 128 * j + 128].T for s in range(3)], 1)
        hp = np.repeat(np.array(hd), 64)
        nega = -np.exp(inp["dn_a_log"][l][:, hp]).T; dtb = inp["dn_dt_bias"][l][:, hp].T
        md = dict(m); md.update(wa=np.ascontiguousarray(wa), wz=np.ascontiguousarray(wz), cw=np.ascontiguousarray(cw), nega=np.ascontiguousarray(nega.astype(f)),
                                dtb=np.ascontiguousarray(dtb), gdn=np.ascontiguousarray(np.broadcast_to(inp["dn_out_g"][l], (128, 64))), blk1=K["blk1"], masks=K["masks"])
        dn.append(md)
        kv = j // 2
        ws = np.concatenate([w_in[:, 2080 + hd[0] * 64:2080 + hd[0] * 64 + 128], w_in[:, 2592 + kv * 64:2592 + (kv + 1) * 64], w_in[:, 2720 + kv * 64:2720 + (kv + 1) * 64]], 1)
        gqk = np.broadcast_to(np.stack([inp["q_norm_g"][l], inp["q_norm_g"][l], inp["k_norm_g"][l]], 0), (128, 3, 64))
        ms = dict(m); ms.update(ws=np.ascontiguousarray(ws), gqk=np.ascontiguousarray(gqk), ropet=K["ropet"],
                                sinkb=np.ascontiguousarray(np.broadcast_to(inp["sinks"][l][hd], (128, 2))), maskw=K["maskw"])
        sw.append(ms)
    return dn, sw
def gather_a(res_dn, res_sw):
    om_x = np.zeros((2, 8192, 1024), np.float32); om_c = np.zeros((2, 256, 1024), np.float32)
    for c in range(8):
        b, j = c // 4, c % 4
        od = res_dn[c]["o_dn"]; os_ = res_sw[c]["o_sw"]
        om_c[b, :, 128 * j:128 * j + 128] = od[:256]; om_x[b, :, 128 * j:128 * j + 128] = od[256:]
        om_c[b, :, 512 + 128 * j:512 + 128 * j + 128] = os_[:256]; om_x[b, :, 512 + 128 * j:512 + 128 * j + 128] = os_[256:]
    return om_x, om_c


def prep_b(inp, l, x, xc, om_x, om_c, last):
    f = np.float32
    ada_w = np.ascontiguousarray(inp["ada_w"][l][:, 2048:6144]); ada_b = inp["ada_b"][l][2048:6144]
    common = dict(
        adaw=ada_w, adab=np.ascontiguousarray(ada_b[None, :]),
        adabT=np.ascontiguousarray(ada_b[1024:3072].reshape(16, 128).T),
        g2T=np.ascontiguousarray(inp["norm2_g"][l].reshape(8, 128).T),
        wout=inp["w_out"][l], rw=inp["router_w"][l], rb=np.ascontiguousarray(inp["router_b"][l][None, :]),
        wgu=inp["w_gate_up"][l], bguT=np.ascontiguousarray(inp["b_gate_up"][l].reshape(32, 16, 128).transpose(0, 2, 1)),
        wd=inp["w_down"][l], bd=inp["b_down"][l], ident=np.eye(128, dtype=f))
    maps = []
    for c in range(8):
        b, q = c // 4, c % 4
        rows = [x[b, q * 2048:(q + 1) * 2048]]; oms = [om_x[b, q * 2048:(q + 1) * 2048]]
        if not last:
            rows.append(xc[b, q * 64:(q + 1) * 64]); oms.append(om_c[b, q * 64:(q + 1) * 64])
        xs = np.ascontiguousarray(np.concatenate(rows, 0)); om = np.concatenate(oms, 0)
        cv = np.stack([inp["c"][b], inp["c_ctx"]], -1)
        m = dict(common)
        m.update(xs=xs, omT=np.ascontiguousarray(om.T), cvT=np.ascontiguousarray(cv.reshape(8, 128, 2).transpose(1, 0, 2)))
        maps.append(m)
    return maps
def gather_b(results, last):
    x = np.zeros((2, 8192, 1024), np.float32); xc = np.zeros((2, 256, 1024), np.float32)
    for c in range(8):
        b, q = c // 4, c % 4
        xo = results[c]["xo"]
        x[b, q * 2048:(q + 1) * 2048] = xo[:2048]
        if not last:
            xc[b, q * 64:(q + 1) * 64] = xo[2048:]
    return x, xc


def prep_fused(inp, nex=32):
    f = np.float32
    K = consts()
    x = np.ascontiguousarray(inp["x"], dtype=f); xc = np.ascontiguousarray(inp["ctx"], dtype=f)
    A = [prep_a(inp, l, x, xc, K) for l in range(2)]
    zx = np.zeros((2, 8192, 1024), f); zc = np.zeros((2, 256, 1024), f)
    B = [prep_b(inp, l, zx, zc, zx, zc, False) for l in range(2)]
    wgu = [np.ascontiguousarray(inp["w_gate_up"][l][:nex], dtype=f) for l in range(2)]; wd = [np.ascontiguousarray(inp["w_down"][l][:nex], dtype=f) for l in range(2)]
    maps = []
    for c in range(8):
        b, q = c // 4, c % 4
        m = dict(xall=A[0][0][c]["xall"], cvT=A[0][0][c]["cvT"], ident=K["ident"], blk1=K["blk1"], masks=K["masks"], maskw=K["maskw"], ropet=K["ropet"])
        m["xs0"] = np.ascontiguousarray(np.concatenate([x[b, q * 2048:(q + 1) * 2048], xc[b, q * 64:(q + 1) * 64]], 0))
        seli = np.zeros((128, 4, 128), f); seli[:, q, :] = np.eye(128, dtype=f); m["seli"] = seli
        for l in range(2):
            dn, sw = A[l][0][c], A[l][1][c]; bb = B[l][c]
            for nm, src, key in [("adaw1", dn, "adaw"), ("adab1T", dn, "adabT"), ("g1T", dn, "g1T"), ("wa", dn, "wa"), ("wz", dn, "wz"), ("cw", dn, "cw"),
                                 ("nega", dn, "nega"), ("dtb", dn, "dtb"), ("gdn", dn, "gdn"), ("ws", sw, "ws"), ("gqk", sw, "gqk"), ("sinkb", sw, "sinkb"),
                                 ("adaw2", bb, "adaw"), ("adab2", bb, "adab"), ("adab2T", bb, "adabT"), ("g2T", bb, "g2T"), ("wout", bb, "wout"),
                                 ("rw", bb, "rw"), ("rb", bb, "rb"), ("bguT", bb, "bguT"), ("bd", bb, "bd")]:
                m["%s_%d" % (nm, l)] = np.ascontiguousarray(src[key], dtype=f)
        for l in range(2):
            m["wgu_%d" % l] = wgu[l]; m["wd_%d" % l] = wd[l]
        maps.append(m)
    return maps
def gather_fused(results):
    x = np.zeros((2, 8192, 1024), np.float32)
    for c in range(8):
        b, q = c // 4, c % 4
        x[b, q * 2048:(q + 1) * 2048] = results[c]["xo"]
    return x


_NC = None


def kernel(**inputs):
    global _NC
    inp = {k: np.asarray(v) for k, v in inputs.items()}
    if _NC is None:
        _NC = build_fused(32)
    maps = prep_fused(inp, 32)
    res = run_bass_kernel_spmd(_NC, maps, core_ids=list(range(8)))
    return gather_fused(res.results).astype(np.float32)
```
